# Optimizing a Trainium2 kernel written in Bass

```python
import math
import functools
import jax
import jax.numpy as jnp
from jax import lax
import numpy as np

D_MODEL = 1024
BATCH = 4
SEQ = 4096
DEPTH = 2

CTX_LEN = 256
GRID_W = 64
EPS = 1e-6
N_DIR = 2
CHUNK = 64

GDN_HEADS = D_MODEL // 256
GDN_DK = 128
GDN_DV = 128
CONV_W = 3

MLSTM_HEADS = D_MODEL // 256
MLSTM_DK = 128
MLSTM_DV = 128

RET_HEADS = D_MODEL // 128
RET_DK = 128
RET_DV = 256
ROPE_BASE = 10000.0

N_EXPERTS = 32
TOP_K = 4
D_FF = D_MODEL
SWIGLU_ALPHA = 1.702
SWIGLU_LIMIT = 7.0
MOE_BLOCK = 256

GDN_QK_W = GDN_HEADS * GDN_DK
GDN_V_W = GDN_HEADS * GDN_DV
GDN_CONV_CH = 2 * GDN_QK_W + GDN_V_W
ML_QK_W = MLSTM_HEADS * MLSTM_DK
ML_V_W = MLSTM_HEADS * MLSTM_DV
EVEN_SPLITS = (GDN_CONV_CH, GDN_V_W, N_DIR * GDN_HEADS, N_DIR * GDN_HEADS,
               ML_QK_W, ML_QK_W, ML_V_W, ML_V_W, N_DIR * MLSTM_HEADS, N_DIR * MLSTM_HEADS)
EVEN_IN = sum(EVEN_SPLITS)
EVEN_OUT = GDN_V_W + ML_V_W
RET_QK_W = RET_HEADS * RET_DK
RET_V_W = RET_HEADS * RET_DV
ODD_SPLITS = (RET_QK_W, RET_QK_W, RET_V_W, RET_V_W)
ODD_IN = sum(ODD_SPLITS)
N_EVEN = (DEPTH + 1) // 2
N_ODD = DEPTH // 2

kernel_name = 'hybrid_gdn_mlstm_retention_moe_dit'


def rms_norm(x, g):
    xf = x.astype(jnp.float32)
    y = xf * lax.rsqrt(jnp.mean(xf * xf, axis=-1, keepdims=True) + EPS)
    return (y * g.astype(jnp.float32)).astype(x.dtype)


def modulate(h, shift, scale):
    return h * (1.0 + scale) + shift


def ada_modulation(cond, w_mod, b_mod):
    m = jax.nn.silu(cond) @ w_mod + b_mod
    return jnp.split(m[:, None, :], 6, axis=-1)


def split_cols(t, sizes):
    bounds = []
    acc = 0
    for s_ in sizes[:-1]:
        acc += s_
        bounds.append(acc)
    return jnp.split(t, bounds, axis=-1)


def to_heads(t, heads):
    b, n, _ = t.shape
    return jnp.swapaxes(t.reshape(b, n, heads, -1), 1, 2).astype(jnp.float32)


def dir_gates(t, heads):
    b, n, _ = t.shape
    return jnp.transpose(t.reshape(b, n, N_DIR, heads), (2, 0, 3, 1)).astype(jnp.float32)


def l2_normalize(t):
    return t * lax.rsqrt(jnp.sum(t * t, axis=-1, keepdims=True) + EPS)


def headwise_norm(o, gain, center=False):
    o = jnp.swapaxes(o, 1, 2)
    if center:
        o = o - jnp.mean(o, axis=-1, keepdims=True)
    o = o * lax.rsqrt(jnp.mean(o * o, axis=-1, keepdims=True) + EPS) * gain.astype(jnp.float32)
    return o.reshape(o.shape[0], o.shape[1], -1)


def centred_short_conv(u, w, on_grid):
    b, t, ch = u.shape
    if on_grid:
        rows = t // GRID_W
        u = u.reshape(b, rows, GRID_W, ch)
    n = u.shape[-2]
    pad = CONV_W // 2
    up = jnp.pad(u, [(0, 0)] * (u.ndim - 2) + [(pad, pad), (0, 0)])
    y = up[..., 0:n, :] * w[0]
    for j in range(1, CONV_W):
        y = y + up[..., j:j + n, :] * w[j]
    return jax.nn.silu(y).reshape(b, t, ch)


def rope(t, pos):
    half = t.shape[-1] // 2
    freqs = ROPE_BASE ** (-jnp.arange(half, dtype=jnp.float32) / half)
    ang = pos[:, None] * freqs[None, :]
    cos, sin = jnp.cos(ang), jnp.sin(ang)
    t1, t2 = t[..., :half], t[..., half:]
    return jnp.concatenate([t1 * cos - t2 * sin, t1 * sin + t2 * cos], axis=-1)


def to_chunks(t):
    b, h, n = t.shape[:3]
    t = t.reshape(b, h, n // CHUNK, CHUNK, *t.shape[3:])
    return jnp.moveaxis(t, 2, 0)


def from_chunks(o):
    o = jnp.moveaxis(o, 0, 2)
    return o.reshape(o.shape[0], o.shape[1], -1, o.shape[-1])


def gdn_chunk_scan(q, k, v, log_alpha, beta, state):
    q, k, v, log_alpha, beta = (to_chunks(t) for t in (q, k, v, log_alpha, beta))
    dv = v.shape[-1]
    g = jnp.cumsum(log_alpha, axis=-1)
    idx = jnp.arange(CHUNK)
    incl = idx[:, None] >= idx[None, :]
    strict = idx[:, None] > idx[None, :]
    decay = jnp.exp(jnp.where(incl, g[..., :, None] - g[..., None, :], -jnp.inf))
    k_beta = k * beta[..., None]
    lower = jnp.where(strict, jnp.einsum('nbhcd,nbhsd->nbhcs', k_beta, k) * decay, 0.0)
    rhs = jnp.concatenate([v * beta[..., None], k_beta * jnp.exp(g)[..., None]], axis=-1)
    sol = lax.linalg.triangular_solve(lower, rhs, left_side=True, lower=True, unit_diagonal=True)
    u, w = sol[..., :dv], sol[..., dv:]
    qk = jnp.where(incl, jnp.einsum('nbhcd,nbhsd->nbhcs', q, k) * decay, 0.0)
    q_dec = q * jnp.exp(g)[..., None]
    k_dec = k * jnp.exp(g[..., -1:] - g)[..., None]
    chunk_decay = jnp.exp(g[..., -1])

    def step(s, xs):
        u_c, w_c, qk_c, qd_c, kd_c, cd_c = xs
        v_new = u_c - jnp.einsum('bhcd,bhde->bhce', w_c, s)
        o = jnp.einsum('bhcd,bhde->bhce', qd_c, s) + jnp.einsum('bhcs,bhse->bhce', qk_c, v_new)
        s = s * cd_c[..., None, None] + jnp.einsum('bhcd,bhce->bhde', kd_c, v_new)
        return s, o

    s_fin, o = lax.scan(step, state, (u, w, qk, q_dec, k_dec, chunk_decay))
    return from_chunks(o), s_fin


def mlstm_chunk_scan(q, k, v, log_i, log_f, state):
    q, k, v, log_i, log_f = (to_chunks(t) for t in (q, k, v, log_i, log_f))
    idx = jnp.arange(CHUNK)
    incl = idx[:, None] >= idx[None, :]

    def step(carry, xs):
        c, n, m = carry
        qc, kc, vc, ic, fc = xs
        b = jnp.cumsum(fc, axis=-1)
        d_in = jnp.where(incl, b[..., :, None] - b[..., None, :] + ic[..., None, :], -jnp.inf)
        d_carry = b + m[..., None]
        m_t = jnp.maximum(d_carry, jnp.max(d_in, axis=-1))
        s = jnp.einsum('bhcd,bhsd->bhcs', qc, kc) * jnp.exp(d_in - m_t[..., None])
        w_carry = jnp.exp(d_carry - m_t)
        num = w_carry[..., None] * jnp.einsum('bhcd,bhde->bhce', qc, c) + jnp.einsum('bhcs,bhse->bhce', s, vc)
        den = w_carry * jnp.einsum('bhcd,bhd->bhc', qc, n) + jnp.sum(s, axis=-1)
        h = num / jnp.maximum(jnp.abs(den), jnp.exp(-m_t))[..., None]
        d_end = b[..., -1:] - b + ic
        carry_end = b[..., -1] + m
        m_new = jnp.maximum(carry_end, jnp.max(d_end, axis=-1))
        w_end = jnp.exp(d_end - m_new[..., None])
        f_end = jnp.exp(carry_end - m_new)
        c = f_end[..., None, None] * c + jnp.einsum('bhs,bhsd,bhse->bhde', w_end, kc, vc)
        n = f_end[..., None] * n + jnp.einsum('bhs,bhsd->bhd', w_end, kc)
        return (c, n, m_new), h

    state, h = lax.scan(step, state, (q, k, v, log_i, log_f))
    return from_chunks(h), state


def retention_chunk_scan(q, k, v, state, log_gamma):
    q, k, v = (to_chunks(t) for t in (q, k, v))
    pos = jnp.arange(CHUNK, dtype=jnp.float32)
    lg = log_gamma[:, None]
    diff = pos[:, None] - pos[None, :]
    intra = jnp.exp(jnp.where(diff >= 0, diff * lg[..., None], -jnp.inf))
    cross = jnp.exp((pos + 1.0) * lg)
    tail = jnp.exp((CHUNK - 1.0 - pos) * lg)
    chunk_decay = jnp.exp(CHUNK * log_gamma)

    def step(s, xs):
        qc, kc, vc = xs
        scores = jnp.einsum('bhcd,bhsd->bhcs', qc, kc) * intra
        o = jnp.einsum('bhcs,bhse->bhce', scores, vc) + jnp.einsum('bhcd,bhde->bhce', qc, s) * cross[..., None]
        s = chunk_decay[:, None, None] * s + jnp.einsum('bhsd,bhse->bhde', kc * tail[..., None], vc)
        return s, o

    s_fin, o = lax.scan(step, state, (q, k, v))
    return from_chunks(o), s_fin


def retention_log_decay(reverse):
    expo = 5.0 + jnp.arange(RET_HEADS, dtype=jnp.float32)
    if reverse:
        expo = expo[::-1]
    return jnp.log1p(-jnp.exp2(-expo))


def bidirectional_scan(scan_f, scan_b, ctx_f, lat_f, ctx_b, lat_b, state0):
    def run(fn, ctx_seq, lat_seq):
        o_ctx, s_ctx = fn(*ctx_seq, state0)
        o_lat, _ = fn(*lat_seq, s_ctx)
        return o_ctx, o_lat

    def rev(seq):
        return tuple(jnp.flip(t, axis=2) for t in seq)

    cf, lf = run(scan_f, ctx_f, lat_f)
    cb, lb = run(scan_b, rev(ctx_b), rev(lat_b))
    return cf + jnp.flip(cb, axis=2), lf + jnp.flip(lb, axis=2)


def gdn_mlstm_mixer(h_ctx, h_lat, w_in, conv_w, a_log, dt_bias, gdn_g, i_bias, f_bias, ml_g, w_out, last):
    f32 = jnp.float32
    a_rate = jnp.exp(a_log.astype(f32))[:, None, :, None]
    dt_b = dt_bias.astype(f32)[:, None, :, None]
    i_b = i_bias.astype(f32)[:, None, :, None]
    f_b = f_bias.astype(f32)[:, None, :, None]

    def prepare(h, on_grid):
        qkv, z, a_gate, b_gate, mq, mk, mv, mo, i_gate, f_gate = split_cols(h @ w_in, EVEN_SPLITS)
        qkv = centred_short_conv(qkv, conv_w, on_grid)
        aq, ak, av = split_cols(qkv, (GDN_QK_W, GDN_QK_W, GDN_V_W))
        aq = l2_normalize(to_heads(aq, GDN_HEADS)) * GDN_DK ** -0.5
        ak = l2_normalize(to_heads(ak, GDN_HEADS))
        av = to_heads(av, GDN_HEADS)
        log_alpha = -a_rate * jax.nn.softplus(dir_gates(a_gate, GDN_HEADS) + dt_b)
        beta = jax.nn.sigmoid(dir_gates(b_gate, GDN_HEADS))
        mq = to_heads(mq, MLSTM_HEADS)
        mk = to_heads(mk, MLSTM_HEADS) * MLSTM_DK ** -0.5
        mv = to_heads(mv, MLSTM_HEADS)
        log_i = dir_gates(i_gate, MLSTM_HEADS) + i_b
        log_f = jax.nn.log_sigmoid(dir_gates(f_gate, MLSTM_HEADS) + f_b)
        gdn_seq = tuple((aq, ak, av, log_alpha[d], beta[d]) for d in range(N_DIR))
        ml_seq = tuple((mq, mk, mv, log_i[d], log_f[d]) for d in range(N_DIR))
        return gdn_seq, ml_seq, z, mo

    ctx_gdn, ctx_ml, ctx_z, ctx_o = prepare(h_ctx, False)
    lat_gdn, lat_ml, lat_z, lat_o = prepare(h_lat, True)
    b = h_lat.shape[0]
    gdn_s0 = jnp.zeros((b, GDN_HEADS, GDN_DK, GDN_DV), f32)
    ml_s0 = (jnp.zeros((b, MLSTM_HEADS, MLSTM_DK, MLSTM_DV), f32),
             jnp.zeros((b, MLSTM_HEADS, MLSTM_DK), f32),
             jnp.zeros((b, MLSTM_HEADS), f32))
    gdn_c, gdn_l = bidirectional_scan(gdn_chunk_scan, gdn_chunk_scan,
                                      ctx_gdn[0], lat_gdn[0], ctx_gdn[1], lat_gdn[1], gdn_s0)
    ml_c, ml_l = bidirectional_scan(mlstm_chunk_scan, mlstm_chunk_scan,
                                    ctx_ml[0], lat_ml[0], ctx_ml[1], lat_ml[1], ml_s0)

    def merge(o_gdn, o_ml, z, og, dtype):
        a = headwise_norm(o_gdn, gdn_g) * jax.nn.silu(z.astype(f32))
        m = headwise_norm(o_ml, ml_g.reshape(MLSTM_HEADS, MLSTM_DV)) * jax.nn.sigmoid(og.astype(f32))
        return jnp.concatenate([a, m], axis=-1).astype(dtype) @ w_out

    y_lat = merge(gdn_l, ml_l, lat_z, lat_o, h_lat.dtype)
    y_ctx = None if last else merge(gdn_c, ml_c, ctx_z, ctx_o, h_ctx.dtype)
    return y_ctx, y_lat


def retention_mixer(h_ctx, h_lat, pos_ctx, pos_lat, w_in, norm_g, w_out, last):
    def prepare(h, pos):
        q, k, v, g = split_cols(h @ w_in, ODD_SPLITS)
        q = rope(to_heads(q, RET_HEADS), pos)
        k = rope(to_heads(k, RET_HEADS), pos) * RET_DK ** -0.5
        return (q, k, to_heads(v, RET_HEADS)), g

    ctx_seq, ctx_g = prepare(h_ctx, pos_ctx)
    lat_seq, lat_g = prepare(h_lat, pos_lat)
    s0 = jnp.zeros((h_lat.shape[0], RET_HEADS, RET_DK, RET_DV), jnp.float32)
    scan_f = functools.partial(retention_chunk_scan, log_gamma=retention_log_decay(False))
    scan_b = functools.partial(retention_chunk_scan, log_gamma=retention_log_decay(True))
    o_ctx, o_lat = bidirectional_scan(scan_f, scan_b, ctx_seq, lat_seq, ctx_seq, lat_seq, s0)

    def merge(o, g, dtype):
        y = headwise_norm(o, norm_g.reshape(RET_HEADS, RET_DV), center=True) * jax.nn.silu(g.astype(jnp.float32))
        return y.astype(dtype) @ w_out

    y_lat = merge(o_lat, lat_g, h_lat.dtype)
    y_ctx = None if last else merge(o_ctx, ctx_g, h_ctx.dtype)
    return y_ctx, y_lat


def moe_ffn(h, router_w, router_b, w_gu, b_gu, w_dn, b_dn):
    n, d = h.shape
    logits = (h @ router_w + router_b).astype(jnp.float32)
    top_val, top_idx = lax.top_k(logits, TOP_K)
    top_w = jax.nn.softmax(top_val, axis=-1)
    n_assign = n * TOP_K
    n_blocks = -(-n_assign // MOE_BLOCK) + N_EXPERTS
    flat_e = top_idx.reshape(-1)
    order = jnp.argsort(flat_e)
    sorted_e = flat_e[order]
    counts = jnp.bincount(flat_e, length=N_EXPERTS)
    padded = -(-counts // MOE_BLOCK) * MOE_BLOCK
    ends = jnp.cumsum(padded)
    dest = (ends - padded)[sorted_e] + jnp.arange(n_assign) - (jnp.cumsum(counts) - counts)[sorted_e]
    rows = n_blocks * MOE_BLOCK
    row_tok = jnp.zeros((rows,), jnp.int32).at[dest].set((order // TOP_K).astype(jnp.int32))
    row_w = jnp.zeros((rows,), jnp.float32).at[dest].set(top_w.reshape(-1)[order])
    block_e = jnp.minimum(jnp.searchsorted(ends, jnp.arange(n_blocks) * MOE_BLOCK, side='right'), N_EXPERTS - 1)

    def block_step(acc, xs):
        tok, wt, e = xs
        gu = h[tok] @ w_gu[e] + b_gu[e]
        gate = jnp.minimum(gu[:, 0::2], SWIGLU_LIMIT)
        up = jnp.clip(gu[:, 1::2], -SWIGLU_LIMIT, SWIGLU_LIMIT)
        act = (up + 1.0) * gate * jax.nn.sigmoid(SWIGLU_ALPHA * gate)
        y = act @ w_dn[e] + b_dn[e]
        return acc.at[tok].add(y * wt[:, None].astype(y.dtype)), None

    out, _ = lax.scan(block_step, jnp.zeros_like(h),
                      (row_tok.reshape(n_blocks, MOE_BLOCK), row_w.reshape(n_blocks, MOE_BLOCK), block_e))
    return out


def setup_inputs(seed: int = 0) -> dict:
    key = jax.random.key(seed)
    ks = iter(jax.random.split(key, 32))
    f32 = jnp.float32

    def nrm(shape, scale):
        return jax.random.normal(next(ks), shape, f32) * scale

    x = nrm((BATCH, SEQ, D_MODEL), 1.0)
    c = nrm((BATCH, D_MODEL), 1.0)
    ctx = nrm((BATCH, CTX_LEN, D_MODEL), 1.0)
    c_ctx = nrm((D_MODEL,), 1.0)
    mod_w = nrm((DEPTH, D_MODEL, 6 * D_MODEL), 0.5 * D_MODEL ** -0.5)
    mod_b = nrm((DEPTH, 6 * D_MODEL), 0.02)
    norm1_g = 1.0 + nrm((DEPTH, D_MODEL), 0.02)
    norm2_g = 1.0 + nrm((DEPTH, D_MODEL), 0.02)
    ev_w_in = nrm((N_EVEN, D_MODEL, EVEN_IN), D_MODEL ** -0.5)
    ev_conv_w = nrm((N_EVEN, CONV_W, GDN_CONV_CH), CONV_W ** -0.5)
    gdn_a_log = jnp.log(jax.random.uniform(next(ks), (N_EVEN, N_DIR, GDN_HEADS), f32, 1.0, 16.0))
    dt = jnp.exp(jax.random.uniform(next(ks), (N_EVEN, N_DIR, GDN_HEADS), f32, math.log(1e-3), math.log(1e-1)))
    gdn_dt_bias = dt + jnp.log(-jnp.expm1(-dt))
    gdn_norm_g = 1.0 + nrm((N_EVEN, GDN_DV), 0.02)
    ml_i_bias = nrm((N_EVEN, N_DIR, MLSTM_HEADS), 0.1)
    ml_f_bias = jnp.linspace(3.0, 6.0, MLSTM_HEADS, dtype=f32) + nrm((N_EVEN, N_DIR, MLSTM_HEADS), 0.1)
    ml_norm_g = 1.0 + nrm((N_EVEN, ML_V_W), 0.02)
    ev_w_out = nrm((N_EVEN, EVEN_OUT, D_MODEL), EVEN_OUT ** -0.5)
    od_w_in = nrm((N_ODD, D_MODEL, ODD_IN), D_MODEL ** -0.5)
    ret_norm_g = 1.0 + nrm((N_ODD, RET_V_W), 0.02)
    od_w_out = nrm((N_ODD, RET_V_W, D_MODEL), RET_V_W ** -0.5)
    router_w = nrm((DEPTH, D_MODEL, N_EXPERTS), D_MODEL ** -0.5)
    router_b = nrm((DEPTH, N_EXPERTS), 0.01)
    moe_w_gu = nrm((DEPTH, N_EXPERTS, D_MODEL, 2 * D_FF), D_MODEL ** -0.5)
    moe_b_gu = nrm((DEPTH, N_EXPERTS, 2 * D_FF), 0.02)
    moe_w_dn = nrm((DEPTH, N_EXPERTS, D_FF, D_MODEL), D_FF ** -0.5)
    moe_b_dn = nrm((DEPTH, N_EXPERTS, D_MODEL), 0.02)
    final_g = 1.0 + nrm((D_MODEL,), 0.02)
    return {'x': x, 'c': c, 'ctx': ctx, 'c_ctx': c_ctx, 'mod_w': mod_w, 'mod_b': mod_b,
            'norm1_g': norm1_g, 'norm2_g': norm2_g, 'ev_w_in': ev_w_in, 'ev_conv_w': ev_conv_w,
            'gdn_a_log': gdn_a_log, 'gdn_dt_bias': gdn_dt_bias, 'gdn_norm_g': gdn_norm_g,
            'ml_i_bias': ml_i_bias, 'ml_f_bias': ml_f_bias, 'ml_norm_g': ml_norm_g, 'ev_w_out': ev_w_out,
            'od_w_in': od_w_in, 'ret_norm_g': ret_norm_g, 'od_w_out': od_w_out,
            'router_w': router_w, 'router_b': router_b, 'moe_w_gu': moe_w_gu, 'moe_b_gu': moe_b_gu,
            'moe_w_dn': moe_w_dn, 'moe_b_dn': moe_b_dn, 'final_g': final_g}


def reference(x, c, ctx, c_ctx, mod_w, mod_b, norm1_g, norm2_g, ev_w_in, ev_conv_w, gdn_a_log, gdn_dt_bias,
              gdn_norm_g, ml_i_bias, ml_f_bias, ml_norm_g, ev_w_out, od_w_in, ret_norm_g, od_w_out,
              router_w, router_b, moe_w_gu, moe_b_gu, moe_w_dn, moe_b_dn, final_g):
    b, s, d = x.shape
    n_ctx_tok = ctx.shape[1]
    pos_ctx = jnp.arange(n_ctx_tok, dtype=jnp.float32)
    pos_lat = n_ctx_tok + jnp.arange(s, dtype=jnp.float32)
    x_lat, x_ctx = x, ctx
    for layer in range(DEPTH):
        last = layer == DEPTH - 1
        j = layer // 2
        sh1, sc1, g1, sh2, sc2, g2 = ada_modulation(c, mod_w[layer], mod_b[layer])
        csh1, csc1, cg1, csh2, csc2, cg2 = ada_modulation(c_ctx[None, :], mod_w[layer], mod_b[layer])
        h_lat = modulate(rms_norm(x_lat, norm1_g[layer]), sh1, sc1)
        h_ctx = modulate(rms_norm(x_ctx, norm1_g[layer]), csh1, csc1)
        if layer % 2 == 0:
            y_ctx, y_lat = gdn_mlstm_mixer(h_ctx, h_lat, ev_w_in[j], ev_conv_w[j], gdn_a_log[j], gdn_dt_bias[j],
                                           gdn_norm_g[j], ml_i_bias[j], ml_f_bias[j], ml_norm_g[j],
                                           ev_w_out[j], last)
        else:
            y_ctx, y_lat = retention_mixer(h_ctx, h_lat, pos_ctx, pos_lat, od_w_in[j], ret_norm_g[j],
                                           od_w_out[j], last)
        x_lat = x_lat + g1 * y_lat
        h_lat = modulate(rms_norm(x_lat, norm2_g[layer]), sh2, sc2)
        moe_params = (router_w[layer], router_b[layer], moe_w_gu[layer], moe_b_gu[layer],
                      moe_w_dn[layer], moe_b_dn[layer])
        if last:
            x_lat = x_lat + g2 * moe_ffn(h_lat.reshape(-1, d), *moe_params).reshape(b, s, d)
        else:
            x_ctx = x_ctx + cg1 * y_ctx
            h_ctx = modulate(rms_norm(x_ctx, norm2_g[layer]), csh2, csc2)
            tokens = jnp.concatenate([h_ctx.reshape(-1, d), h_lat.reshape(-1, d)], axis=0)
            out = moe_ffn(tokens, *moe_params)
            n_c = b * n_ctx_tok
            x_ctx = x_ctx + cg2 * out[:n_c].reshape(b, n_ctx_tok, d)
            x_lat = x_lat + g2 * out[n_c:].reshape(b, s, d)
    return rms_norm(x_lat, final_g)
```

```python
from contextlib import ExitStack
import numpy as np
import concourse.bass as bass
import concourse.mybir as mybir
from concourse.bass_utils import run_bass_kernel_spmd

F32 = mybir.dt.float32
BF16 = mybir.dt.bfloat16
I32 = mybir.dt.int32
AF = mybir.ActivationFunctionType
ALU = mybir.AluOpType
AX = mybir.AxisListType

D = 1024
NCH = 8
EPS = 1e-6
N_EXP = 32
N_DMA_SEMS = 8
DEBUG = False
SERIAL = False
DEBUG_NEXP = None
EVEN_STOP = 99
MMDT = BF16


class Prog:
    def __init__(self, nc, stack):
        self.nc = nc
        self.stack = stack
        self.nrenew = 0
        self.names = ["pe", "act", "dve", "pool", "sp"]
        self.ops = {e: [] for e in self.names}
        self.cnt = {e: 0 for e in self.names}
        self.esem = {e: stack.enter_context(nc.semaphore("s_" + e)) for e in self.names}
        self.dsem, self.dval, self.dnext = {}, {}, {}
        for q in ("sp", "pool", "act"):
            self.dsem[q] = [stack.enter_context(nc.semaphore(f"d_{q}{i}")) for i in range(N_DMA_SEMS)]
            self.dval[q] = [0] * N_DMA_SEMS
            self.dnext[q] = 0
        self.csem = [stack.enter_context(nc.semaphore("c_%d" % i)) for i in range(4)]
        self.cval = [0] * 4
        self.cnext = 0
        self.res = {}
        self.seen = {e: {} for e in self.names}
        self.sem_by_id = {}

    def _need(self, eng, toks):
        best = {}
        for t in toks:
            if t is None:
                continue
            sem, v = t
            k = id(sem)
            self.sem_by_id[k] = sem
            if v > best.get(k, 0):
                best[k] = v
        waits = []
        for k, v in best.items():
            if self.seen[eng].get(k, 0) >= v:
                continue
            self.seen[eng][k] = v
            waits.append((self.sem_by_id[k], v))
        return waits

    def _deps(self, reads, writes):
        toks = []
        for r in reads:
            e = self.res.get(r)
            if e is not None:
                toks.append(e[0])
                if r.startswith("ps"):
                    toks.extend(e[1])
        for w in writes:
            e = self.res.get(w)
            if e is not None:
                toks.append(e[0])
                toks.extend(e[1])
        return toks

    def _commit(self, tok, reads, writes):
        for r in reads:
            e = self.res.setdefault(r, [None, []])
            e[1].append(tok)
        for w in writes:
            self.res[w] = [tok, []]

    def op(self, eng, fn, reads=(), writes=()):
        toks = self._deps(reads, writes)
        if eng == "pe":
            toks = [t for t in toks if t is None or t[0] is not self.esem["pe"]]
        waits = self._need(eng, toks)
        self.cnt[eng] += 1
        tok = (self.esem[eng], self.cnt[eng])
        self.ops[eng].append((fn, waits, (self.esem[eng], 1)))
        self._commit(tok, reads, writes)
        if SERIAL:
            self.barrier()
        return tok

    def dma(self, q, fn, reads=(), writes=()):
        toks = self._deps(reads, writes)
        i = self.dnext[q]
        self.dnext[q] = (i + 1) % N_DMA_SEMS
        sem = self.dsem[q][i]
        if self.dval[q][i] > 0:
            toks.append((sem, self.dval[q][i]))
        waits = self._need(q, toks)
        self.dval[q][i] += 16
        tok = (sem, self.dval[q][i])
        self.ops[q].append((fn, waits, (sem, 16)))
        self._commit(tok, reads, writes)
        if SERIAL:
            self.barrier()
        return tok

    def coll(self, fn, reads=(), writes=()):
        toks = self._deps(reads, writes)
        i = self.cnext
        self.cnext = (i + 1) % len(self.csem)
        sem = self.csem[i]
        if self.cval[i] > 0:
            toks.append((sem, self.cval[i]))
        waits = self._need("pool", toks)
        self.cval[i] += 1
        tok = (sem, self.cval[i])
        self.ops["pool"].append((fn, waits, (sem, None)))
        self._commit(tok, reads, writes)
        return tok

    def _all_dma_toks(self):
        toks = []
        for q in self.dsem:
            for i, sm in enumerate(self.dsem[q]):
                if self.dval[q][i] > 0:
                    toks.append((sm, self.dval[q][i]))
        for i, sm in enumerate(self.csem):
            if self.cval[i] > 0:
                toks.append((sm, self.cval[i]))
        return toks

    def barrier(self):
        toks = [(self.esem[e], self.cnt[e]) for e in self.names if self.cnt[e] > 0]
        toks += self._all_dma_toks()
        for e in self.names:
            waits = self._need(e, toks)
            if waits:
                self.ops[e].append((None, waits, None))
        self.res = {}
        for e in self.names:
            if self.cnt[e] > 12000:
                self.nrenew += 1
                self.esem[e] = self.stack.enter_context(self.nc.semaphore("s_%s_%d" % (e, self.nrenew)))
                self.cnt[e] = 0

    def finish(self):
        toks = self._all_dma_toks()
        waits = self._need("sp", toks)
        self.ops["sp"].append((None, waits, None))

    def emit(self):
        nc = self.nc
        with nc.Block() as block:
            def run(ename):
                def body(eng):
                    for fn, waits, inc in self.ops[ename]:
                        for sem, v in waits:
                            eng.wait_ge(sem, v)
                        if fn is not None:
                            if inc[1] is None:
                                fn(eng).then_inc(inc[0])
                            else:
                                fn(eng).then_inc(inc[0], inc[1])
                return body
            block.tensor(run("pe"))
            block.scalar(run("act"))
            block.vector(run("dve"))
            block.gpsimd(run("pool"))
            block.sync(run("sp"))


class Ctx:
    def __init__(self, nc, st):
        self.nc, self.st = nc, st
        self.P = Prog(nc, st)
        self.psn = 0
        self.sfx = ""

    def sb(self, name, shape, dt=F32):
        return self.st.enter_context(self.nc.sbuf_tensor("sb_" + name + self.sfx, shape, dt))

    def ps(self, name):
        return self.st.enter_context(self.nc.psum_tensor(name, [128, 512], F32))


class Env:
    def __init__(self, nc, C):
        self.nc, self.C = nc, C
        self.sfx = ""
        self.over = {}
        self.xload = None
        self.ywrite = None


def _din_factory(nc, env):
    sfx = env.sfx if env else ""

    def din(name, shape, dt=F32):
        if env is not None and name in env.over:
            return env.over[name]
        return nc.dram_tensor(name + sfx, shape, dt, kind="ExternalInput").ap()
    return din


def _begin(env, st, masks):
    if env is not None:
        C = env.C
        C.st = st
        C.sfx = env.sfx
        return C
    C = Ctx(env_nc_holder[0], st)
    emit_consts(C)
    if masks:
        emit_masks(C)
    C.pss_all = [(C.ps("ps%d" % i), "ps%d" % i) for i in range(8)]
    return C


env_nc_holder = [None]


def token_tiles(n_lat, n_ctx):
    tiles = []
    for s in range(0, n_lat, 512):
        tiles.append((s, min(512, n_lat - s), 0))
    for s in range(0, n_ctx, 512):
        tiles.append((n_lat + s, min(512, n_ctx - s), 1))
    return tiles


def emit_consts(C):
    P, nc = C.P, C.nc
    C.ones = C.sb("ones", [128, 128])
    C.ident = C.sb("ident", [128, 128])
    P.op("pool", lambda e: e.memset(C.ones[:], 1.0), writes=["ones"])
    P.op("pool", lambda e: e.memset(C.ident[:], 1.0), writes=["ident"])
    P.op("pool", lambda e: e.affine_select(out=C.ident[:], in_=C.ident[:], pattern=[[-1, 128]],
                                            compare_op=ALU.is_equal, fill=0.0, base=0, channel_multiplier=1),
         reads=["ident"], writes=["ident"])


def emit_mod(C, cv_d, modw_d, modb_d, ncols, psb, pskey):
    P, nc = C.P, C.nc
    nj = ncols // 128
    cv = C.sb("cv", [128, NCH, 2])
    sc = C.sb("sc", [128, NCH, 2])
    modb = C.sb("modb", [128, nj])
    modv = C.sb("modv", [128, nj, 2])
    P.dma("sp", lambda e: e.dma_start(out=cv[:], in_=cv_d), writes=["cv"])
    P.dma("sp", lambda e: e.dma_start(out=modb[:], in_=modb_d), writes=["modb"])
    P.op("act", lambda e: e.activation(out=sc[:], in_=cv[:], func=AF.Silu), reads=["cv"], writes=["sc"])
    wbufs = [C.sb("modw%d" % i, [128, NCH, 512]) for i in range(2)]
    mw = modw_d.rearrange("(c p) n -> p c n", p=128)
    for blk in range(ncols // 512):
        wb = wbufs[blk % 2]
        key = "modw%d" % (blk % 2)
        P.dma("sp", (lambda wb, blk: lambda e: e.dma_start(out=wb[:], in_=mw[:, :, blk * 512:(blk + 1) * 512]))(wb, blk),
              writes=[key])
        for jj in range(4):
            j = blk * 4 + jj
            for c in range(NCH):
                P.op("pe", (lambda wb, jj, c, j: lambda e: e.matmul(psb[:, 2 * j:2 * j + 2], lhsT=wb[:, c, jj * 128:(jj + 1) * 128],
                                                                      rhs=sc[:, c, :], start=(c == 0), stop=(c == NCH - 1)))(wb, jj, c, j),
                     reads=[key, "sc"], writes=[pskey])
    for w in range(2):
        P.op("dve", (lambda w: lambda e: e.tensor_tensor(out=modv[:, :, w], in0=psb[:, w:2 * nj:2], in1=modb[:], op=ALU.add))(w),
             reads=[pskey, "modb"], writes=["modv"])
    return modv


def emit_rstd(C, src, srckey, N, sq, sqkey, psb, pskey, rstd, rstdkey):
    P = C.P
    for c in range(NCH):
        P.op("act", (lambda c: lambda e: e.activation(out=sq[:, c, :N], in_=src[:, c, :N], func=AF.Square))(c), reads=[srckey], writes=[sqkey])
    for c in range(NCH):
        P.op("pe", (lambda c: lambda e: e.matmul(psb[:, :N], lhsT=C.ones[:], rhs=sq[:, c, :N], start=(c == 0), stop=(c == NCH - 1)))(c),
             reads=["ones", sqkey], writes=[pskey])
    P.op("act", lambda e: e.activation(out=rstd[:, :N], in_=psb[:, :N], func=AF.Sqrt, bias=EPS, scale=1.0 / D),
         reads=[pskey], writes=[rstdkey])
    P.op("dve", lambda e: e.reciprocal(out=rstd[:, :N], in_=rstd[:, :N]), reads=[rstdkey], writes=[rstdkey])


def build_ffn(n_lat, n_ctx, last, env=None):
    NT = n_lat + n_ctx
    tiles = token_tiles(n_lat, n_ctx)
    sfx = env.sfx if env else ""
    nc = env.nc if env else bass.Bass("TRN2", target_bir_lowering=False)
    env_nc_holder[0] = nc
    dt_in = _din_factory(nc, env)
    xT_d = dt_in("xT", [D, NT]); ypa_d = dt_in("ypa", [D, NT]); ypb_d = dt_in("ypb", [D, NT])
    cv_d = dt_in("cv", [128, NCH, 2]); modw_d = dt_in("modw", [D, 4096]); modb_d = dt_in("modb", [128, 32])
    n2g_d = dt_in("n2g", [128, NCH]); fing_d = dt_in("fing", [128, NCH])
    rw_d = dt_in("rw", [D, N_EXP]); rb_d = dt_in("rb", [1, N_EXP])
    wgu_d = dt_in("wgu", [N_EXP, D, 2 * D]); bgu_d = dt_in("bgu", [128, N_EXP, NCH, 2])
    wdn_d = dt_in("wdn", [N_EXP, D, D]); bdn_d = dt_in("bdn", [128, N_EXP, NCH])
    if env is not None and "out" in env.over:
        out_d = env.over["out"]
    else:
        out_d = nc.dram_tensor("out", [D, NT], F32, kind="ExternalOutput").ap()
    xmid_d = nc.dram_tensor("xmid_scr" + sfx, [D, NT], F32, kind="ExternalOutput" if DEBUG else "Internal").ap()
    if DEBUG:
        gwT_dbg = nc.dram_tensor("gwT_dbg", [N_EXP, NT], F32, kind="ExternalOutput").ap()
        acc_dbg = nc.dram_tensor("acc_dbg", [D, NT], F32, kind="ExternalOutput").ap()
        h2_dbg = nc.dram_tensor("h2_dbg", [D, NT], F32, kind="ExternalOutput").ap()
        modv_dbg = nc.dram_tensor("modv_dbg", [128, 64], F32, kind="ExternalOutput").ap()
    r3 = lambda ap: ap.rearrange("(c p) n -> p c n", p=128)

    with ExitStack() as st:
        C = _begin(env, st, False)
        P = C.P
        psA = [C.pss_all[i][0] for i in (0, 1)]
        psB = [C.pss_all[i][0] for i in (2, 3)]
        psY = [C.pss_all[i][0] for i in (4, 5)]
        psM = [C.pss_all[i][0] for i in (6, 7)]
        h2b = C.sb("h2b", [128, NCH, NT], MMDT)
        gwT = C.sb("gwT", [N_EXP, NT])
        n2g = C.sb("n2g", [128, NCH]); fing = C.sb("fing", [128, NCH])
        A2 = C.sb("A2", [128, NCH, 2])
        g2v = C.sb("g2v", [128, NCH, 2])
        tmp32 = C.sb("tmp32", [N_EXP, 512])
        rw = C.sb("rw", [128, NCH, N_EXP]); rb = C.sb("rb", [1, N_EXP])
        bgu = C.sb("bgu", [128, N_EXP, NCH, 2]); bdn = C.sb("bdn", [128, N_EXP, NCH])
        P.dma("sp", lambda e: e.dma_start(out=n2g[:], in_=n2g_d), writes=["n2g"])
        P.dma("sp", lambda e: e.dma_start(out=fing[:], in_=fing_d), writes=["fing"])
        P.dma("sp", lambda e: e.dma_start(out=rw[:], in_=r3(rw_d)), writes=["rw"])
        P.dma("sp", lambda e: e.dma_start(out=rb[:], in_=rb_d), writes=["rb"])
        P.dma("sp", lambda e: e.dma_start(out=bgu[:], in_=bgu_d), writes=["bgu"])
        P.dma("sp", lambda e: e.dma_start(out=bdn[:], in_=bdn_d), writes=["bdn"])

        with ExitStack() as st2:
            C.st = st2
            modv = emit_mod(C, cv_d, modw_d, modb_d, 4096, psM[0], "psM0")
            P.op("dve", lambda e: e.tensor_copy(out=g2v[:], in_=modv[:, 24:32, :]), reads=["modv"], writes=["g2v"])
            for w in range(2):
                P.op("dve", (lambda w: lambda e: e.scalar_tensor_tensor(out=A2[:, :, w], in0=modv[:, 16:24, w], scalar=1.0, in1=n2g[:],
                                                                         op0=ALU.add, op1=ALU.mult))(w),
                     reads=["modv", "n2g"], writes=["A2"])
            xt = [C.sb("xt%d" % i, [128, NCH, 512]) for i in range(2)]
            ya = [C.sb("ya0", [128, NCH, 512])] * 2
            yb = [C.sb("yb0", [128, NCH, 512])] * 2
            sq = C.sb("sq", [128, NCH, 512])
            rstd = C.sb("rstd", [128, 512])
            h2f = C.sb("h2f", [128, NCH, 512])
            lg = C.sb("lg", [128, N_EXP]); top8 = C.sb("top8", [128, 8]); negm = C.sb("negm", [128, 1])
            exl = C.sb("exl", [128, N_EXP]); msk = C.sb("msk", [128, N_EXP]); den = C.sb("den", [128, 1])
            gw = C.sb("gw", [128, N_EXP])
            for ti, (t0, N, w) in enumerate(tiles):
                b = ti % 2
                kx, ka, kb = "xt%d" % b, "ya0", "yb0"
                P.dma("sp", (lambda b, t0, N: lambda e: e.dma_start(out=xt[b][:, :, :N], in_=r3(xT_d)[:, :, t0:t0 + N]))(b, t0, N), writes=[kx])
                P.dma("sp", (lambda b, t0, N: lambda e: e.dma_start(out=ya[b][:, :, :N], in_=r3(ypa_d)[:, :, t0:t0 + N]))(b, t0, N), writes=[ka])
                P.dma("sp", (lambda b, t0, N: lambda e: e.dma_start(out=yb[b][:, :, :N], in_=r3(ypb_d)[:, :, t0:t0 + N]))(b, t0, N), writes=[kb])
                P.op("pool", (lambda b, N: lambda e: e.tensor_tensor(out=ya[b][:, :, :N], in0=ya[b][:, :, :N], in1=yb[b][:, :, :N], op=ALU.add))(b, N),
                     reads=[ka, kb], writes=[ka])
                for c in range(NCH):
                    P.op("dve", (lambda b, N, c, w: lambda e: e.scalar_tensor_tensor(out=xt[b][:, c, :N], in0=ya[b][:, c, :N], scalar=modv[:, c, w:w + 1],
                                                                                      in1=xt[b][:, c, :N], op0=ALU.mult, op1=ALU.add))(b, N, c, w),
                         reads=[ka, kx, "modv"], writes=[kx])
                P.dma("sp", (lambda b, t0, N: lambda e: e.dma_start(out=r3(xmid_d)[:, :, t0:t0 + N], in_=xt[b][:, :, :N]))(b, t0, N),
                      reads=[kx], writes=["xmid_d%d" % ti])
                emit_rstd(C, xt[b], kx, N, sq, "sq", psM[1], "psM1", rstd, "rstd")
                for c in range(NCH):
                    P.op("pool", (lambda b, N, c: lambda e: e.tensor_tensor(out=h2f[:, c, :N], in0=xt[b][:, c, :N], in1=rstd[:, :N], op=ALU.mult))(b, N, c),
                         reads=[kx, "rstd"], writes=["h2f"])
                    P.op("dve", (lambda N, c, w: lambda e: e.tensor_scalar(out=h2f[:, c, :N], in0=h2f[:, c, :N], scalar1=A2[:, c, w:w + 1],
                                                                            scalar2=modv[:, 8 + c, w:w + 1], op0=ALU.mult, op1=ALU.add))(N, c, w),
                         reads=["h2f", "A2", "modv"], writes=["h2f"])
                for c in range(NCH):
                    P.op("act", (lambda t0, N, c: lambda e: e.copy(out=h2b[:, c, t0:t0 + N], in_=h2f[:, c, :N]))(t0, N, c), reads=["h2f"], writes=["h2b"])
                if DEBUG:
                    P.dma("sp", (lambda t0, N: lambda e: e.dma_start(out=r3(h2_dbg)[:, :, t0:t0 + N], in_=h2f[:, :, :N]))(t0, N), reads=["h2f"], writes=["dbgh%d" % ti])
                    if ti == 0:
                        P.dma("sp", lambda e: e.dma_start(out=modv_dbg, in_=modv[:].rearrange("p j w -> p (j w)")), reads=["modv"], writes=["dbgm"])
                        a2_dbg = nc.dram_tensor("a2_dbg", [128, 16], F32, kind="ExternalOutput").ap()
                        P.dma("sp", lambda e: e.dma_start(out=a2_dbg, in_=A2[:].rearrange("p j w -> p (j w)")), reads=["A2"], writes=["dbga2"])
                        rstd_dbg = nc.dram_tensor("rstd_dbg", [128, 512], F32, kind="ExternalOutput").ap()
                        P.dma("sp", lambda e: e.dma_start(out=rstd_dbg, in_=rstd[:]), reads=["rstd"], writes=["dbgr"])
                        sq_dbg = nc.dram_tensor("sq_dbg", [128, NCH, 512], F32, kind="ExternalOutput").ap()
                        P.dma("sp", lambda e: e.dma_start(out=sq_dbg, in_=sq[:]), reads=["sq"], writes=["dbgsq"])
                for blk in range(N // 128):
                    o = blk * 128
                    for c in range(NCH):
                        P.op("pe", (lambda o, c: lambda e: e.matmul(psM[0][:, 0:N_EXP], lhsT=h2f[:, c, o:o + 128], rhs=rw[:, c, :], start=(c == 0), stop=False))(o, c),
                             reads=["h2f", "rw"], writes=["psM0"])
                    P.op("pe", lambda e: e.matmul(psM[0][:, 0:N_EXP], lhsT=C.ones[0:1, :], rhs=rb[:], start=False, stop=True),
                         reads=["ones", "rb"], writes=["psM0"])
                    P.op("dve", lambda e: e.tensor_copy(out=lg[:], in_=psM[0][:, 0:N_EXP]), reads=["psM0"], writes=["lg"])
                    P.op("dve", lambda e: e.max(out=top8[:], in_=lg[:]), reads=["lg"], writes=["top8"])
                    P.op("dve", lambda e: e.tensor_scalar(out=negm[:], in0=top8[:, 0:1], scalar1=-1.0, scalar2=None, op0=ALU.mult),
                         reads=["top8"], writes=["negm"])
                    P.op("act", lambda e: e.activation(out=exl[:], in_=lg[:], func=AF.Exp, bias=negm[:], scale=1.0),
                         reads=["lg", "negm"], writes=["exl"])
                    P.op("dve", lambda e: e.tensor_scalar(out=msk[:], in0=lg[:], scalar1=top8[:, 3:4], scalar2=None, op0=ALU.is_ge),
                         reads=["lg", "top8"], writes=["msk"])
                    P.op("dve", lambda e: e.tensor_tensor(out=exl[:], in0=exl[:], in1=msk[:], op=ALU.mult), reads=["exl", "msk"], writes=["exl"])
                    P.op("dve", lambda e: e.reduce_sum(out=den[:], in_=exl[:], axis=AX.X), reads=["exl"], writes=["den"])
                    P.op("dve", lambda e: e.reciprocal(out=den[:], in_=den[:]), reads=["den"], writes=["den"])
                    P.op("dve", lambda e: e.tensor_scalar(out=gw[:], in0=exl[:], scalar1=den[:, 0:1], scalar2=None, op0=ALU.mult),
                         reads=["exl", "den"], writes=["gw"])
                    P.op("pe", lambda e: e.transpose(out=psM[1][0:N_EXP, 0:128], in_=gw[:], identity=C.ident[:]), reads=["gw", "ident"], writes=["psM1"])
                    P.op("act", (lambda t0, o: lambda e: e.copy(out=gwT[:, t0 + o:t0 + o + 128], in_=psM[1][0:N_EXP, 0:128]))(t0, o),
                         reads=["psM1"], writes=["gwT"])
            P.barrier()
        C.st = st

        stacc = st.enter_context(ExitStack())
        C.st = stacc
        acc = C.sb("acc", [128, NCH, NT])
        P.op("pool", lambda e: e.memset(acc[:], 0.0), writes=["acc"])
        with ExitStack() as st3:
            C.st = st3
            NRING = 4
            wgu_r = [C.sb("wgu_r%d" % i, [128, NCH, 256], MMDT) for i in range(NRING)]
            wdn_r = [C.sb("wdn_r%d" % i, [128, NCH, 128], MMDT) for i in range(NRING)]
            actT = C.sb("actT", [128, NCH, NT], MMDT)
            gwb = C.sb("gwb", [128, NT])
            gt = [C.sb("gt%d" % i, [128, 512]) for i in range(2)]
            ut = [C.sb("ut%d" % i, [128, 512]) for i in range(2)]
            sg = [C.sb("sg%d" % i, [128, 512]) for i in range(2)]
            yt = [C.sb("yt%d" % i, [128, 512]) for i in range(2)]
            wguv = wgu_d.rearrange("e (c p) n -> e p c n", p=128)
            wdnv = wdn_d.rearrange("e (c p) n -> e p c n", p=128)
            pg = 0
            pd = 0
            it = 0
            for ex in range(N_EXP if DEBUG_NEXP is None else DEBUG_NEXP):
                for ti, (t0, N, w) in enumerate(tiles):
                    _ts(C, "dve", tmp32[:, :N], gwT[:, t0:t0 + N], C.ident[0:N_EXP, ex:ex + 1], None, ALU.mult, None, ["gwT", "ident"], ["tmp32"])
                    _mm(C, psM[0][:, :N], C.ones[0:N_EXP, :], tmp32[:, :N], True, True, ["ones", "tmp32"], ["psM0"])
                    _cp(C, "act", gwb[:, t0:t0 + N], psM[0][:, :N], ["psM0"], ["gwb%d" % ti])
                for fc in range(NCH):
                    rg_ = pg % NRING
                    pg += 1
                    kw = "wgu_r%d" % rg_
                    _dma(C, "pool", wgu_r[rg_][:], wguv[ex, :, :, fc * 256:(fc + 1) * 256], [], [kw])
                    for ti, (t0, N, w) in enumerate(tiles):
                        pb = it % 2
                        it += 1
                        ka, kb = "psA%d" % pb, "psB%d" % pb
                        for two, (pst, kk) in enumerate(((psA[pb], ka), (psB[pb], kb))):
                            for c in range(NCH):
                                _mm(C, pst[:, :N], wgu_r[rg_][:, c, two:256:2], h2b[:, c, t0:t0 + N], c == 0, c == NCH - 1, [kw, "h2b"], [kk])
                        kg, ku, ks = "gt%d" % pb, "ut%d" % pb, "sg%d" % pb
                        _act(C, ut[pb][:, :N], psB[pb][:, :N], AF.Identity, [kb], [ku], bias=bgu[:, ex, fc, 1:2])
                        _ts(C, "dve", gt[pb][:, :N], psA[pb][:, :N], bgu[:, ex, fc, 0:1], 7.0, ALU.add, ALU.min, [ka, "bgu"], [kg])
                        _act(C, sg[pb][:, :N], gt[pb][:, :N], AF.Sigmoid, [kg], [ks], scale=1.702)
                        _ts(C, "dve", ut[pb][:, :N], ut[pb][:, :N], -7.0, 7.0, ALU.max, ALU.min, [ku], [ku])
                        _tt(C, "pool", gt[pb][:, :N], gt[pb][:, :N], sg[pb][:, :N], ALU.mult, [kg, ks], [kg])
                        _stt(C, actT[:, fc, t0:t0 + N], ut[pb][:, :N], 1.0, gt[pb][:, :N], ALU.add, ALU.mult, [ku, kg], ["actT%d_%d" % (fc, ti)])
                for dmc in range(NCH):
                    rd_ = pd % NRING
                    pd += 1
                    kw = "wdn_r%d" % rd_
                    _dma(C, "pool", wdn_r[rd_][:], wdnv[ex, :, :, dmc * 128:(dmc + 1) * 128], [], [kw])
                    for ti, (t0, N, w) in enumerate(tiles):
                        pb = it % 2
                        it += 1
                        ky, kyt = "psY%d" % pb, "yt%d" % pb
                        for fc in range(NCH):
                            _mm(C, psY[pb][:, :N], wdn_r[rd_][:, fc, :], actT[:, fc, t0:t0 + N], fc == 0, fc == NCH - 1, [kw, "actT%d_%d" % (fc, ti)], [ky])
                        _stt(C, yt[pb][:, :N], psY[pb][:, :N], bdn[:, ex, dmc:dmc + 1], gwb[:, t0:t0 + N], ALU.add, ALU.mult, [ky, "bdn", "gwb%d" % ti], [kyt])
                        _tt(C, "pool" if (dmc + ti) % 2 else "dve", acc[:, dmc, t0:t0 + N], acc[:, dmc, t0:t0 + N], yt[pb][:, :N], ALU.add, [kyt, "acc", "acc%d_%d" % (dmc, ti)], ["acc%d_%d" % (dmc, ti)])
            P.barrier()
        C.st = st
        if DEBUG:
            P.dma("sp", lambda e: e.dma_start(out=gwT_dbg, in_=gwT[:]), writes=["dbg1"])
            P.dma("sp", lambda e: e.dma_start(out=r3(acc_dbg), in_=acc[:]), writes=["dbg2"])
        with ExitStack() as st4:
            C.st = st4
            xm = [C.sb("xm%d" % i, [128, NCH, 512]) for i in range(2)]
            sqD = C.sb("sq2", [128, NCH, 512])
            rstdD = C.sb("rstd2", [128, 512])
            for ti, (t0, N, w) in enumerate(tiles):
                b = ti % 2
                kx = "xm%d" % b
                P.dma("sp", (lambda b, t0, N: lambda e: e.dma_start(out=xm[b][:, :, :N], in_=r3(xmid_d)[:, :, t0:t0 + N]))(b, t0, N), writes=[kx])
                for c in range(NCH):
                    P.op("dve", (lambda b, N, c, w, t0: lambda e: e.scalar_tensor_tensor(out=xm[b][:, c, :N], in0=acc[:, c, t0:t0 + N], scalar=g2v[:, c, w:w + 1],
                                                                                          in1=xm[b][:, c, :N], op0=ALU.mult, op1=ALU.add))(b, N, c, w, t0),
                         reads=[kx], writes=[kx])
                if last:
                    emit_rstd(C, xm[b], kx, N, sqD, "sq2", psM[1], "psM1", rstdD, "rstd2")
                    for c in range(NCH):
                        P.op("dve", (lambda b, N, c: lambda e: e.scalar_tensor_tensor(out=xm[b][:, c, :N], in0=xm[b][:, c, :N], scalar=fing[:, c:c + 1],
                                                                                       in1=rstdD[:, :N], op0=ALU.mult, op1=ALU.mult))(b, N, c),
                             reads=[kx, "rstd2"], writes=[kx])
                P.dma("sp", (lambda b, t0, N: lambda e: e.dma_start(out=r3(out_d)[:, :, t0:t0 + N], in_=xm[b][:, :, :N]))(b, t0, N),
                      reads=[kx], writes=["out%d" % ti])
            if env is None:
                P.finish()
            else:
                P.barrier()
        if env is None:
            P.emit()
    return nc


def ffn_inputs(layer, inputs, xT, ypa, ypb, b):
    pc = lambda v: np.ascontiguousarray(v.reshape(-1, 128).T)
    cv = np.stack([pc(inputs["c"][b]), pc(inputs["c_ctx"])], axis=-1)
    bgu = np.ascontiguousarray(inputs["moe_b_gu"][layer].reshape(N_EXP, NCH, 128, 2).transpose(2, 0, 1, 3))
    bdn = np.ascontiguousarray(inputs["moe_b_dn"][layer].reshape(N_EXP, NCH, 128).transpose(2, 0, 1))
    return {
        "xT": None if xT is None else np.ascontiguousarray(xT), "ypa": None if ypa is None else np.ascontiguousarray(ypa),
        "ypb": None if ypb is None else np.ascontiguousarray(ypb),
        "cv": np.ascontiguousarray(cv),
        "modw": np.ascontiguousarray(inputs["mod_w"][layer][:, 2048:6144]),
        "modb": pc(inputs["mod_b"][layer][2048:6144]),
        "n2g": pc(inputs["norm2_g"][layer]), "fing": pc(inputs["final_g"]),
        "rw": np.ascontiguousarray(inputs["router_w"][layer]), "rb": np.ascontiguousarray(inputs["router_b"][layer][None, :]),
        "wgu": np.ascontiguousarray(inputs["moe_w_gu"][layer]), "bgu": bgu,
        "wdn": np.ascontiguousarray(inputs["moe_w_dn"][layer]), "bdn": bdn,
    }


def _mm(C, out, lhsT, rhs, start, stop, r, w):
    C.P.op("pe", lambda e: e.matmul(out, lhsT=lhsT, rhs=rhs, start=start, stop=stop), reads=r, writes=w)


def _tr(C, out, in_, r, w):
    ident = C.ident[0:in_.shape[0], 0:in_.shape[0]]
    C.P.op("pe", lambda e: e.transpose(out=out, in_=in_, identity=ident), reads=list(r) + ["ident"], writes=w)


def _ts(C, eng, out, in0, s1, s2, op0, op1, r, w):
    if s2 is None:
        C.P.op(eng, lambda e: e.tensor_scalar(out=out, in0=in0, scalar1=s1, scalar2=None, op0=op0), reads=r, writes=w)
    else:
        C.P.op(eng, lambda e: e.tensor_scalar(out=out, in0=in0, scalar1=s1, scalar2=s2, op0=op0, op1=op1), reads=r, writes=w)


def _tt(C, eng, out, in0, in1, op, r, w):
    C.P.op(eng, lambda e: e.tensor_tensor(out=out, in0=in0, in1=in1, op=op), reads=r, writes=w)


def _stt(C, out, in0, scalar, in1, op0, op1, r, w):
    C.P.op("dve", lambda e: e.scalar_tensor_tensor(out=out, in0=in0, scalar=scalar, in1=in1, op0=op0, op1=op1), reads=r, writes=w)


def _act(C, out, in_, func, r, w, bias=None, scale=1.0):
    if bias is None:
        C.P.op("act", lambda e: e.activation(out=out, in_=in_, func=func, scale=scale), reads=r, writes=w)
    else:
        C.P.op("act", lambda e: e.activation(out=out, in_=in_, func=func, bias=bias, scale=scale), reads=r, writes=w)


def _cp(C, eng, out, in_, r, w):
    if eng == "act":
        C.P.op("act", lambda e: e.copy(out=out, in_=in_), reads=r, writes=w)
    else:
        C.P.op(eng, lambda e: e.tensor_copy(out=out, in_=in_), reads=r, writes=w)


def _dma(C, q, out, in_, r, w):
    C.P.dma(q, lambda e: e.dma_start(out=out, in_=in_), reads=r, writes=w)


T_ALL = 4352
NB = 34
N_CTXB = 2


def scan_order(direction):
    if direction == 0:
        return list(range(NB))
    return [1, 0] + list(range(NB - 1, N_CTXB - 1, -1))


def emit_masks(C):
    P = C.P
    C.masks = {}
    for name, op, sgn in (("F", ALU.is_ge, 1), ("Fs", ALU.is_gt, 1), ("B", ALU.is_ge, -1), ("Bs", ALU.is_gt, -1)):
        m = C.sb("mask" + name, [128, 128])
        P.op("pool", (lambda m: lambda e: e.memset(m[:], 1.0))(m), writes=["mask" + name])
        P.op("pool", (lambda m, op, sgn: lambda e: e.affine_select(out=m[:], in_=m[:], pattern=[[sgn, 128]], compare_op=op, fill=0.0,
                                                               base=0, channel_multiplier=-sgn))(m, op, sgn),
             reads=["mask" + name], writes=["mask" + name])
        C.masks[name] = m
    for name, row in (("sel0", 0), ("sel127", 127)):
        m = C.sb(name, [128, 128])
        P.op("pool", (lambda m: lambda e: e.memset(m[:], 1.0))(m), writes=[name])
        P.op("pool", (lambda m, row: lambda e: e.affine_select(out=m[:], in_=m[:], pattern=[[0, 128]], compare_op=ALU.is_equal, fill=0.0,
                                                                base=-row, channel_multiplier=1))(m, row),
             reads=[name], writes=[name])
        C.masks[name] = m


def emit_stream_tables(C, g, colb, direction, tabs, psb, pskey, sid):
    P = C.P
    kf, rowf, ctab, gF, gL, tmp = tabs
    first, last = ("sel0", "sel127") if direction == 0 else ("sel127", "sel0")
    kk = "tabs%s" % sid
    _mm(C, psb[:, 0:NB], C.masks[first][:], g[:], True, True, [first, "g" + sid], [pskey])
    _cp(C, "dve", gF[:], psb[:, 0:NB], [pskey], [kk + "gF"])
    _mm(C, psb[:, 0:NB], C.masks[last][:], g[:], True, True, [last, "g" + sid], [pskey])
    _cp(C, "dve", gL[:], psb[:, 0:NB], [pskey], [kk + "gL"])
    _tt(C, "dve", kf[:], gL[:], colb[:], ALU.add, [kk + "gL", "colb" + sid], [kk + "kf"])
    _ts(C, "dve", kf[:], kf[:], 0.0, None, ALU.min, None, [kk + "kf"], [kk + "kf"])
    _act(C, kf[:], kf[:], AF.Exp, [kk + "kf"], [kk + "kf"])
    _tt(C, "dve", rowf[:], g[:], gF[:], ALU.subtract, ["g" + sid, kk + "gF"], [kk + "rowf"])
    _ts(C, "dve", rowf[:], rowf[:], 0.0, None, ALU.min, None, [kk + "rowf"], [kk + "rowf"])
    _act(C, rowf[:], rowf[:], AF.Exp, [kk + "rowf"], [kk + "rowf"])
    _tt(C, "dve", ctab[:], gF[:, :, None].to_broadcast([128, NB, NB]), gL[:, None, :].to_broadcast([128, NB, NB]), ALU.subtract,
        [kk + "gF", kk + "gL"], [kk + "ctab"])
    for I in range(NB):
        _ts(C, "dve", ctab[:, I, :], ctab[:, I, :], 0.0, None, ALU.min, None, [kk + "ctab"], [kk + "ctab"])
        _act(C, ctab[:, I, :], ctab[:, I, :], AF.Exp, [kk + "ctab"], [kk + "ctab"])


def emit_diag_E(C, g, colb, I, direction, strict, Et, etkey, dg, psb, pskey, sid):
    mk = C.masks[("F" if direction == 0 else "B") + ("s" if strict else "")]
    mkey = "mask" + ("F" if direction == 0 else "B") + ("s" if strict else "")
    _ts(C, "dve", dg[:], C.ident[:], g[:, I:I + 1], None, ALU.mult, None, ["ident", "g" + sid], ["dg"])
    _mm(C, psb[:, 0:128], C.ones[:], dg[:], True, True, ["ones", "dg"], [pskey])
    _ts(C, "dve", Et[:], psb[:, 0:128], colb[:, I:I + 1], 0.0, ALU.add, ALU.min, [pskey, "colb" + sid], [etkey])
    _act(C, Et[:], Et[:], AF.Exp, [etkey], [etkey])
    _tt(C, "pool", Et[:], Et[:], mk[:], ALU.mult, [etkey, mkey], [etkey])


def emit_attn_block(C, I, order_pos, order, AT, BT, VP, Vraw, dvp, tabs, g, colb, direction, bufs, sid, out, outkey, skip_off_rowscale=None):
    kf, rowf, ctab, gF, gL, tmp = tabs
    psS, psO, psD, psR, wts, Et, dg, offb = bufs
    before = order[:order_pos]
    isl = slice(I * 128, (I + 1) * 128)
    kk = "tabs%s" % sid
    for n, J in enumerate(before):
        sb_i = C.rot % len(psS)
        C.rot += 1
        ps, pk = psS[sb_i]
        wt, wk = wts[sb_i % len(wts)]
        _mm(C, ps[:, 0:128], AT[:, J * 128:(J + 1) * 128], BT[:, isl], True, True, ["AT" + sid, "BT" + sid], [pk])
        _ts(C, "dve", wt[:], ps[:, 0:128], ctab[:, I, J:J + 1], None, ALU.mult, None, [pk, kk + "ctab"], [wk])
        _mm(C, psO[0][:, 0:dvp], wt[:], VP[:, J, :], n == 0, n == len(before) - 1, [wk, "VP" + sid], [psO[1]])
    if before:
        _ts(C, "dve", offb[:, 0:dvp], psO[0][:, 0:dvp], rowf[:, I:I + 1], None, ALU.mult, None, [psO[1], kk + "rowf"], ["offb"])
    emit_diag_E(C, g, colb, I, direction, False, Et, "Et", dg, psR[0], psR[1], sid)
    sb_i = C.rot % len(psS)
    C.rot += 1
    ps, pk = psS[sb_i]
    wt, wk = wts[sb_i % len(wts)]
    _mm(C, ps[:, 0:128], AT[:, isl], BT[:, isl], True, True, ["AT" + sid, "BT" + sid], [pk])
    _tt(C, "dve", wt[:], ps[:, 0:128], Et[:], ALU.mult, [pk, "Et"], [wk])
    _mm(C, psD[0][:, 0:dvp], wt[:], Vraw[:, I, :], True, True, [wk, "Vraw" + sid], [psD[1]])
    if before:
        _tt(C, "dve", out, psD[0][:, 0:dvp], offb[:, 0:dvp], ALU.add, [psD[1], "offb"], [outkey])
    else:
        _cp(C, "dve", out, psD[0][:, 0:dvp], [psD[1]], [outkey])


def emit_norm1_tile(C, xt, kx, N, w, A1, modv, sq, rstd, psb, pskey, h1b, tmp):
    emit_rstd(C, xt, kx, N, sq, "sq", psb, pskey, rstd, "rstd")
    for c in range(NCH):
        _tt(C, "pool", tmp[:, c, :N], xt[:, c, :N], rstd[:, :N], ALU.mult, [kx, "rstd"], ["sq"])
        _ts(C, "dve", h1b[:, c, :N], tmp[:, c, :N], A1[:, c, w:w + 1], modv[:, c, w:w + 1], ALU.mult, ALU.add, ["sq", "A1", "modv"], ["h1b"])


def mixer_tiles():
    return [(0, 256, 1)] + [(256 + 512 * k, 512, 0) for k in range(8)]


def build_mixer_odd(env=None):
    T = T_ALL
    sfx = env.sfx if env else ""
    nc = env.nc if env else bass.Bass("TRN2", target_bir_lowering=False)
    env_nc_holder[0] = nc
    din = _din_factory(nc, env)
    xT_d = din("xT", [D, T]); cv_d = din("cv", [128, NCH, 2]); modw_d = din("modw", [D, 2048]); modb_d = din("modb", [128, 16])
    n1g_d = din("n1g", [128, NCH])
    wfm_d = din("wfm", [D, 1024]); wtm_d = din("wtm", [D, 2048]); wout_d = din("wout", [1024, D])
    rope_d = din("rope", [4, 128, T]); rperm_d = din("rperm", [128, 128]); gtab_d = din("gtab", [128, 8, NB]); gainb_d = din("gainb", [128, 1024])
    yT_d = None if env else nc.dram_tensor("yT", [D, T], F32, kind="ExternalOutput").ap()
    FT_d = nc.dram_tensor("FT_scr" + sfx, [8, 128, T], F32, kind="Internal").ap()
    TM_d = nc.dram_tensor("TM_scr" + sfx, [T, 2048], F32, kind="Internal").ap()
    UT_d = nc.dram_tensor("UT_scr" + sfx, [8, 128, T], MMDT, kind="Internal").ap()
    r3 = lambda ap: ap.rearrange("(c p) n -> p c n", p=128)
    tiles = mixer_tiles()
    with ExitStack() as st:
        C = _begin(env, st, True)
        P = C.P
        C.rot = 0
        pss = C.pss_all
        n1g = C.sb("n1g", [128, NCH]); A1 = C.sb("A1", [128, NCH, 2])
        _dma(C, "sp", n1g[:], n1g_d, [], ["n1g"])
        with ExitStack() as st2:
            C.st = st2
            modv = emit_mod(C, cv_d, modw_d, modb_d, 2048, pss[0][0], "ps0")
            for w in range(2):
                _stt(C, A1[:, :, w], modv[:, 8:16, w], 1.0, n1g[:], ALU.add, ALU.mult, ["modv", "n1g"], ["A1"])
            wfm = C.sb("wfm", [128, NCH, 1024], MMDT); wtm = C.sb("wtm", [128, NCH, 2048], MMDT)
            rperm = C.sb("rperm", [128, 128])
            _dma(C, "sp", rperm[:], rperm_d, [], ["rperm"])
            for c in range(NCH):
                _dma(C, "pool", wfm[:, c, :], wfm_d[c * 128:(c + 1) * 128, :], [], ["wfm"])
                _dma(C, "pool", wtm[:, c, :], wtm_d[c * 128:(c + 1) * 128, :], [], ["wtm"])
            xt = C.sb("xt", [128, NCH, 512]); sq = C.sb("sq", [128, NCH, 512]); rstd = C.sb("rstd", [128, 512])
            h1b = C.sb("h1b", [128, NCH, 512], MMDT)
            ropet = C.sb("ropet", [128, 4, 512])
            raw = [C.sb("raw%d" % i, [128, 512]) for i in range(2)]
            r1 = [C.sb("r1%d" % i, [128, 512]) for i in range(2)]
            r2 = [C.sb("r2%d" % i, [128, 512]) for i in range(2)]
            tmo = [C.sb("tmo%d" % i, [128, 512]) for i in range(2)]
            for ti, (t0, N, w) in enumerate(tiles):
                if env is not None and env.xload is not None:
                    env.xload(C, xt, t0, N)
                else:
                    _dma(C, "sp", xt[:, :, :N], r3(xT_d)[:, :, t0:t0 + N], [], ["xt"])
                _dma(C, "sp", ropet[:, :, :N], rope_d.rearrange("f p t -> p f t")[:, :, t0:t0 + N], [], ["ropet"])
                emit_norm1_tile(C, xt, "xt", N, w, A1, modv, sq, rstd, pss[1][0], "ps1", h1b, sq)
                for fm in range(8):
                    b = fm % 2
                    ps, pk = pss[2 + b]
                    ps2, pk2 = pss[4 + b]
                    for c in range(NCH):
                        _mm(C, ps[:, :N], wfm[:, c, fm * 128:(fm + 1) * 128], h1b[:, c, :N], c == 0, c == NCH - 1, ["wfm", "h1b"], [pk])
                    _cp(C, "act", raw[b][:, :N], ps[:, :N], [pk], ["raw%d" % b])
                    _mm(C, ps2[:, :N], rperm[:], raw[b][:, :N], True, True, ["rperm", "raw%d" % b], [pk2])
                    tb = 0 if fm < 4 else 2
                    _tt(C, "pool", r1[b][:, :N], raw[b][:, :N], ropet[:, tb, :N], ALU.mult, ["raw%d" % b, "ropet"], ["r1%d" % b])
                    _tt(C, "dve", r2[b][:, :N], ps2[:, :N], ropet[:, tb + 1, :N], ALU.mult, [pk2, "ropet"], ["r2%d" % b])
                    _tt(C, "pool", r1[b][:, :N], r1[b][:, :N], r2[b][:, :N], ALU.add, ["r1%d" % b, "r2%d" % b], ["r1%d" % b])
                    _dma(C, "sp", FT_d[fm, :, t0:t0 + N], r1[b][:, :N], ["r1%d" % b], ["FT_d"])
                for blk in range(N // 128):
                    for half in range(4):
                        b = (blk * 4 + half) % 2
                        ps, pk = pss[6 + b]
                        for c in range(NCH):
                            _mm(C, ps[:, :512], h1b[:, c, blk * 128:(blk + 1) * 128], wtm[:, c, half * 512:(half + 1) * 512], c == 0, c == NCH - 1, ["h1b", "wtm"], [pk])
                        _cp(C, "act" if half % 2 else "dve", tmo[b][:], ps[:, :512], [pk], ["tmo%d" % b])
                        _dma(C, "sp", TM_d[t0 + blk * 128:t0 + (blk + 1) * 128, half * 512:(half + 1) * 512], tmo[b][:], ["tmo%d" % b], ["TM_d"])
            P.barrier()
        C.st = st
        with ExitStack() as st3:
            C.st = st3
            AT = C.sb("AT", [128, T]); BT = C.sb("BT", [128, T])
            Vraw = C.sb("Vraw", [128, NB, 256]); VP = C.sb("VP", [128, NB, 256]); OH = C.sb("OH", [128, NB, 256])
            gtab = C.sb("gtab", [128, 8, NB]); gainb = C.sb("gainb", [128, 1024])
            _dma(C, "sp", gtab[:], gtab_d, [], ["gtab", "g"])
            _dma(C, "sp", gainb[:], gainb_d, [], ["gainb"])
            colb = C.sb("colb", [128, NB])
            tabs = (C.sb("kf", [128, NB]), C.sb("rowf", [128, NB]), C.sb("ctab", [128, NB, NB]), C.sb("gF", [128, NB]), C.sb("gL", [128, NB]), None)
            wts = [(C.sb("wt%d" % i, [128, 128]), "wt%d" % i) for i in range(4)]
            Et = C.sb("Et", [128, 128]); dg = C.sb("dg", [128, 128]); offb = C.sb("offb", [128, 256]); otmp = C.sb("otmp", [128, 256])
            bufs = (pss[0:4], pss[4], pss[5], pss[6], wts, Et, dg, offb)
            Gt = C.sb("Gt", [128, 256]); cen = C.sb("cen", [128, 256]); st1 = C.sb("st1", [128, 4]); uT = C.sb("uT", [128, 2, 128], MMDT)
            TMv = TM_d.rearrange("(n p) w -> p n w", p=128)
            for h in range(4):
                _dma(C, "sp", AT[:], FT_d[4 + h], ["FT_d"], ["AT"])
                _dma(C, "sp", BT[:], FT_d[h], ["FT_d"], ["BT"])
                _dma(C, "sp", Vraw[:], TMv[:, :, h * 256:(h + 1) * 256], ["TM_d"], ["Vraw"])
                for d in range(2):
                    s = h * 2 + d
                    g = gtab[:, s, :]
                    _ts(C, "dve", colb[:], g, -1.0, None, ALU.mult, None, ["gtab"], ["colb"])
                    emit_stream_tables(C, g, colb, d, tabs, pss[7][0], "ps7", "")
                    for J in range(NB):
                        _ts(C, "pool" if J % 2 else "dve", VP[:, J, :], Vraw[:, J, :], tabs[0][:, J:J + 1], None, ALU.mult, None, ["Vraw", "tabskf"], ["VP"])
                    order = scan_order(d)
                    for pos, I in enumerate(order):
                        if d == 0:
                            emit_attn_block(C, I, pos, order, AT, BT, VP, Vraw, 256, tabs, g, colb, d, bufs, "", OH[:, I, :], "OH%d" % I)
                        else:
                            emit_attn_block(C, I, pos, order, AT, BT, VP, Vraw, 256, tabs, g, colb, d, bufs, "", otmp[:], "otmp")
                            _tt(C, "pool", OH[:, I, :], OH[:, I, :], otmp[:], ALU.add, ["otmp", "OH%d" % I], ["OH%d" % I])
                for I in range(NB):
                    k = "OH%d" % I
                    _dma(C, "sp", Gt[:], TM_d[I * 128:(I + 1) * 128, 1024 + h * 256:1024 + (h + 1) * 256], ["TM_d"], ["Gt"])
                    P.op("dve", (lambda I: lambda e: e.reduce_sum(out=st1[:, 0:1], in_=OH[:, I, :], axis=AX.X))(I), reads=[k], writes=["st1"])
                    _ts(C, "dve", st1[:, 0:1], st1[:, 0:1], -1.0 / 256, None, ALU.mult, None, ["st1"], ["st1"])
                    _ts(C, "dve", cen[:], OH[:, I, :], st1[:, 0:1], None, ALU.add, None, [k, "st1"], ["cen"])
                    P.op("act", lambda e: e.activation(out=otmp[:], in_=cen[:], func=AF.Square, accum_out=st1[:, 1:2]), reads=["cen"], writes=["otmp", "st1b"])
                    _act(C, st1[:, 2:3], st1[:, 1:2], AF.Sqrt, ["st1b"], ["st1c"], bias=EPS, scale=1.0 / 256)
                    P.op("dve", lambda e: e.reciprocal(out=st1[:, 3:4], in_=st1[:, 2:3]), reads=["st1c"], writes=["st1d"])
                    _act(C, Gt[:], Gt[:], AF.Silu, ["Gt"], ["Gt"])
                    _tt(C, "pool", Gt[:], Gt[:], gainb[:, h * 256:(h + 1) * 256], ALU.mult, ["Gt", "gainb"], ["Gt"])
                    _stt(C, cen[:], cen[:], st1[:, 3:4], Gt[:], ALU.mult, ALU.mult, ["cen", "st1d", "Gt"], ["cen"])
                    for ec in range(2):
                        _tr(C, pss[7][0][:, ec * 128:(ec + 1) * 128], cen[:, ec * 128:(ec + 1) * 128], ["cen"], ["ps7"])
                    _cp(C, "act", uT[:].rearrange("p a b -> p (a b)"), pss[7][0][:, 0:256], ["ps7"], ["uT"])
                    for ec in range(2):
                        _dma(C, "sp", UT_d[h * 2 + ec, :, I * 128:(I + 1) * 128], uT[:, ec, :], ["uT"], ["UT_d"])
            P.barrier()
        C.st = st
        with ExitStack() as st4:
            C.st = st4
            wout = C.sb("wout", [128, 8, D], MMDT)
            for hc in range(8):
                _dma(C, "pool", wout[:, hc, :], wout_d[hc * 128:(hc + 1) * 128, :], [], ["wout"])
            ut = [C.sb("utile%d" % i, [128, 8, 512], MMDT) for i in range(2)]
            yo = [C.sb("yo%d" % i, [128, 512]) for i in range(2)]
            for ti, (t0, N, w) in enumerate(tiles):
                b = ti % 2
                _dma(C, "sp", ut[b][:, :, :N], UT_d.rearrange("f p t -> p f t")[:, :, t0:t0 + N], [], ["ut%d" % b])
                for dmc in range(NCH):
                    pb = dmc % 2
                    ps, pk = pss[pb]
                    for hc in range(8):
                        _mm(C, ps[:, :N], wout[:, hc, dmc * 128:(dmc + 1) * 128], ut[b][:, hc, :N], hc == 0, hc == 7, ["wout", "ut%d" % b], [pk])
                    _cp(C, "act" if pb else "dve", yo[pb][:, :N], ps[:, :N], [pk], ["yo%d" % pb])
                    if env is not None:
                        env.ywrite(C, dmc, t0, N, yo[pb], "yo%d" % pb)
                    else:
                        _dma(C, "sp", yT_d[dmc * 128:(dmc + 1) * 128, t0:t0 + N], yo[pb][:, :N], ["yo%d" % pb], ["yT%d_%d" % (ti, dmc)])
            if env is None:
                P.finish()
            else:
                P.barrier()
        if env is None:
            P.emit()
    return nc


RET_HEADS = 8
ROPE_BASE = 10000.0


def mixer_common_inputs(layer, inputs, b, x_lat, x_ctx):
    pc = lambda v: np.ascontiguousarray(v.reshape(-1, 128).T)
    cv = np.stack([pc(inputs["c"][b]), pc(inputs["c_ctx"])], axis=-1)
    xT = np.ascontiguousarray(np.concatenate([x_ctx[b], x_lat[b]], 0).T)
    return {"xT": xT, "cv": np.ascontiguousarray(cv), "modw": np.ascontiguousarray(inputs["mod_w"][layer][:, 0:2048]),
            "modb": pc(inputs["mod_b"][layer][0:2048]), "n1g": pc(inputs["norm1_g"][layer])}


def odd_inputs(layer, inputs, b, hg, x_lat, x_ctx):
    j = layer // 2
    m = mixer_common_inputs(layer, inputs, b, x_lat, x_ctx)
    w_in = inputs["od_w_in"][j]
    hs = slice(hg * 4, hg * 4 + 4)
    wq = w_in[:, 0:1024].reshape(D, 8, 128)[:, hs].reshape(D, 512)
    wk = w_in[:, 1024:2048].reshape(D, 8, 128)[:, hs].reshape(D, 512)
    wv = w_in[:, 2048:4096].reshape(D, 8, 256)[:, hs].reshape(D, 1024)
    wg = w_in[:, 4096:6144].reshape(D, 8, 256)[:, hs].reshape(D, 1024)
    m["wfm"] = np.ascontiguousarray(np.concatenate([wq, wk], 1))
    m["wtm"] = np.ascontiguousarray(np.concatenate([wv, wg], 1))
    m["wout"] = np.ascontiguousarray(inputs["od_w_out"][j].reshape(8, 256, D)[hs].reshape(1024, D))
    half = 64
    freqs = (np.float32(ROPE_BASE) ** (-np.arange(half, dtype=np.float32) / np.float32(half))).astype(np.float32)
    pos = np.arange(T_ALL, dtype=np.float32)
    ang = (pos[:, None] * freqs[None, :]).astype(np.float32)
    cos, sin = np.cos(ang).astype(np.float32).T, np.sin(ang).astype(np.float32).T
    cosT = np.concatenate([cos, cos], 0); sinT = np.concatenate([-sin, sin], 0)
    ks = np.float32(128.0 ** -0.5)
    m["rope"] = np.ascontiguousarray(np.stack([cosT, sinT, cosT * ks, sinT * ks], 0).astype(np.float32))
    m["rperm"] = np.ascontiguousarray(np.roll(np.eye(128, dtype=np.float32), 64, axis=0))
    expo = 5.0 + np.arange(RET_HEADS, dtype=np.float32)
    lg_f = np.log1p(-np.exp2(-expo)).astype(np.float32)
    lg_b = np.log1p(-np.exp2(-expo[::-1])).astype(np.float32)
    sp_f = np.arange(T_ALL, dtype=np.float32)
    sp_b = np.concatenate([255.0 - np.arange(256), 256.0 + 4095.0 - np.arange(4096)]).astype(np.float32)
    gt = np.zeros((128, 8, NB), np.float32)
    for hl in range(4):
        h = hg * 4 + hl
        gt[:, hl * 2 + 0, :] = (sp_f * lg_f[h]).reshape(NB, 128).T
        gt[:, hl * 2 + 1, :] = (sp_b * lg_b[h]).reshape(NB, 128).T
    m["gtab"] = gt
    m["gainb"] = np.ascontiguousarray(np.broadcast_to(inputs["ret_norm_g"][j].reshape(8, 256)[hs].reshape(1, 1024), (128, 1024)))
    return m


def emit_offdiag(C, I, order_pos, order, AT, BT, VP, dvp, ctab, bufs, sid):
    psS, psO, psD, psR, wts, Et, dg, offb = bufs
    before = order[:order_pos]
    isl = slice(I * 128, (I + 1) * 128)
    kk = "tabs%s" % sid
    for n, J in enumerate(before):
        sb_i = C.rot % len(psS)
        C.rot += 1
        ps, pk = psS[sb_i]
        wt, wk = wts[sb_i % len(wts)]
        _mm(C, ps[:, 0:128], AT[:, J * 128:(J + 1) * 128], BT[:, isl], True, True, ["AT" + sid, "BT" + sid], [pk])
        _ts(C, "dve", wt[:], ps[:, 0:128], ctab[:, I, J:J + 1], None, ALU.mult, None, [pk, kk + "ctab"], [wk])
        _mm(C, psO[0][:, 0:dvp], wt[:], VP[:, J, :], n == 0, n == len(before) - 1, [wk, "VP" + sid], [psO[1]])
    return len(before) > 0


def emit_cumsum(C, x, xkey, direction, out, outkey, tmpA, tmpB, psb, pskey):
    n = NB * 4
    flat = lambda t: t[:].rearrange("p a b -> p (a b)")
    m = C.masks["F" if direction == 0 else "B"]
    mkey = "maskF" if direction == 0 else "maskB"
    _mm(C, psb[:, 0:n], C.ones[:], flat(x), True, True, ["ones", xkey], [pskey])
    _cp(C, "dve", flat(tmpA), psb[:, 0:n], [pskey], ["cs_tot"])
    _mm(C, psb[:, 0:n], m[:], flat(x), True, True, [mkey, xkey], [pskey])
    for k in range(4):
        C.P.op("dve", (lambda k: lambda e: e.tensor_tensor_scan(out=tmpB[:, :, k], data0=C.ones[:, 0:NB], data1=tmpA[:, :, k], initial=0.0,
                                                                 op0=ALU.mult, op1=ALU.add))(k), reads=["cs_tot", "ones"], writes=["cs_cs"])
    if direction == 0:
        _tt(C, "dve", flat(tmpB), flat(tmpB), flat(tmpA), ALU.subtract, ["cs_cs", "cs_tot"], ["cs_carry"])
    else:
        _tt(C, "dve", tmpA[:, 0, :], tmpB[:, 1, :], tmpB[:, NB - 1, :], ALU.add, ["cs_cs", "cs_tot"], ["cs_tot"])
        _cp(C, "dve", tmpA[:, 2, :], tmpA[:, 1, :], ["cs_tot"], ["cs_tot"])
        _tt(C, "dve", tmpB[:, 2:NB, :], tmpA[:, 0:1, :].to_broadcast([128, NB - 2, 4]), tmpB[:, 2:NB, :], ALU.subtract, ["cs_cs", "cs_tot"], ["cs_carry"])
        _cp(C, "dve", tmpB[:, 0, :], tmpA[:, 2, :], ["cs_tot", "cs_carry"], ["cs_carry"])
        C.P.op("dve", lambda e: e.memset(tmpB[:, 1, :], 0.0), reads=["cs_carry"], writes=["cs_carry"])
    _tt(C, "dve", flat(out), psb[:, 0:n], flat(tmpB), ALU.add, [pskey, "cs_carry"], [outkey])


def build_mixer_even(env=None):
    T = T_ALL
    sfx = env.sfx if env else ""
    nc = env.nc if env else bass.Bass("TRN2", target_bir_lowering=False)
    env_nc_holder[0] = nc
    din = _din_factory(nc, env)
    xT_d = din("xT", [D, T]); cv_d = din("cv", [128, NCH, 2]); modw_d = din("modw", [D, 2048]); modb_d = din("modb", [128, 16])
    n1g_d = din("n1g", [128, NCH])
    wfm_d = din("wfm", [D, 1280]); wtm_d = din("wtm", [D, 784]); wout_d = din("wout", [512, D])
    convw_d = din("convw", [128, 6, 3]); gconst_d = din("gconst", [128, 4, 4]); gainb_d = din("gainb", [128, 512])
    yT_d = None if env else nc.dram_tensor("yT", [D, T], F32, kind="ExternalOutput").ap()
    FT_d = nc.dram_tensor("FT_scr" + sfx, [10, 128, T], F32, kind="Internal").ap()
    TM_d = nc.dram_tensor("TM_scr" + sfx, [T, 784], F32, kind="Internal").ap()
    UT_d = nc.dram_tensor("UT_scr" + sfx, [4, 128, T], MMDT, kind="Internal").ap()
    r3 = lambda ap: ap.rearrange("(c p) n -> p c n", p=128)
    tiles = mixer_tiles()
    with ExitStack() as st:
        C = _begin(env, st, True)
        P = C.P
        C.rot = 0
        pss = C.pss_all
        n1g = C.sb("n1g", [128, NCH]); A1 = C.sb("A1", [128, NCH, 2])
        _dma(C, "sp", n1g[:], n1g_d, [], ["n1g"])
        with ExitStack() as st2:
            C.st = st2
            modv = emit_mod(C, cv_d, modw_d, modb_d, 2048, pss[0][0], "ps0")
            for w in range(2):
                _stt(C, A1[:, :, w], modv[:, 8:16, w], 1.0, n1g[:], ALU.add, ALU.mult, ["modv", "n1g"], ["A1"])
            wfm = C.sb("wfm", [128, NCH, 1280], MMDT); wtm = C.sb("wtm", [128, NCH, 784], MMDT)
            convw = C.sb("convw", [128, 6, 3])
            _dma(C, "sp", convw[:], convw_d, [], ["convw"])
            for c in range(NCH):
                _dma(C, "pool", wfm[:, c, :], wfm_d[c * 128:(c + 1) * 128, :], [], ["wfm"])
                _dma(C, "pool", wtm[:, c, :], wtm_d[c * 128:(c + 1) * 128, :], [], ["wtm"])
            xt = C.sb("xt", [128, NCH, 512]); sq = C.sb("sq", [128, NCH, 512]); rstd = C.sb("rstd", [128, 512])
            h1b = C.sb("h1b", [128, NCH, 512], MMDT)
            raw = [C.sb("raw%d" % i, [128, 512]) for i in range(2)]
            r1 = [C.sb("r1%d" % i, [128, 512]) for i in range(2)]
            r2 = [C.sb("r2%d" % i, [128, 512]) for i in range(2)]
            tmo = [C.sb("tmo%d" % i, [128, 512]) for i in range(2)]
            for ti, (t0, N, w) in enumerate(tiles):
                if env is not None and env.xload is not None:
                    env.xload(C, xt, t0, N)
                else:
                    _dma(C, "sp", xt[:, :, :N], r3(xT_d)[:, :, t0:t0 + N], [], ["xt"])
                emit_norm1_tile(C, xt, "xt", N, w, A1, modv, sq, rstd, pss[1][0], "ps1", h1b, sq)
                R, RL = (1, 256) if w == 1 else (N // 64, 64)
                v3 = lambda t: t[:, :N].rearrange("p (r l) -> p r l", l=RL)
                for fm in range(10):
                    b = fm % 2
                    ps, pk = pss[2 + b]
                    ps2, pk2 = pss[4 + b]
                    kr, k1, k2 = "raw%d" % b, "r1%d" % b, "r2%d" % b
                    for c in range(NCH):
                        _mm(C, ps[:, :N], wfm[:, c, fm * 128:(fm + 1) * 128], h1b[:, c, :N], c == 0, c == NCH - 1, ["wfm", "h1b"], [pk])
                    if fm < 6:
                        _cp(C, "act", raw[b][:, :N], ps[:, :N], [pk], [kr])
                        _ts(C, "dve", r1[b][:, :N], raw[b][:, :N], convw[:, fm, 1:2], None, ALU.mult, None, [kr, "convw"], [k1])
                        _stt(C, v3(r1[b])[:, :, 1:RL], v3(raw[b])[:, :, 0:RL - 1], convw[:, fm, 0:1], v3(r1[b])[:, :, 1:RL], ALU.mult, ALU.add, [kr, k1, "convw"], [k1])
                        _stt(C, v3(r1[b])[:, :, 0:RL - 1], v3(raw[b])[:, :, 1:RL], convw[:, fm, 2:3], v3(r1[b])[:, :, 0:RL - 1], ALU.mult, ALU.add, [kr, k1, "convw"], [k1])
                        _act(C, r1[b][:, :N], r1[b][:, :N], AF.Silu, [k1], [k1])
                        if fm < 4:
                            _act(C, r2[b][:, :N], r1[b][:, :N], AF.Square, [k1], [k2])
                            _mm(C, ps2[:, :N], C.ones[:], r2[b][:, :N], True, True, ["ones", k2], [pk2])
                            _act(C, r2[b][:, :N], ps2[:, :N], AF.Sqrt, [pk2], [k2], bias=EPS, scale=1.0)
                            P.op("dve", (lambda b, N: lambda e: e.reciprocal(out=r2[b][:, :N], in_=r2[b][:, :N]))(b, N), reads=[k2], writes=[k2])
                            _stt(C, r1[b][:, :N], r1[b][:, :N], (128.0 ** -0.5) if fm < 2 else 1.0, r2[b][:, :N], ALU.mult, ALU.mult, [k1, k2], [k1])
                    elif fm < 8:
                        _cp(C, "act", r1[b][:, :N], ps[:, :N], [pk], [k1])
                    else:
                        _act(C, r1[b][:, :N], ps[:, :N], AF.Identity, [pk], [k1], scale=128.0 ** -0.5)
                    _dma(C, "sp", FT_d[fm, :, t0:t0 + N], r1[b][:, :N], [k1], ["FT_d"])
                for blk in range(N // 128):
                    for half, (c0, c1) in enumerate(((0, 512), (512, 784))):
                        b = (blk * 2 + half) % 2
                        ps, pk = pss[6 + b]
                        for c in range(NCH):
                            _mm(C, ps[:, :c1 - c0], h1b[:, c, blk * 128:(blk + 1) * 128], wtm[:, c, c0:c1], c == 0, c == NCH - 1, ["h1b", "wtm"], [pk])
                        _cp(C, "act" if half % 2 else "dve", tmo[b][:, :c1 - c0], ps[:, :c1 - c0], [pk], ["tmo%d" % b])
                        _dma(C, "sp", TM_d[t0 + blk * 128:t0 + (blk + 1) * 128, c0:c1], tmo[b][:, :c1 - c0], ["tmo%d" % b], ["TM_d"])
            P.barrier()
        C.st = st
        with ExitStack() as st3:
            C.st = st3
            TMv = TM_d.rearrange("(n p) w -> p n w", p=128)
            gates = C.sb("gates", [128, NB, 16]); gconst = C.sb("gconst", [128, 4, 4]); gainb = C.sb("gainb", [128, 512])
            _dma(C, "sp", gates[:], TMv[:, :, 768:784], [], ["gates"])
            _dma(C, "sp", gconst[:], gconst_d, [], ["gconst"])
            _dma(C, "sp", gainb[:], gainb_d, [], ["gainb"])
            la = C.sb("la", [128, NB, 4]); beta = C.sb("beta", [128, NB, 4]); ic = C.sb("ic", [128, NB, 4]); lf = C.sb("lf", [128, NB, 4])
            gG = C.sb("gG", [128, NB, 4]); gF_ = C.sb("gFm", [128, NB, 4]); tA = C.sb("tA", [128, NB, 4]); tB = C.sb("tB", [128, NB, 4])
            arate = C.sb("arate", [128, 4]); cmax = C.sb("cmax", [128, 4]); ecneg = C.sb("ecneg", [128, 4]); c1 = C.sb("c1", [128, 4])
            bc = lambda col: gconst[:, col:col + 1, :].to_broadcast([128, NB, 4])
            _act(C, arate[:], gconst[:, 0, :], AF.Exp, ["gconst"], ["arate"])
            _tt(C, "dve", la[:], gates[:, :, 0:4], bc(1), ALU.add, ["gates", "gconst"], ["la"])
            _act(C, la[:], la[:], AF.Exp, ["la"], ["la"])
            _act(C, la[:], la[:], AF.Ln, ["la"], ["la"], bias=1.0)
            _tt(C, "dve", la[:], la[:], arate[:, None, :].to_broadcast([128, NB, 4]), ALU.mult, ["la", "arate"], ["la"])
            _ts(C, "dve", la[:], la[:], -1.0, None, ALU.mult, None, ["la"], ["la"])
            _act(C, beta[:], gates[:, :, 4:8], AF.Sigmoid, ["gates"], ["beta"])
            _tt(C, "dve", ic[:], gates[:, :, 8:12], bc(2), ALU.add, ["gates", "gconst"], ["ic"])
            _tt(C, "dve", lf[:], gates[:, :, 12:16], bc(3), ALU.add, ["gates", "gconst"], ["lf"])
            _act(C, lf[:], lf[:], AF.Exp, ["lf"], ["lf"], scale=-1.0)
            _act(C, lf[:], lf[:], AF.Ln, ["lf"], ["lf"], bias=1.0)
            _ts(C, "dve", lf[:], lf[:], -1.0, None, ALU.mult, None, ["lf"], ["lf"])
            P.op("dve", lambda e: e.tensor_reduce(out=c1[:], in_=ic[:].rearrange("p n k -> p k n"), axis=AX.X, op=ALU.max), reads=["ic"], writes=["c1"])
            _tr(C, pss[7][0][0:4, 0:128], c1[:], ["c1"], ["ps7"])
            c2 = C.sb("c2", [4, 1]); c3 = C.sb("c3", [4, 4])
            P.op("dve", lambda e: e.reduce_max(out=c2[:], in_=pss[7][0][0:4, 0:128], axis=AX.X), reads=["ps7"], writes=["c2"])
            _ts(C, "dve", c3[:], C.ident[0:4, 0:4], c2[:, 0:1], None, ALU.mult, None, ["ident", "c2"], ["c3"])
            _mm(C, pss[7][0][:, 0:4], C.ones[0:4, :], c3[:], True, True, ["ones", "c3"], ["ps7"])
            _cp(C, "dve", cmax[:], pss[7][0][:, 0:4], ["ps7"], ["cmax"])
            _act(C, ecneg[:], cmax[:], AF.Exp, ["cmax"], ["ecneg"], scale=-1.0)
            ncmax = C.sb("ncmax", [128, 4])
            _ts(C, "dve", ncmax[:], cmax[:], -1.0, None, ALU.mult, None, ["cmax"], ["ncmax"])

            KT = C.sb("KT", [128, T]); QT = C.sb("QT", [128, T]); VT = C.sb("VT", [128, T])
            gdn_heads = 2 if EVEN_STOP >= 3 else 0
            ml_heads = 2 if EVEN_STOP >= 7 else 0
            Vtm = C.sb("Vtm", [128, NB, 129]); X = C.sb("X", [128, NB, 129]); XP = C.sb("XP", [128, NB, 129])
            TT = C.sb("TT", [128, NB, 128]); OH = C.sb("OH", [128, NB, 128])
            g = C.sb("g", [128, NB]); colb = C.sb("colb", [128, NB]); rowfb = C.sb("rowfb", [128, NB])
            tabs = (C.sb("kf", [128, NB]), C.sb("rowf", [128, NB]), C.sb("ctab", [128, NB, NB]), C.sb("gF", [128, NB]), C.sb("gL", [128, NB]), None)
            kf, rowf, ctab = tabs[0], tabs[1], tabs[2]
            wts = [(C.sb("wt%d" % i, [128, 128]), "wt%d" % i) for i in range(4)]
            Et = C.sb("Et", [128, 128]); dg = C.sb("dg", [128, 128]); offb = C.sb("offb", [128, 129]); otmp = C.sb("otmp", [128, 129])
            Lm = C.sb("Lm", [128, 128]); Am = [C.sb("Am%d" % i, [128, 128]) for i in range(2)]; Bm = [C.sb("Bm%d" % i, [128, 128]) for i in range(2)]
            Pm = C.sb("Pm", [128, 128]); Rm = C.sb("Rm", [128, 128])
            bufs = (pss[0:4], pss[4], pss[5], pss[6], wts, Et, dg, offb)
            Gt = C.sb("Gt", [128, 128]); cen = C.sb("cen", [128, 128]); st1 = C.sb("st1", [128, 4]); uT = C.sb("uT", [128, 128], MMDT)
            ps7 = pss[7][0]

            def head_tail(hidx, gate_col, gate_func, gain_off):
                for I in range(NB):
                    k = "OH%d" % I
                    _dma(C, "sp", Gt[:], TM_d[I * 128:(I + 1) * 128, gate_col:gate_col + 128], ["TM_d"], ["Gt"])
                    P.op("act", (lambda I: lambda e: e.activation(out=otmp[:, 0:128], in_=OH[:, I, :], func=AF.Square, accum_out=st1[:, 1:2]))(I), reads=[k], writes=["otmp", "st1b"])
                    _act(C, st1[:, 2:3], st1[:, 1:2], AF.Sqrt, ["st1b"], ["st1c"], bias=EPS, scale=1.0 / 128)
                    P.op("dve", lambda e: e.reciprocal(out=st1[:, 3:4], in_=st1[:, 2:3]), reads=["st1c"], writes=["st1d"])
                    _act(C, Gt[:], Gt[:], gate_func, ["Gt"], ["Gt"])
                    _tt(C, "pool", Gt[:], Gt[:], gainb[:, gain_off:gain_off + 128], ALU.mult, ["Gt", "gainb"], ["Gt"])
                    _stt(C, cen[:], OH[:, I, :], st1[:, 3:4], Gt[:], ALU.mult, ALU.mult, [k, "st1d", "Gt"], ["cen"])
                    _tr(C, ps7[:, 0:128], cen[:], ["cen"], ["ps7"])
                    _cp(C, "act", uT[:], ps7[:, 0:128], ["ps7"], ["uT"])
                    _dma(C, "sp", UT_d[hidx, :, I * 128:(I + 1) * 128], uT[:], ["uT"], ["UT_d"])

            for hl in range(gdn_heads):
                _dma(C, "sp", KT[:], FT_d[2 + hl], ["FT_d"], ["AT"])
                _dma(C, "sp", QT[:], FT_d[hl], ["FT_d"], ["BT"])
                _dma(C, "sp", VT[:], FT_d[4 + hl], ["FT_d"], ["VT"])
                for I in range(NB):
                    _tr(C, ps7[:, 0:128], VT[:, I * 128:(I + 1) * 128], ["VT"], ["ps7"])
                    _cp(C, "act", Vtm[:, I, 0:128], ps7[:, 0:128], ["ps7"], ["Vtm"])
                for d in range(2):
                    col = d * 2 + hl
                    order = scan_order(d)
                    emit_cumsum(C, la, "la", d, gG, "gG", tA, tB, ps7, "ps7")
                    _cp(C, "dve", g[:], gG[:, :, col], ["gG"], ["g"])
                    _ts(C, "dve", colb[:], g[:], -1.0, None, ALU.mult, None, ["g"], ["colb"])
                    emit_stream_tables(C, g[:], colb, d, tabs, ps7, "ps7", "")
                    _tt(C, "dve", rowfb[:], rowf[:], beta[:, :, col], ALU.mult, ["tabsrowf", "beta"], ["rowfb"])
                    smask = C.masks["Bs" if d == 0 else "Fs"]
                    smkey = "maskBs" if d == 0 else "maskFs"
                    for I in range(NB):
                        isl = slice(I * 128, (I + 1) * 128)
                        _ts(C, "dve", dg[:], C.ident[:], g[:, I:I + 1], None, ALU.mult, None, ["ident", "g"], ["dg"])
                        _mm(C, pss[6][0][:, 0:128], C.ones[:], dg[:], True, True, ["ones", "dg"], ["ps6"])
                        _ts(C, "dve", Et[:], pss[6][0][:, 0:128], colb[:, I:I + 1], 0.0, ALU.add, ALU.max, ["ps6", "colb"], ["Et"])
                        _act(C, Et[:], Et[:], AF.Exp, ["Et"], ["Et"], scale=-1.0)
                        _tt(C, "pool", Et[:], Et[:], smask[:], ALU.mult, ["Et", smkey], ["Et"])
                        _mm(C, pss[0][0][:, 0:128], KT[:, isl], KT[:, isl], True, True, ["AT"], ["ps0"])
                        _stt(C, Bm[0][:], pss[0][0][:, 0:128], beta[:, I, col:col + 1], Et[:], ALU.mult, ALU.mult, ["ps0", "beta", "Et"], ["Bm0"])
                        _tr(C, pss[1][0][:, 0:128], Bm[0][:], ["Bm0"], ["ps1"])
                        _cp(C, "act", Am[0][:], pss[1][0][:, 0:128], ["ps1"], ["Am0"])
                        _tt(C, "dve", Pm[:], C.ident[:], Am[0][:], ALU.subtract, ["ident", "Am0"], ["Pm"])
                        for lev in range(1, 7):
                            a0, a1 = (lev - 1) % 2, lev % 2
                            _mm(C, pss[2][0][:, 0:128], Bm[a0][:], Am[a0][:], True, True, ["Bm%d" % a0, "Am%d" % a0], ["ps2"])
                            _mm(C, pss[3][0][:, 0:128], Am[a0][:], Bm[a0][:], True, True, ["Bm%d" % a0, "Am%d" % a0], ["ps3"])
                            _cp(C, "act", Am[a1][:], pss[2][0][:, 0:128], ["ps2"], ["Am%d" % a1])
                            _cp(C, "dve", Bm[a1][:], pss[3][0][:, 0:128], ["ps3"], ["Bm%d" % a1])
                            _mm(C, pss[1][0][:, 0:128], Bm[a1][:], Pm[:], True, True, ["Bm%d" % a1, "Pm"], ["ps1"])
                            _tt(C, "dve", Pm[:], Pm[:], pss[1][0][:, 0:128], ALU.add, ["Pm", "ps1"], ["Pm"])
                        _cp(C, "pool", TT[:, I, :], Pm[:], ["Pm"], ["TT"])
                    for pos, I in enumerate(order if EVEN_STOP >= 4 else []):
                        anyj = emit_offdiag(C, I, pos, order, KT, KT, XP[:, :, 0:128], 128, ctab, bufs, "")
                        _ts(C, "dve", Rm[:], Vtm[:, I, 0:128], beta[:, I, col:col + 1], None, ALU.mult, None, ["Vtm", "beta"], ["Rm"])
                        if anyj:
                            _ts(C, "dve", offb[:, 0:128], pss[4][0][:, 0:128], rowfb[:, I:I + 1], None, ALU.mult, None, ["ps4", "rowfb"], ["offb"])
                            _tt(C, "pool", Rm[:], Rm[:], offb[:, 0:128], ALU.subtract, ["Rm", "offb"], ["Rm"])
                        _mm(C, pss[5][0][:, 0:128], TT[:, I, :], Rm[:], True, True, ["TT", "Rm"], ["ps5"])
                        _cp(C, "act", X[:, I, 0:128], pss[5][0][:, 0:128], ["ps5"], ["Vraw"])
                        _ts(C, "dve", XP[:, I, 0:128], pss[5][0][:, 0:128], kf[:, I:I + 1], None, ALU.mult, None, ["ps5", "tabskf"], ["VP"])
                    for pos, I in enumerate(order if EVEN_STOP >= 5 else []):
                        if d == 0:
                            emit_attn_block(C, I, pos, order, KT, QT, XP[:, :, 0:128], X[:, :, 0:128], 128, tabs, g, colb, d, bufs, "", OH[:, I, :], "OH%d" % I)
                        else:
                            emit_attn_block(C, I, pos, order, KT, QT, XP[:, :, 0:128], X[:, :, 0:128], 128, tabs, g, colb, d, bufs, "", otmp[:, 0:128], "otmp")
                            _tt(C, "pool", OH[:, I, :], OH[:, I, :], otmp[:, 0:128], ALU.add, ["otmp", "OH%d" % I], ["OH%d" % I])
                if EVEN_STOP >= 6:
                    head_tail(hl, hl * 128, AF.Silu, 0)
            for hl in range(ml_heads):
                _dma(C, "sp", KT[:], FT_d[8 + hl], ["FT_d"], ["AT"])
                _dma(C, "sp", QT[:], FT_d[6 + hl], ["FT_d"], ["BT"])
                _dma(C, "sp", Vtm[:, :, 0:128], TMv[:, :, 256 + hl * 128:256 + (hl + 1) * 128], ["TM_d"], ["Vraw", "Vtm"])
                P.op("pool", lambda e: e.memset(Vtm[:, :, 128:129], 1.0), reads=["Vraw"], writes=["Vraw"])
                for d in range(2):
                    col = d * 2 + hl
                    order = scan_order(d)
                    emit_cumsum(C, lf, "lf", d, gF_, "gFm", tA, tB, ps7, "ps7")
                    _cp(C, "dve", g[:], gF_[:, :, col], ["gFm"], ["g"])
                    _tt(C, "dve", colb[:], ic[:, :, col], g[:], ALU.subtract, ["ic", "g"], ["colb"])
                    _ts(C, "dve", colb[:], colb[:], ncmax[:, col:col + 1], None, ALU.add, None, ["colb", "ncmax"], ["colb"])
                    emit_stream_tables(C, g[:], colb, d, tabs, ps7, "ps7", "")
                    for J in range(NB):
                        _ts(C, "pool" if J % 2 else "dve", XP[:, J, :], Vtm[:, J, :], kf[:, J:J + 1], None, ALU.mult, None, ["Vraw", "tabskf"], ["VP"])
                    for pos, I in enumerate(order):
                        emit_attn_block(C, I, pos, order, KT, QT, XP, Vtm, 129, tabs, g, colb, d, bufs, "", otmp[:], "otmp")
                        _act(C, st1[:, 0:1], otmp[:, 128:129], AF.Abs, ["otmp"], ["st1"])
                        _ts(C, "dve", st1[:, 0:1], st1[:, 0:1], ecneg[:, col:col + 1], None, ALU.max, None, ["st1", "ecneg"], ["st1"])
                        P.op("dve", lambda e: e.reciprocal(out=st1[:, 0:1], in_=st1[:, 0:1]), reads=["st1"], writes=["st1"])
                        if d == 0:
                            _ts(C, "dve", OH[:, I, :], otmp[:, 0:128], st1[:, 0:1], None, ALU.mult, None, ["otmp", "st1"], ["OH%d" % I])
                        else:
                            _stt(C, OH[:, I, :], otmp[:, 0:128], st1[:, 0:1], OH[:, I, :], ALU.mult, ALU.add, ["otmp", "st1", "OH%d" % I], ["OH%d" % I])
                head_tail(2 + hl, 512 + hl * 128, AF.Sigmoid, 128 + hl * 128)
            P.barrier()
        C.st = st
        with ExitStack() as st4:
            C.st = st4
            wout = C.sb("wout", [128, 4, D], MMDT)
            for hc in range(4):
                _dma(C, "pool", wout[:, hc, :], wout_d[hc * 128:(hc + 1) * 128, :], [], ["wout"])
            ut = [C.sb("utile%d" % i, [128, 4, 512], MMDT) for i in range(2)]
            yo = [C.sb("yo%d" % i, [128, 512]) for i in range(2)]
            for ti, (t0, N, w) in enumerate(tiles):
                b = ti % 2
                _dma(C, "sp", ut[b][:, :, :N], UT_d.rearrange("f p t -> p f t")[:, :, t0:t0 + N], [], ["ut%d" % b])
                for dmc in range(NCH):
                    pb = dmc % 2
                    ps, pk = pss[pb]
                    for hc in range(4):
                        _mm(C, ps[:, :N], wout[:, hc, dmc * 128:(dmc + 1) * 128], ut[b][:, hc, :N], hc == 0, hc == 3, ["wout", "ut%d" % b], [pk])
                    _cp(C, "act" if pb else "dve", yo[pb][:, :N], ps[:, :N], [pk], ["yo%d" % pb])
                    if env is not None:
                        env.ywrite(C, dmc, t0, N, yo[pb], "yo%d" % pb)
                    else:
                        _dma(C, "sp", yT_d[dmc * 128:(dmc + 1) * 128, t0:t0 + N], yo[pb][:, :N], ["yo%d" % pb], ["yT%d_%d" % (ti, dmc)])
            if env is None:
                P.finish()
            else:
                P.barrier()
        if env is None:
            P.emit()
    return nc


def even_inputs(layer, inputs, b, hg, x_lat, x_ctx):
    j = layer // 2
    m = mixer_common_inputs(layer, inputs, b, x_lat, x_ctx)
    w_in = inputs["ev_w_in"][j]
    hs = [hg * 2, hg * 2 + 1]
    hcols = lambda off, h: w_in[:, off + h * 128: off + (h + 1) * 128]
    fm = [hcols(0, h) for h in hs] + [hcols(512, h) for h in hs] + [hcols(1024, h) for h in hs] + \
         [hcols(2064, h) for h in hs] + [hcols(2576, h) for h in hs]
    m["wfm"] = np.ascontiguousarray(np.concatenate(fm, 1))
    gate_cols = []
    for off in (2048, 2056, 4112, 4120):
        for d in range(2):
            for h in hs:
                gate_cols.append(w_in[:, off + d * 4 + h: off + d * 4 + h + 1])
    tm = [hcols(1536, h) for h in hs] + [hcols(3088, h) for h in hs] + [hcols(3600, h) for h in hs] + gate_cols
    m["wtm"] = np.ascontiguousarray(np.concatenate(tm, 1))
    w_out = inputs["ev_w_out"][j]
    m["wout"] = np.ascontiguousarray(np.concatenate([w_out[h * 128:(h + 1) * 128] for h in hs] + [w_out[512 + h * 128:512 + (h + 1) * 128] for h in hs], 0))
    cw = inputs["ev_conv_w"][j]
    conv = np.zeros((128, 6, 3), np.float32)
    for qi, off in enumerate((0, 512, 1024)):
        for hi, h in enumerate(hs):
            conv[:, qi * 2 + hi, :] = cw[:, off + h * 128: off + (h + 1) * 128].T
    m["convw"] = conv
    gc = np.zeros((128, 4, 4), np.float32)
    for r, name in enumerate(("gdn_a_log", "gdn_dt_bias", "ml_i_bias", "ml_f_bias")):
        for d in range(2):
            for hi, h in enumerate(hs):
                gc[:, r, d * 2 + hi] = inputs[name][j][d, h]
    m["gconst"] = gc
    gb = np.concatenate([inputs["gdn_norm_g"][j]] + [inputs["ml_norm_g"][j][h * 128:(h + 1) * 128] for h in hs] + [np.zeros(128, np.float32)])
    m["gainb"] = np.ascontiguousarray(np.broadcast_to(gb[None, :], (128, 512)).astype(np.float32))
    return m


NTH = 2176
PAIRS = [[0, 1], [2, 3], [4, 5], [6, 7]]


def build_fused():
    nc = bass.Bass("TRN2", target_bir_lowering=False)
    env_nc_holder[0] = nc
    idx_d = nc.dram_tensor("idx_tab", [128, 16], I32, kind="ExternalInput").ap()
    ysend = nc.dram_tensor("ysend", [2, D, NTH], F32, kind="Internal").ap()
    yrecv = nc.dram_tensor("yrecv", [16 * 256, NTH], F32, kind="Internal", addr_space="Local").ap()
    ysel = nc.dram_tensor("ysel", [2, D, NTH], F32, kind="Internal").ap()
    xo0 = nc.dram_tensor("xo0", [D, NTH], F32, kind="Internal").ap()
    g2 = nc.dram_tensor("g2", [8 * 256, NTH], F32, kind="Internal", addr_space="Local").ap()
    with ExitStack() as gst:
        C = Ctx(nc, gst)
        C.sfx = "_g"
        P = C.P
        emit_consts(C)
        emit_masks(C)
        C.pss_all = [(C.ps("ps%d" % i), "ps%d" % i) for i in range(8)]
        idx_sb = C.sb("idx", [128, 16], I32)
        _dma(C, "sp", idx_sb[:], idx_d, [], ["idx"])
        env = Env(nc, C)

        def ywrite(C, dmc, t0, N, yo, yokey):
            rows = slice(dmc * 128, (dmc + 1) * 128)
            if t0 == 0:
                _dma(C, "sp", ysend[0, rows, 2048:2176], yo[:, 0:128], [yokey], ["ysend"])
                _dma(C, "sp", ysend[1, rows, 2048:2176], yo[:, 128:256], [yokey], ["ysend"])
            else:
                j0 = t0 - 256
                _dma(C, "sp", ysend[j0 // 2048, rows, j0 % 2048:j0 % 2048 + N], yo[:, :N], [yokey], ["ysend"])

        def exchange_y(tag):
            for k in range(16):
                h, c = k // 8, k % 8
                P.coll((lambda h, c, k: lambda e: e.collective_compute("AllGather", ALU.bypass, replica_groups=PAIRS,
                                                                         ins=[ysend[h, c * 128:(c + 1) * 128, :]],
                                                                         outs=[yrecv[k * 256:(k + 1) * 256, :]]))(h, c, k),
                       reads=["ysend"], writes=["yrecv"])
            with ExitStack() as stx:
                C.st = stx
                C.sfx = "_x" + tag
                selb = [C.sb("selb%d" % i, [128, NTH]) for i in range(2)]
                for c in range(8):
                    for r in range(2):
                        b = (c * 2 + r) % 2
                        col = c * 2 + r
                        P.dma("pool", (lambda b, col: lambda e: e.indirect_dma_start(
                            out=selb[b][:, :], out_offset=None, in_=yrecv[:, :],
                            in_offset=bass.IndirectOffsetOnAxis(ap=idx_sb[:, col:col + 1], axis=0)))(b, col),
                            reads=["yrecv", "idx"], writes=["selb%d" % b])
                        _dma(C, "sp", ysel[r, c * 128:(c + 1) * 128, :], selb[b][:], ["selb%d" % b], ["ysel"])
                P.barrier()
            C.st = gst

        def exchange_x():
            for c in range(8):
                P.coll((lambda c: lambda e: e.collective_compute("AllGather", ALU.bypass, replica_groups=PAIRS,
                                                                   ins=[xo0[c * 128:(c + 1) * 128, :]],
                                                                   outs=[g2[c * 256:(c + 1) * 256, :]]))(c),
                       reads=["xo0"], writes=["g2"])
            P.barrier()

        g2v = g2.rearrange("(c r p) n -> p c r n", c=8, r=2, p=128)

        def xload_g2(C, xt, t0, N):
            if t0 == 0:
                _dma(C, "sp", xt[:, :, 0:128], g2v[:, :, 0, 2048:2176], [], ["xt"])
                _dma(C, "sp", xt[:, :, 128:256], g2v[:, :, 1, 2048:2176], [], ["xt"])
            else:
                j0 = t0 - 256
                _dma(C, "sp", xt[:, :, :N], g2v[:, :, j0 // 2048, j0 % 2048:j0 % 2048 + N], [], ["xt"])

        env.sfx, env.over, env.xload, env.ywrite = "_m0", {}, None, ywrite
        build_mixer_even(env)
        C.st = gst
        exchange_y("0")
        env.sfx, env.over = "_f0", {"ypa": ysel[0], "ypb": ysel[1], "out": xo0}
        build_ffn(2048, 128, False, env)
        C.st = gst
        exchange_x()
        env.sfx, env.over, env.xload = "_m1", {"xT": None}, xload_g2
        build_mixer_odd(env)
        C.st = gst
        exchange_y("1")
        env.sfx, env.over = "_f1", {"xT": xo0[:, 0:2048], "ypa": ysel[0][:, 0:2048], "ypb": ysel[1][:, 0:2048]}
        build_ffn(2048, 0, True, env)
        C.st = gst
        P.finish()
        P.emit()
    return nc


def fused_inputs(inputs, c):
    b, r = c // 2, c % 2
    x_lat, x_ctx = inputs["x"], inputs["ctx"]
    m = {}
    for k, v in even_inputs(0, inputs, b, r, x_lat, x_ctx).items():
        m[k + "_m0"] = v
    xh = np.concatenate([x_lat[b, r * 2048:(r + 1) * 2048], x_ctx[b, r * 128:(r + 1) * 128]], 0).T
    for k, v in ffn_inputs(0, inputs, xh, None, None, b).items():
        if k not in ("ypa", "ypb"):
            m[k + "_f0"] = v
    for k, v in odd_inputs(1, inputs, b, r, x_lat, x_ctx).items():
        if k != "xT":
            m[k + "_m1"] = v
    for k, v in ffn_inputs(1, inputs, None, None, None, b).items():
        if k not in ("xT", "ypa", "ypb"):
            m[k + "_f1"] = v
    idx = np.zeros((128, 16), np.int32)
    for cc in range(8):
        for rr in range(2):
            idx[:, cc * 2 + rr] = ((r * 8 + cc) * 2 + rr) * 128 + np.arange(128)
    m["idx_tab"] = idx
    return m


_CACHE = {}


def _prog(name, fn):
    if name not in _CACHE:
        _CACHE[name] = fn()
    return _CACHE[name]


def _run(nc, maps):
    res = run_bass_kernel_spmd(nc, maps, core_ids=list(range(8)))
    return res.results


FUSED = True


def kernel(**inputs):
    inputs = {k: np.asarray(v) for k, v in inputs.items()}
    if FUSED:
        nc = _prog("fused", build_fused)
        maps = [fused_inputs(inputs, c) for c in range(8)]
        res = _run(nc, maps)
        out = np.empty((4, 4096, D), np.float32)
        for c in range(8):
            out[c // 2, (c % 2) * 2048:(c % 2 + 1) * 2048] = res[c]["out"].T
        return out
    x_lat = np.ascontiguousarray(inputs["x"], dtype=np.float32)
    x_ctx = np.ascontiguousarray(inputs["ctx"], dtype=np.float32)
    B = x_lat.shape[0]
    depth = inputs["mod_w"].shape[0]
    out_final = None
    for layer in range(depth):
        last = layer == depth - 1
        if layer % 2 == 0:
            nc = _prog("even", build_mixer_even)
            maps = [even_inputs(layer, inputs, c // 2, c % 2, x_lat, x_ctx) for c in range(8)]
        else:
            nc = _prog("odd", build_mixer_odd)
            maps = [odd_inputs(layer, inputs, c // 2, c % 2, x_lat, x_ctx) for c in range(8)]
        res = _run(nc, maps)
        yparts = [res[c]["yT"] for c in range(8)]
        n_lat, n_ctx = 2048, (0 if last else 128)
        nc = _prog("ffn_last" if last else "ffn", lambda: build_ffn(n_lat, n_ctx, last))
        maps = []
        for c in range(8):
            b, hf = c // 2, c % 2
            cols = [np.arange(256 + hf * 2048, 256 + (hf + 1) * 2048)]
            xs = [x_lat[b, hf * 2048:(hf + 1) * 2048]]
            if not last:
                cols.append(np.arange(hf * 128, (hf + 1) * 128))
                xs.append(x_ctx[b, hf * 128:(hf + 1) * 128])
            cols = np.concatenate(cols)
            xT = np.concatenate(xs, 0).T
            maps.append(ffn_inputs(layer, inputs, xT, yparts[2 * b][:, cols], yparts[2 * b + 1][:, cols], b))
        res = _run(nc, maps)
        if last:
            out_final = np.empty_like(x_lat)
            for c in range(8):
                b, hf = c // 2, c % 2
                out_final[b, hf * 2048:(hf + 1) * 2048] = res[c]["out"].T
        else:
            nx_lat = np.empty_like(x_lat)
            nx_ctx = np.empty_like(x_ctx)
            for c in range(8):
                b, hf = c // 2, c % 2
                o = res[c]["out"]
                nx_lat[b, hf * 2048:(hf + 1) * 2048] = o[:, :2048].T
                nx_ctx[b, hf * 128:(hf + 1) * 128] = o[:, 2048:].T
            x_lat, x_ctx = nx_lat, nx_ctx
    return out_final.astype(np.float32)
```

```python
from contextlib import ExitStack
import numpy as np
import concourse.bass as bass
import concourse.mybir as mybir
from concourse.bass_utils import run_bass_kernel_spmd

F32 = mybir.dt.float32
BF16 = mybir.dt.bfloat16
I32 = mybir.dt.int32
AF = mybir.ActivationFunctionType
ALU = mybir.AluOpType
AX = mybir.AxisListType

D = 1024
NCH = 8
EPS = 1e-6
N_EXP = 32
N_DMA_SEMS = 8
DEBUG = False
SERIAL = False
DEBUG_NEXP = None
EVEN_STOP = 99
MMDT = BF16


class Prog:
    def __init__(self, nc, stack):
        self.nc = nc
        self.stack = stack
        self.nrenew = 0
        self.names = ["pe", "act", "dve", "pool", "sp"]
        self.ops = {e: [] for e in self.names}
        self.cnt = {e: 0 for e in self.names}
        self.esem = {e: stack.enter_context(nc.semaphore("s_" + e)) for e in self.names}
        self.dsem, self.dval, self.dnext = {}, {}, {}
        for q in ("sp", "pool", "act"):
            self.dsem[q] = [stack.enter_context(nc.semaphore(f"d_{q}{i}")) for i in range(N_DMA_SEMS)]
            self.dval[q] = [0] * N_DMA_SEMS
            self.dnext[q] = 0
        self.csem = [stack.enter_context(nc.semaphore("c_%d" % i)) for i in range(4)]
        self.cval = [0] * 4
        self.cnext = 0
        self.res = {}
        self.seen = {e: {} for e in self.names}
        self.sem_by_id = {}

    def _need(self, eng, toks):
        best = {}
        for t in toks:
            if t is None:
                continue
            sem, v = t
            k = id(sem)
            self.sem_by_id[k] = sem
            if v > best.get(k, 0):
                best[k] = v
        waits = []
        for k, v in best.items():
            if self.seen[eng].get(k, 0) >= v:
                continue
            self.seen[eng][k] = v
            waits.append((self.sem_by_id[k], v))
        return waits

    def _deps(self, reads, writes):
        toks = []
        for r in reads:
            e = self.res.get(r)
            if e is not None:
                toks.append(e[0])
                if r.startswith("ps"):
                    toks.extend(e[1])
        for w in writes:
            e = self.res.get(w)
            if e is not None:
                toks.append(e[0])
                toks.extend(e[1])
        return toks

    def _commit(self, tok, reads, writes):
        for r in reads:
            e = self.res.setdefault(r, [None, []])
            e[1].append(tok)
        for w in writes:
            self.res[w] = [tok, []]

    def op(self, eng, fn, reads=(), writes=()):
        toks = self._deps(reads, writes)
        if eng == "pe":
            toks = [t for t in toks if t is None or t[0] is not self.esem["pe"]]
        waits = self._need(eng, toks)
        self.cnt[eng] += 1
        tok = (self.esem[eng], self.cnt[eng])
        self.ops[eng].append((fn, waits, (self.esem[eng], 1)))
        self._commit(tok, reads, writes)
        if SERIAL:
            self.barrier()
        return tok

    def dma(self, q, fn, reads=(), writes=()):
        toks = self._deps(reads, writes)
        i = self.dnext[q]
        self.dnext[q] = (i + 1) % N_DMA_SEMS
        sem = self.dsem[q][i]
        if self.dval[q][i] > 0:
            toks.append((sem, self.dval[q][i]))
        waits = self._need(q, toks)
        self.dval[q][i] += 16
        tok = (sem, self.dval[q][i])
        self.ops[q].append((fn, waits, (sem, 16)))
        self._commit(tok, reads, writes)
        if SERIAL:
            self.barrier()
        return tok

    def coll(self, fn, reads=(), writes=()):
        toks = self._deps(reads, writes)
        i = self.cnext
        self.cnext = (i + 1) % len(self.csem)
        sem = self.csem[i]
        if self.cval[i] > 0:
            toks.append((sem, self.cval[i]))
        waits = self._need("pool", toks)
        self.cval[i] += 1
        tok = (sem, self.cval[i])
        self.ops["pool"].append((fn, waits, (sem, None)))
        self._commit(tok, reads, writes)
        return tok

    def _all_dma_toks(self):
        toks = []
        for q in self.dsem:
            for i, sm in enumerate(self.dsem[q]):
                if self.dval[q][i] > 0:
                    toks.append((sm, self.dval[q][i]))
        for i, sm in enumerate(self.csem):
            if self.cval[i] > 0:
                toks.append((sm, self.cval[i]))
        return toks

    def barrier(self):
        toks = [(self.esem[e], self.cnt[e]) for e in self.names if self.cnt[e] > 0]
        toks += self._all_dma_toks()
        for e in self.names:
            waits = self._need(e, toks)
            if waits:
                self.ops[e].append((None, waits, None))
        self.res = {}
        for e in self.names:
            if self.cnt[e] > 12000:
                self.nrenew += 1
                self.esem[e] = self.stack.enter_context(self.nc.semaphore("s_%s_%d" % (e, self.nrenew)))
                self.cnt[e] = 0

    def finish(self):
        toks = self._all_dma_toks()
        waits = self._need("sp", toks)
        self.ops["sp"].append((None, waits, None))

    def emit(self):
        nc = self.nc
        with nc.Block() as block:
            def run(ename):
                def body(eng):
                    for fn, waits, inc in self.ops[ename]:
                        for sem, v in waits:
                            eng.wait_ge(sem, v)
                        if fn is not None:
                            if inc[1] is None:
                                fn(eng).then_inc(inc[0])
                            else:
                                fn(eng).then_inc(inc[0], inc[1])
                return body
            block.tensor(run("pe"))
            block.scalar(run("act"))
            block.vector(run("dve"))
            block.gpsimd(run("pool"))
            block.sync(run("sp"))


class Ctx:
    def __init__(self, nc, st):
        self.nc, self.st = nc, st
        self.P = Prog(nc, st)
        self.psn = 0
        self.sfx = ""

    def sb(self, name, shape, dt=F32):
        return self.st.enter_context(self.nc.sbuf_tensor("sb_" + name + self.sfx, shape, dt))

    def ps(self, name):
        return self.st.enter_context(self.nc.psum_tensor(name, [128, 512], F32))


class Env:
    def __init__(self, nc, C):
        self.nc, self.C = nc, C
        self.sfx = ""
        self.over = {}
        self.xload = None
        self.ywrite = None


def _din_factory(nc, env):
    sfx = env.sfx if env else ""

    def din(name, shape, dt=F32):
        if env is not None and name in env.over:
            return env.over[name]
        return nc.dram_tensor(name + sfx, shape, dt, kind="ExternalInput").ap()
    return din


def _begin(env, st, masks):
    if env is not None:
        C = env.C
        C.st = st
        C.sfx = env.sfx
        return C
    C = Ctx(env_nc_holder[0], st)
    emit_consts(C)
    if masks:
        emit_masks(C)
    C.pss_all = [(C.ps("ps%d" % i), "ps%d" % i) for i in range(8)]
    return C


env_nc_holder = [None]


def token_tiles(n_lat, n_ctx):
    tiles = []
    for s in range(0, n_lat, 512):
        tiles.append((s, min(512, n_lat - s), 0))
    for s in range(0, n_ctx, 512):
        tiles.append((n_lat + s, min(512, n_ctx - s), 1))
    return tiles


def emit_consts(C):
    P, nc = C.P, C.nc
    C.ones = C.sb("ones", [128, 128])
    C.ident = C.sb("ident", [128, 128])
    P.op("pool", lambda e: e.memset(C.ones[:], 1.0), writes=["ones"])
    P.op("pool", lambda e: e.memset(C.ident[:], 1.0), writes=["ident"])
    P.op("pool", lambda e: e.affine_select(out=C.ident[:], in_=C.ident[:], pattern=[[-1, 128]],
                                            compare_op=ALU.is_equal, fill=0.0, base=0, channel_multiplier=1),
         reads=["ident"], writes=["ident"])


def emit_mod(C, cv_d, modw_d, modb_d, ncols, psb, pskey):
    P, nc = C.P, C.nc
    nj = ncols // 128
    cv = C.sb("cv", [128, NCH, 2])
    sc = C.sb("sc", [128, NCH, 2])
    modb = C.sb("modb", [128, nj])
    modv = C.sb("modv", [128, nj, 2])
    P.dma("sp", lambda e: e.dma_start(out=cv[:], in_=cv_d), writes=["cv"])
    P.dma("sp", lambda e: e.dma_start(out=modb[:], in_=modb_d), writes=["modb"])
    P.op("act", lambda e: e.activation(out=sc[:], in_=cv[:], func=AF.Silu), reads=["cv"], writes=["sc"])
    wbufs = [C.sb("modw%d" % i, [128, NCH, 512]) for i in range(2)]
    mw = modw_d.rearrange("(c p) n -> p c n", p=128)
    for blk in range(ncols // 512):
        wb = wbufs[blk % 2]
        key = "modw%d" % (blk % 2)
        P.dma("sp", (lambda wb, blk: lambda e: e.dma_start(out=wb[:], in_=mw[:, :, blk * 512:(blk + 1) * 512]))(wb, blk),
              writes=[key])
        for jj in range(4):
            j = blk * 4 + jj
            for c in range(NCH):
                P.op("pe", (lambda wb, jj, c, j: lambda e: e.matmul(psb[:, 2 * j:2 * j + 2], lhsT=wb[:, c, jj * 128:(jj + 1) * 128],
                                                                      rhs=sc[:, c, :], start=(c == 0), stop=(c == NCH - 1)))(wb, jj, c, j),
                     reads=[key, "sc"], writes=[pskey])
    for w in range(2):
        P.op("dve", (lambda w: lambda e: e.tensor_tensor(out=modv[:, :, w], in0=psb[:, w:2 * nj:2], in1=modb[:], op=ALU.add))(w),
             reads=[pskey, "modb"], writes=["modv"])
    return modv


def emit_rstd(C, src, srckey, N, sq, sqkey, psb, pskey, rstd, rstdkey):
    P = C.P
    for c in range(NCH):
        P.op("act", (lambda c: lambda e: e.activation(out=sq[:, c, :N], in_=src[:, c, :N], func=AF.Square))(c), reads=[srckey], writes=[sqkey])
    for c in range(NCH):
        P.op("pe", (lambda c: lambda e: e.matmul(psb[:, :N], lhsT=C.ones[:], rhs=sq[:, c, :N], start=(c == 0), stop=(c == NCH - 1)))(c),
             reads=["ones", sqkey], writes=[pskey])
    P.op("act", lambda e: e.activation(out=rstd[:, :N], in_=psb[:, :N], func=AF.Sqrt, bias=EPS, scale=1.0 / D),
         reads=[pskey], writes=[rstdkey])
    P.op("dve", lambda e: e.reciprocal(out=rstd[:, :N], in_=rstd[:, :N]), reads=[rstdkey], writes=[rstdkey])


def build_ffn(n_lat, n_ctx, last, env=None):
    NT = n_lat + n_ctx
    tiles = token_tiles(n_lat, n_ctx)
    sfx = env.sfx if env else ""
    nc = env.nc if env else bass.Bass("TRN2", target_bir_lowering=False)
    env_nc_holder[0] = nc
    dt_in = _din_factory(nc, env)
    xT_d = dt_in("xT", [D, NT]); ypa_d = dt_in("ypa", [D, NT]); ypb_d = dt_in("ypb", [D, NT])
    cv_d = dt_in("cv", [128, NCH, 2]); modw_d = dt_in("modw", [D, 4096]); modb_d = dt_in("modb", [128, 32])
    n2g_d = dt_in("n2g", [128, NCH]); fing_d = dt_in("fing", [128, NCH])
    rw_d = dt_in("rw", [D, N_EXP]); rb_d = dt_in("rb", [1, N_EXP])
    wgu_d = dt_in("wgu", [N_EXP, D, 2 * D]); bgu_d = dt_in("bgu", [128, N_EXP, NCH, 2])
    wdn_d = dt_in("wdn", [N_EXP, D, D]); bdn_d = dt_in("bdn", [128, N_EXP, NCH])
    if env is not None and "out" in env.over:
        out_d = env.over["out"]
    else:
        out_d = nc.dram_tensor("out", [D, NT], F32, kind="ExternalOutput").ap()
    xmid_d = nc.dram_tensor("xmid_scr" + sfx, [D, NT], F32, kind="ExternalOutput" if DEBUG else "Internal").ap()
    if DEBUG:
        gwT_dbg = nc.dram_tensor("gwT_dbg", [N_EXP, NT], F32, kind="ExternalOutput").ap()
        acc_dbg = nc.dram_tensor("acc_dbg", [D, NT], F32, kind="ExternalOutput").ap()
        h2_dbg = nc.dram_tensor("h2_dbg", [D, NT], F32, kind="ExternalOutput").ap()
        modv_dbg = nc.dram_tensor("modv_dbg", [128, 64], F32, kind="ExternalOutput").ap()
    r3 = lambda ap: ap.rearrange("(c p) n -> p c n", p=128)

    with ExitStack() as st:
        C = _begin(env, st, False)
        P = C.P
        psA = [C.pss_all[i][0] for i in (0, 1)]
        psB = [C.pss_all[i][0] for i in (2, 3)]
        psY = [C.pss_all[i][0] for i in (4, 5)]
        psM = [C.pss_all[i][0] for i in (6, 7)]
        h2b = C.sb("h2b", [128, NCH, NT], MMDT)
        gwT = C.sb("gwT", [N_EXP, NT])
        n2g = C.sb("n2g", [128, NCH]); fing = C.sb("fing", [128, NCH])
        A2 = C.sb("A2", [128, NCH, 2])
        g2v = C.sb("g2v", [128, NCH, 2])
        tmp32 = C.sb("tmp32", [N_EXP, 512])
        rw = C.sb("rw", [128, NCH, N_EXP]); rb = C.sb("rb", [1, N_EXP])
        bgu = C.sb("bgu", [128, N_EXP, NCH, 2]); bdn = C.sb("bdn", [128, N_EXP, NCH])
        P.dma("sp", lambda e: e.dma_start(out=n2g[:], in_=n2g_d), writes=["n2g"])
        P.dma("sp", lambda e: e.dma_start(out=fing[:], in_=fing_d), writes=["fing"])
        P.dma("sp", lambda e: e.dma_start(out=rw[:], in_=r3(rw_d)), writes=["rw"])
        P.dma("sp", lambda e: e.dma_start(out=rb[:], in_=rb_d), writes=["rb"])
        P.dma("sp", lambda e: e.dma_start(out=bgu[:], in_=bgu_d), writes=["bgu"])
        P.dma("sp", lambda e: e.dma_start(out=bdn[:], in_=bdn_d), writes=["bdn"])

        with ExitStack() as st2:
            C.st = st2
            modv = emit_mod(C, cv_d, modw_d, modb_d, 4096, psM[0], "psM0")
            P.op("dve", lambda e: e.tensor_copy(out=g2v[:], in_=modv[:, 24:32, :]), reads=["modv"], writes=["g2v"])
            for w in range(2):
                P.op("dve", (lambda w: lambda e: e.scalar_tensor_tensor(out=A2[:, :, w], in0=modv[:, 16:24, w], scalar=1.0, in1=n2g[:],
                                                                         op0=ALU.add, op1=ALU.mult))(w),
                     reads=["modv", "n2g"], writes=["A2"])
            xt = [C.sb("xt%d" % i, [128, NCH, 512]) for i in range(2)]
            ya = [C.sb("ya0", [128, NCH, 512])] * 2
            yb = [C.sb("yb0", [128, NCH, 512])] * 2
            sq = C.sb("sq", [128, NCH, 512])
            rstd = C.sb("rstd", [128, 512])
            h2f = C.sb("h2f", [128, NCH, 512])
            lg = C.sb("lg", [128, N_EXP]); top8 = C.sb("top8", [128, 8]); negm = C.sb("negm", [128, 1])
            exl = C.sb("exl", [128, N_EXP]); msk = C.sb("msk", [128, N_EXP]); den = C.sb("den", [128, 1])
            gw = C.sb("gw", [128, N_EXP])
            for ti, (t0, N, w) in enumerate(tiles):
                b = ti % 2
                kx, ka, kb = "xt%d" % b, "ya0", "yb0"
                P.dma("sp", (lambda b, t0, N: lambda e: e.dma_start(out=xt[b][:, :, :N], in_=r3(xT_d)[:, :, t0:t0 + N]))(b, t0, N), writes=[kx])
                P.dma("sp", (lambda b, t0, N: lambda e: e.dma_start(out=ya[b][:, :, :N], in_=r3(ypa_d)[:, :, t0:t0 + N]))(b, t0, N), writes=[ka])
                P.dma("sp", (lambda b, t0, N: lambda e: e.dma_start(out=yb[b][:, :, :N], in_=r3(ypb_d)[:, :, t0:t0 + N]))(b, t0, N), writes=[kb])
                P.op("pool", (lambda b, N: lambda e: e.tensor_tensor(out=ya[b][:, :, :N], in0=ya[b][:, :, :N], in1=yb[b][:, :, :N], op=ALU.add))(b, N),
                     reads=[ka, kb], writes=[ka])
                for c in range(NCH):
                    P.op("dve", (lambda b, N, c, w: lambda e: e.scalar_tensor_tensor(out=xt[b][:, c, :N], in0=ya[b][:, c, :N], scalar=modv[:, c, w:w + 1],
                                                                                      in1=xt[b][:, c, :N], op0=ALU.mult, op1=ALU.add))(b, N, c, w),
                         reads=[ka, kx, "modv"], writes=[kx])
                P.dma("sp", (lambda b, t0, N: lambda e: e.dma_start(out=r3(xmid_d)[:, :, t0:t0 + N], in_=xt[b][:, :, :N]))(b, t0, N),
                      reads=[kx], writes=["xmid_d%d" % ti])
                emit_rstd(C, xt[b], kx, N, sq, "sq", psM[1], "psM1", rstd, "rstd")
                for c in range(NCH):
                    P.op("pool", (lambda b, N, c: lambda e: e.tensor_tensor(out=h2f[:, c, :N], in0=xt[b][:, c, :N], in1=rstd[:, :N], op=ALU.mult))(b, N, c),
                         reads=[kx, "rstd"], writes=["h2f"])
                    P.op("dve", (lambda N, c, w: lambda e: e.tensor_scalar(out=h2f[:, c, :N], in0=h2f[:, c, :N], scalar1=A2[:, c, w:w + 1],
                                                                            scalar2=modv[:, 8 + c, w:w + 1], op0=ALU.mult, op1=ALU.add))(N, c, w),
                         reads=["h2f", "A2", "modv"], writes=["h2f"])
                for c in range(NCH):
                    P.op("act", (lambda t0, N, c: lambda e: e.copy(out=h2b[:, c, t0:t0 + N], in_=h2f[:, c, :N]))(t0, N, c), reads=["h2f"], writes=["h2b"])
                if DEBUG:
                    P.dma("sp", (lambda t0, N: lambda e: e.dma_start(out=r3(h2_dbg)[:, :, t0:t0 + N], in_=h2f[:, :, :N]))(t0, N), reads=["h2f"], writes=["dbgh%d" % ti])
                    if ti == 0:
                        P.dma("sp", lambda e: e.dma_start(out=modv_dbg, in_=modv[:].rearrange("p j w -> p (j w)")), reads=["modv"], writes=["dbgm"])
                        a2_dbg = nc.dram_tensor("a2_dbg", [128, 16], F32, kind="ExternalOutput").ap()
                        P.dma("sp", lambda e: e.dma_start(out=a2_dbg, in_=A2[:].rearrange("p j w -> p (j w)")), reads=["A2"], writes=["dbga2"])
                        rstd_dbg = nc.dram_tensor("rstd_dbg", [128, 512], F32, kind="ExternalOutput").ap()
                        P.dma("sp", lambda e: e.dma_start(out=rstd_dbg, in_=rstd[:]), reads=["rstd"], writes=["dbgr"])
                        sq_dbg = nc.dram_tensor("sq_dbg", [128, NCH, 512], F32, kind="ExternalOutput").ap()
                        P.dma("sp", lambda e: e.dma_start(out=sq_dbg, in_=sq[:]), reads=["sq"], writes=["dbgsq"])
                for blk in range(N // 128):
                    o = blk * 128
                    for c in range(NCH):
                        P.op("pe", (lambda o, c: lambda e: e.matmul(psM[0][:, 0:N_EXP], lhsT=h2f[:, c, o:o + 128], rhs=rw[:, c, :], start=(c == 0), stop=False))(o, c),
                             reads=["h2f", "rw"], writes=["psM0"])
                    P.op("pe", lambda e: e.matmul(psM[0][:, 0:N_EXP], lhsT=C.ones[0:1, :], rhs=rb[:], start=False, stop=True),
                         reads=["ones", "rb"], writes=["psM0"])
                    P.op("dve", lambda e: e.tensor_copy(out=lg[:], in_=psM[0][:, 0:N_EXP]), reads=["psM0"], writes=["lg"])
                    P.op("dve", lambda e: e.max(out=top8[:], in_=lg[:]), reads=["lg"], writes=["top8"])
                    P.op("dve", lambda e: e.tensor_scalar(out=negm[:], in0=top8[:, 0:1], scalar1=-1.0, scalar2=None, op0=ALU.mult),
                         reads=["top8"], writes=["negm"])
                    P.op("act", lambda e: e.activation(out=exl[:], in_=lg[:], func=AF.Exp, bias=negm[:], scale=1.0),
                         reads=["lg", "negm"], writes=["exl"])
                    P.op("dve", lambda e: e.tensor_scalar(out=msk[:], in0=lg[:], scalar1=top8[:, 3:4], scalar2=None, op0=ALU.is_ge),
                         reads=["lg", "top8"], writes=["msk"])
                    P.op("dve", lambda e: e.tensor_tensor(out=exl[:], in0=exl[:], in1=msk[:], op=ALU.mult), reads=["exl", "msk"], writes=["exl"])
                    P.op("dve", lambda e: e.reduce_sum(out=den[:], in_=exl[:], axis=AX.X), reads=["exl"], writes=["den"])
                    P.op("dve", lambda e: e.reciprocal(out=den[:], in_=den[:]), reads=["den"], writes=["den"])
                    P.op("dve", lambda e: e.tensor_scalar(out=gw[:], in0=exl[:], scalar1=den[:, 0:1], scalar2=None, op0=ALU.mult),
                         reads=["exl", "den"], writes=["gw"])
                    P.op("pe", lambda e: e.transpose(out=psM[1][0:N_EXP, 0:128], in_=gw[:], identity=C.ident[:]), reads=["gw", "ident"], writes=["psM1"])
                    P.op("act", (lambda t0, o: lambda e: e.copy(out=gwT[:, t0 + o:t0 + o + 128], in_=psM[1][0:N_EXP, 0:128]))(t0, o),
                         reads=["psM1"], writes=["gwT"])
            P.barrier()
        C.st = st

        stacc = st.enter_context(ExitStack())
        C.st = stacc
        acc = C.sb("acc", [128, NCH, NT])
        P.op("pool", lambda e: e.memset(acc[:], 0.0), writes=["acc"])
        with ExitStack() as st3:
            C.st = st3
            NRING = 4
            wgu_r = [C.sb("wgu_r%d" % i, [128, NCH, 256], MMDT) for i in range(NRING)]
            wdn_r = [C.sb("wdn_r%d" % i, [128, NCH, 128], MMDT) for i in range(NRING)]
            actT = C.sb("actT", [128, NCH, NT], MMDT)
            gwb = C.sb("gwb", [128, NT])
            gt = [C.sb("gt%d" % i, [128, 512]) for i in range(2)]
            ut = [C.sb("ut%d" % i, [128, 512]) for i in range(2)]
            sg = [C.sb("sg%d" % i, [128, 512]) for i in range(2)]
            yt = [C.sb("yt%d" % i, [128, 512]) for i in range(2)]
            wguv = wgu_d.rearrange("e (c p) n -> e p c n", p=128)
            wdnv = wdn_d.rearrange("e (c p) n -> e p c n", p=128)
            pg = 0
            pd = 0
            it = 0
            for ex in range(N_EXP if DEBUG_NEXP is None else DEBUG_NEXP):
                for ti, (t0, N, w) in enumerate(tiles):
                    _ts(C, "dve", tmp32[:, :N], gwT[:, t0:t0 + N], C.ident[0:N_EXP, ex:ex + 1], None, ALU.mult, None, ["gwT", "ident"], ["tmp32"])
                    _mm(C, psM[0][:, :N], C.ones[0:N_EXP, :], tmp32[:, :N], True, True, ["ones", "tmp32"], ["psM0"])
                    _cp(C, "act", gwb[:, t0:t0 + N], psM[0][:, :N], ["psM0"], ["gwb%d" % ti])
                for fc in range(NCH):
                    rg_ = pg % NRING
                    pg += 1
                    kw = "wgu_r%d" % rg_
                    _dma(C, "pool", wgu_r[rg_][:], wguv[ex, :, :, fc * 256:(fc + 1) * 256], [], [kw])
                    for ti, (t0, N, w) in enumerate(tiles):
                        pb = it % 2
                        it += 1
                        ka, kb = "psA%d" % pb, "psB%d" % pb
                        for two, (pst, kk) in enumerate(((psA[pb], ka), (psB[pb], kb))):
                            for c in range(NCH):
                                _mm(C, pst[:, :N], wgu_r[rg_][:, c, two:256:2], h2b[:, c, t0:t0 + N], c == 0, c == NCH - 1, [kw, "h2b"], [kk])
                        kg, ku, ks = "gt%d" % pb, "ut%d" % pb, "sg%d" % pb
                        _act(C, ut[pb][:, :N], psB[pb][:, :N], AF.Identity, [kb], [ku], bias=bgu[:, ex, fc, 1:2])
                        _ts(C, "dve", gt[pb][:, :N], psA[pb][:, :N], bgu[:, ex, fc, 0:1], 7.0, ALU.add, ALU.min, [ka, "bgu"], [kg])
                        _act(C, sg[pb][:, :N], gt[pb][:, :N], AF.Sigmoid, [kg], [ks], scale=1.702)
                        _ts(C, "dve", ut[pb][:, :N], ut[pb][:, :N], -7.0, 7.0, ALU.max, ALU.min, [ku], [ku])
                        _tt(C, "pool", gt[pb][:, :N], gt[pb][:, :N], sg[pb][:, :N], ALU.mult, [kg, ks], [kg])
                        _stt(C, actT[:, fc, t0:t0 + N], ut[pb][:, :N], 1.0, gt[pb][:, :N], ALU.add, ALU.mult, [ku, kg], ["actT%d_%d" % (fc, ti)])
                for dmc in range(NCH):
                    rd_ = pd % NRING
                    pd += 1
                    kw = "wdn_r%d" % rd_
                    _dma(C, "pool", wdn_r[rd_][:], wdnv[ex, :, :, dmc * 128:(dmc + 1) * 128], [], [kw])
                    for ti, (t0, N, w) in enumerate(tiles):
                        pb = it % 2
                        it += 1
                        ky, kyt = "psY%d" % pb, "yt%d" % pb
                        for fc in range(NCH):
                            _mm(C, psY[pb][:, :N], wdn_r[rd_][:, fc, :], actT[:, fc, t0:t0 + N], fc == 0, fc == NCH - 1, [kw, "actT%d_%d" % (fc, ti)], [ky])
                        _stt(C, yt[pb][:, :N], psY[pb][:, :N], bdn[:, ex, dmc:dmc + 1], gwb[:, t0:t0 + N], ALU.add, ALU.mult, [ky, "bdn", "gwb%d" % ti], [kyt])
                        _tt(C, "pool" if (dmc + ti) % 2 else "dve", acc[:, dmc, t0:t0 + N], acc[:, dmc, t0:t0 + N], yt[pb][:, :N], ALU.add, [kyt, "acc", "acc%d_%d" % (dmc, ti)], ["acc%d_%d" % (dmc, ti)])
            P.barrier()
        C.st = st
        if DEBUG:
            P.dma("sp", lambda e: e.dma_start(out=gwT_dbg, in_=gwT[:]), writes=["dbg1"])
            P.dma("sp", lambda e: e.dma_start(out=r3(acc_dbg), in_=acc[:]), writes=["dbg2"])
        with ExitStack() as st4:
            C.st = st4
            xm = [C.sb("xm%d" % i, [128, NCH, 512]) for i in range(2)]
            sqD = C.sb("sq2", [128, NCH, 512])
            rstdD = C.sb("rstd2", [128, 512])
            for ti, (t0, N, w) in enumerate(tiles):
                b = ti % 2
                kx = "xm%d" % b
                P.dma("sp", (lambda b, t0, N: lambda e: e.dma_start(out=xm[b][:, :, :N], in_=r3(xmid_d)[:, :, t0:t0 + N]))(b, t0, N), writes=[kx])
                for c in range(NCH):
                    P.op("dve", (lambda b, N, c, w, t0: lambda e: e.scalar_tensor_tensor(out=xm[b][:, c, :N], in0=acc[:, c, t0:t0 + N], scalar=g2v[:, c, w:w + 1],
                                                                                          in1=xm[b][:, c, :N], op0=ALU.mult, op1=ALU.add))(b, N, c, w, t0),
                         reads=[kx], writes=[kx])
                if last:
                    emit_rstd(C, xm[b], kx, N, sqD, "sq2", psM[1], "psM1", rstdD, "rstd2")
                    for c in range(NCH):
                        P.op("dve", (lambda b, N, c: lambda e: e.scalar_tensor_tensor(out=xm[b][:, c, :N], in0=xm[b][:, c, :N], scalar=fing[:, c:c + 1],
                                                                                       in1=rstdD[:, :N], op0=ALU.mult, op1=ALU.mult))(b, N, c),
                             reads=[kx, "rstd2"], writes=[kx])
                P.dma("sp", (lambda b, t0, N: lambda e: e.dma_start(out=r3(out_d)[:, :, t0:t0 + N], in_=xm[b][:, :, :N]))(b, t0, N),
                      reads=[kx], writes=["out%d" % ti])
            if env is None:
                P.finish()
            else:
                P.barrier()
        if env is None:
            P.emit()
    return nc


def ffn_inputs(layer, inputs, xT, ypa, ypb, b):
    pc = lambda v: np.ascontiguousarray(v.reshape(-1, 128).T)
    cv = np.stack([pc(inputs["c"][b]), pc(inputs["c_ctx"])], axis=-1)
    bgu = np.ascontiguousarray(inputs["moe_b_gu"][layer].reshape(N_EXP, NCH, 128, 2).transpose(2, 0, 1, 3))
    bdn = np.ascontiguousarray(inputs["moe_b_dn"][layer].reshape(N_EXP, NCH, 128).transpose(2, 0, 1))
    return {
        "xT": None if xT is None else np.ascontiguousarray(xT), "ypa": None if ypa is None else np.ascontiguousarray(ypa),
        "ypb": None if ypb is None else np.ascontiguousarray(ypb),
        "cv": np.ascontiguousarray(cv),
        "modw": np.ascontiguousarray(inputs["mod_w"][layer][:, 2048:6144]),
        "modb": pc(inputs["mod_b"][layer][2048:6144]),
        "n2g": pc(inputs["norm2_g"][layer]), "fing": pc(inputs["final_g"]),
        "rw": np.ascontiguousarray(inputs["router_w"][layer]), "rb": np.ascontiguousarray(inputs["router_b"][layer][None, :]),
        "wgu": np.ascontiguousarray(inputs["moe_w_gu"][layer]), "bgu": bgu,
        "wdn": np.ascontiguousarray(inputs["moe_w_dn"][layer]), "bdn": bdn,
    }


def _mm(C, out, lhsT, rhs, start, stop, r, w):
    C.P.op("pe", lambda e: e.matmul(out, lhsT=lhsT, rhs=rhs, start=start, stop=stop), reads=r, writes=w)


def _tr(C, out, in_, r, w):
    ident = C.ident[0:in_.shape[0], 0:in_.shape[0]]
    C.P.op("pe", lambda e: e.transpose(out=out, in_=in_, identity=ident), reads=list(r) + ["ident"], writes=w)


def _ts(C, eng, out, in0, s1, s2, op0, op1, r, w):
    if s2 is None:
        C.P.op(eng, lambda e: e.tensor_scalar(out=out, in0=in0, scalar1=s1, scalar2=None, op0=op0), reads=r, writes=w)
    else:
        C.P.op(eng, lambda e: e.tensor_scalar(out=out, in0=in0, scalar1=s1, scalar2=s2, op0=op0, op1=op1), reads=r, writes=w)


def _tt(C, eng, out, in0, in1, op, r, w):
    C.P.op(eng, lambda e: e.tensor_tensor(out=out, in0=in0, in1=in1, op=op), reads=r, writes=w)


def _stt(C, out, in0, scalar, in1, op0, op1, r, w):
    C.P.op("dve", lambda e: e.scalar_tensor_tensor(out=out, in0=in0, scalar=scalar, in1=in1, op0=op0, op1=op1), reads=r, writes=w)


def _act(C, out, in_, func, r, w, bias=None, scale=1.0):
    if bias is None:
        C.P.op("act", lambda e: e.activation(out=out, in_=in_, func=func, scale=scale), reads=r, writes=w)
    else:
        C.P.op("act", lambda e: e.activation(out=out, in_=in_, func=func, bias=bias, scale=scale), reads=r, writes=w)


def _cp(C, eng, out, in_, r, w):
    if eng == "act":
        C.P.op("act", lambda e: e.copy(out=out, in_=in_), reads=r, writes=w)
    else:
        C.P.op(eng, lambda e: e.tensor_copy(out=out, in_=in_), reads=r, writes=w)


def _dma(C, q, out, in_, r, w):
    C.P.dma(q, lambda e: e.dma_start(out=out, in_=in_), reads=r, writes=w)


T_ALL = 4352
NB = 34
N_CTXB = 2


def scan_order(direction):
    if direction == 0:
        return list(range(NB))
    return [1, 0] + list(range(NB - 1, N_CTXB - 1, -1))


def emit_masks(C):
    P = C.P
    C.masks = {}
    for name, op, sgn in (("F", ALU.is_ge, 1), ("Fs", ALU.is_gt, 1), ("B", ALU.is_ge, -1), ("Bs", ALU.is_gt, -1)):
        m = C.sb("mask" + name, [128, 128])
        P.op("pool", (lambda m: lambda e: e.memset(m[:], 1.0))(m), writes=["mask" + name])
        P.op("pool", (lambda m, op, sgn: lambda e: e.affine_select(out=m[:], in_=m[:], pattern=[[sgn, 128]], compare_op=op, fill=0.0,
                                                               base=0, channel_multiplier=-sgn))(m, op, sgn),
             reads=["mask" + name], writes=["mask" + name])
        C.masks[name] = m
    for name, row in (("sel0", 0), ("sel127", 127)):
        m = C.sb(name, [128, 128])
        P.op("pool", (lambda m: lambda e: e.memset(m[:], 1.0))(m), writes=[name])
        P.op("pool", (lambda m, row: lambda e: e.affine_select(out=m[:], in_=m[:], pattern=[[0, 128]], compare_op=ALU.is_equal, fill=0.0,
                                                                base=-row, channel_multiplier=1))(m, row),
             reads=[name], writes=[name])
        C.masks[name] = m


def emit_stream_tables(C, g, colb, direction, tabs, psb, pskey, sid):
    P = C.P
    kf, rowf, ctab, gF, gL, tmp = tabs
    first, last = ("sel0", "sel127") if direction == 0 else ("sel127", "sel0")
    kk = "tabs%s" % sid
    _mm(C, psb[:, 0:NB], C.masks[first][:], g[:], True, True, [first, "g" + sid], [pskey])
    _cp(C, "dve", gF[:], psb[:, 0:NB], [pskey], [kk + "gF"])
    _mm(C, psb[:, 0:NB], C.masks[last][:], g[:], True, True, [last, "g" + sid], [pskey])
    _cp(C, "dve", gL[:], psb[:, 0:NB], [pskey], [kk + "gL"])
    _tt(C, "dve", kf[:], gL[:], colb[:], ALU.add, [kk + "gL", "colb" + sid], [kk + "kf"])
    _ts(C, "dve", kf[:], kf[:], 0.0, None, ALU.min, None, [kk + "kf"], [kk + "kf"])
    _act(C, kf[:], kf[:], AF.Exp, [kk + "kf"], [kk + "kf"])
    _tt(C, "dve", rowf[:], g[:], gF[:], ALU.subtract, ["g" + sid, kk + "gF"], [kk + "rowf"])
    _ts(C, "dve", rowf[:], rowf[:], 0.0, None, ALU.min, None, [kk + "rowf"], [kk + "rowf"])
    _act(C, rowf[:], rowf[:], AF.Exp, [kk + "rowf"], [kk + "rowf"])
    _tt(C, "dve", ctab[:], gF[:, :, None].to_broadcast([128, NB, NB]), gL[:, None, :].to_broadcast([128, NB, NB]), ALU.subtract,
        [kk + "gF", kk + "gL"], [kk + "ctab"])
    for I in range(NB):
        _ts(C, "dve", ctab[:, I, :], ctab[:, I, :], 0.0, None, ALU.min, None, [kk + "ctab"], [kk + "ctab"])
        _act(C, ctab[:, I, :], ctab[:, I, :], AF.Exp, [kk + "ctab"], [kk + "ctab"])


def emit_diag_E(C, g, colb, I, direction, strict, Et, etkey, dg, psb, pskey, sid):
    mk = C.masks[("F" if direction == 0 else "B") + ("s" if strict else "")]
    mkey = "mask" + ("F" if direction == 0 else "B") + ("s" if strict else "")
    _ts(C, "dve", dg[:], C.ident[:], g[:, I:I + 1], None, ALU.mult, None, ["ident", "g" + sid], ["dg"])
    _mm(C, psb[:, 0:128], C.ones[:], dg[:], True, True, ["ones", "dg"], [pskey])
    _ts(C, "dve", Et[:], psb[:, 0:128], colb[:, I:I + 1], 0.0, ALU.add, ALU.min, [pskey, "colb" + sid], [etkey])
    _act(C, Et[:], Et[:], AF.Exp, [etkey], [etkey])
    _tt(C, "pool", Et[:], Et[:], mk[:], ALU.mult, [etkey, mkey], [etkey])


def emit_attn_block(C, I, order_pos, order, AT, BT, VP, Vraw, dvp, tabs, g, colb, direction, bufs, sid, out, outkey, skip_off_rowscale=None, kx=""):
    kf, rowf, ctab, gF, gL, tmp = tabs
    psS, psO, psD, psR, wts, Et, dg, offb = bufs
    before = order[:order_pos]
    isl = slice(I * 128, (I + 1) * 128)
    kk = "tabs%s" % sid
    for n, J in enumerate(before):
        sb_i = C.rot % len(psS)
        C.rot += 1
        ps, pk = psS[sb_i]
        wt, wk = wts[sb_i % len(wts)]
        _mm(C, ps[:, 0:128], AT[:, J * 128:(J + 1) * 128], BT[:, isl], True, True, ["AT" + kx + sid, "BT" + kx + sid], [pk])
        _ts(C, "dve", wt[:], ps[:, 0:128], ctab[:, I, J:J + 1], None, ALU.mult, None, [pk, kk + "ctab"], [wk])
        _mm(C, psO[0][:, 0:dvp], wt[:], VP[:, J, :], n == 0, n == len(before) - 1, [wk, "VP" + kx + sid], [psO[1]])
    if before:
        _ts(C, "dve", offb[:, 0:dvp], psO[0][:, 0:dvp], rowf[:, I:I + 1], None, ALU.mult, None, [psO[1], kk + "rowf"], ["offb"])
    emit_diag_E(C, g, colb, I, direction, False, Et, "Et", dg, psR[0], psR[1], sid)
    sb_i = C.rot % len(psS)
    C.rot += 1
    ps, pk = psS[sb_i]
    wt, wk = wts[sb_i % len(wts)]
    _mm(C, ps[:, 0:128], AT[:, isl], BT[:, isl], True, True, ["AT" + kx + sid, "BT" + kx + sid], [pk])
    _tt(C, "dve", wt[:], ps[:, 0:128], Et[:], ALU.mult, [pk, "Et"], [wk])
    _mm(C, psD[0][:, 0:dvp], wt[:], Vraw[:, I, :], True, True, [wk, "Vraw" + kx + sid], [psD[1]])
    if before:
        _tt(C, "dve", out, psD[0][:, 0:dvp], offb[:, 0:dvp], ALU.add, [psD[1], "offb"], [outkey])
    else:
        _cp(C, "dve", out, psD[0][:, 0:dvp], [psD[1]], [outkey])


def emit_stream_pipelined(C, order, AT, BT, VP, Vraw, dvp, tabs, g, colb, direction, bufs, kx, tail, diag=True, lookahead=3):
    kf, rowf, ctab, gF, gL, _ = tabs
    psS, psO2, psD, psR, wts, Et2, dg2, offb = bufs
    kk = "tabs"
    items = []
    for pos, I in enumerate(order):
        before = order[:pos]
        for n, J in enumerate(before):
            items.append(("off", I, pos, J, n == 0, n == len(before) - 1, False))
        if diag:
            items.append(("diag", I, pos, I, True, True, True))
        if items and items[-1][1] == I:
            it = items[-1]
            items[-1] = it[:6] + (True,)
        elif not before and not diag:
            items.append(("none", I, pos, I, True, True, True))

    def emit_s(idx):
        kind, I, pos, J, first, last, lastI = items[idx]
        if kind == "none":
            return
        ps, pk = psS[idx % len(psS)]
        if kind == "diag" or (kind == "off" and first and not diag):
            pass
        if kind == "diag":
            Et, ek = Et2[pos % 2]
            dg, dk = dg2[pos % 2]
            mk = C.masks["F" if direction == 0 else "B"]
            mkey = "maskF" if direction == 0 else "maskB"
            _ts(C, "dve", dg[:], C.ident[:], g[:, I:I + 1], None, ALU.mult, None, ["ident", "g"], [dk])
            _mm(C, psR[0][:, 0:128], C.ones[:], dg[:], True, True, ["ones", dk], [psR[1]])
            _ts(C, "dve", Et[:], psR[0][:, 0:128], colb[:, I:I + 1], 0.0, ALU.add, ALU.min, [psR[1], "colb"], [ek])
            _act(C, Et[:], Et[:], AF.Exp, [ek], [ek])
            _tt(C, "pool", Et[:], Et[:], mk[:], ALU.mult, [ek, mkey], [ek])
        _mm(C, ps[:, 0:128], AT[:, J * 128:(J + 1) * 128], BT[:, I * 128:(I + 1) * 128], True, True, ["AT" + kx, "BT" + kx], [pk])

    def emit_da(idx):
        kind, I, pos, J, first, last, lastI = items[idx]
        psO = psO2[pos % 2]
        if kind != "none":
            ps, pk = psS[idx % len(psS)]
            wt, wk = wts[idx % len(wts)]
            if kind == "off":
                _ts(C, "dve", wt[:], ps[:, 0:128], ctab[:, I, J:J + 1], None, ALU.mult, None, [pk, kk + "ctab"], [wk])
                _mm(C, psO[0][:, 0:dvp], wt[:], VP[:, J, :], first, last, [wk, "VP" + kx], [psO[1]])
            else:
                Et, ek = Et2[pos % 2]
                _tt(C, "dve", wt[:], ps[:, 0:128], Et[:], ALU.mult, [pk, ek], [wk])
                _mm(C, psD[0][:, 0:dvp], wt[:], Vraw[:, I, :], True, True, [wk, "Vraw" + kx], [psD[1]])
        if lastI:
            tail(I, pos, pos > 0, psO[0], psO[1])

    n = len(items)
    for idx in range(min(lookahead, n)):
        emit_s(idx)
    for idx in range(n):
        emit_da(idx)
        if idx + lookahead < n:
            emit_s(idx + lookahead)


def std_tail(C, tabs, bufs, dvp, out_fn):
    kf, rowf, ctab, gF, gL, _ = tabs
    psS, psO2, psD, psR, wts, Et2, dg2, offb = bufs

    def tail(I, pos, has_off, psO, psOkey):
        res = C.tailbuf
        if has_off:
            _ts(C, "dve", offb[:, 0:dvp], psO[:, 0:dvp], rowf[:, I:I + 1], None, ALU.mult, None, [psOkey, "tabsrowf"], ["offb"])
            _tt(C, "dve", res[:, 0:dvp], psD[0][:, 0:dvp], offb[:, 0:dvp], ALU.add, [psD[1], "offb"], ["tailbuf"])
        else:
            _cp(C, "dve", res[:, 0:dvp], psD[0][:, 0:dvp], [psD[1]], ["tailbuf"])
        out_fn(I, res, "tailbuf")
    return tail


def emit_norm1_tile(C, xt, kx, N, w, A1, modv, sq, rstd, psb, pskey, h1b, tmp):
    emit_rstd(C, xt, kx, N, sq, "sq", psb, pskey, rstd, "rstd")
    for c in range(NCH):
        _tt(C, "pool", tmp[:, c, :N], xt[:, c, :N], rstd[:, :N], ALU.mult, [kx, "rstd"], ["sq"])
        _ts(C, "dve", h1b[:, c, :N], tmp[:, c, :N], A1[:, c, w:w + 1], modv[:, c, w:w + 1], ALU.mult, ALU.add, ["sq", "A1", "modv"], ["h1b"])


def mixer_tiles():
    return [(0, 256, 1)] + [(256 + 512 * k, 512, 0) for k in range(8)]


def build_mixer_odd(env=None):
    T = T_ALL
    sfx = env.sfx if env else ""
    nc = env.nc if env else bass.Bass("TRN2", target_bir_lowering=False)
    env_nc_holder[0] = nc
    din = _din_factory(nc, env)
    xT_d = din("xT", [D, T]); cv_d = din("cv", [128, NCH, 2]); modw_d = din("modw", [D, 2048]); modb_d = din("modb", [128, 16])
    n1g_d = din("n1g", [128, NCH])
    wfm_d = din("wfm", [D, 1024]); wtm_d = din("wtm", [D, 2048]); wout_d = din("wout", [1024, D])
    rope_d = din("rope", [4, 128, T]); rperm_d = din("rperm", [128, 128]); gtab_d = din("gtab", [128, 8, NB]); gainb_d = din("gainb", [128, 1024])
    yT_d = None if env else nc.dram_tensor("yT", [D, T], F32, kind="ExternalOutput").ap()
    FT_d = nc.dram_tensor("FT_scr" + sfx, [8, 128, T], F32, kind="Internal").ap()
    TM_d = nc.dram_tensor("TM_scr" + sfx, [T, 2048], F32, kind="Internal").ap()
    UT_d = nc.dram_tensor("UT_scr" + sfx, [8, 128, T], MMDT, kind="Internal").ap()
    r3 = lambda ap: ap.rearrange("(c p) n -> p c n", p=128)
    tiles = mixer_tiles()
    with ExitStack() as st:
        C = _begin(env, st, True)
        P = C.P
        C.rot = 0
        pss = C.pss_all
        n1g = C.sb("n1g", [128, NCH]); A1 = C.sb("A1", [128, NCH, 2])
        _dma(C, "sp", n1g[:], n1g_d, [], ["n1g"])
        with ExitStack() as st2:
            C.st = st2
            modv = emit_mod(C, cv_d, modw_d, modb_d, 2048, pss[0][0], "ps0")
            for w in range(2):
                _stt(C, A1[:, :, w], modv[:, 8:16, w], 1.0, n1g[:], ALU.add, ALU.mult, ["modv", "n1g"], ["A1"])
            wfm = C.sb("wfm", [128, NCH, 1024], MMDT); wtm = C.sb("wtm", [128, NCH, 2048], MMDT)
            rperm = C.sb("rperm", [128, 128])
            _dma(C, "sp", rperm[:], rperm_d, [], ["rperm"])
            for c in range(NCH):
                _dma(C, "pool", wfm[:, c, :], wfm_d[c * 128:(c + 1) * 128, :], [], ["wfm"])
                _dma(C, "pool", wtm[:, c, :], wtm_d[c * 128:(c + 1) * 128, :], [], ["wtm"])
            xt = C.sb("xt", [128, NCH, 512]); sq = C.sb("sq", [128, NCH, 512]); rstd = C.sb("rstd", [128, 512])
            h1b = C.sb("h1b", [128, NCH, 512], MMDT)
            ropet = C.sb("ropet", [128, 4, 512])
            raw = [C.sb("raw%d" % i, [128, 512]) for i in range(2)]
            r1 = [C.sb("r1%d" % i, [128, 512]) for i in range(2)]
            r2 = [C.sb("r2%d" % i, [128, 512]) for i in range(2)]
            tmo = [C.sb("tmo%d" % i, [128, 512]) for i in range(2)]
            for ti, (t0, N, w) in enumerate(tiles):
                if env is not None and env.xload is not None:
                    env.xload(C, xt, t0, N)
                else:
                    _dma(C, "sp", xt[:, :, :N], r3(xT_d)[:, :, t0:t0 + N], [], ["xt"])
                _dma(C, "sp", ropet[:, :, :N], rope_d.rearrange("f p t -> p f t")[:, :, t0:t0 + N], [], ["ropet"])
                emit_norm1_tile(C, xt, "xt", N, w, A1, modv, sq, rstd, pss[1][0], "ps1", h1b, sq)
                for fm in range(8):
                    b = fm % 2
                    ps, pk = pss[2 + b]
                    ps2, pk2 = pss[4 + b]
                    for c in range(NCH):
                        _mm(C, ps[:, :N], wfm[:, c, fm * 128:(fm + 1) * 128], h1b[:, c, :N], c == 0, c == NCH - 1, ["wfm", "h1b"], [pk])
                    _cp(C, "act", raw[b][:, :N], ps[:, :N], [pk], ["raw%d" % b])
                    _mm(C, ps2[:, :N], rperm[:], raw[b][:, :N], True, True, ["rperm", "raw%d" % b], [pk2])
                    tb = 0 if fm < 4 else 2
                    _tt(C, "pool", r1[b][:, :N], raw[b][:, :N], ropet[:, tb, :N], ALU.mult, ["raw%d" % b, "ropet"], ["r1%d" % b])
                    _tt(C, "dve", r2[b][:, :N], ps2[:, :N], ropet[:, tb + 1, :N], ALU.mult, [pk2, "ropet"], ["r2%d" % b])
                    _tt(C, "pool", r1[b][:, :N], r1[b][:, :N], r2[b][:, :N], ALU.add, ["r1%d" % b, "r2%d" % b], ["r1%d" % b])
                    _dma(C, "sp", FT_d[fm, :, t0:t0 + N], r1[b][:, :N], ["r1%d" % b], ["FT_d"])
                for blk in range(N // 128):
                    for half in range(4):
                        b = (blk * 4 + half) % 2
                        ps, pk = pss[6 + b]
                        for c in range(NCH):
                            _mm(C, ps[:, :512], h1b[:, c, blk * 128:(blk + 1) * 128], wtm[:, c, half * 512:(half + 1) * 512], c == 0, c == NCH - 1, ["h1b", "wtm"], [pk])
                        _cp(C, "act" if half % 2 else "dve", tmo[b][:], ps[:, :512], [pk], ["tmo%d" % b])
                        _dma(C, "sp", TM_d[t0 + blk * 128:t0 + (blk + 1) * 128, half * 512:(half + 1) * 512], tmo[b][:], ["tmo%d" % b], ["TM_d"])
            P.barrier()
        C.st = st
        with ExitStack() as st3:
            C.st = st3
            AT = C.sb("AT", [128, T], MMDT); BT = C.sb("BT", [128, T], MMDT)
            Vraw = C.sb("Vraw", [128, NB, 256], MMDT); VP = C.sb("VP", [128, NB, 256], MMDT); OH = C.sb("OH", [128, NB, 256])
            gtab = C.sb("gtab", [128, 8, NB]); gainb = C.sb("gainb", [128, 1024])
            _dma(C, "sp", gtab[:], gtab_d, [], ["gtab", "g"])
            _dma(C, "sp", gainb[:], gainb_d, [], ["gainb"])
            colb = C.sb("colb", [128, NB])
            tabs = (C.sb("kf", [128, NB]), C.sb("rowf", [128, NB]), C.sb("ctab", [128, NB, NB]), C.sb("gF", [128, NB]), C.sb("gL", [128, NB]), None)
            wts = [(C.sb("wt%d" % i, [128, 128], MMDT), "wt%d" % i) for i in range(4)]
            Et2 = [(C.sb("Et%d" % i, [128, 128]), "Et%d" % i) for i in range(2)]; dg2 = [(C.sb("dg%d" % i, [128, 128]), "dg%d" % i) for i in range(2)]
            offb = C.sb("offb", [128, 256]); otmp = C.sb("otmp", [128, 256]); C.tailbuf = C.sb("tailbuf", [128, 256])
            bufs = (pss[0:4], [pss[4], pss[6]], pss[5], pss[7], wts, Et2, dg2, offb)
            Gt = C.sb("Gt", [128, 256]); cen = C.sb("cen", [128, 256]); st1 = C.sb("st1", [128, 4]); uT = C.sb("uT", [128, 2, 128], MMDT)
            TMv = TM_d.rearrange("(n p) w -> p n w", p=128)
            for h in range(4):
                _dma(C, "pool", AT[:], FT_d[4 + h], ["FT_d"], ["AT"])
                _dma(C, "pool", BT[:], FT_d[h], ["FT_d"], ["BT"])
                for q4 in range(2):
                    _dma(C, "pool", Vraw[:, q4 * 17:(q4 + 1) * 17, :], TMv[:, q4 * 17:(q4 + 1) * 17, h * 256:(h + 1) * 256], ["TM_d"], ["Vraw"])
                for d in range(2):
                    s = h * 2 + d
                    g = gtab[:, s, :]
                    _ts(C, "dve", colb[:], g, -1.0, None, ALU.mult, None, ["gtab"], ["colb"])
                    emit_stream_tables(C, g, colb, d, tabs, pss[7][0], "ps7", "")
                    for J in range(NB):
                        _ts(C, "pool" if J % 2 else "dve", VP[:, J, :], Vraw[:, J, :], tabs[0][:, J:J + 1], None, ALU.mult, None, ["Vraw", "tabskf"], ["VP"])
                    order = scan_order(d)

                    def out_fn(I, res, rk, d=d):
                        if d == 0:
                            _cp(C, "pool", OH[:, I, :], res[:, 0:256], [rk], ["OH%d" % I])
                        else:
                            _tt(C, "pool", OH[:, I, :], OH[:, I, :], res[:, 0:256], ALU.add, [rk, "OH%d" % I], ["OH%d" % I])
                    emit_stream_pipelined(C, order, AT, BT, VP, Vraw, 256, tabs, g, colb, d, bufs, "", std_tail(C, tabs, bufs, 256, out_fn))
                for I in range(NB):
                    k = "OH%d" % I
                    _dma(C, "sp", Gt[:], TM_d[I * 128:(I + 1) * 128, 1024 + h * 256:1024 + (h + 1) * 256], ["TM_d"], ["Gt"])
                    P.op("dve", (lambda I: lambda e: e.reduce_sum(out=st1[:, 0:1], in_=OH[:, I, :], axis=AX.X))(I), reads=[k], writes=["st1"])
                    _ts(C, "dve", st1[:, 0:1], st1[:, 0:1], -1.0 / 256, None, ALU.mult, None, ["st1"], ["st1"])
                    _ts(C, "dve", cen[:], OH[:, I, :], st1[:, 0:1], None, ALU.add, None, [k, "st1"], ["cen"])
                    P.op("act", lambda e: e.activation(out=otmp[:], in_=cen[:], func=AF.Square, accum_out=st1[:, 1:2]), reads=["cen"], writes=["otmp", "st1b"])
                    _act(C, st1[:, 2:3], st1[:, 1:2], AF.Sqrt, ["st1b"], ["st1c"], bias=EPS, scale=1.0 / 256)
                    P.op("dve", lambda e: e.reciprocal(out=st1[:, 3:4], in_=st1[:, 2:3]), reads=["st1c"], writes=["st1d"])
                    _act(C, Gt[:], Gt[:], AF.Silu, ["Gt"], ["Gt"])
                    _tt(C, "pool", Gt[:], Gt[:], gainb[:, h * 256:(h + 1) * 256], ALU.mult, ["Gt", "gainb"], ["Gt"])
                    _stt(C, cen[:], cen[:], st1[:, 3:4], Gt[:], ALU.mult, ALU.mult, ["cen", "st1d", "Gt"], ["cen"])
                    for ec in range(2):
                        _tr(C, pss[7][0][:, ec * 128:(ec + 1) * 128], cen[:, ec * 128:(ec + 1) * 128], ["cen"], ["ps7"])
                    _cp(C, "act", uT[:].rearrange("p a b -> p (a b)"), pss[7][0][:, 0:256], ["ps7"], ["uT"])
                    for ec in range(2):
                        _dma(C, "sp", UT_d[h * 2 + ec, :, I * 128:(I + 1) * 128], uT[:, ec, :], ["uT"], ["UT_d"])
            P.barrier()
        C.st = st
        with ExitStack() as st4:
            C.st = st4
            wout = C.sb("wout", [128, 8, D], MMDT)
            for hc in range(8):
                _dma(C, "pool", wout[:, hc, :], wout_d[hc * 128:(hc + 1) * 128, :], [], ["wout"])
            ut = [C.sb("utile%d" % i, [128, 8, 512], MMDT) for i in range(2)]
            yo = [C.sb("yo%d" % i, [128, 512]) for i in range(2)]
            for ti, (t0, N, w) in enumerate(tiles):
                b = ti % 2
                _dma(C, "sp", ut[b][:, :, :N], UT_d.rearrange("f p t -> p f t")[:, :, t0:t0 + N], [], ["ut%d" % b])
                for dmc in range(NCH):
                    pb = dmc % 2
                    ps, pk = pss[pb]
                    for hc in range(8):
                        _mm(C, ps[:, :N], wout[:, hc, dmc * 128:(dmc + 1) * 128], ut[b][:, hc, :N], hc == 0, hc == 7, ["wout", "ut%d" % b], [pk])
                    _cp(C, "act" if pb else "dve", yo[pb][:, :N], ps[:, :N], [pk], ["yo%d" % pb])
                    if env is not None:
                        env.ywrite(C, dmc, t0, N, yo[pb], "yo%d" % pb)
                    else:
                        _dma(C, "sp", yT_d[dmc * 128:(dmc + 1) * 128, t0:t0 + N], yo[pb][:, :N], ["yo%d" % pb], ["yT%d_%d" % (ti, dmc)])
            if env is None:
                P.finish()
            else:
                P.barrier()
        if env is None:
            P.emit()
    return nc


RET_HEADS = 8
ROPE_BASE = 10000.0


def mixer_common_inputs(layer, inputs, b, x_lat, x_ctx):
    pc = lambda v: np.ascontiguousarray(v.reshape(-1, 128).T)
    cv = np.stack([pc(inputs["c"][b]), pc(inputs["c_ctx"])], axis=-1)
    xT = np.ascontiguousarray(np.concatenate([x_ctx[b], x_lat[b]], 0).T)
    return {"xT": xT, "cv": np.ascontiguousarray(cv), "modw": np.ascontiguousarray(inputs["mod_w"][layer][:, 0:2048]),
            "modb": pc(inputs["mod_b"][layer][0:2048]), "n1g": pc(inputs["norm1_g"][layer])}


def odd_inputs(layer, inputs, b, hg, x_lat, x_ctx):
    j = layer // 2
    m = mixer_common_inputs(layer, inputs, b, x_lat, x_ctx)
    w_in = inputs["od_w_in"][j]
    hs = slice(hg * 4, hg * 4 + 4)
    wq = w_in[:, 0:1024].reshape(D, 8, 128)[:, hs].reshape(D, 512)
    wk = w_in[:, 1024:2048].reshape(D, 8, 128)[:, hs].reshape(D, 512)
    wv = w_in[:, 2048:4096].reshape(D, 8, 256)[:, hs].reshape(D, 1024)
    wg = w_in[:, 4096:6144].reshape(D, 8, 256)[:, hs].reshape(D, 1024)
    m["wfm"] = np.ascontiguousarray(np.concatenate([wq, wk], 1))
    m["wtm"] = np.ascontiguousarray(np.concatenate([wv, wg], 1))
    m["wout"] = np.ascontiguousarray(inputs["od_w_out"][j].reshape(8, 256, D)[hs].reshape(1024, D))
    half = 64
    freqs = (np.float32(ROPE_BASE) ** (-np.arange(half, dtype=np.float32) / np.float32(half))).astype(np.float32)
    pos = np.arange(T_ALL, dtype=np.float32)
    ang = (pos[:, None] * freqs[None, :]).astype(np.float32)
    cos, sin = np.cos(ang).astype(np.float32).T, np.sin(ang).astype(np.float32).T
    cosT = np.concatenate([cos, cos], 0); sinT = np.concatenate([-sin, sin], 0)
    ks = np.float32(128.0 ** -0.5)
    m["rope"] = np.ascontiguousarray(np.stack([cosT, sinT, cosT * ks, sinT * ks], 0).astype(np.float32))
    m["rperm"] = np.ascontiguousarray(np.roll(np.eye(128, dtype=np.float32), 64, axis=0))
    expo = 5.0 + np.arange(RET_HEADS, dtype=np.float32)
    lg_f = np.log1p(-np.exp2(-expo)).astype(np.float32)
    lg_b = np.log1p(-np.exp2(-expo[::-1])).astype(np.float32)
    sp_f = np.arange(T_ALL, dtype=np.float32)
    sp_b = np.concatenate([255.0 - np.arange(256), 256.0 + 4095.0 - np.arange(4096)]).astype(np.float32)
    gt = np.zeros((128, 8, NB), np.float32)
    for hl in range(4):
        h = hg * 4 + hl
        gt[:, hl * 2 + 0, :] = (sp_f * lg_f[h]).reshape(NB, 128).T
        gt[:, hl * 2 + 1, :] = (sp_b * lg_b[h]).reshape(NB, 128).T
    m["gtab"] = gt
    m["gainb"] = np.ascontiguousarray(np.broadcast_to(inputs["ret_norm_g"][j].reshape(8, 256)[hs].reshape(1, 1024), (128, 1024)))
    return m


def emit_offdiag(C, I, order_pos, order, AT, BT, VP, dvp, ctab, bufs, sid):
    psS, psO, psD, psR, wts, Et, dg, offb = bufs
    before = order[:order_pos]
    isl = slice(I * 128, (I + 1) * 128)
    kk = "tabs%s" % sid
    for n, J in enumerate(before):
        sb_i = C.rot % len(psS)
        C.rot += 1
        ps, pk = psS[sb_i]
        wt, wk = wts[sb_i % len(wts)]
        _mm(C, ps[:, 0:128], AT[:, J * 128:(J + 1) * 128], BT[:, isl], True, True, ["AT" + sid, "BT" + sid], [pk])
        _ts(C, "dve", wt[:], ps[:, 0:128], ctab[:, I, J:J + 1], None, ALU.mult, None, [pk, kk + "ctab"], [wk])
        _mm(C, psO[0][:, 0:dvp], wt[:], VP[:, J, :], n == 0, n == len(before) - 1, [wk, "VP" + sid], [psO[1]])
    return len(before) > 0


def emit_cumsum(C, x, xkey, direction, out, outkey, tmpA, tmpB, psb, pskey):
    n = NB * 4
    flat = lambda t: t[:].rearrange("p a b -> p (a b)")
    m = C.masks["F" if direction == 0 else "B"]
    mkey = "maskF" if direction == 0 else "maskB"
    _mm(C, psb[:, 0:n], C.ones[:], flat(x), True, True, ["ones", xkey], [pskey])
    _cp(C, "dve", flat(tmpA), psb[:, 0:n], [pskey], ["cs_tot"])
    _mm(C, psb[:, 0:n], m[:], flat(x), True, True, [mkey, xkey], [pskey])
    for k in range(4):
        C.P.op("dve", (lambda k: lambda e: e.tensor_tensor_scan(out=tmpB[:, :, k], data0=C.ones[:, 0:NB], data1=tmpA[:, :, k], initial=0.0,
                                                                 op0=ALU.mult, op1=ALU.add))(k), reads=["cs_tot", "ones"], writes=["cs_cs"])
    if direction == 0:
        _tt(C, "dve", flat(tmpB), flat(tmpB), flat(tmpA), ALU.subtract, ["cs_cs", "cs_tot"], ["cs_carry"])
    else:
        _tt(C, "dve", tmpA[:, 0, :], tmpB[:, 1, :], tmpB[:, NB - 1, :], ALU.add, ["cs_cs", "cs_tot"], ["cs_tot"])
        _cp(C, "dve", tmpA[:, 2, :], tmpA[:, 1, :], ["cs_tot"], ["cs_tot"])
        _tt(C, "dve", tmpB[:, 2:NB, :], tmpA[:, 0:1, :].to_broadcast([128, NB - 2, 4]), tmpB[:, 2:NB, :], ALU.subtract, ["cs_cs", "cs_tot"], ["cs_carry"])
        _cp(C, "dve", tmpB[:, 0, :], tmpA[:, 2, :], ["cs_tot", "cs_carry"], ["cs_carry"])
        C.P.op("dve", lambda e: e.memset(tmpB[:, 1, :], 0.0), reads=["cs_carry"], writes=["cs_carry"])
    _tt(C, "dve", flat(out), psb[:, 0:n], flat(tmpB), ALU.add, [pskey, "cs_carry"], [outkey])


def build_mixer_even(env=None):
    T = T_ALL
    sfx = env.sfx if env else ""
    nc = env.nc if env else bass.Bass("TRN2", target_bir_lowering=False)
    env_nc_holder[0] = nc
    din = _din_factory(nc, env)
    xT_d = din("xT", [D, T]); cv_d = din("cv", [128, NCH, 2]); modw_d = din("modw", [D, 2048]); modb_d = din("modb", [128, 16])
    n1g_d = din("n1g", [128, NCH])
    wfm_d = din("wfm", [D, 1280]); wtm_d = din("wtm", [D, 784]); wout_d = din("wout", [512, D])
    convw_d = din("convw", [128, 6, 3]); gconst_d = din("gconst", [128, 4, 4]); gainb_d = din("gainb", [128, 512])
    yT_d = None if env else nc.dram_tensor("yT", [D, T], F32, kind="ExternalOutput").ap()
    FT_d = nc.dram_tensor("FT_scr" + sfx, [10, 128, T], F32, kind="Internal").ap()
    TM_d = nc.dram_tensor("TM_scr" + sfx, [T, 784], F32, kind="Internal").ap()
    UT_d = nc.dram_tensor("UT_scr" + sfx, [4, 128, T], MMDT, kind="Internal").ap()
    r3 = lambda ap: ap.rearrange("(c p) n -> p c n", p=128)
    tiles = mixer_tiles()
    with ExitStack() as st:
        C = _begin(env, st, True)
        P = C.P
        C.rot = 0
        pss = C.pss_all
        n1g = C.sb("n1g", [128, NCH]); A1 = C.sb("A1", [128, NCH, 2])
        _dma(C, "sp", n1g[:], n1g_d, [], ["n1g"])
        with ExitStack() as st2:
            C.st = st2
            modv = emit_mod(C, cv_d, modw_d, modb_d, 2048, pss[0][0], "ps0")
            for w in range(2):
                _stt(C, A1[:, :, w], modv[:, 8:16, w], 1.0, n1g[:], ALU.add, ALU.mult, ["modv", "n1g"], ["A1"])
            wfm = C.sb("wfm", [128, NCH, 1280], MMDT); wtm = C.sb("wtm", [128, NCH, 784], MMDT)
            convw = C.sb("convw", [128, 6, 3])
            _dma(C, "sp", convw[:], convw_d, [], ["convw"])
            for c in range(NCH):
                _dma(C, "pool", wfm[:, c, :], wfm_d[c * 128:(c + 1) * 128, :], [], ["wfm"])
                _dma(C, "pool", wtm[:, c, :], wtm_d[c * 128:(c + 1) * 128, :], [], ["wtm"])
            xt = C.sb("xt", [128, NCH, 512]); sq = C.sb("sq", [128, NCH, 512]); rstd = C.sb("rstd", [128, 512])
            h1b = C.sb("h1b", [128, NCH, 512], MMDT)
            raw = [C.sb("raw%d" % i, [128, 512]) for i in range(2)]
            r1 = [C.sb("r1%d" % i, [128, 512]) for i in range(2)]
            r2 = [C.sb("r2%d" % i, [128, 512]) for i in range(2)]
            tmo = [C.sb("tmo%d" % i, [128, 512]) for i in range(2)]
            for ti, (t0, N, w) in enumerate(tiles):
                if env is not None and env.xload is not None:
                    env.xload(C, xt, t0, N)
                else:
                    _dma(C, "sp", xt[:, :, :N], r3(xT_d)[:, :, t0:t0 + N], [], ["xt"])
                emit_norm1_tile(C, xt, "xt", N, w, A1, modv, sq, rstd, pss[1][0], "ps1", h1b, sq)
                R, RL = (1, 256) if w == 1 else (N // 64, 64)
                v3 = lambda t: t[:, :N].rearrange("p (r l) -> p r l", l=RL)
                for fm in range(10):
                    b = fm % 2
                    ps, pk = pss[2 + b]
                    ps2, pk2 = pss[4 + b]
                    kr, k1, k2 = "raw%d" % b, "r1%d" % b, "r2%d" % b
                    for c in range(NCH):
                        _mm(C, ps[:, :N], wfm[:, c, fm * 128:(fm + 1) * 128], h1b[:, c, :N], c == 0, c == NCH - 1, ["wfm", "h1b"], [pk])
                    if fm < 6:
                        _cp(C, "act", raw[b][:, :N], ps[:, :N], [pk], [kr])
                        _ts(C, "dve", r1[b][:, :N], raw[b][:, :N], convw[:, fm, 1:2], None, ALU.mult, None, [kr, "convw"], [k1])
                        _stt(C, v3(r1[b])[:, :, 1:RL], v3(raw[b])[:, :, 0:RL - 1], convw[:, fm, 0:1], v3(r1[b])[:, :, 1:RL], ALU.mult, ALU.add, [kr, k1, "convw"], [k1])
                        _stt(C, v3(r1[b])[:, :, 0:RL - 1], v3(raw[b])[:, :, 1:RL], convw[:, fm, 2:3], v3(r1[b])[:, :, 0:RL - 1], ALU.mult, ALU.add, [kr, k1, "convw"], [k1])
                        _act(C, r1[b][:, :N], r1[b][:, :N], AF.Silu, [k1], [k1])
                        if fm < 4:
                            _act(C, r2[b][:, :N], r1[b][:, :N], AF.Square, [k1], [k2])
                            _mm(C, ps2[:, :N], C.ones[:], r2[b][:, :N], True, True, ["ones", k2], [pk2])
                            _act(C, r2[b][:, :N], ps2[:, :N], AF.Sqrt, [pk2], [k2], bias=EPS, scale=1.0)
                            P.op("dve", (lambda b, N: lambda e: e.reciprocal(out=r2[b][:, :N], in_=r2[b][:, :N]))(b, N), reads=[k2], writes=[k2])
                            _stt(C, r1[b][:, :N], r1[b][:, :N], (128.0 ** -0.5) if fm < 2 else 1.0, r2[b][:, :N], ALU.mult, ALU.mult, [k1, k2], [k1])
                    elif fm < 8:
                        _cp(C, "act", r1[b][:, :N], ps[:, :N], [pk], [k1])
                    else:
                        _act(C, r1[b][:, :N], ps[:, :N], AF.Identity, [pk], [k1], scale=128.0 ** -0.5)
                    _dma(C, "sp", FT_d[fm, :, t0:t0 + N], r1[b][:, :N], [k1], ["FT_d"])
                for blk in range(N // 128):
                    for half, (c0, c1) in enumerate(((0, 512), (512, 784))):
                        b = (blk * 2 + half) % 2
                        ps, pk = pss[6 + b]
                        for c in range(NCH):
                            _mm(C, ps[:, :c1 - c0], h1b[:, c, blk * 128:(blk + 1) * 128], wtm[:, c, c0:c1], c == 0, c == NCH - 1, ["h1b", "wtm"], [pk])
                        _cp(C, "act" if half % 2 else "dve", tmo[b][:, :c1 - c0], ps[:, :c1 - c0], [pk], ["tmo%d" % b])
                        _dma(C, "sp", TM_d[t0 + blk * 128:t0 + (blk + 1) * 128, c0:c1], tmo[b][:, :c1 - c0], ["tmo%d" % b], ["TM_d"])
            P.barrier()
        C.st = st
        with ExitStack() as st3:
            C.st = st3
            TMv = TM_d.rearrange("(n p) w -> p n w", p=128)
            gates = C.sb("gates", [128, NB, 16]); gconst = C.sb("gconst", [128, 4, 4]); gainb = C.sb("gainb", [128, 512])
            _dma(C, "sp", gates[:], TMv[:, :, 768:784], [], ["gates"])
            _dma(C, "sp", gconst[:], gconst_d, [], ["gconst"])
            _dma(C, "sp", gainb[:], gainb_d, [], ["gainb"])
            la = C.sb("la", [128, NB, 4]); beta = C.sb("beta", [128, NB, 4]); ic = C.sb("ic", [128, NB, 4]); lf = C.sb("lf", [128, NB, 4])
            gG = C.sb("gG", [128, NB, 4]); gF_ = C.sb("gFm", [128, NB, 4]); tA = C.sb("tA", [128, NB, 4]); tB = C.sb("tB", [128, NB, 4])
            arate = C.sb("arate", [128, 4]); cmax = C.sb("cmax", [128, 4]); ecneg = C.sb("ecneg", [128, 4]); c1 = C.sb("c1", [128, 4])
            bc = lambda col: gconst[:, col:col + 1, :].to_broadcast([128, NB, 4])
            _act(C, arate[:], gconst[:, 0, :], AF.Exp, ["gconst"], ["arate"])
            _tt(C, "dve", la[:], gates[:, :, 0:4], bc(1), ALU.add, ["gates", "gconst"], ["la"])
            _act(C, la[:], la[:], AF.Exp, ["la"], ["la"])
            _act(C, la[:], la[:], AF.Ln, ["la"], ["la"], bias=1.0)
            _tt(C, "dve", la[:], la[:], arate[:, None, :].to_broadcast([128, NB, 4]), ALU.mult, ["la", "arate"], ["la"])
            _ts(C, "dve", la[:], la[:], -1.0, None, ALU.mult, None, ["la"], ["la"])
            _act(C, beta[:], gates[:, :, 4:8], AF.Sigmoid, ["gates"], ["beta"])
            _tt(C, "dve", ic[:], gates[:, :, 8:12], bc(2), ALU.add, ["gates", "gconst"], ["ic"])
            _tt(C, "dve", lf[:], gates[:, :, 12:16], bc(3), ALU.add, ["gates", "gconst"], ["lf"])
            _act(C, lf[:], lf[:], AF.Exp, ["lf"], ["lf"], scale=-1.0)
            _act(C, lf[:], lf[:], AF.Ln, ["lf"], ["lf"], bias=1.0)
            _ts(C, "dve", lf[:], lf[:], -1.0, None, ALU.mult, None, ["lf"], ["lf"])
            P.op("dve", lambda e: e.tensor_reduce(out=c1[:], in_=ic[:].rearrange("p n k -> p k n"), axis=AX.X, op=ALU.max), reads=["ic"], writes=["c1"])
            _tr(C, pss[7][0][0:4, 0:128], c1[:], ["c1"], ["ps7"])
            c2 = C.sb("c2", [4, 1]); c3 = C.sb("c3", [4, 4])
            P.op("dve", lambda e: e.reduce_max(out=c2[:], in_=pss[7][0][0:4, 0:128], axis=AX.X), reads=["ps7"], writes=["c2"])
            _ts(C, "dve", c3[:], C.ident[0:4, 0:4], c2[:, 0:1], None, ALU.mult, None, ["ident", "c2"], ["c3"])
            _mm(C, pss[7][0][:, 0:4], C.ones[0:4, :], c3[:], True, True, ["ones", "c3"], ["ps7"])
            _cp(C, "dve", cmax[:], pss[7][0][:, 0:4], ["ps7"], ["cmax"])
            _act(C, ecneg[:], cmax[:], AF.Exp, ["cmax"], ["ecneg"], scale=-1.0)
            ncmax = C.sb("ncmax", [128, 4])
            _ts(C, "dve", ncmax[:], cmax[:], -1.0, None, ALU.mult, None, ["cmax"], ["ncmax"])

            KT = C.sb("KT", [128, T]); QT = C.sb("QT", [128, T], MMDT); VT = C.sb("VT", [128, T]); KTb = C.sb("KTb", [128, T], MMDT)
            Xb = C.sb("Xb", [128, NB, 129], MMDT); XPb = C.sb("XPb", [128, NB, 129], MMDT)
            gdn_heads = 2 if EVEN_STOP >= 3 else 0
            ml_heads = 2 if EVEN_STOP >= 7 else 0
            Vtm = C.sb("Vtm", [128, NB, 129]); X = C.sb("X", [128, NB, 129]); XP = C.sb("XP", [128, NB, 129])
            TT = C.sb("TT", [128, NB, 128]); OH = C.sb("OH", [128, NB, 128])
            g = C.sb("g", [128, NB]); colb = C.sb("colb", [128, NB]); rowfb = C.sb("rowfb", [128, NB])
            tabs = (C.sb("kf", [128, NB]), C.sb("rowf", [128, NB]), C.sb("ctab", [128, NB, NB]), C.sb("gF", [128, NB]), C.sb("gL", [128, NB]), None)
            kf, rowf, ctab = tabs[0], tabs[1], tabs[2]
            wts = [(C.sb("wt%d" % i, [128, 128]), "wt%d" % i) for i in range(4)]
            wtsb = [(C.sb("wtb%d" % i, [128, 128], MMDT), "wtb%d" % i) for i in range(4)]
            Et = C.sb("Et", [128, 128]); dg = C.sb("dg", [128, 128]); offb = C.sb("offb", [128, 129]); otmp = C.sb("otmp", [128, 129])
            Et2 = [(C.sb("Etp%d" % i, [128, 128]), "Etp%d" % i) for i in range(2)]; dg2 = [(C.sb("dgp%d" % i, [128, 128]), "dgp%d" % i) for i in range(2)]
            C.tailbuf = C.sb("tailbuf", [128, 129])
            Lm = C.sb("Lm", [128, 128]); Am = [C.sb("Am%d" % i, [128, 128]) for i in range(2)]; Bm = [C.sb("Bm%d" % i, [128, 128]) for i in range(2)]
            Pm = C.sb("Pm", [128, 128]); Rm = C.sb("Rm", [128, 128])
            bufs = (pss[0:4], pss[4], pss[5], pss[6], wts, Et, dg, offb)
            bufsb = (pss[0:4], pss[4], pss[5], pss[6], wtsb, Et, dg, offb)
            pbufs = (pss[0:4], [pss[4], pss[6]], pss[5], pss[7], wts, Et2, dg2, offb)
            pbufsb = (pss[0:4], [pss[4], pss[6]], pss[5], pss[7], wtsb, Et2, dg2, offb)
            Gt = C.sb("Gt", [128, 128]); cen = C.sb("cen", [128, 128]); st1 = C.sb("st1", [128, 4]); uT = C.sb("uT", [128, 128], MMDT)
            ps7 = pss[7][0]

            def head_tail(hidx, gate_col, gate_func, gain_off):
                for I in range(NB):
                    k = "OH%d" % I
                    _dma(C, "sp", Gt[:], TM_d[I * 128:(I + 1) * 128, gate_col:gate_col + 128], ["TM_d"], ["Gt"])
                    P.op("act", (lambda I: lambda e: e.activation(out=otmp[:, 0:128], in_=OH[:, I, :], func=AF.Square, accum_out=st1[:, 1:2]))(I), reads=[k], writes=["otmp", "st1b"])
                    _act(C, st1[:, 2:3], st1[:, 1:2], AF.Sqrt, ["st1b"], ["st1c"], bias=EPS, scale=1.0 / 128)
                    P.op("dve", lambda e: e.reciprocal(out=st1[:, 3:4], in_=st1[:, 2:3]), reads=["st1c"], writes=["st1d"])
                    _act(C, Gt[:], Gt[:], gate_func, ["Gt"], ["Gt"])
                    _tt(C, "pool", Gt[:], Gt[:], gainb[:, gain_off:gain_off + 128], ALU.mult, ["Gt", "gainb"], ["Gt"])
                    _stt(C, cen[:], OH[:, I, :], st1[:, 3:4], Gt[:], ALU.mult, ALU.mult, [k, "st1d", "Gt"], ["cen"])
                    _tr(C, ps7[:, 0:128], cen[:], ["cen"], ["ps7"])
                    _cp(C, "act", uT[:], ps7[:, 0:128], ["ps7"], ["uT"])
                    _dma(C, "sp", UT_d[hidx, :, I * 128:(I + 1) * 128], uT[:], ["uT"], ["UT_d"])

            for hl in range(gdn_heads):
                _dma(C, "sp", KT[:], FT_d[2 + hl], ["FT_d"], ["AT"])
                _dma(C, "pool", KTb[:], FT_d[2 + hl], ["FT_d"], ["ATb"])
                _dma(C, "pool", QT[:], FT_d[hl], ["FT_d"], ["BTb"])
                _dma(C, "sp", VT[:], FT_d[4 + hl], ["FT_d"], ["VT"])
                for I in range(NB):
                    _tr(C, ps7[:, 0:128], VT[:, I * 128:(I + 1) * 128], ["VT"], ["ps7"])
                    _cp(C, "act", Vtm[:, I, 0:128], ps7[:, 0:128], ["ps7"], ["Vtm"])
                for d in range(2):
                    col = d * 2 + hl
                    order = scan_order(d)
                    emit_cumsum(C, la, "la", d, gG, "gG", tA, tB, ps7, "ps7")
                    _cp(C, "dve", g[:], gG[:, :, col], ["gG"], ["g"])
                    _ts(C, "dve", colb[:], g[:], -1.0, None, ALU.mult, None, ["g"], ["colb"])
                    emit_stream_tables(C, g[:], colb, d, tabs, ps7, "ps7", "")
                    _tt(C, "dve", rowfb[:], rowf[:], beta[:, :, col], ALU.mult, ["tabsrowf", "beta"], ["rowfb"])
                    smask = C.masks["Bs" if d == 0 else "Fs"]
                    smkey = "maskBs" if d == 0 else "maskFs"
                    for I in range(NB):
                        isl = slice(I * 128, (I + 1) * 128)
                        _ts(C, "dve", dg[:], C.ident[:], g[:, I:I + 1], None, ALU.mult, None, ["ident", "g"], ["dg"])
                        _mm(C, pss[6][0][:, 0:128], C.ones[:], dg[:], True, True, ["ones", "dg"], ["ps6"])
                        _ts(C, "dve", Et[:], pss[6][0][:, 0:128], colb[:, I:I + 1], 0.0, ALU.add, ALU.max, ["ps6", "colb"], ["Et"])
                        _act(C, Et[:], Et[:], AF.Exp, ["Et"], ["Et"], scale=-1.0)
                        _tt(C, "pool", Et[:], Et[:], smask[:], ALU.mult, ["Et", smkey], ["Et"])
                        _mm(C, pss[0][0][:, 0:128], KT[:, isl], KT[:, isl], True, True, ["AT"], ["ps0"])
                        _stt(C, Bm[0][:], pss[0][0][:, 0:128], beta[:, I, col:col + 1], Et[:], ALU.mult, ALU.mult, ["ps0", "beta", "Et"], ["Bm0"])
                        _tr(C, pss[1][0][:, 0:128], Bm[0][:], ["Bm0"], ["ps1"])
                        _cp(C, "act", Am[0][:], pss[1][0][:, 0:128], ["ps1"], ["Am0"])
                        _tt(C, "dve", Pm[:], C.ident[:], Am[0][:], ALU.subtract, ["ident", "Am0"], ["Pm"])
                        for lev in range(1, 7):
                            a0, a1 = (lev - 1) % 2, lev % 2
                            _mm(C, pss[2][0][:, 0:128], Bm[a0][:], Am[a0][:], True, True, ["Bm%d" % a0, "Am%d" % a0], ["ps2"])
                            _mm(C, pss[3][0][:, 0:128], Am[a0][:], Bm[a0][:], True, True, ["Bm%d" % a0, "Am%d" % a0], ["ps3"])
                            _cp(C, "act", Am[a1][:], pss[2][0][:, 0:128], ["ps2"], ["Am%d" % a1])
                            _cp(C, "dve", Bm[a1][:], pss[3][0][:, 0:128], ["ps3"], ["Bm%d" % a1])
                            _mm(C, pss[1][0][:, 0:128], Bm[a1][:], Pm[:], True, True, ["Bm%d" % a1, "Pm"], ["ps1"])
                            _tt(C, "dve", Pm[:], Pm[:], pss[1][0][:, 0:128], ALU.add, ["Pm", "ps1"], ["Pm"])
                        _cp(C, "pool", TT[:, I, :], Pm[:], ["Pm"], ["TT"])
                    def tail1(I, pos, has_off, psO, psOkey, col=col):
                        _ts(C, "dve", Rm[:], Vtm[:, I, 0:128], beta[:, I, col:col + 1], None, ALU.mult, None, ["Vtm", "beta"], ["Rm"])
                        if has_off:
                            _ts(C, "dve", offb[:, 0:128], psO[:, 0:128], rowfb[:, I:I + 1], None, ALU.mult, None, [psOkey, "rowfb"], ["offb"])
                            _tt(C, "pool", Rm[:], Rm[:], offb[:, 0:128], ALU.subtract, ["Rm", "offb"], ["Rm"])
                        _mm(C, pss[5][0][:, 0:128], TT[:, I, :], Rm[:], True, True, ["TT", "Rm"], ["ps5"])
                        _cp(C, "act", X[:, I, 0:128], pss[5][0][:, 0:128], ["ps5"], ["Vraw"])
                        _ts(C, "dve", XP[:, I, 0:128], pss[5][0][:, 0:128], kf[:, I:I + 1], None, ALU.mult, None, ["ps5", "tabskf"], ["VP"])
                        _cp(C, "pool", Xb[:, I, 0:128], X[:, I, 0:128], ["Vraw"], ["Vrawb"])
                        _cp(C, "pool", XPb[:, I, 0:128], XP[:, I, 0:128], ["VP"], ["VPb"])
                    emit_stream_pipelined(C, order, KT, KT, XP[:, :, 0:128], X[:, :, 0:128], 128, tabs, g, colb, d, pbufs, "", tail1, diag=False)
                    def out2(I, res, rk, d=d):
                        if d == 0:
                            _cp(C, "pool", OH[:, I, :], res[:, 0:128], [rk], ["OH%d" % I])
                        else:
                            _tt(C, "pool", OH[:, I, :], OH[:, I, :], res[:, 0:128], ALU.add, [rk, "OH%d" % I], ["OH%d" % I])
                    emit_stream_pipelined(C, order, KTb, QT, XPb[:, :, 0:128], Xb[:, :, 0:128], 128, tabs, g, colb, d, pbufsb, "b", std_tail(C, tabs, pbufsb, 128, out2))
                if EVEN_STOP >= 6:
                    head_tail(hl, hl * 128, AF.Silu, 0)
            for hl in range(ml_heads):
                _dma(C, "pool", KTb[:], FT_d[8 + hl], ["FT_d"], ["ATb"])
                _dma(C, "pool", QT[:], FT_d[6 + hl], ["FT_d"], ["BTb"])
                _dma(C, "sp", Vtm[:, :, 0:128], TMv[:, :, 256 + hl * 128:256 + (hl + 1) * 128], ["TM_d"], ["Vraw", "Vtm"])
                P.op("pool", lambda e: e.memset(Vtm[:, :, 128:129], 1.0), reads=["Vraw"], writes=["Vraw"])
                for J in range(NB):
                    _cp(C, "act", Xb[:, J, :], Vtm[:, J, :], ["Vraw"], ["Vrawb"])
                for d in range(2):
                    col = d * 2 + hl
                    order = scan_order(d)
                    emit_cumsum(C, lf, "lf", d, gF_, "gFm", tA, tB, ps7, "ps7")
                    _cp(C, "dve", g[:], gF_[:, :, col], ["gFm"], ["g"])
                    _tt(C, "dve", colb[:], ic[:, :, col], g[:], ALU.subtract, ["ic", "g"], ["colb"])
                    _ts(C, "dve", colb[:], colb[:], ncmax[:, col:col + 1], None, ALU.add, None, ["colb", "ncmax"], ["colb"])
                    emit_stream_tables(C, g[:], colb, d, tabs, ps7, "ps7", "")
                    for J in range(NB):
                        _ts(C, "pool" if J % 2 else "dve", XPb[:, J, :], Vtm[:, J, :], kf[:, J:J + 1], None, ALU.mult, None, ["Vraw", "tabskf"], ["VPb"])
                    def out3(I, res, rk, d=d, col=col):
                        _act(C, st1[:, 0:1], res[:, 128:129], AF.Abs, [rk], ["st1"])
                        _ts(C, "dve", st1[:, 0:1], st1[:, 0:1], ecneg[:, col:col + 1], None, ALU.max, None, ["st1", "ecneg"], ["st1"])
                        P.op("dve", lambda e: e.reciprocal(out=st1[:, 0:1], in_=st1[:, 0:1]), reads=["st1"], writes=["st1"])
                        if d == 0:
                            _ts(C, "dve", OH[:, I, :], res[:, 0:128], st1[:, 0:1], None, ALU.mult, None, [rk, "st1"], ["OH%d" % I])
                        else:
                            _stt(C, OH[:, I, :], res[:, 0:128], st1[:, 0:1], OH[:, I, :], ALU.mult, ALU.add, [rk, "st1", "OH%d" % I], ["OH%d" % I])
                    emit_stream_pipelined(C, order, KTb, QT, XPb, Xb, 129, tabs, g, colb, d, pbufsb, "b", std_tail(C, tabs, pbufsb, 129, out3))
                head_tail(2 + hl, 512 + hl * 128, AF.Sigmoid, 128 + hl * 128)
            P.barrier()
        C.st = st
        with ExitStack() as st4:
            C.st = st4
            wout = C.sb("wout", [128, 4, D], MMDT)
            for hc in range(4):
                _dma(C, "pool", wout[:, hc, :], wout_d[hc * 128:(hc + 1) * 128, :], [], ["wout"])
            ut = [C.sb("utile%d" % i, [128, 4, 512], MMDT) for i in range(2)]
            yo = [C.sb("yo%d" % i, [128, 512]) for i in range(2)]
            for ti, (t0, N, w) in enumerate(tiles):
                b = ti % 2
                _dma(C, "sp", ut[b][:, :, :N], UT_d.rearrange("f p t -> p f t")[:, :, t0:t0 + N], [], ["ut%d" % b])
                for dmc in range(NCH):
                    pb = dmc % 2
                    ps, pk = pss[pb]
                    for hc in range(4):
                        _mm(C, ps[:, :N], wout[:, hc, dmc * 128:(dmc + 1) * 128], ut[b][:, hc, :N], hc == 0, hc == 3, ["wout", "ut%d" % b], [pk])
                    _cp(C, "act" if pb else "dve", yo[pb][:, :N], ps[:, :N], [pk], ["yo%d" % pb])
                    if env is not None:
                        env.ywrite(C, dmc, t0, N, yo[pb], "yo%d" % pb)
                    else:
                        _dma(C, "sp", yT_d[dmc * 128:(dmc + 1) * 128, t0:t0 + N], yo[pb][:, :N], ["yo%d" % pb], ["yT%d_%d" % (ti, dmc)])
            if env is None:
                P.finish()
            else:
                P.barrier()
        if env is None:
            P.emit()
    return nc


def even_inputs(layer, inputs, b, hg, x_lat, x_ctx):
    j = layer // 2
    m = mixer_common_inputs(layer, inputs, b, x_lat, x_ctx)
    w_in = inputs["ev_w_in"][j]
    hs = [hg * 2, hg * 2 + 1]
    hcols = lambda off, h: w_in[:, off + h * 128: off + (h + 1) * 128]
    fm = [hcols(0, h) for h in hs] + [hcols(512, h) for h in hs] + [hcols(1024, h) for h in hs] + \
         [hcols(2064, h) for h in hs] + [hcols(2576, h) for h in hs]
    m["wfm"] = np.ascontiguousarray(np.concatenate(fm, 1))
    gate_cols = []
    for off in (2048, 2056, 4112, 4120):
        for d in range(2):
            for h in hs:
                gate_cols.append(w_in[:, off + d * 4 + h: off + d * 4 + h + 1])
    tm = [hcols(1536, h) for h in hs] + [hcols(3088, h) for h in hs] + [hcols(3600, h) for h in hs] + gate_cols
    m["wtm"] = np.ascontiguousarray(np.concatenate(tm, 1))
    w_out = inputs["ev_w_out"][j]
    m["wout"] = np.ascontiguousarray(np.concatenate([w_out[h * 128:(h + 1) * 128] for h in hs] + [w_out[512 + h * 128:512 + (h + 1) * 128] for h in hs], 0))
    cw = inputs["ev_conv_w"][j]
    conv = np.zeros((128, 6, 3), np.float32)
    for qi, off in enumerate((0, 512, 1024)):
        for hi, h in enumerate(hs):
            conv[:, qi * 2 + hi, :] = cw[:, off + h * 128: off + (h + 1) * 128].T
    m["convw"] = conv
    gc = np.zeros((128, 4, 4), np.float32)
    for r, name in enumerate(("gdn_a_log", "gdn_dt_bias", "ml_i_bias", "ml_f_bias")):
        for d in range(2):
            for hi, h in enumerate(hs):
                gc[:, r, d * 2 + hi] = inputs[name][j][d, h]
    m["gconst"] = gc
    gb = np.concatenate([inputs["gdn_norm_g"][j]] + [inputs["ml_norm_g"][j][h * 128:(h + 1) * 128] for h in hs] + [np.zeros(128, np.float32)])
    m["gainb"] = np.ascontiguousarray(np.broadcast_to(gb[None, :], (128, 512)).astype(np.float32))
    return m


NTH = 2176
PAIRS = [[0, 1], [2, 3], [4, 5], [6, 7]]


def build_fused():
    nc = bass.Bass("TRN2", target_bir_lowering=False)
    env_nc_holder[0] = nc
    idx_d = nc.dram_tensor("idx_tab", [128, 16], I32, kind="ExternalInput").ap()
    ysend = nc.dram_tensor("ysend", [2, D, NTH], F32, kind="Internal").ap()
    yrecv = nc.dram_tensor("yrecv", [16 * 256, NTH], F32, kind="Internal", addr_space="Local").ap()
    ysel = nc.dram_tensor("ysel", [2, D, NTH], F32, kind="Internal").ap()
    xo0 = nc.dram_tensor("xo0", [D, NTH], F32, kind="Internal").ap()
    g2 = nc.dram_tensor("g2", [8 * 256, NTH], F32, kind="Internal", addr_space="Local").ap()
    with ExitStack() as gst:
        C = Ctx(nc, gst)
        C.sfx = "_g"
        P = C.P
        emit_consts(C)
        emit_masks(C)
        C.pss_all = [(C.ps("ps%d" % i), "ps%d" % i) for i in range(8)]
        idx_sb = C.sb("idx", [128, 16], I32)
        _dma(C, "sp", idx_sb[:], idx_d, [], ["idx"])
        env = Env(nc, C)

        def ywrite(C, dmc, t0, N, yo, yokey):
            rows = slice(dmc * 128, (dmc + 1) * 128)
            if t0 == 0:
                _dma(C, "sp", ysend[0, rows, 2048:2176], yo[:, 0:128], [yokey], ["ysend"])
                _dma(C, "sp", ysend[1, rows, 2048:2176], yo[:, 128:256], [yokey], ["ysend"])
            else:
                j0 = t0 - 256
                _dma(C, "sp", ysend[j0 // 2048, rows, j0 % 2048:j0 % 2048 + N], yo[:, :N], [yokey], ["ysend"])

        def exchange_y(tag):
            for k in range(16):
                h, c = k // 8, k % 8
                P.coll((lambda h, c, k: lambda e: e.collective_compute("AllGather", ALU.bypass, replica_groups=PAIRS,
                                                                         ins=[ysend[h, c * 128:(c + 1) * 128, :]],
                                                                         outs=[yrecv[k * 256:(k + 1) * 256, :]]))(h, c, k),
                       reads=["ysend"], writes=["yrecv"])
            with ExitStack() as stx:
                C.st = stx
                C.sfx = "_x" + tag
                selb = [C.sb("selb%d" % i, [128, NTH]) for i in range(2)]
                for c in range(8):
                    for r in range(2):
                        b = (c * 2 + r) % 2
                        col = c * 2 + r
                        P.dma("pool", (lambda b, col: lambda e: e.indirect_dma_start(
                            out=selb[b][:, :], out_offset=None, in_=yrecv[:, :],
                            in_offset=bass.IndirectOffsetOnAxis(ap=idx_sb[:, col:col + 1], axis=0)))(b, col),
                            reads=["yrecv", "idx"], writes=["selb%d" % b])
                        _dma(C, "sp", ysel[r, c * 128:(c + 1) * 128, :], selb[b][:], ["selb%d" % b], ["ysel"])
                P.barrier()
            C.st = gst

        def exchange_x():
            for c in range(8):
                P.coll((lambda c: lambda e: e.collective_compute("AllGather", ALU.bypass, replica_groups=PAIRS,
                                                                   ins=[xo0[c * 128:(c + 1) * 128, :]],
                                                                   outs=[g2[c * 256:(c + 1) * 256, :]]))(c),
                       reads=["xo0"], writes=["g2"])
            P.barrier()

        g2v = g2.rearrange("(c r p) n -> p c r n", c=8, r=2, p=128)

        def xload_g2(C, xt, t0, N):
            if t0 == 0:
                _dma(C, "sp", xt[:, :, 0:128], g2v[:, :, 0, 2048:2176], [], ["xt"])
                _dma(C, "sp", xt[:, :, 128:256], g2v[:, :, 1, 2048:2176], [], ["xt"])
            else:
                j0 = t0 - 256
                _dma(C, "sp", xt[:, :, :N], g2v[:, :, j0 // 2048, j0 % 2048:j0 % 2048 + N], [], ["xt"])

        env.sfx, env.over, env.xload, env.ywrite = "_m0", {}, None, ywrite
        build_mixer_even(env)
        C.st = gst
        exchange_y("0")
        env.sfx, env.over = "_f0", {"ypa": ysel[0], "ypb": ysel[1], "out": xo0}
        build_ffn(2048, 128, False, env)
        C.st = gst
        exchange_x()
        env.sfx, env.over, env.xload = "_m1", {"xT": None}, xload_g2
        build_mixer_odd(env)
        C.st = gst
        exchange_y("1")
        env.sfx, env.over = "_f1", {"xT": xo0[:, 0:2048], "ypa": ysel[0][:, 0:2048], "ypb": ysel[1][:, 0:2048]}
        build_ffn(2048, 0, True, env)
        C.st = gst
        P.finish()
        P.emit()
    return nc


def fused_inputs(inputs, c):
    b, r = c // 2, c % 2
    x_lat, x_ctx = inputs["x"], inputs["ctx"]
    m = {}
    for k, v in even_inputs(0, inputs, b, r, x_lat, x_ctx).items():
        m[k + "_m0"] = v
    xh = np.concatenate([x_lat[b, r * 2048:(r + 1) * 2048], x_ctx[b, r * 128:(r + 1) * 128]], 0).T
    for k, v in ffn_inputs(0, inputs, xh, None, None, b).items():
        if k not in ("ypa", "ypb"):
            m[k + "_f0"] = v
    for k, v in odd_inputs(1, inputs, b, r, x_lat, x_ctx).items():
        if k != "xT":
            m[k + "_m1"] = v
    for k, v in ffn_inputs(1, inputs, None, None, None, b).items():
        if k not in ("xT", "ypa", "ypb"):
            m[k + "_f1"] = v
    idx = np.zeros((128, 16), np.int32)
    for cc in range(8):
        for rr in range(2):
            idx[:, cc * 2 + rr] = ((r * 8 + cc) * 2 + rr) * 128 + np.arange(128)
    m["idx_tab"] = idx
    return m


_CACHE = {}


def _prog(name, fn):
    if name not in _CACHE:
        _CACHE[name] = fn()
    return _CACHE[name]


def _run(nc, maps):
    res = run_bass_kernel_spmd(nc, maps, core_ids=list(range(8)))
    return res.results


FUSED = True


def kernel(**inputs):
    inputs = {k: np.asarray(v) for k, v in inputs.items()}
    if FUSED:
        nc = _prog("fused", build_fused)
        maps = [fused_inputs(inputs, c) for c in range(8)]
        res = _run(nc, maps)
        out = np.empty((4, 4096, D), np.float32)
        for c in range(8):
            out[c // 2, (c % 2) * 2048:(c % 2 + 1) * 2048] = res[c]["out"].T
        return out
    x_lat = np.ascontiguousarray(inputs["x"], dtype=np.float32)
    x_ctx = np.ascontiguousarray(inputs["ctx"], dtype=np.float32)
    B = x_lat.shape[0]
    depth = inputs["mod_w"].shape[0]
    out_final = None
    for layer in range(depth):
        last = layer == depth - 1
        if layer % 2 == 0:
            nc = _prog("even", build_mixer_even)
            maps = [even_inputs(layer, inputs, c // 2, c % 2, x_lat, x_ctx) for c in range(8)]
        else:
            nc = _prog("odd", build_mixer_odd)
            maps = [odd_inputs(layer, inputs, c // 2, c % 2, x_lat, x_ctx) for c in range(8)]
        res = _run(nc, maps)
        yparts = [res[c]["yT"] for c in range(8)]
        n_lat, n_ctx = 2048, (0 if last else 128)
        nc = _prog("ffn_last" if last else "ffn", lambda: build_ffn(n_lat, n_ctx, last))
        maps = []
        for c in range(8):
            b, hf = c // 2, c % 2
            cols = [np.arange(256 + hf * 2048, 256 + (hf + 1) * 2048)]
            xs = [x_lat[b, hf * 2048:(hf + 1) * 2048]]
            if not last:
                cols.append(np.arange(hf * 128, (hf + 1) * 128))
                xs.append(x_ctx[b, hf * 128:(hf + 1) * 128])
            cols = np.concatenate(cols)
            xT = np.concatenate(xs, 0).T
            maps.append(ffn_inputs(layer, inputs, xT, yparts[2 * b][:, cols], yparts[2 * b + 1][:, cols], b))
        res = _run(nc, maps)
        if last:
            out_final = np.empty_like(x_lat)
            for c in range(8):
                b, hf = c // 2, c % 2
                out_final[b, hf * 2048:(hf + 1) * 2048] = res[c]["out"].T
        else:
            nx_lat = np.empty_like(x_lat)
            nx_ctx = np.empty_like(x_ctx)
            for c in range(8):
                b, hf = c // 2, c % 2
                o = res[c]["out"]
                nx_lat[b, hf * 2048:(hf + 1) * 2048] = o[:, :2048].T
                nx_ctx[b, hf * 128:(hf + 1) * 128] = o[:, 2048:].T
            x_lat, x_ctx = nx_lat, nx_ctx
    return out_final.astype(np.float32)
```

```python
from contextlib import ExitStack
import numpy as np
import concourse.bass as bass
import concourse.mybir as mybir
from concourse.bass_utils import run_bass_kernel_spmd

F32 = mybir.dt.float32
BF16 = mybir.dt.bfloat16
I32 = mybir.dt.int32
AF = mybir.ActivationFunctionType
ALU = mybir.AluOpType
AX = mybir.AxisListType

D = 1024
NCH = 8
EPS = 1e-6
N_EXP = 32
N_DMA_SEMS = 8
DEBUG = False
SERIAL = False
DEBUG_NEXP = None
EVEN_STOP = 99
MMDT = BF16


class Prog:
    def __init__(self, nc, stack):
        self.nc = nc
        self.stack = stack
        self.nrenew = 0
        self.names = ["pe", "act", "dve", "pool", "sp"]
        self.ops = {e: [] for e in self.names}
        self.cnt = {e: 0 for e in self.names}
        self.esem = {e: stack.enter_context(nc.semaphore("s_" + e)) for e in self.names}
        self.dsem, self.dval, self.dnext = {}, {}, {}
        for q in ("sp", "pool", "act"):
            self.dsem[q] = [stack.enter_context(nc.semaphore(f"d_{q}{i}")) for i in range(N_DMA_SEMS)]
            self.dval[q] = [0] * N_DMA_SEMS
            self.dnext[q] = 0
        self.csem = [stack.enter_context(nc.semaphore("c_%d" % i)) for i in range(4)]
        self.cval = [0] * 4
        self.cnext = 0
        self.res = {}
        self.seen = {e: {} for e in self.names}
        self.sem_by_id = {}

    def _need(self, eng, toks):
        best = {}
        for t in toks:
            if t is None:
                continue
            sem, v = t
            k = id(sem)
            self.sem_by_id[k] = sem
            if v > best.get(k, 0):
                best[k] = v
        waits = []
        for k, v in best.items():
            if self.seen[eng].get(k, 0) >= v:
                continue
            self.seen[eng][k] = v
            waits.append((self.sem_by_id[k], v))
        return waits

    def _deps(self, reads, writes):
        toks = []
        for r in reads:
            e = self.res.get(r)
            if e is not None:
                toks.append(e[0])
                if r.startswith("ps"):
                    toks.extend(e[1])
        for w in writes:
            e = self.res.get(w)
            if e is not None:
                toks.append(e[0])
                toks.extend(e[1])
        return toks

    def _commit(self, tok, reads, writes):
        for r in reads:
            e = self.res.setdefault(r, [None, []])
            e[1].append(tok)
        for w in writes:
            self.res[w] = [tok, []]

    def op(self, eng, fn, reads=(), writes=()):
        toks = self._deps(reads, writes)
        if eng == "pe":
            toks = [t for t in toks if t is None or t[0] is not self.esem["pe"]]
        waits = self._need(eng, toks)
        self.cnt[eng] += 1
        tok = (self.esem[eng], self.cnt[eng])
        self.ops[eng].append((fn, waits, (self.esem[eng], 1)))
        self._commit(tok, reads, writes)
        if SERIAL:
            self.barrier()
        return tok

    def dma(self, q, fn, reads=(), writes=()):
        toks = self._deps(reads, writes)
        i = self.dnext[q]
        self.dnext[q] = (i + 1) % N_DMA_SEMS
        sem = self.dsem[q][i]
        if self.dval[q][i] > 0:
            toks.append((sem, self.dval[q][i]))
        waits = self._need(q, toks)
        self.dval[q][i] += 16
        tok = (sem, self.dval[q][i])
        self.ops[q].append((fn, waits, (sem, 16)))
        self._commit(tok, reads, writes)
        if SERIAL:
            self.barrier()
        return tok

    def coll(self, fn, reads=(), writes=()):
        toks = self._deps(reads, writes)
        i = self.cnext
        self.cnext = (i + 1) % len(self.csem)
        sem = self.csem[i]
        if self.cval[i] > 0:
            toks.append((sem, self.cval[i]))
        waits = self._need("pool", toks)
        self.cval[i] += 1
        tok = (sem, self.cval[i])
        self.ops["pool"].append((fn, waits, (sem, None)))
        self._commit(tok, reads, writes)
        return tok

    def _all_dma_toks(self):
        toks = []
        for q in self.dsem:
            for i, sm in enumerate(self.dsem[q]):
                if self.dval[q][i] > 0:
                    toks.append((sm, self.dval[q][i]))
        for i, sm in enumerate(self.csem):
            if self.cval[i] > 0:
                toks.append((sm, self.cval[i]))
        return toks

    def barrier(self):
        toks = [(self.esem[e], self.cnt[e]) for e in self.names if self.cnt[e] > 0]
        toks += self._all_dma_toks()
        for e in self.names:
            waits = self._need(e, toks)
            if waits:
                self.ops[e].append((None, waits, None))
        self.res = {}
        for e in self.names:
            if self.cnt[e] > 12000:
                self.nrenew += 1
                self.esem[e] = self.stack.enter_context(self.nc.semaphore("s_%s_%d" % (e, self.nrenew)))
                self.cnt[e] = 0

    def finish(self):
        toks = self._all_dma_toks()
        waits = self._need("sp", toks)
        self.ops["sp"].append((None, waits, None))

    def emit(self):
        nc = self.nc
        with nc.Block() as block:
            def run(ename):
                def body(eng):
                    for fn, waits, inc in self.ops[ename]:
                        for sem, v in waits:
                            eng.wait_ge(sem, v)
                        if fn is not None:
                            if inc[1] is None:
                                fn(eng).then_inc(inc[0])
                            else:
                                fn(eng).then_inc(inc[0], inc[1])
                return body
            block.tensor(run("pe"))
            block.scalar(run("act"))
            block.vector(run("dve"))
            block.gpsimd(run("pool"))
            block.sync(run("sp"))


class Ctx:
    def __init__(self, nc, st):
        self.nc, self.st = nc, st
        self.P = Prog(nc, st)
        self.psn = 0
        self.sfx = ""

    def sb(self, name, shape, dt=F32):
        return self.st.enter_context(self.nc.sbuf_tensor("sb_" + name + self.sfx, shape, dt))

    def ps(self, name):
        return self.st.enter_context(self.nc.psum_tensor(name, [128, 512], F32))


class Env:
    def __init__(self, nc, C):
        self.nc, self.C = nc, C
        self.sfx = ""
        self.over = {}
        self.xload = None
        self.ywrite = None


def _din_factory(nc, env):
    sfx = env.sfx if env else ""

    def din(name, shape, dt=F32):
        if env is not None and name in env.over:
            return env.over[name]
        return nc.dram_tensor(name + sfx, shape, dt, kind="ExternalInput").ap()
    return din


def _begin(env, st, masks):
    if env is not None:
        C = env.C
        C.st = st
        C.sfx = env.sfx
        return C
    C = Ctx(env_nc_holder[0], st)
    emit_consts(C)
    if masks:
        emit_masks(C)
    C.pss_all = [(C.ps("ps%d" % i), "ps%d" % i) for i in range(8)]
    return C


env_nc_holder = [None]


def token_tiles(n_lat, n_ctx):
    tiles = []
    for s in range(0, n_lat, 512):
        tiles.append((s, min(512, n_lat - s), 0))
    for s in range(0, n_ctx, 512):
        tiles.append((n_lat + s, min(512, n_ctx - s), 1))
    return tiles


def emit_consts(C):
    P, nc = C.P, C.nc
    C.ones = C.sb("ones", [128, 128])
    C.ident = C.sb("ident", [128, 128])
    P.op("pool", lambda e: e.memset(C.ones[:], 1.0), writes=["ones"])
    P.op("pool", lambda e: e.memset(C.ident[:], 1.0), writes=["ident"])
    P.op("pool", lambda e: e.affine_select(out=C.ident[:], in_=C.ident[:], pattern=[[-1, 128]],
                                            compare_op=ALU.is_equal, fill=0.0, base=0, channel_multiplier=1),
         reads=["ident"], writes=["ident"])


def emit_mod(C, cv_d, modw_d, modb_d, ncols, psb, pskey):
    P, nc = C.P, C.nc
    nj = ncols // 128
    cv = C.sb("cv", [128, NCH, 2])
    sc = C.sb("sc", [128, NCH, 2])
    modb = C.sb("modb", [128, nj])
    modv = C.sb("modv", [128, nj, 2])
    P.dma("sp", lambda e: e.dma_start(out=cv[:], in_=cv_d), writes=["cv"])
    P.dma("sp", lambda e: e.dma_start(out=modb[:], in_=modb_d), writes=["modb"])
    P.op("act", lambda e: e.activation(out=sc[:], in_=cv[:], func=AF.Silu), reads=["cv"], writes=["sc"])
    wbufs = [C.sb("modw%d" % i, [128, NCH, 512]) for i in range(2)]
    mw = modw_d.rearrange("(c p) n -> p c n", p=128)
    for blk in range(ncols // 512):
        wb = wbufs[blk % 2]
        key = "modw%d" % (blk % 2)
        P.dma("sp", (lambda wb, blk: lambda e: e.dma_start(out=wb[:], in_=mw[:, :, blk * 512:(blk + 1) * 512]))(wb, blk),
              writes=[key])
        for jj in range(4):
            j = blk * 4 + jj
            for c in range(NCH):
                P.op("pe", (lambda wb, jj, c, j: lambda e: e.matmul(psb[:, 2 * j:2 * j + 2], lhsT=wb[:, c, jj * 128:(jj + 1) * 128],
                                                                      rhs=sc[:, c, :], start=(c == 0), stop=(c == NCH - 1)))(wb, jj, c, j),
                     reads=[key, "sc"], writes=[pskey])
    for w in range(2):
        P.op("dve", (lambda w: lambda e: e.tensor_tensor(out=modv[:, :, w], in0=psb[:, w:2 * nj:2], in1=modb[:], op=ALU.add))(w),
             reads=[pskey, "modb"], writes=["modv"])
    return modv


def emit_rstd(C, src, srckey, N, sq, sqkey, psb, pskey, rstd, rstdkey):
    P = C.P
    for c in range(NCH):
        P.op("act", (lambda c: lambda e: e.activation(out=sq[:, c, :N], in_=src[:, c, :N], func=AF.Square))(c), reads=[srckey], writes=[sqkey])
    for c in range(NCH):
        P.op("pe", (lambda c: lambda e: e.matmul(psb[:, :N], lhsT=C.ones[:], rhs=sq[:, c, :N], start=(c == 0), stop=(c == NCH - 1)))(c),
             reads=["ones", sqkey], writes=[pskey])
    P.op("act", lambda e: e.activation(out=rstd[:, :N], in_=psb[:, :N], func=AF.Sqrt, bias=EPS, scale=1.0 / D),
         reads=[pskey], writes=[rstdkey])
    P.op("dve", lambda e: e.reciprocal(out=rstd[:, :N], in_=rstd[:, :N]), reads=[rstdkey], writes=[rstdkey])


def build_ffn(n_lat, n_ctx, last, env=None):
    NT = n_lat + n_ctx
    tiles = token_tiles(n_lat, n_ctx)
    sfx = env.sfx if env else ""
    nc = env.nc if env else bass.Bass("TRN2", target_bir_lowering=False)
    env_nc_holder[0] = nc
    dt_in = _din_factory(nc, env)
    xT_d = dt_in("xT", [D, NT]); ypa_d = dt_in("ypa", [D, NT]); ypb_d = dt_in("ypb", [D, NT])
    cv_d = dt_in("cv", [128, NCH, 2]); modw_d = dt_in("modw", [D, 4096]); modb_d = dt_in("modb", [128, 32])
    n2g_d = dt_in("n2g", [128, NCH]); fing_d = dt_in("fing", [128, NCH])
    rw_d = dt_in("rw", [D, N_EXP]); rb_d = dt_in("rb", [1, N_EXP])
    wgu_d = dt_in("wgu", [N_EXP, D, 2 * D]); bgu_d = dt_in("bgu", [128, N_EXP, NCH, 2])
    wdn_d = dt_in("wdn", [N_EXP, D, D]); bdn_d = dt_in("bdn", [128, N_EXP, NCH]); bdnT_d = dt_in("bdnT", [N_EXP, D])
    if env is not None and "out" in env.over:
        out_d = env.over["out"]
    else:
        out_d = nc.dram_tensor("out", [D, NT], F32, kind="ExternalOutput").ap()
    xmid_d = nc.dram_tensor("xmid_scr" + sfx, [D, NT], F32, kind="ExternalOutput" if DEBUG else "Internal").ap()
    if DEBUG:
        gwT_dbg = nc.dram_tensor("gwT_dbg", [N_EXP, NT], F32, kind="ExternalOutput").ap()
        acc_dbg = nc.dram_tensor("acc_dbg", [D, NT], F32, kind="ExternalOutput").ap()
        h2_dbg = nc.dram_tensor("h2_dbg", [D, NT], F32, kind="ExternalOutput").ap()
        modv_dbg = nc.dram_tensor("modv_dbg", [128, 64], F32, kind="ExternalOutput").ap()
    r3 = lambda ap: ap.rearrange("(c p) n -> p c n", p=128)

    with ExitStack() as st:
        C = _begin(env, st, False)
        P = C.P
        psA = [C.pss_all[i][0] for i in (0, 1)]
        psB = [C.pss_all[i][0] for i in (2, 3)]
        psY = [C.pss_all[i][0] for i in (4, 5)]
        psM = [C.pss_all[i][0] for i in (6, 7)]
        h2b = C.sb("h2b", [128, NCH, NT], MMDT)
        gwT = C.sb("gwT", [N_EXP, NT])
        n2g = C.sb("n2g", [128, NCH]); fing = C.sb("fing", [128, NCH])
        A2 = C.sb("A2", [128, NCH, 2])
        g2v = C.sb("g2v", [128, NCH, 2])
        tmp32 = C.sb("tmp32", [N_EXP, 512])
        rw = C.sb("rw", [128, NCH, N_EXP]); rb = C.sb("rb", [1, N_EXP])
        bgu = C.sb("bgu", [128, N_EXP, NCH, 2]); bdn = C.sb("bdn", [128, N_EXP, NCH])
        P.dma("sp", lambda e: e.dma_start(out=n2g[:], in_=n2g_d), writes=["n2g"])
        P.dma("sp", lambda e: e.dma_start(out=fing[:], in_=fing_d), writes=["fing"])
        P.dma("sp", lambda e: e.dma_start(out=rw[:], in_=r3(rw_d)), writes=["rw"])
        P.dma("sp", lambda e: e.dma_start(out=rb[:], in_=rb_d), writes=["rb"])
        P.dma("sp", lambda e: e.dma_start(out=bgu[:], in_=bgu_d), writes=["bgu"])
        P.dma("sp", lambda e: e.dma_start(out=bdn[:], in_=bdn_d), writes=["bdn"])

        with ExitStack() as st2:
            C.st = st2
            modv = emit_mod(C, cv_d, modw_d, modb_d, 4096, psM[0], "psM0")
            P.op("dve", lambda e: e.tensor_copy(out=g2v[:], in_=modv[:, 24:32, :]), reads=["modv"], writes=["g2v"])
            for w in range(2):
                P.op("dve", (lambda w: lambda e: e.scalar_tensor_tensor(out=A2[:, :, w], in0=modv[:, 16:24, w], scalar=1.0, in1=n2g[:],
                                                                         op0=ALU.add, op1=ALU.mult))(w),
                     reads=["modv", "n2g"], writes=["A2"])
            xt = [C.sb("xt%d" % i, [128, NCH, 512]) for i in range(2)]
            ya = [C.sb("ya0", [128, NCH, 512])] * 2
            yb = [C.sb("yb0", [128, NCH, 512])] * 2
            sq = C.sb("sq", [128, NCH, 512])
            rstd = C.sb("rstd", [128, 512])
            h2f = C.sb("h2f", [128, NCH, 512])
            lg = C.sb("lg", [128, N_EXP]); top8 = C.sb("top8", [128, 8]); negm = C.sb("negm", [128, 1])
            exl = C.sb("exl", [128, N_EXP]); msk = C.sb("msk", [128, N_EXP]); den = C.sb("den", [128, 1])
            gw = C.sb("gw", [128, N_EXP])
            for ti, (t0, N, w) in enumerate(tiles):
                b = ti % 2
                kx, ka, kb = "xt%d" % b, "ya0", "yb0"
                P.dma("sp", (lambda b, t0, N: lambda e: e.dma_start(out=xt[b][:, :, :N], in_=r3(xT_d)[:, :, t0:t0 + N]))(b, t0, N), writes=[kx])
                P.dma("sp", (lambda b, t0, N: lambda e: e.dma_start(out=ya[b][:, :, :N], in_=r3(ypa_d)[:, :, t0:t0 + N]))(b, t0, N), writes=[ka])
                P.dma("sp", (lambda b, t0, N: lambda e: e.dma_start(out=yb[b][:, :, :N], in_=r3(ypb_d)[:, :, t0:t0 + N]))(b, t0, N), writes=[kb])
                P.op("pool", (lambda b, N: lambda e: e.tensor_tensor(out=ya[b][:, :, :N], in0=ya[b][:, :, :N], in1=yb[b][:, :, :N], op=ALU.add))(b, N),
                     reads=[ka, kb], writes=[ka])
                for c in range(NCH):
                    P.op("dve", (lambda b, N, c, w: lambda e: e.scalar_tensor_tensor(out=xt[b][:, c, :N], in0=ya[b][:, c, :N], scalar=modv[:, c, w:w + 1],
                                                                                      in1=xt[b][:, c, :N], op0=ALU.mult, op1=ALU.add))(b, N, c, w),
                         reads=[ka, kx, "modv"], writes=[kx])
                P.dma("sp", (lambda b, t0, N: lambda e: e.dma_start(out=r3(xmid_d)[:, :, t0:t0 + N], in_=xt[b][:, :, :N]))(b, t0, N),
                      reads=[kx], writes=["xmid_d%d" % ti])
                emit_rstd(C, xt[b], kx, N, sq, "sq", psM[1], "psM1", rstd, "rstd")
                for c in range(NCH):
                    P.op("pool", (lambda b, N, c: lambda e: e.tensor_tensor(out=h2f[:, c, :N], in0=xt[b][:, c, :N], in1=rstd[:, :N], op=ALU.mult))(b, N, c),
                         reads=[kx, "rstd"], writes=["h2f"])
                    P.op("dve", (lambda N, c, w: lambda e: e.tensor_scalar(out=h2f[:, c, :N], in0=h2f[:, c, :N], scalar1=A2[:, c, w:w + 1],
                                                                            scalar2=modv[:, 8 + c, w:w + 1], op0=ALU.mult, op1=ALU.add))(N, c, w),
                         reads=["h2f", "A2", "modv"], writes=["h2f"])
                for c in range(NCH):
                    P.op("act", (lambda t0, N, c: lambda e: e.copy(out=h2b[:, c, t0:t0 + N], in_=h2f[:, c, :N]))(t0, N, c), reads=["h2f"], writes=["h2b"])
                if DEBUG:
                    P.dma("sp", (lambda t0, N: lambda e: e.dma_start(out=r3(h2_dbg)[:, :, t0:t0 + N], in_=h2f[:, :, :N]))(t0, N), reads=["h2f"], writes=["dbgh%d" % ti])
                    if ti == 0:
                        P.dma("sp", lambda e: e.dma_start(out=modv_dbg, in_=modv[:].rearrange("p j w -> p (j w)")), reads=["modv"], writes=["dbgm"])
                        a2_dbg = nc.dram_tensor("a2_dbg", [128, 16], F32, kind="ExternalOutput").ap()
                        P.dma("sp", lambda e: e.dma_start(out=a2_dbg, in_=A2[:].rearrange("p j w -> p (j w)")), reads=["A2"], writes=["dbga2"])
                        rstd_dbg = nc.dram_tensor("rstd_dbg", [128, 512], F32, kind="ExternalOutput").ap()
                        P.dma("sp", lambda e: e.dma_start(out=rstd_dbg, in_=rstd[:]), reads=["rstd"], writes=["dbgr"])
                        sq_dbg = nc.dram_tensor("sq_dbg", [128, NCH, 512], F32, kind="ExternalOutput").ap()
                        P.dma("sp", lambda e: e.dma_start(out=sq_dbg, in_=sq[:]), reads=["sq"], writes=["dbgsq"])
                for blk in range(N // 128):
                    o = blk * 128
                    for c in range(NCH):
                        P.op("pe", (lambda o, c: lambda e: e.matmul(psM[0][:, 0:N_EXP], lhsT=h2f[:, c, o:o + 128], rhs=rw[:, c, :], start=(c == 0), stop=False))(o, c),
                             reads=["h2f", "rw"], writes=["psM0"])
                    P.op("pe", lambda e: e.matmul(psM[0][:, 0:N_EXP], lhsT=C.ones[0:1, :], rhs=rb[:], start=False, stop=True),
                         reads=["ones", "rb"], writes=["psM0"])
                    P.op("dve", lambda e: e.tensor_copy(out=lg[:], in_=psM[0][:, 0:N_EXP]), reads=["psM0"], writes=["lg"])
                    P.op("dve", lambda e: e.max(out=top8[:], in_=lg[:]), reads=["lg"], writes=["top8"])
                    P.op("dve", lambda e: e.tensor_scalar(out=negm[:], in0=top8[:, 0:1], scalar1=-1.0, scalar2=None, op0=ALU.mult),
                         reads=["top8"], writes=["negm"])
                    P.op("act", lambda e: e.activation(out=exl[:], in_=lg[:], func=AF.Exp, bias=negm[:], scale=1.0),
                         reads=["lg", "negm"], writes=["exl"])
                    P.op("dve", lambda e: e.tensor_scalar(out=msk[:], in0=lg[:], scalar1=top8[:, 3:4], scalar2=None, op0=ALU.is_ge),
                         reads=["lg", "top8"], writes=["msk"])
                    P.op("dve", lambda e: e.tensor_tensor(out=exl[:], in0=exl[:], in1=msk[:], op=ALU.mult), reads=["exl", "msk"], writes=["exl"])
                    P.op("dve", lambda e: e.reduce_sum(out=den[:], in_=exl[:], axis=AX.X), reads=["exl"], writes=["den"])
                    P.op("dve", lambda e: e.reciprocal(out=den[:], in_=den[:]), reads=["den"], writes=["den"])
                    P.op("dve", lambda e: e.tensor_scalar(out=gw[:], in0=exl[:], scalar1=den[:, 0:1], scalar2=None, op0=ALU.mult),
                         reads=["exl", "den"], writes=["gw"])
                    P.op("pe", lambda e: e.transpose(out=psM[1][0:N_EXP, 0:128], in_=gw[:], identity=C.ident[:]), reads=["gw", "ident"], writes=["psM1"])
                    P.op("act", (lambda t0, o: lambda e: e.copy(out=gwT[:, t0 + o:t0 + o + 128], in_=psM[1][0:N_EXP, 0:128]))(t0, o),
                         reads=["psM1"], writes=["gwT"])
            P.barrier()
        C.st = st

        stacc = st.enter_context(ExitStack())
        C.st = stacc
        acc = C.sb("acc", [128, NCH, NT])
        with ExitStack() as stb:
            C.st = stb
            bdnT = C.sb("bdnT", [N_EXP, D])
            _dma(C, "sp", bdnT[:], bdnT_d, [], ["bdnT"])
            nb_ = 0
            for ti, (t0, N, w) in enumerate(tiles):
                for dmc in range(NCH):
                    pb = nb_ % 2
                    nb_ += 1
                    _mm(C, psY[pb][:, :N], bdnT[:, dmc * 128:(dmc + 1) * 128], gwT[:, t0:t0 + N], True, True, ["bdnT", "gwT"], ["psY%d" % pb])
                    _cp(C, "act" if pb else "dve", acc[:, dmc, t0:t0 + N], psY[pb][:, :N], ["psY%d" % pb], ["acc"])
            P.barrier()
        C.st = stacc
        with ExitStack() as st3:
            C.st = st3
            NRING = 2
            wgu_r = [C.sb("wgu_r%d" % i, [128, NCH, 256], MMDT) for i in range(NRING)]
            wdn_r = [C.sb("wdn_r%d" % i, [128, NCH, 128], MMDT) for i in range(NRING)]
            wst = [C.sb("wst%d" % i, [128, NCH, 256]) for i in range(2)]
            actT = C.sb("actT", [128, NCH, NT], MMDT)
            gwb = C.sb("gwb", [128, NT])
            gt = [C.sb("gt%d" % i, [128, 512]) for i in range(2)]
            ut = [C.sb("ut%d" % i, [128, 512]) for i in range(2)]
            sg = [C.sb("sg%d" % i, [128, 512]) for i in range(2)]
            wguv = wgu_d.rearrange("e (c p) n -> e p c n", p=128)
            wdnv = wdn_d.rearrange("e (c p) n -> e p c n", p=128)
            pg = 0
            pd = 0
            pw = 0
            it = 0
            pending = []
            for ex in range(N_EXP if DEBUG_NEXP is None else DEBUG_NEXP):
                for ti, (t0, N, w) in enumerate(tiles):
                    _ts(C, "dve", tmp32[:, :N], gwT[:, t0:t0 + N], C.ident[0:N_EXP, ex:ex + 1], None, ALU.mult, None, ["gwT", "ident"], ["tmp32"])
                    _mm(C, psM[0][:, :N], C.ones[0:N_EXP, :], tmp32[:, :N], True, True, ["ones", "tmp32"], ["psM0"])
                    _cp(C, "act", gwb[:, t0:t0 + N], psM[0][:, :N], ["psM0"], ["gwb%d" % ti])
                for fc in range(NCH):
                    rg_ = pg % NRING
                    pg += 1
                    ws_ = pw % 2
                    pw += 1
                    kw, kst = "wgu_r%d" % rg_, "wst%d" % ws_
                    _dma(C, "sp", wst[ws_][:], wguv[ex, :, :, fc * 256:(fc + 1) * 256], [], [kst])
                    for c in range(NCH):
                        _cp(C, "act" if c % 2 else "pool", wgu_r[rg_][:, c, :], wst[ws_][:, c, :], [kst], [kw])
                    for ti, (t0, N, w) in enumerate(tiles):
                        pb = it % 2
                        it += 1
                        ka, kb = "psA%d" % pb, "psB%d" % pb
                        for two, (pst, kk) in enumerate(((psA[pb], ka), (psB[pb], kb))):
                            for c in range(NCH):
                                _mm(C, pst[:, :N], wgu_r[rg_][:, c, two:256:2], h2b[:, c, t0:t0 + N], c == 0, c == NCH - 1, [kw, "h2b"], [kk])
                        kg, ku, ks = "gt%d" % pb, "ut%d" % pb, "sg%d" % pb
                        _act(C, ut[pb][:, :N], psB[pb][:, :N], AF.Identity, [kb], [ku], bias=bgu[:, ex, fc, 1:2])
                        _ts(C, "dve", gt[pb][:, :N], psA[pb][:, :N], bgu[:, ex, fc, 0:1], 7.0, ALU.add, ALU.min, [ka, "bgu"], [kg])
                        _act(C, sg[pb][:, :N], gt[pb][:, :N], AF.Sigmoid, [kg], [ks], scale=1.702)
                        _ts(C, "dve", ut[pb][:, :N], ut[pb][:, :N], -7.0, 7.0, ALU.max, ALU.min, [ku], [ku])
                        for fn_ in pending:
                            fn_()
                        pending = [
                            (lambda pb=pb, N=N, kg=kg, ks=ks: _tt(C, "pool", gt[pb][:, :N], gt[pb][:, :N], sg[pb][:, :N], ALU.mult, [kg, ks], [kg])),
                            (lambda pb=pb, N=N, kg=kg, t0=t0, ti=ti: _tt(C, "pool", gt[pb][:, :N], gt[pb][:, :N], gwb[:, t0:t0 + N], ALU.mult, [kg, "gwb%d" % ti], [kg])),
                            (lambda pb=pb, N=N, kg=kg, ku=ku, t0=t0, ti=ti, fc=fc: _stt(C, actT[:, fc, t0:t0 + N], ut[pb][:, :N], 1.0, gt[pb][:, :N], ALU.add, ALU.mult, [ku, kg], ["actT%d_%d" % (fc, ti)])),
                        ]
                for fn_ in pending:
                    fn_()
                pending = []
                for dmc in range(NCH):
                    rd_ = pd % NRING
                    pd += 1
                    ws_ = pw % 2
                    pw += 1
                    kw, kst = "wdn_r%d" % rd_, "wst%d" % ws_
                    _dma(C, "sp", wst[ws_][:, :, 0:128], wdnv[ex, :, :, dmc * 128:(dmc + 1) * 128], [], [kst])
                    for hh in range(2):
                        _cp(C, "pool" if hh else "act", wdn_r[rd_][:, hh * 4:(hh + 1) * 4, :], wst[ws_][:, hh * 4:(hh + 1) * 4, 0:128], [kst], [kw])
                    for ti, (t0, N, w) in enumerate(tiles):
                        pb = it % 2
                        it += 1
                        ky = "psY%d" % pb
                        for fc in range(NCH):
                            _mm(C, psY[pb][:, :N], wdn_r[rd_][:, fc, :], actT[:, fc, t0:t0 + N], fc == 0, fc == NCH - 1, [kw, "actT%d_%d" % (fc, ti)], [ky])
                        _tt(C, "dve", acc[:, dmc, t0:t0 + N], acc[:, dmc, t0:t0 + N], psY[pb][:, :N], ALU.add, [ky, "acc", "acc%d_%d" % (dmc, ti)], ["acc%d_%d" % (dmc, ti)])
            P.barrier()
        C.st = st
        if DEBUG:
            P.dma("sp", lambda e: e.dma_start(out=gwT_dbg, in_=gwT[:]), writes=["dbg1"])
            P.dma("sp", lambda e: e.dma_start(out=r3(acc_dbg), in_=acc[:]), writes=["dbg2"])
        with ExitStack() as st4:
            C.st = st4
            xm = [C.sb("xm%d" % i, [128, NCH, 512]) for i in range(2)]
            sqD = C.sb("sq2", [128, NCH, 512])
            rstdD = C.sb("rstd2", [128, 512])
            for ti, (t0, N, w) in enumerate(tiles):
                b = ti % 2
                kx = "xm%d" % b
                P.dma("sp", (lambda b, t0, N: lambda e: e.dma_start(out=xm[b][:, :, :N], in_=r3(xmid_d)[:, :, t0:t0 + N]))(b, t0, N), writes=[kx])
                for c in range(NCH):
                    P.op("dve", (lambda b, N, c, w, t0: lambda e: e.scalar_tensor_tensor(out=xm[b][:, c, :N], in0=acc[:, c, t0:t0 + N], scalar=g2v[:, c, w:w + 1],
                                                                                          in1=xm[b][:, c, :N], op0=ALU.mult, op1=ALU.add))(b, N, c, w, t0),
                         reads=[kx], writes=[kx])
                if last:
                    emit_rstd(C, xm[b], kx, N, sqD, "sq2", psM[1], "psM1", rstdD, "rstd2")
                    for c in range(NCH):
                        P.op("dve", (lambda b, N, c: lambda e: e.scalar_tensor_tensor(out=xm[b][:, c, :N], in0=xm[b][:, c, :N], scalar=fing[:, c:c + 1],
                                                                                       in1=rstdD[:, :N], op0=ALU.mult, op1=ALU.mult))(b, N, c),
                             reads=[kx, "rstd2"], writes=[kx])
                P.dma("sp", (lambda b, t0, N: lambda e: e.dma_start(out=r3(out_d)[:, :, t0:t0 + N], in_=xm[b][:, :, :N]))(b, t0, N),
                      reads=[kx], writes=["out%d" % ti])
            if env is None:
                P.finish()
            else:
                P.barrier()
        if env is None:
            P.emit()
    return nc


def ffn_inputs(layer, inputs, xT, ypa, ypb, b):
    pc = lambda v: np.ascontiguousarray(v.reshape(-1, 128).T)
    cv = np.stack([pc(inputs["c"][b]), pc(inputs["c_ctx"])], axis=-1)
    bgu = np.ascontiguousarray(inputs["moe_b_gu"][layer].reshape(N_EXP, NCH, 128, 2).transpose(2, 0, 1, 3))
    bdn = np.ascontiguousarray(inputs["moe_b_dn"][layer].reshape(N_EXP, NCH, 128).transpose(2, 0, 1))
    return {
        "xT": None if xT is None else np.ascontiguousarray(xT), "ypa": None if ypa is None else np.ascontiguousarray(ypa),
        "ypb": None if ypb is None else np.ascontiguousarray(ypb),
        "cv": np.ascontiguousarray(cv),
        "modw": np.ascontiguousarray(inputs["mod_w"][layer][:, 2048:6144]),
        "modb": pc(inputs["mod_b"][layer][2048:6144]),
        "n2g": pc(inputs["norm2_g"][layer]), "fing": pc(inputs["final_g"]),
        "rw": np.ascontiguousarray(inputs["router_w"][layer]), "rb": np.ascontiguousarray(inputs["router_b"][layer][None, :]),
        "wgu": np.ascontiguousarray(inputs["moe_w_gu"][layer]), "bgu": bgu,
        "wdn": np.ascontiguousarray(inputs["moe_w_dn"][layer]), "bdn": bdn, "bdnT": np.ascontiguousarray(inputs["moe_b_dn"][layer]),
    }


def _mm(C, out, lhsT, rhs, start, stop, r, w):
    C.P.op("pe", lambda e: e.matmul(out, lhsT=lhsT, rhs=rhs, start=start, stop=stop), reads=r, writes=w)


def _tr(C, out, in_, r, w):
    ident = C.ident[0:in_.shape[0], 0:in_.shape[0]]
    C.P.op("pe", lambda e: e.transpose(out=out, in_=in_, identity=ident), reads=list(r) + ["ident"], writes=w)


def _ts(C, eng, out, in0, s1, s2, op0, op1, r, w):
    if s2 is None:
        C.P.op(eng, lambda e: e.tensor_scalar(out=out, in0=in0, scalar1=s1, scalar2=None, op0=op0), reads=r, writes=w)
    else:
        C.P.op(eng, lambda e: e.tensor_scalar(out=out, in0=in0, scalar1=s1, scalar2=s2, op0=op0, op1=op1), reads=r, writes=w)


def _tt(C, eng, out, in0, in1, op, r, w):
    C.P.op(eng, lambda e: e.tensor_tensor(out=out, in0=in0, in1=in1, op=op), reads=r, writes=w)


def _stt(C, out, in0, scalar, in1, op0, op1, r, w):
    C.P.op("dve", lambda e: e.scalar_tensor_tensor(out=out, in0=in0, scalar=scalar, in1=in1, op0=op0, op1=op1), reads=r, writes=w)


def _act(C, out, in_, func, r, w, bias=None, scale=1.0):
    if bias is None:
        C.P.op("act", lambda e: e.activation(out=out, in_=in_, func=func, scale=scale), reads=r, writes=w)
    else:
        C.P.op("act", lambda e: e.activation(out=out, in_=in_, func=func, bias=bias, scale=scale), reads=r, writes=w)


def _cp(C, eng, out, in_, r, w):
    if eng == "act":
        C.P.op("act", lambda e: e.copy(out=out, in_=in_), reads=r, writes=w)
    else:
        C.P.op(eng, lambda e: e.tensor_copy(out=out, in_=in_), reads=r, writes=w)


def _dma(C, q, out, in_, r, w):
    C.P.dma(q, lambda e: e.dma_start(out=out, in_=in_), reads=r, writes=w)


T_ALL = 4352
NB = 34
N_CTXB = 2


def scan_order(direction):
    if direction == 0:
        return list(range(NB))
    return [1, 0] + list(range(NB - 1, N_CTXB - 1, -1))


def emit_masks(C):
    P = C.P
    C.masks = {}
    for name, op, sgn in (("F", ALU.is_ge, 1), ("Fs", ALU.is_gt, 1), ("B", ALU.is_ge, -1), ("Bs", ALU.is_gt, -1)):
        m = C.sb("mask" + name, [128, 128])
        P.op("pool", (lambda m: lambda e: e.memset(m[:], 1.0))(m), writes=["mask" + name])
        P.op("pool", (lambda m, op, sgn: lambda e: e.affine_select(out=m[:], in_=m[:], pattern=[[sgn, 128]], compare_op=op, fill=0.0,
                                                               base=0, channel_multiplier=-sgn))(m, op, sgn),
             reads=["mask" + name], writes=["mask" + name])
        C.masks[name] = m
    for name, row in (("sel0", 0), ("sel127", 127)):
        m = C.sb(name, [128, 128])
        P.op("pool", (lambda m: lambda e: e.memset(m[:], 1.0))(m), writes=[name])
        P.op("pool", (lambda m, row: lambda e: e.affine_select(out=m[:], in_=m[:], pattern=[[0, 128]], compare_op=ALU.is_equal, fill=0.0,
                                                                base=-row, channel_multiplier=1))(m, row),
             reads=[name], writes=[name])
        C.masks[name] = m


def emit_stream_tables(C, g, colb, direction, tabs, psb, pskey, sid):
    P = C.P
    kf, rowf, ctab, gF, gL, tmp = tabs
    first, last = ("sel0", "sel127") if direction == 0 else ("sel127", "sel0")
    kk = "tabs%s" % sid
    _mm(C, psb[:, 0:NB], C.masks[first][:], g[:], True, True, [first, "g" + sid], [pskey])
    _cp(C, "dve", gF[:], psb[:, 0:NB], [pskey], [kk + "gF"])
    _mm(C, psb[:, 0:NB], C.masks[last][:], g[:], True, True, [last, "g" + sid], [pskey])
    _cp(C, "dve", gL[:], psb[:, 0:NB], [pskey], [kk + "gL"])
    _tt(C, "dve", kf[:], gL[:], colb[:], ALU.add, [kk + "gL", "colb" + sid], [kk + "kf"])
    _ts(C, "dve", kf[:], kf[:], 0.0, None, ALU.min, None, [kk + "kf"], [kk + "kf"])
    _act(C, kf[:], kf[:], AF.Exp, [kk + "kf"], [kk + "kf"])
    _tt(C, "dve", rowf[:], g[:], gF[:], ALU.subtract, ["g" + sid, kk + "gF"], [kk + "rowf"])
    _ts(C, "dve", rowf[:], rowf[:], 0.0, None, ALU.min, None, [kk + "rowf"], [kk + "rowf"])
    _act(C, rowf[:], rowf[:], AF.Exp, [kk + "rowf"], [kk + "rowf"])
    _tt(C, "dve", ctab[:], gF[:, :, None].to_broadcast([128, NB, NB]), gL[:, None, :].to_broadcast([128, NB, NB]), ALU.subtract,
        [kk + "gF", kk + "gL"], [kk + "ctab"])
    for I in range(NB):
        _ts(C, "dve", ctab[:, I, :], ctab[:, I, :], 0.0, None, ALU.min, None, [kk + "ctab"], [kk + "ctab"])
        _act(C, ctab[:, I, :], ctab[:, I, :], AF.Exp, [kk + "ctab"], [kk + "ctab"])


def emit_diag_E(C, g, colb, I, direction, strict, Et, etkey, dg, psb, pskey, sid):
    mk = C.masks[("F" if direction == 0 else "B") + ("s" if strict else "")]
    mkey = "mask" + ("F" if direction == 0 else "B") + ("s" if strict else "")
    _ts(C, "dve", dg[:], C.ident[:], g[:, I:I + 1], None, ALU.mult, None, ["ident", "g" + sid], ["dg"])
    _mm(C, psb[:, 0:128], C.ones[:], dg[:], True, True, ["ones", "dg"], [pskey])
    _ts(C, "dve", Et[:], psb[:, 0:128], colb[:, I:I + 1], 0.0, ALU.add, ALU.min, [pskey, "colb" + sid], [etkey])
    _act(C, Et[:], Et[:], AF.Exp, [etkey], [etkey])
    _tt(C, "pool", Et[:], Et[:], mk[:], ALU.mult, [etkey, mkey], [etkey])


def emit_attn_block(C, I, order_pos, order, AT, BT, VP, Vraw, dvp, tabs, g, colb, direction, bufs, sid, out, outkey, skip_off_rowscale=None, kx=""):
    kf, rowf, ctab, gF, gL, tmp = tabs
    psS, psO, psD, psR, wts, Et, dg, offb = bufs
    before = order[:order_pos]
    isl = slice(I * 128, (I + 1) * 128)
    kk = "tabs%s" % sid
    for n, J in enumerate(before):
        sb_i = C.rot % len(psS)
        C.rot += 1
        ps, pk = psS[sb_i]
        wt, wk = wts[sb_i % len(wts)]
        _mm(C, ps[:, 0:128], AT[:, J * 128:(J + 1) * 128], BT[:, isl], True, True, ["AT" + kx + sid, "BT" + kx + sid], [pk])
        _ts(C, "dve", wt[:], ps[:, 0:128], ctab[:, I, J:J + 1], None, ALU.mult, None, [pk, kk + "ctab"], [wk])
        _mm(C, psO[0][:, 0:dvp], wt[:], VP[:, J, :], n == 0, n == len(before) - 1, [wk, "VP" + kx + sid], [psO[1]])
    if before:
        _ts(C, "dve", offb[:, 0:dvp], psO[0][:, 0:dvp], rowf[:, I:I + 1], None, ALU.mult, None, [psO[1], kk + "rowf"], ["offb"])
    emit_diag_E(C, g, colb, I, direction, False, Et, "Et", dg, psR[0], psR[1], sid)
    sb_i = C.rot % len(psS)
    C.rot += 1
    ps, pk = psS[sb_i]
    wt, wk = wts[sb_i % len(wts)]
    _mm(C, ps[:, 0:128], AT[:, isl], BT[:, isl], True, True, ["AT" + kx + sid, "BT" + kx + sid], [pk])
    _tt(C, "dve", wt[:], ps[:, 0:128], Et[:], ALU.mult, [pk, "Et"], [wk])
    _mm(C, psD[0][:, 0:dvp], wt[:], Vraw[:, I, :], True, True, [wk, "Vraw" + kx + sid], [psD[1]])
    if before:
        _tt(C, "dve", out, psD[0][:, 0:dvp], offb[:, 0:dvp], ALU.add, [psD[1], "offb"], [outkey])
    else:
        _cp(C, "dve", out, psD[0][:, 0:dvp], [psD[1]], [outkey])


def emit_stream_pipelined(C, order, AT, BT, VP, Vraw, dvp, tabs, g, colb, direction, bufs, kx, tail, diag=True, lookahead=3):
    kf, rowf, ctab, gF, gL, _ = tabs
    psS, psO2, psD, psR, wts, Et2, dg2, offb = bufs
    kk = "tabs"
    items = []
    for pos, I in enumerate(order):
        before = order[:pos]
        for n, J in enumerate(before):
            items.append(("off", I, pos, J, n == 0, n == len(before) - 1, False))
        if diag:
            items.append(("diag", I, pos, I, True, True, True))
        if items and items[-1][1] == I:
            it = items[-1]
            items[-1] = it[:6] + (True,)
        elif not before and not diag:
            items.append(("none", I, pos, I, True, True, True))

    def emit_s(idx):
        kind, I, pos, J, first, last, lastI = items[idx]
        if kind == "none":
            return
        ps, pk = psS[idx % len(psS)]
        if kind == "diag" or (kind == "off" and first and not diag):
            pass
        if kind == "diag":
            Et, ek = Et2[pos % 2]
            dg, dk = dg2[pos % 2]
            mk = C.masks["F" if direction == 0 else "B"]
            mkey = "maskF" if direction == 0 else "maskB"
            _ts(C, "dve", dg[:], C.ident[:], g[:, I:I + 1], None, ALU.mult, None, ["ident", "g"], [dk])
            _mm(C, psR[0][:, 0:128], C.ones[:], dg[:], True, True, ["ones", dk], [psR[1]])
            _ts(C, "dve", Et[:], psR[0][:, 0:128], colb[:, I:I + 1], 0.0, ALU.add, ALU.min, [psR[1], "colb"], [ek])
            _act(C, Et[:], Et[:], AF.Exp, [ek], [ek])
            _tt(C, "pool", Et[:], Et[:], mk[:], ALU.mult, [ek, mkey], [ek])
        _mm(C, ps[:, 0:128], AT[:, J * 128:(J + 1) * 128], BT[:, I * 128:(I + 1) * 128], True, True, ["AT" + kx, "BT" + kx], [pk])

    def emit_da(idx):
        kind, I, pos, J, first, last, lastI = items[idx]
        psO = psO2[pos % 2]
        if kind != "none":
            ps, pk = psS[idx % len(psS)]
            wt, wk = wts[idx % len(wts)]
            if kind == "off":
                _ts(C, "dve", wt[:], ps[:, 0:128], ctab[:, I, J:J + 1], None, ALU.mult, None, [pk, kk + "ctab"], [wk])
                _mm(C, psO[0][:, 0:dvp], wt[:], VP[:, J, :], first, last, [wk, "VP" + kx], [psO[1]])
            else:
                Et, ek = Et2[pos % 2]
                _tt(C, "dve", wt[:], ps[:, 0:128], Et[:], ALU.mult, [pk, ek], [wk])
                _mm(C, psD[0][:, 0:dvp], wt[:], Vraw[:, I, :], True, True, [wk, "Vraw" + kx], [psD[1]])
        if lastI:
            tail(I, pos, pos > 0, psO[0], psO[1])

    n = len(items)
    for idx in range(min(lookahead, n)):
        emit_s(idx)
    for idx in range(n):
        emit_da(idx)
        if idx + lookahead < n:
            emit_s(idx + lookahead)


def std_tail(C, tabs, bufs, dvp, out_fn):
    kf, rowf, ctab, gF, gL, _ = tabs
    psS, psO2, psD, psR, wts, Et2, dg2, offb = bufs

    def tail(I, pos, has_off, psO, psOkey):
        res = C.tailbuf
        if has_off:
            _ts(C, "dve", offb[:, 0:dvp], psO[:, 0:dvp], rowf[:, I:I + 1], None, ALU.mult, None, [psOkey, "tabsrowf"], ["offb"])
            _tt(C, "dve", res[:, 0:dvp], psD[0][:, 0:dvp], offb[:, 0:dvp], ALU.add, [psD[1], "offb"], ["tailbuf"])
        else:
            _cp(C, "dve", res[:, 0:dvp], psD[0][:, 0:dvp], [psD[1]], ["tailbuf"])
        out_fn(I, res, "tailbuf")
    return tail


def emit_norm1_tile(C, xt, kx, N, w, A1, modv, sq, rstd, psb, pskey, h1b, tmp):
    emit_rstd(C, xt, kx, N, sq, "sq", psb, pskey, rstd, "rstd")
    for c in range(NCH):
        _tt(C, "pool", tmp[:, c, :N], xt[:, c, :N], rstd[:, :N], ALU.mult, [kx, "rstd"], ["sq"])
        _ts(C, "dve", h1b[:, c, :N], tmp[:, c, :N], A1[:, c, w:w + 1], modv[:, c, w:w + 1], ALU.mult, ALU.add, ["sq", "A1", "modv"], ["h1b"])


def mixer_tiles():
    return [(0, 256, 1)] + [(256 + 512 * k, 512, 0) for k in range(8)]


def build_mixer_odd(env=None):
    T = T_ALL
    sfx = env.sfx if env else ""
    nc = env.nc if env else bass.Bass("TRN2", target_bir_lowering=False)
    env_nc_holder[0] = nc
    din = _din_factory(nc, env)
    xT_d = din("xT", [D, T]); cv_d = din("cv", [128, NCH, 2]); modw_d = din("modw", [D, 2048]); modb_d = din("modb", [128, 16])
    n1g_d = din("n1g", [128, NCH])
    wfm_d = din("wfm", [D, 1024]); wtm_d = din("wtm", [D, 2048]); wout_d = din("wout", [1024, D])
    rope_d = din("rope", [4, 128, T]); rperm_d = din("rperm", [128, 128]); gtab_d = din("gtab", [128, 8, NB]); gainb_d = din("gainb", [128, 1024])
    yT_d = None if env else nc.dram_tensor("yT", [D, T], F32, kind="ExternalOutput").ap()
    FT_d = nc.dram_tensor("FT_scr" + sfx, [8, 128, T], F32, kind="Internal").ap()
    TM_d = nc.dram_tensor("TM_scr" + sfx, [T, 2048], F32, kind="Internal").ap()
    UT_d = nc.dram_tensor("UT_scr" + sfx, [8, 128, T], MMDT, kind="Internal").ap()
    r3 = lambda ap: ap.rearrange("(c p) n -> p c n", p=128)
    tiles = mixer_tiles()
    with ExitStack() as st:
        C = _begin(env, st, True)
        P = C.P
        C.rot = 0
        pss = C.pss_all
        n1g = C.sb("n1g", [128, NCH]); A1 = C.sb("A1", [128, NCH, 2])
        _dma(C, "sp", n1g[:], n1g_d, [], ["n1g"])
        with ExitStack() as st2:
            C.st = st2
            modv = emit_mod(C, cv_d, modw_d, modb_d, 2048, pss[0][0], "ps0")
            for w in range(2):
                _stt(C, A1[:, :, w], modv[:, 8:16, w], 1.0, n1g[:], ALU.add, ALU.mult, ["modv", "n1g"], ["A1"])
            wfm = C.sb("wfm", [128, NCH, 1024], MMDT); wtm = C.sb("wtm", [128, NCH, 2048], MMDT)
            rperm = C.sb("rperm", [128, 128])
            _dma(C, "sp", rperm[:], rperm_d, [], ["rperm"])
            for c in range(NCH):
                _dma(C, "pool", wfm[:, c, :], wfm_d[c * 128:(c + 1) * 128, :], [], ["wfm"])
                _dma(C, "pool", wtm[:, c, :], wtm_d[c * 128:(c + 1) * 128, :], [], ["wtm"])
            xt = C.sb("xt", [128, NCH, 512]); sq = C.sb("sq", [128, NCH, 512]); rstd = C.sb("rstd", [128, 512])
            h1b = C.sb("h1b", [128, NCH, 512], MMDT)
            ropet = C.sb("ropet", [128, 4, 512])
            raw = [C.sb("raw%d" % i, [128, 512]) for i in range(2)]
            r1 = [C.sb("r1%d" % i, [128, 512]) for i in range(2)]
            r2 = [C.sb("r2%d" % i, [128, 512]) for i in range(2)]
            tmo = [C.sb("tmo%d" % i, [128, 512]) for i in range(2)]
            for ti, (t0, N, w) in enumerate(tiles):
                if env is not None and env.xload is not None:
                    env.xload(C, xt, t0, N)
                else:
                    _dma(C, "sp", xt[:, :, :N], r3(xT_d)[:, :, t0:t0 + N], [], ["xt"])
                _dma(C, "sp", ropet[:, :, :N], rope_d.rearrange("f p t -> p f t")[:, :, t0:t0 + N], [], ["ropet"])
                emit_norm1_tile(C, xt, "xt", N, w, A1, modv, sq, rstd, pss[1][0], "ps1", h1b, sq)
                for fm in range(8):
                    b = fm % 2
                    ps, pk = pss[2 + b]
                    ps2, pk2 = pss[4 + b]
                    for c in range(NCH):
                        _mm(C, ps[:, :N], wfm[:, c, fm * 128:(fm + 1) * 128], h1b[:, c, :N], c == 0, c == NCH - 1, ["wfm", "h1b"], [pk])
                    _cp(C, "act", raw[b][:, :N], ps[:, :N], [pk], ["raw%d" % b])
                    _mm(C, ps2[:, :N], rperm[:], raw[b][:, :N], True, True, ["rperm", "raw%d" % b], [pk2])
                    tb = 0 if fm < 4 else 2
                    _tt(C, "pool", r1[b][:, :N], raw[b][:, :N], ropet[:, tb, :N], ALU.mult, ["raw%d" % b, "ropet"], ["r1%d" % b])
                    _tt(C, "dve", r2[b][:, :N], ps2[:, :N], ropet[:, tb + 1, :N], ALU.mult, [pk2, "ropet"], ["r2%d" % b])
                    _tt(C, "pool", r1[b][:, :N], r1[b][:, :N], r2[b][:, :N], ALU.add, ["r1%d" % b, "r2%d" % b], ["r1%d" % b])
                    _dma(C, "sp", FT_d[fm, :, t0:t0 + N], r1[b][:, :N], ["r1%d" % b], ["FT_d"])
                for blk in range(N // 128):
                    for half in range(4):
                        b = (blk * 4 + half) % 2
                        ps, pk = pss[6 + b]
                        for c in range(NCH):
                            _mm(C, ps[:, :512], h1b[:, c, blk * 128:(blk + 1) * 128], wtm[:, c, half * 512:(half + 1) * 512], c == 0, c == NCH - 1, ["h1b", "wtm"], [pk])
                        _cp(C, "act" if half % 2 else "dve", tmo[b][:], ps[:, :512], [pk], ["tmo%d" % b])
                        _dma(C, "sp", TM_d[t0 + blk * 128:t0 + (blk + 1) * 128, half * 512:(half + 1) * 512], tmo[b][:], ["tmo%d" % b], ["TM_d"])
            P.barrier()
        C.st = st
        with ExitStack() as st3:
            C.st = st3
            AT = C.sb("AT", [128, T], MMDT); BT = C.sb("BT", [128, T], MMDT)
            Vraw = C.sb("Vraw", [128, NB, 256], MMDT); VP = C.sb("VP", [128, NB, 256], MMDT); OH = C.sb("OH", [128, NB, 256])
            gtab = C.sb("gtab", [128, 8, NB]); gainb = C.sb("gainb", [128, 1024])
            _dma(C, "sp", gtab[:], gtab_d, [], ["gtab", "g"])
            _dma(C, "sp", gainb[:], gainb_d, [], ["gainb"])
            colb = C.sb("colb", [128, NB])
            tabs = (C.sb("kf", [128, NB]), C.sb("rowf", [128, NB]), C.sb("ctab", [128, NB, NB]), C.sb("gF", [128, NB]), C.sb("gL", [128, NB]), None)
            wts = [(C.sb("wt%d" % i, [128, 128], MMDT), "wt%d" % i) for i in range(4)]
            Et2 = [(C.sb("Et%d" % i, [128, 128]), "Et%d" % i) for i in range(2)]; dg2 = [(C.sb("dg%d" % i, [128, 128]), "dg%d" % i) for i in range(2)]
            offb = C.sb("offb", [128, 256]); otmp = C.sb("otmp", [128, 256]); C.tailbuf = C.sb("tailbuf", [128, 256])
            bufs = (pss[0:4], [pss[4], pss[6]], pss[5], pss[7], wts, Et2, dg2, offb)
            Gt = C.sb("Gt", [128, 256]); cen = C.sb("cen", [128, 256]); st1 = C.sb("st1", [128, 4]); uT = C.sb("uT", [128, 2, 128], MMDT)
            TMv = TM_d.rearrange("(n p) w -> p n w", p=128)
            for h in range(4):
                _dma(C, "pool", AT[:], FT_d[4 + h], ["FT_d"], ["AT"])
                _dma(C, "pool", BT[:], FT_d[h], ["FT_d"], ["BT"])
                for q4 in range(2):
                    _dma(C, "pool", Vraw[:, q4 * 17:(q4 + 1) * 17, :], TMv[:, q4 * 17:(q4 + 1) * 17, h * 256:(h + 1) * 256], ["TM_d"], ["Vraw"])
                for d in range(2):
                    s = h * 2 + d
                    g = gtab[:, s, :]
                    _ts(C, "dve", colb[:], g, -1.0, None, ALU.mult, None, ["gtab"], ["colb"])
                    emit_stream_tables(C, g, colb, d, tabs, pss[7][0], "ps7", "")
                    for J in range(NB):
                        _ts(C, "pool" if J % 2 else "dve", VP[:, J, :], Vraw[:, J, :], tabs[0][:, J:J + 1], None, ALU.mult, None, ["Vraw", "tabskf"], ["VP"])
                    order = scan_order(d)

                    def out_fn(I, res, rk, d=d):
                        if d == 0:
                            _cp(C, "pool", OH[:, I, :], res[:, 0:256], [rk], ["OH%d" % I])
                        else:
                            _tt(C, "pool", OH[:, I, :], OH[:, I, :], res[:, 0:256], ALU.add, [rk, "OH%d" % I], ["OH%d" % I])
                    emit_stream_pipelined(C, order, AT, BT, VP, Vraw, 256, tabs, g, colb, d, bufs, "", std_tail(C, tabs, bufs, 256, out_fn))
                for I in range(NB):
                    k = "OH%d" % I
                    _dma(C, "sp", Gt[:], TM_d[I * 128:(I + 1) * 128, 1024 + h * 256:1024 + (h + 1) * 256], ["TM_d"], ["Gt"])
                    P.op("dve", (lambda I: lambda e: e.reduce_sum(out=st1[:, 0:1], in_=OH[:, I, :], axis=AX.X))(I), reads=[k], writes=["st1"])
                    _ts(C, "dve", st1[:, 0:1], st1[:, 0:1], -1.0 / 256, None, ALU.mult, None, ["st1"], ["st1"])
                    _ts(C, "dve", cen[:], OH[:, I, :], st1[:, 0:1], None, ALU.add, None, [k, "st1"], ["cen"])
                    P.op("act", lambda e: e.activation(out=otmp[:], in_=cen[:], func=AF.Square, accum_out=st1[:, 1:2]), reads=["cen"], writes=["otmp", "st1b"])
                    _act(C, st1[:, 2:3], st1[:, 1:2], AF.Sqrt, ["st1b"], ["st1c"], bias=EPS, scale=1.0 / 256)
                    P.op("dve", lambda e: e.reciprocal(out=st1[:, 3:4], in_=st1[:, 2:3]), reads=["st1c"], writes=["st1d"])
                    _act(C, Gt[:], Gt[:], AF.Silu, ["Gt"], ["Gt"])
                    _tt(C, "pool", Gt[:], Gt[:], gainb[:, h * 256:(h + 1) * 256], ALU.mult, ["Gt", "gainb"], ["Gt"])
                    _stt(C, cen[:], cen[:], st1[:, 3:4], Gt[:], ALU.mult, ALU.mult, ["cen", "st1d", "Gt"], ["cen"])
                    for ec in range(2):
                        _tr(C, pss[7][0][:, ec * 128:(ec + 1) * 128], cen[:, ec * 128:(ec + 1) * 128], ["cen"], ["ps7"])
                    _cp(C, "act", uT[:].rearrange("p a b -> p (a b)"), pss[7][0][:, 0:256], ["ps7"], ["uT"])
                    for ec in range(2):
                        _dma(C, "sp", UT_d[h * 2 + ec, :, I * 128:(I + 1) * 128], uT[:, ec, :], ["uT"], ["UT_d"])
            P.barrier()
        C.st = st
        with ExitStack() as st4:
            C.st = st4
            wout = C.sb("wout", [128, 8, D], MMDT)
            for hc in range(8):
                _dma(C, "pool", wout[:, hc, :], wout_d[hc * 128:(hc + 1) * 128, :], [], ["wout"])
            ut = [C.sb("utile%d" % i, [128, 8, 512], MMDT) for i in range(2)]
            yo = [C.sb("yo%d" % i, [128, 512]) for i in range(2)]
            for ti, (t0, N, w) in enumerate(tiles):
                b = ti % 2
                _dma(C, "sp", ut[b][:, :, :N], UT_d.rearrange("f p t -> p f t")[:, :, t0:t0 + N], [], ["ut%d" % b])
                for dmc in range(NCH):
                    pb = dmc % 2
                    ps, pk = pss[pb]
                    for hc in range(8):
                        _mm(C, ps[:, :N], wout[:, hc, dmc * 128:(dmc + 1) * 128], ut[b][:, hc, :N], hc == 0, hc == 7, ["wout", "ut%d" % b], [pk])
                    _cp(C, "act" if pb else "dve", yo[pb][:, :N], ps[:, :N], [pk], ["yo%d" % pb])
                    if env is not None:
                        env.ywrite(C, dmc, t0, N, yo[pb], "yo%d" % pb)
                    else:
                        _dma(C, "sp", yT_d[dmc * 128:(dmc + 1) * 128, t0:t0 + N], yo[pb][:, :N], ["yo%d" % pb], ["yT%d_%d" % (ti, dmc)])
            if env is None:
                P.finish()
            else:
                P.barrier()
        if env is None:
            P.emit()
    return nc


RET_HEADS = 8
ROPE_BASE = 10000.0


def mixer_common_inputs(layer, inputs, b, x_lat, x_ctx):
    pc = lambda v: np.ascontiguousarray(v.reshape(-1, 128).T)
    cv = np.stack([pc(inputs["c"][b]), pc(inputs["c_ctx"])], axis=-1)
    xT = np.ascontiguousarray(np.concatenate([x_ctx[b], x_lat[b]], 0).T)
    return {"xT": xT, "cv": np.ascontiguousarray(cv), "modw": np.ascontiguousarray(inputs["mod_w"][layer][:, 0:2048]),
            "modb": pc(inputs["mod_b"][layer][0:2048]), "n1g": pc(inputs["norm1_g"][layer])}


def odd_inputs(layer, inputs, b, hg, x_lat, x_ctx):
    j = layer // 2
    m = mixer_common_inputs(layer, inputs, b, x_lat, x_ctx)
    w_in = inputs["od_w_in"][j]
    hs = slice(hg * 4, hg * 4 + 4)
    wq = w_in[:, 0:1024].reshape(D, 8, 128)[:, hs].reshape(D, 512)
    wk = w_in[:, 1024:2048].reshape(D, 8, 128)[:, hs].reshape(D, 512)
    wv = w_in[:, 2048:4096].reshape(D, 8, 256)[:, hs].reshape(D, 1024)
    wg = w_in[:, 4096:6144].reshape(D, 8, 256)[:, hs].reshape(D, 1024)
    m["wfm"] = np.ascontiguousarray(np.concatenate([wq, wk], 1))
    m["wtm"] = np.ascontiguousarray(np.concatenate([wv, wg], 1))
    m["wout"] = np.ascontiguousarray(inputs["od_w_out"][j].reshape(8, 256, D)[hs].reshape(1024, D))
    half = 64
    freqs = (np.float32(ROPE_BASE) ** (-np.arange(half, dtype=np.float32) / np.float32(half))).astype(np.float32)
    pos = np.arange(T_ALL, dtype=np.float32)
    ang = (pos[:, None] * freqs[None, :]).astype(np.float32)
    cos, sin = np.cos(ang).astype(np.float32).T, np.sin(ang).astype(np.float32).T
    cosT = np.concatenate([cos, cos], 0); sinT = np.concatenate([-sin, sin], 0)
    ks = np.float32(128.0 ** -0.5)
    m["rope"] = np.ascontiguousarray(np.stack([cosT, sinT, cosT * ks, sinT * ks], 0).astype(np.float32))
    m["rperm"] = np.ascontiguousarray(np.roll(np.eye(128, dtype=np.float32), 64, axis=0))
    expo = 5.0 + np.arange(RET_HEADS, dtype=np.float32)
    lg_f = np.log1p(-np.exp2(-expo)).astype(np.float32)
    lg_b = np.log1p(-np.exp2(-expo[::-1])).astype(np.float32)
    sp_f = np.arange(T_ALL, dtype=np.float32)
    sp_b = np.concatenate([255.0 - np.arange(256), 256.0 + 4095.0 - np.arange(4096)]).astype(np.float32)
    gt = np.zeros((128, 8, NB), np.float32)
    for hl in range(4):
        h = hg * 4 + hl
        gt[:, hl * 2 + 0, :] = (sp_f * lg_f[h]).reshape(NB, 128).T
        gt[:, hl * 2 + 1, :] = (sp_b * lg_b[h]).reshape(NB, 128).T
    m["gtab"] = gt
    m["gainb"] = np.ascontiguousarray(np.broadcast_to(inputs["ret_norm_g"][j].reshape(8, 256)[hs].reshape(1, 1024), (128, 1024)))
    return m


def emit_offdiag(C, I, order_pos, order, AT, BT, VP, dvp, ctab, bufs, sid):
    psS, psO, psD, psR, wts, Et, dg, offb = bufs
    before = order[:order_pos]
    isl = slice(I * 128, (I + 1) * 128)
    kk = "tabs%s" % sid
    for n, J in enumerate(before):
        sb_i = C.rot % len(psS)
        C.rot += 1
        ps, pk = psS[sb_i]
        wt, wk = wts[sb_i % len(wts)]
        _mm(C, ps[:, 0:128], AT[:, J * 128:(J + 1) * 128], BT[:, isl], True, True, ["AT" + sid, "BT" + sid], [pk])
        _ts(C, "dve", wt[:], ps[:, 0:128], ctab[:, I, J:J + 1], None, ALU.mult, None, [pk, kk + "ctab"], [wk])
        _mm(C, psO[0][:, 0:dvp], wt[:], VP[:, J, :], n == 0, n == len(before) - 1, [wk, "VP" + sid], [psO[1]])
    return len(before) > 0


def emit_cumsum(C, x, xkey, direction, out, outkey, tmpA, tmpB, psb, pskey):
    n = NB * 4
    flat = lambda t: t[:].rearrange("p a b -> p (a b)")
    m = C.masks["F" if direction == 0 else "B"]
    mkey = "maskF" if direction == 0 else "maskB"
    _mm(C, psb[:, 0:n], C.ones[:], flat(x), True, True, ["ones", xkey], [pskey])
    _cp(C, "dve", flat(tmpA), psb[:, 0:n], [pskey], ["cs_tot"])
    _mm(C, psb[:, 0:n], m[:], flat(x), True, True, [mkey, xkey], [pskey])
    for k in range(4):
        C.P.op("dve", (lambda k: lambda e: e.tensor_tensor_scan(out=tmpB[:, :, k], data0=C.ones[:, 0:NB], data1=tmpA[:, :, k], initial=0.0,
                                                                 op0=ALU.mult, op1=ALU.add))(k), reads=["cs_tot", "ones"], writes=["cs_cs"])
    if direction == 0:
        _tt(C, "dve", flat(tmpB), flat(tmpB), flat(tmpA), ALU.subtract, ["cs_cs", "cs_tot"], ["cs_carry"])
    else:
        _tt(C, "dve", tmpA[:, 0, :], tmpB[:, 1, :], tmpB[:, NB - 1, :], ALU.add, ["cs_cs", "cs_tot"], ["cs_tot"])
        _cp(C, "dve", tmpA[:, 2, :], tmpA[:, 1, :], ["cs_tot"], ["cs_tot"])
        _tt(C, "dve", tmpB[:, 2:NB, :], tmpA[:, 0:1, :].to_broadcast([128, NB - 2, 4]), tmpB[:, 2:NB, :], ALU.subtract, ["cs_cs", "cs_tot"], ["cs_carry"])
        _cp(C, "dve", tmpB[:, 0, :], tmpA[:, 2, :], ["cs_tot", "cs_carry"], ["cs_carry"])
        C.P.op("dve", lambda e: e.memset(tmpB[:, 1, :], 0.0), reads=["cs_carry"], writes=["cs_carry"])
    _tt(C, "dve", flat(out), psb[:, 0:n], flat(tmpB), ALU.add, [pskey, "cs_carry"], [outkey])


def build_mixer_even(env=None):
    T = T_ALL
    sfx = env.sfx if env else ""
    nc = env.nc if env else bass.Bass("TRN2", target_bir_lowering=False)
    env_nc_holder[0] = nc
    din = _din_factory(nc, env)
    xT_d = din("xT", [D, T]); cv_d = din("cv", [128, NCH, 2]); modw_d = din("modw", [D, 2048]); modb_d = din("modb", [128, 16])
    n1g_d = din("n1g", [128, NCH])
    wfm_d = din("wfm", [D, 1280]); wtm_d = din("wtm", [D, 784]); wout_d = din("wout", [512, D])
    convw_d = din("convw", [128, 6, 3]); gconst_d = din("gconst", [128, 4, 4]); gainb_d = din("gainb", [128, 512])
    yT_d = None if env else nc.dram_tensor("yT", [D, T], F32, kind="ExternalOutput").ap()
    FT_d = nc.dram_tensor("FT_scr" + sfx, [10, 128, T], F32, kind="Internal").ap()
    TM_d = nc.dram_tensor("TM_scr" + sfx, [T, 784], F32, kind="Internal").ap()
    UT_d = nc.dram_tensor("UT_scr" + sfx, [4, 128, T], MMDT, kind="Internal").ap()
    r3 = lambda ap: ap.rearrange("(c p) n -> p c n", p=128)
    tiles = mixer_tiles()
    with ExitStack() as st:
        C = _begin(env, st, True)
        P = C.P
        C.rot = 0
        pss = C.pss_all
        n1g = C.sb("n1g", [128, NCH]); A1 = C.sb("A1", [128, NCH, 2])
        _dma(C, "sp", n1g[:], n1g_d, [], ["n1g"])
        with ExitStack() as st2:
            C.st = st2
            modv = emit_mod(C, cv_d, modw_d, modb_d, 2048, pss[0][0], "ps0")
            for w in range(2):
                _stt(C, A1[:, :, w], modv[:, 8:16, w], 1.0, n1g[:], ALU.add, ALU.mult, ["modv", "n1g"], ["A1"])
            wfm = C.sb("wfm", [128, NCH, 1280], MMDT); wtm = C.sb("wtm", [128, NCH, 784], MMDT)
            convw = C.sb("convw", [128, 6, 3])
            _dma(C, "sp", convw[:], convw_d, [], ["convw"])
            for c in range(NCH):
                _dma(C, "pool", wfm[:, c, :], wfm_d[c * 128:(c + 1) * 128, :], [], ["wfm"])
                _dma(C, "pool", wtm[:, c, :], wtm_d[c * 128:(c + 1) * 128, :], [], ["wtm"])
            xt = C.sb("xt", [128, NCH, 512]); sq = C.sb("sq", [128, NCH, 512]); rstd = C.sb("rstd", [128, 512])
            h1b = C.sb("h1b", [128, NCH, 512], MMDT)
            raw = [C.sb("raw%d" % i, [128, 512]) for i in range(2)]
            r1 = [C.sb("r1%d" % i, [128, 512]) for i in range(2)]
            r2 = [C.sb("r2%d" % i, [128, 512]) for i in range(2)]
            tmo = [C.sb("tmo%d" % i, [128, 512]) for i in range(2)]
            for ti, (t0, N, w) in enumerate(tiles):
                if env is not None and env.xload is not None:
                    env.xload(C, xt, t0, N)
                else:
                    _dma(C, "sp", xt[:, :, :N], r3(xT_d)[:, :, t0:t0 + N], [], ["xt"])
                emit_norm1_tile(C, xt, "xt", N, w, A1, modv, sq, rstd, pss[1][0], "ps1", h1b, sq)
                R, RL = (1, 256) if w == 1 else (N // 64, 64)
                v3 = lambda t: t[:, :N].rearrange("p (r l) -> p r l", l=RL)
                for fm in range(10):
                    b = fm % 2
                    ps, pk = pss[2 + b]
                    ps2, pk2 = pss[4 + b]
                    kr, k1, k2 = "raw%d" % b, "r1%d" % b, "r2%d" % b
                    for c in range(NCH):
                        _mm(C, ps[:, :N], wfm[:, c, fm * 128:(fm + 1) * 128], h1b[:, c, :N], c == 0, c == NCH - 1, ["wfm", "h1b"], [pk])
                    if fm < 6:
                        _cp(C, "act", raw[b][:, :N], ps[:, :N], [pk], [kr])
                        _ts(C, "dve", r1[b][:, :N], raw[b][:, :N], convw[:, fm, 1:2], None, ALU.mult, None, [kr, "convw"], [k1])
                        _stt(C, v3(r1[b])[:, :, 1:RL], v3(raw[b])[:, :, 0:RL - 1], convw[:, fm, 0:1], v3(r1[b])[:, :, 1:RL], ALU.mult, ALU.add, [kr, k1, "convw"], [k1])
                        _stt(C, v3(r1[b])[:, :, 0:RL - 1], v3(raw[b])[:, :, 1:RL], convw[:, fm, 2:3], v3(r1[b])[:, :, 0:RL - 1], ALU.mult, ALU.add, [kr, k1, "convw"], [k1])
                        _act(C, r1[b][:, :N], r1[b][:, :N], AF.Silu, [k1], [k1])
                        if fm < 4:
                            _act(C, r2[b][:, :N], r1[b][:, :N], AF.Square, [k1], [k2])
                            _mm(C, ps2[:, :N], C.ones[:], r2[b][:, :N], True, True, ["ones", k2], [pk2])
                            _act(C, r2[b][:, :N], ps2[:, :N], AF.Sqrt, [pk2], [k2], bias=EPS, scale=1.0)
                            P.op("dve", (lambda b, N: lambda e: e.reciprocal(out=r2[b][:, :N], in_=r2[b][:, :N]))(b, N), reads=[k2], writes=[k2])
                            _stt(C, r1[b][:, :N], r1[b][:, :N], (128.0 ** -0.5) if fm < 2 else 1.0, r2[b][:, :N], ALU.mult, ALU.mult, [k1, k2], [k1])
                    elif fm < 8:
                        _cp(C, "act", r1[b][:, :N], ps[:, :N], [pk], [k1])
                    else:
                        _act(C, r1[b][:, :N], ps[:, :N], AF.Identity, [pk], [k1], scale=128.0 ** -0.5)
                    _dma(C, "sp", FT_d[fm, :, t0:t0 + N], r1[b][:, :N], [k1], ["FT_d"])
                for blk in range(N // 128):
                    for half, (c0, c1) in enumerate(((0, 512), (512, 784))):
                        b = (blk * 2 + half) % 2
                        ps, pk = pss[6 + b]
                        for c in range(NCH):
                            _mm(C, ps[:, :c1 - c0], h1b[:, c, blk * 128:(blk + 1) * 128], wtm[:, c, c0:c1], c == 0, c == NCH - 1, ["h1b", "wtm"], [pk])
                        _cp(C, "act" if half % 2 else "dve", tmo[b][:, :c1 - c0], ps[:, :c1 - c0], [pk], ["tmo%d" % b])
                        _dma(C, "sp", TM_d[t0 + blk * 128:t0 + (blk + 1) * 128, c0:c1], tmo[b][:, :c1 - c0], ["tmo%d" % b], ["TM_d"])
            P.barrier()
        C.st = st
        with ExitStack() as st3:
            C.st = st3
            TMv = TM_d.rearrange("(n p) w -> p n w", p=128)
            gates = C.sb("gates", [128, NB, 16]); gconst = C.sb("gconst", [128, 4, 4]); gainb = C.sb("gainb", [128, 512])
            _dma(C, "sp", gates[:], TMv[:, :, 768:784], [], ["gates"])
            _dma(C, "sp", gconst[:], gconst_d, [], ["gconst"])
            _dma(C, "sp", gainb[:], gainb_d, [], ["gainb"])
            la = C.sb("la", [128, NB, 4]); beta = C.sb("beta", [128, NB, 4]); ic = C.sb("ic", [128, NB, 4]); lf = C.sb("lf", [128, NB, 4])
            gG = C.sb("gG", [128, NB, 4]); gF_ = C.sb("gFm", [128, NB, 4]); tA = C.sb("tA", [128, NB, 4]); tB = C.sb("tB", [128, NB, 4])
            arate = C.sb("arate", [128, 4]); cmax = C.sb("cmax", [128, 4]); ecneg = C.sb("ecneg", [128, 4]); c1 = C.sb("c1", [128, 4])
            bc = lambda col: gconst[:, col:col + 1, :].to_broadcast([128, NB, 4])
            _act(C, arate[:], gconst[:, 0, :], AF.Exp, ["gconst"], ["arate"])
            _tt(C, "dve", la[:], gates[:, :, 0:4], bc(1), ALU.add, ["gates", "gconst"], ["la"])
            _act(C, la[:], la[:], AF.Exp, ["la"], ["la"])
            _act(C, la[:], la[:], AF.Ln, ["la"], ["la"], bias=1.0)
            _tt(C, "dve", la[:], la[:], arate[:, None, :].to_broadcast([128, NB, 4]), ALU.mult, ["la", "arate"], ["la"])
            _ts(C, "dve", la[:], la[:], -1.0, None, ALU.mult, None, ["la"], ["la"])
            _act(C, beta[:], gates[:, :, 4:8], AF.Sigmoid, ["gates"], ["beta"])
            _tt(C, "dve", ic[:], gates[:, :, 8:12], bc(2), ALU.add, ["gates", "gconst"], ["ic"])
            _tt(C, "dve", lf[:], gates[:, :, 12:16], bc(3), ALU.add, ["gates", "gconst"], ["lf"])
            _act(C, lf[:], lf[:], AF.Exp, ["lf"], ["lf"], scale=-1.0)
            _act(C, lf[:], lf[:], AF.Ln, ["lf"], ["lf"], bias=1.0)
            _ts(C, "dve", lf[:], lf[:], -1.0, None, ALU.mult, None, ["lf"], ["lf"])
            P.op("dve", lambda e: e.tensor_reduce(out=c1[:], in_=ic[:].rearrange("p n k -> p k n"), axis=AX.X, op=ALU.max), reads=["ic"], writes=["c1"])
            _tr(C, pss[7][0][0:4, 0:128], c1[:], ["c1"], ["ps7"])
            c2 = C.sb("c2", [4, 1]); c3 = C.sb("c3", [4, 4])
            P.op("dve", lambda e: e.reduce_max(out=c2[:], in_=pss[7][0][0:4, 0:128], axis=AX.X), reads=["ps7"], writes=["c2"])
            _ts(C, "dve", c3[:], C.ident[0:4, 0:4], c2[:, 0:1], None, ALU.mult, None, ["ident", "c2"], ["c3"])
            _mm(C, pss[7][0][:, 0:4], C.ones[0:4, :], c3[:], True, True, ["ones", "c3"], ["ps7"])
            _cp(C, "dve", cmax[:], pss[7][0][:, 0:4], ["ps7"], ["cmax"])
            _act(C, ecneg[:], cmax[:], AF.Exp, ["cmax"], ["ecneg"], scale=-1.0)
            ncmax = C.sb("ncmax", [128, 4])
            _ts(C, "dve", ncmax[:], cmax[:], -1.0, None, ALU.mult, None, ["cmax"], ["ncmax"])

            KT = C.sb("KT", [128, T]); QT = C.sb("QT", [128, T], MMDT); VT = C.sb("VT", [128, T]); KTb = C.sb("KTb", [128, T], MMDT)
            Xb = C.sb("Xb", [128, NB, 129], MMDT); XPb = C.sb("XPb", [128, NB, 129], MMDT)
            gdn_heads = 2 if EVEN_STOP >= 3 else 0
            ml_heads = 2 if EVEN_STOP >= 7 else 0
            Vtm = C.sb("Vtm", [128, NB, 129]); X = C.sb("X", [128, NB, 129]); XP = C.sb("XP", [128, NB, 129])
            TT = C.sb("TT", [128, NB, 128]); OH = C.sb("OH", [128, NB, 128])
            g = C.sb("g", [128, NB]); colb = C.sb("colb", [128, NB]); rowfb = C.sb("rowfb", [128, NB])
            tabs = (C.sb("kf", [128, NB]), C.sb("rowf", [128, NB]), C.sb("ctab", [128, NB, NB]), C.sb("gF", [128, NB]), C.sb("gL", [128, NB]), None)
            kf, rowf, ctab = tabs[0], tabs[1], tabs[2]
            wts = [(C.sb("wt%d" % i, [128, 128]), "wt%d" % i) for i in range(4)]
            wtsb = [(C.sb("wtb%d" % i, [128, 128], MMDT), "wtb%d" % i) for i in range(4)]
            Et = C.sb("Et", [128, 128]); dg = C.sb("dg", [128, 128]); offb = C.sb("offb", [128, 129]); otmp = C.sb("otmp", [128, 129])
            Et2 = [(C.sb("Etp%d" % i, [128, 128]), "Etp%d" % i) for i in range(2)]; dg2 = [(C.sb("dgp%d" % i, [128, 128]), "dgp%d" % i) for i in range(2)]
            C.tailbuf = C.sb("tailbuf", [128, 129])
            Lm = C.sb("Lm", [128, 128]); Am = [C.sb("Am%d" % i, [128, 128]) for i in range(2)]; Bm = [C.sb("Bm%d" % i, [128, 128]) for i in range(2)]
            Pm = C.sb("Pm", [128, 128]); Rm = C.sb("Rm", [128, 128])
            bufs = (pss[0:4], pss[4], pss[5], pss[6], wts, Et, dg, offb)
            bufsb = (pss[0:4], pss[4], pss[5], pss[6], wtsb, Et, dg, offb)
            pbufs = (pss[0:4], [pss[4], pss[6]], pss[5], pss[7], wts, Et2, dg2, offb)
            pbufsb = (pss[0:4], [pss[4], pss[6]], pss[5], pss[7], wtsb, Et2, dg2, offb)
            Gt = C.sb("Gt", [128, 128]); cen = C.sb("cen", [128, 128]); st1 = C.sb("st1", [128, 4]); uT = C.sb("uT", [128, 128], MMDT)
            ps7 = pss[7][0]

            def head_tail(hidx, gate_col, gate_func, gain_off):
                for I in range(NB):
                    k = "OH%d" % I
                    _dma(C, "sp", Gt[:], TM_d[I * 128:(I + 1) * 128, gate_col:gate_col + 128], ["TM_d"], ["Gt"])
                    P.op("act", (lambda I: lambda e: e.activation(out=otmp[:, 0:128], in_=OH[:, I, :], func=AF.Square, accum_out=st1[:, 1:2]))(I), reads=[k], writes=["otmp", "st1b"])
                    _act(C, st1[:, 2:3], st1[:, 1:2], AF.Sqrt, ["st1b"], ["st1c"], bias=EPS, scale=1.0 / 128)
                    P.op("dve", lambda e: e.reciprocal(out=st1[:, 3:4], in_=st1[:, 2:3]), reads=["st1c"], writes=["st1d"])
                    _act(C, Gt[:], Gt[:], gate_func, ["Gt"], ["Gt"])
                    _tt(C, "pool", Gt[:], Gt[:], gainb[:, gain_off:gain_off + 128], ALU.mult, ["Gt", "gainb"], ["Gt"])
                    _stt(C, cen[:], OH[:, I, :], st1[:, 3:4], Gt[:], ALU.mult, ALU.mult, [k, "st1d", "Gt"], ["cen"])
                    _tr(C, ps7[:, 0:128], cen[:], ["cen"], ["ps7"])
                    _cp(C, "act", uT[:], ps7[:, 0:128], ["ps7"], ["uT"])
                    _dma(C, "sp", UT_d[hidx, :, I * 128:(I + 1) * 128], uT[:], ["uT"], ["UT_d"])

            for hl in range(gdn_heads):
                _dma(C, "sp", KT[:], FT_d[2 + hl], ["FT_d"], ["AT"])
                _dma(C, "pool", KTb[:], FT_d[2 + hl], ["FT_d"], ["ATb"])
                _dma(C, "pool", QT[:], FT_d[hl], ["FT_d"], ["BTb"])
                _dma(C, "sp", VT[:], FT_d[4 + hl], ["FT_d"], ["VT"])
                for I in range(NB):
                    _tr(C, ps7[:, 0:128], VT[:, I * 128:(I + 1) * 128], ["VT"], ["ps7"])
                    _cp(C, "act", Vtm[:, I, 0:128], ps7[:, 0:128], ["ps7"], ["Vtm"])
                for d in range(2):
                    col = d * 2 + hl
                    order = scan_order(d)
                    emit_cumsum(C, la, "la", d, gG, "gG", tA, tB, ps7, "ps7")
                    _cp(C, "dve", g[:], gG[:, :, col], ["gG"], ["g"])
                    _ts(C, "dve", colb[:], g[:], -1.0, None, ALU.mult, None, ["g"], ["colb"])
                    emit_stream_tables(C, g[:], colb, d, tabs, ps7, "ps7", "")
                    _tt(C, "dve", rowfb[:], rowf[:], beta[:, :, col], ALU.mult, ["tabsrowf", "beta"], ["rowfb"])
                    smask = C.masks["Bs" if d == 0 else "Fs"]
                    smkey = "maskBs" if d == 0 else "maskFs"
                    for I in range(NB):
                        isl = slice(I * 128, (I + 1) * 128)
                        _ts(C, "dve", dg[:], C.ident[:], g[:, I:I + 1], None, ALU.mult, None, ["ident", "g"], ["dg"])
                        _mm(C, pss[6][0][:, 0:128], C.ones[:], dg[:], True, True, ["ones", "dg"], ["ps6"])
                        _ts(C, "dve", Et[:], pss[6][0][:, 0:128], colb[:, I:I + 1], 0.0, ALU.add, ALU.max, ["ps6", "colb"], ["Et"])
                        _act(C, Et[:], Et[:], AF.Exp, ["Et"], ["Et"], scale=-1.0)
                        _tt(C, "pool", Et[:], Et[:], smask[:], ALU.mult, ["Et", smkey], ["Et"])
                        _mm(C, pss[0][0][:, 0:128], KT[:, isl], KT[:, isl], True, True, ["AT"], ["ps0"])
                        _stt(C, Bm[0][:], pss[0][0][:, 0:128], beta[:, I, col:col + 1], Et[:], ALU.mult, ALU.mult, ["ps0", "beta", "Et"], ["Bm0"])
                        _tr(C, pss[1][0][:, 0:128], Bm[0][:], ["Bm0"], ["ps1"])
                        _cp(C, "act", Am[0][:], pss[1][0][:, 0:128], ["ps1"], ["Am0"])
                        _tt(C, "dve", Pm[:], C.ident[:], Am[0][:], ALU.subtract, ["ident", "Am0"], ["Pm"])
                        for lev in range(1, 7):
                            a0, a1 = (lev - 1) % 2, lev % 2
                            _mm(C, pss[2][0][:, 0:128], Bm[a0][:], Am[a0][:], True, True, ["Bm%d" % a0, "Am%d" % a0], ["ps2"])
                            _mm(C, pss[3][0][:, 0:128], Am[a0][:], Bm[a0][:], True, True, ["Bm%d" % a0, "Am%d" % a0], ["ps3"])
                            _cp(C, "act", Am[a1][:], pss[2][0][:, 0:128], ["ps2"], ["Am%d" % a1])
                            _cp(C, "dve", Bm[a1][:], pss[3][0][:, 0:128], ["ps3"], ["Bm%d" % a1])
                            _mm(C, pss[1][0][:, 0:128], Bm[a1][:], Pm[:], True, True, ["Bm%d" % a1, "Pm"], ["ps1"])
                            _tt(C, "dve", Pm[:], Pm[:], pss[1][0][:, 0:128], ALU.add, ["Pm", "ps1"], ["Pm"])
                        _cp(C, "pool", TT[:, I, :], Pm[:], ["Pm"], ["TT"])
                    def tail1(I, pos, has_off, psO, psOkey, col=col):
                        _ts(C, "dve", Rm[:], Vtm[:, I, 0:128], beta[:, I, col:col + 1], None, ALU.mult, None, ["Vtm", "beta"], ["Rm"])
                        if has_off:
                            _ts(C, "dve", offb[:, 0:128], psO[:, 0:128], rowfb[:, I:I + 1], None, ALU.mult, None, [psOkey, "rowfb"], ["offb"])
                            _tt(C, "pool", Rm[:], Rm[:], offb[:, 0:128], ALU.subtract, ["Rm", "offb"], ["Rm"])
                        _mm(C, pss[5][0][:, 0:128], TT[:, I, :], Rm[:], True, True, ["TT", "Rm"], ["ps5"])
                        _cp(C, "act", X[:, I, 0:128], pss[5][0][:, 0:128], ["ps5"], ["Vraw"])
                        _ts(C, "dve", XP[:, I, 0:128], pss[5][0][:, 0:128], kf[:, I:I + 1], None, ALU.mult, None, ["ps5", "tabskf"], ["VP"])
                        _cp(C, "pool", Xb[:, I, 0:128], X[:, I, 0:128], ["Vraw"], ["Vrawb"])
                        _cp(C, "pool", XPb[:, I, 0:128], XP[:, I, 0:128], ["VP"], ["VPb"])
                    emit_stream_pipelined(C, order, KT, KT, XP[:, :, 0:128], X[:, :, 0:128], 128, tabs, g, colb, d, pbufs, "", tail1, diag=False)
                    def out2(I, res, rk, d=d):
                        if d == 0:
                            _cp(C, "pool", OH[:, I, :], res[:, 0:128], [rk], ["OH%d" % I])
                        else:
                            _tt(C, "pool", OH[:, I, :], OH[:, I, :], res[:, 0:128], ALU.add, [rk, "OH%d" % I], ["OH%d" % I])
                    emit_stream_pipelined(C, order, KTb, QT, XPb[:, :, 0:128], Xb[:, :, 0:128], 128, tabs, g, colb, d, pbufsb, "b", std_tail(C, tabs, pbufsb, 128, out2))
                if EVEN_STOP >= 6:
                    head_tail(hl, hl * 128, AF.Silu, 0)
            for hl in range(ml_heads):
                _dma(C, "pool", KTb[:], FT_d[8 + hl], ["FT_d"], ["ATb"])
                _dma(C, "pool", QT[:], FT_d[6 + hl], ["FT_d"], ["BTb"])
                _dma(C, "sp", Vtm[:, :, 0:128], TMv[:, :, 256 + hl * 128:256 + (hl + 1) * 128], ["TM_d"], ["Vraw", "Vtm"])
                P.op("pool", lambda e: e.memset(Vtm[:, :, 128:129], 1.0), reads=["Vraw"], writes=["Vraw"])
                for J in range(NB):
                    _cp(C, "act", Xb[:, J, :], Vtm[:, J, :], ["Vraw"], ["Vrawb"])
                for d in range(2):
                    col = d * 2 + hl
                    order = scan_order(d)
                    emit_cumsum(C, lf, "lf", d, gF_, "gFm", tA, tB, ps7, "ps7")
                    _cp(C, "dve", g[:], gF_[:, :, col], ["gFm"], ["g"])
                    _tt(C, "dve", colb[:], ic[:, :, col], g[:], ALU.subtract, ["ic", "g"], ["colb"])
                    _ts(C, "dve", colb[:], colb[:], ncmax[:, col:col + 1], None, ALU.add, None, ["colb", "ncmax"], ["colb"])
                    emit_stream_tables(C, g[:], colb, d, tabs, ps7, "ps7", "")
                    for J in range(NB):
                        _ts(C, "pool" if J % 2 else "dve", XPb[:, J, :], Vtm[:, J, :], kf[:, J:J + 1], None, ALU.mult, None, ["Vraw", "tabskf"], ["VPb"])
                    def out3(I, res, rk, d=d, col=col):
                        _act(C, st1[:, 0:1], res[:, 128:129], AF.Abs, [rk], ["st1"])
                        _ts(C, "dve", st1[:, 0:1], st1[:, 0:1], ecneg[:, col:col + 1], None, ALU.max, None, ["st1", "ecneg"], ["st1"])
                        P.op("dve", lambda e: e.reciprocal(out=st1[:, 0:1], in_=st1[:, 0:1]), reads=["st1"], writes=["st1"])
                        if d == 0:
                            _ts(C, "dve", OH[:, I, :], res[:, 0:128], st1[:, 0:1], None, ALU.mult, None, [rk, "st1"], ["OH%d" % I])
                        else:
                            _stt(C, OH[:, I, :], res[:, 0:128], st1[:, 0:1], OH[:, I, :], ALU.mult, ALU.add, [rk, "st1", "OH%d" % I], ["OH%d" % I])
                    emit_stream_pipelined(C, order, KTb, QT, XPb, Xb, 129, tabs, g, colb, d, pbufsb, "b", std_tail(C, tabs, pbufsb, 129, out3))
                head_tail(2 + hl, 512 + hl * 128, AF.Sigmoid, 128 + hl * 128)
            P.barrier()
        C.st = st
        with ExitStack() as st4:
            C.st = st4
            wout = C.sb("wout", [128, 4, D], MMDT)
            for hc in range(4):
                _dma(C, "pool", wout[:, hc, :], wout_d[hc * 128:(hc + 1) * 128, :], [], ["wout"])
            ut = [C.sb("utile%d" % i, [128, 4, 512], MMDT) for i in range(2)]
            yo = [C.sb("yo%d" % i, [128, 512]) for i in range(2)]
            for ti, (t0, N, w) in enumerate(tiles):
                b = ti % 2
                _dma(C, "sp", ut[b][:, :, :N], UT_d.rearrange("f p t -> p f t")[:, :, t0:t0 + N], [], ["ut%d" % b])
                for dmc in range(NCH):
                    pb = dmc % 2
                    ps, pk = pss[pb]
                    for hc in range(4):
                        _mm(C, ps[:, :N], wout[:, hc, dmc * 128:(dmc + 1) * 128], ut[b][:, hc, :N], hc == 0, hc == 3, ["wout", "ut%d" % b], [pk])
                    _cp(C, "act" if pb else "dve", yo[pb][:, :N], ps[:, :N], [pk], ["yo%d" % pb])
                    if env is not None:
                        env.ywrite(C, dmc, t0, N, yo[pb], "yo%d" % pb)
                    else:
                        _dma(C, "sp", yT_d[dmc * 128:(dmc + 1) * 128, t0:t0 + N], yo[pb][:, :N], ["yo%d" % pb], ["yT%d_%d" % (ti, dmc)])
            if env is None:
                P.finish()
            else:
                P.barrier()
        if env is None:
            P.emit()
    return nc


def even_inputs(layer, inputs, b, hg, x_lat, x_ctx):
    j = layer // 2
    m = mixer_common_inputs(layer, inputs, b, x_lat, x_ctx)
    w_in = inputs["ev_w_in"][j]
    hs = [hg * 2, hg * 2 + 1]
    hcols = lambda off, h: w_in[:, off + h * 128: off + (h + 1) * 128]
    fm = [hcols(0, h) for h in hs] + [hcols(512, h) for h in hs] + [hcols(1024, h) for h in hs] + \
         [hcols(2064, h) for h in hs] + [hcols(2576, h) for h in hs]
    m["wfm"] = np.ascontiguousarray(np.concatenate(fm, 1))
    gate_cols = []
    for off in (2048, 2056, 4112, 4120):
        for d in range(2):
            for h in hs:
                gate_cols.append(w_in[:, off + d * 4 + h: off + d * 4 + h + 1])
    tm = [hcols(1536, h) for h in hs] + [hcols(3088, h) for h in hs] + [hcols(3600, h) for h in hs] + gate_cols
    m["wtm"] = np.ascontiguousarray(np.concatenate(tm, 1))
    w_out = inputs["ev_w_out"][j]
    m["wout"] = np.ascontiguousarray(np.concatenate([w_out[h * 128:(h + 1) * 128] for h in hs] + [w_out[512 + h * 128:512 + (h + 1) * 128] for h in hs], 0))
    cw = inputs["ev_conv_w"][j]
    conv = np.zeros((128, 6, 3), np.float32)
    for qi, off in enumerate((0, 512, 1024)):
        for hi, h in enumerate(hs):
            conv[:, qi * 2 + hi, :] = cw[:, off + h * 128: off + (h + 1) * 128].T
    m["convw"] = conv
    gc = np.zeros((128, 4, 4), np.float32)
    for r, name in enumerate(("gdn_a_log", "gdn_dt_bias", "ml_i_bias", "ml_f_bias")):
        for d in range(2):
            for hi, h in enumerate(hs):
                gc[:, r, d * 2 + hi] = inputs[name][j][d, h]
    m["gconst"] = gc
    gb = np.concatenate([inputs["gdn_norm_g"][j]] + [inputs["ml_norm_g"][j][h * 128:(h + 1) * 128] for h in hs] + [np.zeros(128, np.float32)])
    m["gainb"] = np.ascontiguousarray(np.broadcast_to(gb[None, :], (128, 512)).astype(np.float32))
    return m


NTH = 2176
PAIRS = [[0, 1], [2, 3], [4, 5], [6, 7]]


def build_fused():
    nc = bass.Bass("TRN2", target_bir_lowering=False)
    env_nc_holder[0] = nc
    idx_d = nc.dram_tensor("idx_tab", [128, 16], I32, kind="ExternalInput").ap()
    ysend = nc.dram_tensor("ysend", [2, D, NTH], F32, kind="Internal").ap()
    yrecv = nc.dram_tensor("yrecv", [16 * 256, NTH], F32, kind="Internal", addr_space="Local").ap()
    ysel = nc.dram_tensor("ysel", [2, D, NTH], F32, kind="Internal").ap()
    xo0 = nc.dram_tensor("xo0", [D, NTH], F32, kind="Internal").ap()
    g2 = nc.dram_tensor("g2", [8 * 256, NTH], F32, kind="Internal", addr_space="Local").ap()
    with ExitStack() as gst:
        C = Ctx(nc, gst)
        C.sfx = "_g"
        P = C.P
        emit_consts(C)
        emit_masks(C)
        C.pss_all = [(C.ps("ps%d" % i), "ps%d" % i) for i in range(8)]
        idx_sb = C.sb("idx", [128, 16], I32)
        _dma(C, "sp", idx_sb[:], idx_d, [], ["idx"])
        env = Env(nc, C)

        def ywrite(C, dmc, t0, N, yo, yokey):
            rows = slice(dmc * 128, (dmc + 1) * 128)
            if t0 == 0:
                _dma(C, "sp", ysend[0, rows, 2048:2176], yo[:, 0:128], [yokey], ["ysend"])
                _dma(C, "sp", ysend[1, rows, 2048:2176], yo[:, 128:256], [yokey], ["ysend"])
            else:
                j0 = t0 - 256
                _dma(C, "sp", ysend[j0 // 2048, rows, j0 % 2048:j0 % 2048 + N], yo[:, :N], [yokey], ["ysend"])

        def exchange_y(tag):
            for k in range(16):
                h, c = k // 8, k % 8
                P.coll((lambda h, c, k: lambda e: e.collective_compute("AllGather", ALU.bypass, replica_groups=PAIRS,
                                                                         ins=[ysend[h, c * 128:(c + 1) * 128, :]],
                                                                         outs=[yrecv[k * 256:(k + 1) * 256, :]]))(h, c, k),
                       reads=["ysend"], writes=["yrecv"])
            with ExitStack() as stx:
                C.st = stx
                C.sfx = "_x" + tag
                selb = [C.sb("selb%d" % i, [128, NTH]) for i in range(2)]
                for c in range(8):
                    for r in range(2):
                        b = (c * 2 + r) % 2
                        col = c * 2 + r
                        P.dma("pool", (lambda b, col: lambda e: e.indirect_dma_start(
                            out=selb[b][:, :], out_offset=None, in_=yrecv[:, :],
                            in_offset=bass.IndirectOffsetOnAxis(ap=idx_sb[:, col:col + 1], axis=0)))(b, col),
                            reads=["yrecv", "idx"], writes=["selb%d" % b])
                        _dma(C, "sp", ysel[r, c * 128:(c + 1) * 128, :], selb[b][:], ["selb%d" % b], ["ysel"])
                P.barrier()
            C.st = gst

        def exchange_x():
            for c in range(8):
                P.coll((lambda c: lambda e: e.collective_compute("AllGather", ALU.bypass, replica_groups=PAIRS,
                                                                   ins=[xo0[c * 128:(c + 1) * 128, :]],
                                                                   outs=[g2[c * 256:(c + 1) * 256, :]]))(c),
                       reads=["xo0"], writes=["g2"])
            P.barrier()

        g2v = g2.rearrange("(c r p) n -> p c r n", c=8, r=2, p=128)

        def xload_g2(C, xt, t0, N):
            if t0 == 0:
                _dma(C, "sp", xt[:, :, 0:128], g2v[:, :, 0, 2048:2176], [], ["xt"])
                _dma(C, "sp", xt[:, :, 128:256], g2v[:, :, 1, 2048:2176], [], ["xt"])
            else:
                j0 = t0 - 256
                _dma(C, "sp", xt[:, :, :N], g2v[:, :, j0 // 2048, j0 % 2048:j0 % 2048 + N], [], ["xt"])

        env.sfx, env.over, env.xload, env.ywrite = "_m0", {}, None, ywrite
        build_mixer_even(env)
        C.st = gst
        exchange_y("0")
        env.sfx, env.over = "_f0", {"ypa": ysel[0], "ypb": ysel[1], "out": xo0}
        build_ffn(2048, 128, False, env)
        C.st = gst
        exchange_x()
        env.sfx, env.over, env.xload = "_m1", {"xT": None}, xload_g2
        build_mixer_odd(env)
        C.st = gst
        exchange_y("1")
        env.sfx, env.over = "_f1", {"xT": xo0[:, 0:2048], "ypa": ysel[0][:, 0:2048], "ypb": ysel[1][:, 0:2048]}
        build_ffn(2048, 0, True, env)
        C.st = gst
        P.finish()
        P.emit()
    return nc


def fused_inputs(inputs, c):
    b, r = c // 2, c % 2
    x_lat, x_ctx = inputs["x"], inputs["ctx"]
    m = {}
    for k, v in even_inputs(0, inputs, b, r, x_lat, x_ctx).items():
        m[k + "_m0"] = v
    xh = np.concatenate([x_lat[b, r * 2048:(r + 1) * 2048], x_ctx[b, r * 128:(r + 1) * 128]], 0).T
    for k, v in ffn_inputs(0, inputs, xh, None, None, b).items():
        if k not in ("ypa", "ypb"):
            m[k + "_f0"] = v
    for k, v in odd_inputs(1, inputs, b, r, x_lat, x_ctx).items():
        if k != "xT":
            m[k + "_m1"] = v
    for k, v in ffn_inputs(1, inputs, None, None, None, b).items():
        if k not in ("xT", "ypa", "ypb"):
            m[k + "_f1"] = v
    idx = np.zeros((128, 16), np.int32)
    for cc in range(8):
        for rr in range(2):
            idx[:, cc * 2 + rr] = ((r * 8 + cc) * 2 + rr) * 128 + np.arange(128)
    m["idx_tab"] = idx
    return m


_CACHE = {}


def _prog(name, fn):
    if name not in _CACHE:
        _CACHE[name] = fn()
    return _CACHE[name]


def _run(nc, maps):
    res = run_bass_kernel_spmd(nc, maps, core_ids=list(range(8)))
    return res.results


FUSED = True


def kernel(**inputs):
    inputs = {k: np.asarray(v) for k, v in inputs.items()}
    if FUSED:
        nc = _prog("fused", build_fused)
        maps = [fused_inputs(inputs, c) for c in range(8)]
        res = _run(nc, maps)
        out = np.empty((4, 4096, D), np.float32)
        for c in range(8):
            out[c // 2, (c % 2) * 2048:(c % 2 + 1) * 2048] = res[c]["out"].T
        return out
    x_lat = np.ascontiguousarray(inputs["x"], dtype=np.float32)
    x_ctx = np.ascontiguousarray(inputs["ctx"], dtype=np.float32)
    B = x_lat.shape[0]
    depth = inputs["mod_w"].shape[0]
    out_final = None
    for layer in range(depth):
        last = layer == depth - 1
        if layer % 2 == 0:
            nc = _prog("even", build_mixer_even)
            maps = [even_inputs(layer, inputs, c // 2, c % 2, x_lat, x_ctx) for c in range(8)]
        else:
            nc = _prog("odd", build_mixer_odd)
            maps = [odd_inputs(layer, inputs, c // 2, c % 2, x_lat, x_ctx) for c in range(8)]
        res = _run(nc, maps)
        yparts = [res[c]["yT"] for c in range(8)]
        n_lat, n_ctx = 2048, (0 if last else 128)
        nc = _prog("ffn_last" if last else "ffn", lambda: build_ffn(n_lat, n_ctx, last))
        maps = []
        for c in range(8):
            b, hf = c // 2, c % 2
            cols = [np.arange(256 + hf * 2048, 256 + (hf + 1) * 2048)]
            xs = [x_lat[b, hf * 2048:(hf + 1) * 2048]]
            if not last:
                cols.append(np.arange(hf * 128, (hf + 1) * 128))
                xs.append(x_ctx[b, hf * 128:(hf + 1) * 128])
            cols = np.concatenate(cols)
            xT = np.concatenate(xs, 0).T
            maps.append(ffn_inputs(layer, inputs, xT, yparts[2 * b][:, cols], yparts[2 * b + 1][:, cols], b))
        res = _run(nc, maps)
        if last:
            out_final = np.empty_like(x_lat)
            for c in range(8):
                b, hf = c // 2, c % 2
                out_final[b, hf * 2048:(hf + 1) * 2048] = res[c]["out"].T
        else:
            nx_lat = np.empty_like(x_lat)
            nx_ctx = np.empty_like(x_ctx)
            for c in range(8):
                b, hf = c // 2, c % 2
                o = res[c]["out"]
                nx_lat[b, hf * 2048:(hf + 1) * 2048] = o[:, :2048].T
                nx_ctx[b, hf * 128:(hf + 1) * 128] = o[:, 2048:].T
            x_lat, x_ctx = nx_lat, nx_ctx
    return out_final.astype(np.float32)
```

```python
from contextlib import ExitStack
import numpy as np
import concourse.bass as bass
import concourse.mybir as mybir
from concourse.bass_utils import run_bass_kernel_spmd

F32 = mybir.dt.float32
BF16 = mybir.dt.bfloat16
I32 = mybir.dt.int32
AF = mybir.ActivationFunctionType
ALU = mybir.AluOpType
AX = mybir.AxisListType

D = 1024
NCH = 8
EPS = 1e-6
N_EXP = 32
N_DMA_SEMS = 8
DEBUG = False
SERIAL = False
DEBUG_NEXP = None
EVEN_STOP = 99
MMDT = BF16


class Prog:
    def __init__(self, nc, stack):
        self.nc = nc
        self.stack = stack
        self.nrenew = 0
        self.names = ["pe", "act", "dve", "pool", "sp"]
        self.ops = {e: [] for e in self.names}
        self.cnt = {e: 0 for e in self.names}
        self.esem = {e: stack.enter_context(nc.semaphore("s_" + e)) for e in self.names}
        self.dsem, self.dval, self.dnext = {}, {}, {}
        for q in ("sp", "pool", "act"):
            self.dsem[q] = [stack.enter_context(nc.semaphore(f"d_{q}{i}")) for i in range(N_DMA_SEMS)]
            self.dval[q] = [0] * N_DMA_SEMS
            self.dnext[q] = 0
        self.csem = [stack.enter_context(nc.semaphore("c_%d" % i)) for i in range(4)]
        self.cval = [0] * 4
        self.cnext = 0
        self.res = {}
        self.seen = {e: {} for e in self.names}
        self.sem_by_id = {}

    def _need(self, eng, toks):
        best = {}
        for t in toks:
            if t is None:
                continue
            sem, v = t
            k = id(sem)
            self.sem_by_id[k] = sem
            if v > best.get(k, 0):
                best[k] = v
        waits = []
        for k, v in best.items():
            if self.seen[eng].get(k, 0) >= v:
                continue
            self.seen[eng][k] = v
            waits.append((self.sem_by_id[k], v))
        return waits

    def _deps(self, reads, writes):
        toks = []
        for r in reads:
            e = self.res.get(r)
            if e is not None:
                toks.append(e[0])
                if r.startswith("ps"):
                    toks.extend(e[1])
        for w in writes:
            e = self.res.get(w)
            if e is not None:
                toks.append(e[0])
                toks.extend(e[1])
        return toks

    def _commit(self, tok, reads, writes):
        for r in reads:
            e = self.res.setdefault(r, [None, []])
            e[1].append(tok)
        for w in writes:
            self.res[w] = [tok, []]

    def op(self, eng, fn, reads=(), writes=()):
        toks = self._deps(reads, writes)
        if eng == "pe":
            toks = [t for t in toks if t is None or t[0] is not self.esem["pe"]]
        waits = self._need(eng, toks)
        self.cnt[eng] += 1
        tok = (self.esem[eng], self.cnt[eng])
        self.ops[eng].append((fn, waits, (self.esem[eng], 1)))
        self._commit(tok, reads, writes)
        if SERIAL:
            self.barrier()
        return tok

    def dma(self, q, fn, reads=(), writes=()):
        toks = self._deps(reads, writes)
        i = self.dnext[q]
        self.dnext[q] = (i + 1) % N_DMA_SEMS
        sem = self.dsem[q][i]
        if self.dval[q][i] > 0:
            toks.append((sem, self.dval[q][i]))
        waits = self._need(q, toks)
        self.dval[q][i] += 16
        tok = (sem, self.dval[q][i])
        self.ops[q].append((fn, waits, (sem, 16)))
        self._commit(tok, reads, writes)
        if SERIAL:
            self.barrier()
        return tok

    def coll(self, fn, reads=(), writes=()):
        toks = self._deps(reads, writes)
        i = self.cnext
        self.cnext = (i + 1) % len(self.csem)
        sem = self.csem[i]
        if self.cval[i] > 0:
            toks.append((sem, self.cval[i]))
        waits = self._need("pool", toks)
        self.cval[i] += 1
        tok = (sem, self.cval[i])
        self.ops["pool"].append((fn, waits, (sem, None)))
        self._commit(tok, reads, writes)
        return tok

    def _all_dma_toks(self):
        toks = []
        for q in self.dsem:
            for i, sm in enumerate(self.dsem[q]):
                if self.dval[q][i] > 0:
                    toks.append((sm, self.dval[q][i]))
        for i, sm in enumerate(self.csem):
            if self.cval[i] > 0:
                toks.append((sm, self.cval[i]))
        return toks

    def barrier(self):
        toks = [(self.esem[e], self.cnt[e]) for e in self.names if self.cnt[e] > 0]
        toks += self._all_dma_toks()
        for e in self.names:
            waits = self._need(e, toks)
            if waits:
                self.ops[e].append((None, waits, None))
        self.res = {}
        for e in self.names:
            if self.cnt[e] > 12000:
                self.nrenew += 1
                self.esem[e] = self.stack.enter_context(self.nc.semaphore("s_%s_%d" % (e, self.nrenew)))
                self.cnt[e] = 0

    def finish(self):
        toks = self._all_dma_toks()
        waits = self._need("sp", toks)
        self.ops["sp"].append((None, waits, None))

    def emit(self):
        nc = self.nc
        with nc.Block() as block:
            def run(ename):
                def body(eng):
                    for fn, waits, inc in self.ops[ename]:
                        for sem, v in waits:
                            eng.wait_ge(sem, v)
                        if fn is not None:
                            if inc[1] is None:
                                fn(eng).then_inc(inc[0])
                            else:
                                fn(eng).then_inc(inc[0], inc[1])
                return body
            block.tensor(run("pe"))
            block.scalar(run("act"))
            block.vector(run("dve"))
            block.gpsimd(run("pool"))
            block.sync(run("sp"))


class Ctx:
    def __init__(self, nc, st):
        self.nc, self.st = nc, st
        self.P = Prog(nc, st)
        self.psn = 0
        self.sfx = ""

    def sb(self, name, shape, dt=F32):
        return self.st.enter_context(self.nc.sbuf_tensor("sb_" + name + self.sfx, shape, dt))

    def ps(self, name):
        return self.st.enter_context(self.nc.psum_tensor(name, [128, 512], F32))


class Env:
    def __init__(self, nc, C):
        self.nc, self.C = nc, C
        self.sfx = ""
        self.over = {}
        self.xload = None
        self.ywrite = None


def _din_factory(nc, env):
    sfx = env.sfx if env else ""

    def din(name, shape, dt=F32):
        if env is not None and name in env.over:
            return env.over[name]
        return nc.dram_tensor(name + sfx, shape, dt, kind="ExternalInput").ap()
    return din


def _begin(env, st, masks):
    if env is not None:
        C = env.C
        C.st = st
        C.sfx = env.sfx
        return C
    C = Ctx(env_nc_holder[0], st)
    emit_consts(C)
    if masks:
        emit_masks(C)
    C.pss_all = [(C.ps("ps%d" % i), "ps%d" % i) for i in range(8)]
    return C


env_nc_holder = [None]


def token_tiles(n_lat, n_ctx):
    tiles = []
    for s in range(0, n_lat, 512):
        tiles.append((s, min(512, n_lat - s), 0))
    for s in range(0, n_ctx, 512):
        tiles.append((n_lat + s, min(512, n_ctx - s), 1))
    return tiles


def emit_consts(C):
    P, nc = C.P, C.nc
    C.ones = C.sb("ones", [128, 128])
    C.ident = C.sb("ident", [128, 128])
    P.op("pool", lambda e: e.memset(C.ones[:], 1.0), writes=["ones"])
    P.op("pool", lambda e: e.memset(C.ident[:], 1.0), writes=["ident"])
    P.op("pool", lambda e: e.affine_select(out=C.ident[:], in_=C.ident[:], pattern=[[-1, 128]],
                                            compare_op=ALU.is_equal, fill=0.0, base=0, channel_multiplier=1),
         reads=["ident"], writes=["ident"])


def emit_mod(C, cv_d, modw_d, modb_d, ncols, psb, pskey):
    P, nc = C.P, C.nc
    nj = ncols // 128
    cv = C.sb("cv", [128, NCH, 2])
    sc = C.sb("sc", [128, NCH, 2])
    modb = C.sb("modb", [128, nj])
    modv = C.sb("modv", [128, nj, 2])
    P.dma("sp", lambda e: e.dma_start(out=cv[:], in_=cv_d), writes=["cv"])
    P.dma("sp", lambda e: e.dma_start(out=modb[:], in_=modb_d), writes=["modb"])
    P.op("act", lambda e: e.activation(out=sc[:], in_=cv[:], func=AF.Silu), reads=["cv"], writes=["sc"])
    wbufs = [C.sb("modw%d" % i, [128, NCH, 512]) for i in range(2)]
    mw = modw_d.rearrange("(c p) n -> p c n", p=128)
    for blk in range(ncols // 512):
        wb = wbufs[blk % 2]
        key = "modw%d" % (blk % 2)
        P.dma("sp", (lambda wb, blk: lambda e: e.dma_start(out=wb[:], in_=mw[:, :, blk * 512:(blk + 1) * 512]))(wb, blk),
              writes=[key])
        for jj in range(4):
            j = blk * 4 + jj
            for c in range(NCH):
                P.op("pe", (lambda wb, jj, c, j: lambda e: e.matmul(psb[:, 2 * j:2 * j + 2], lhsT=wb[:, c, jj * 128:(jj + 1) * 128],
                                                                      rhs=sc[:, c, :], start=(c == 0), stop=(c == NCH - 1)))(wb, jj, c, j),
                     reads=[key, "sc"], writes=[pskey])
    for w in range(2):
        P.op("dve", (lambda w: lambda e: e.tensor_tensor(out=modv[:, :, w], in0=psb[:, w:2 * nj:2], in1=modb[:], op=ALU.add))(w),
             reads=[pskey, "modb"], writes=["modv"])
    return modv


def emit_rstd(C, src, srckey, N, sq, sqkey, psb, pskey, rstd, rstdkey):
    P = C.P
    for c in range(NCH):
        P.op("act", (lambda c: lambda e: e.activation(out=sq[:, c, :N], in_=src[:, c, :N], func=AF.Square))(c), reads=[srckey], writes=[sqkey])
    for c in range(NCH):
        P.op("pe", (lambda c: lambda e: e.matmul(psb[:, :N], lhsT=C.ones[:], rhs=sq[:, c, :N], start=(c == 0), stop=(c == NCH - 1)))(c),
             reads=["ones", sqkey], writes=[pskey])
    P.op("act", lambda e: e.activation(out=rstd[:, :N], in_=psb[:, :N], func=AF.Sqrt, bias=EPS, scale=1.0 / D),
         reads=[pskey], writes=[rstdkey])
    P.op("dve", lambda e: e.reciprocal(out=rstd[:, :N], in_=rstd[:, :N]), reads=[rstdkey], writes=[rstdkey])


def build_ffn(n_lat, n_ctx, last, env=None):
    NT = n_lat + n_ctx
    tiles = token_tiles(n_lat, n_ctx)
    sfx = env.sfx if env else ""
    nc = env.nc if env else bass.Bass("TRN2", target_bir_lowering=False)
    env_nc_holder[0] = nc
    dt_in = _din_factory(nc, env)
    xT_d = dt_in("xT", [D, NT]); ypa_d = dt_in("ypa", [D, NT]); ypb_d = dt_in("ypb", [D, NT])
    cv_d = dt_in("cv", [128, NCH, 2]); modw_d = dt_in("modw", [D, 4096]); modb_d = dt_in("modb", [128, 32])
    n2g_d = dt_in("n2g", [128, NCH]); fing_d = dt_in("fing", [128, NCH])
    rw_d = dt_in("rw", [D, N_EXP]); rb_d = dt_in("rb", [1, N_EXP])
    wgu_d = dt_in("wgu", [N_EXP, D, 2 * D]); bgu_d = dt_in("bgu", [128, N_EXP, NCH, 2])
    wdn_d = dt_in("wdn", [N_EXP, D, D]); bdn_d = dt_in("bdn", [128, N_EXP, NCH]); bdnT_d = dt_in("bdnT", [N_EXP, D])
    if env is not None and "out" in env.over:
        out_d = env.over["out"]
    else:
        out_d = nc.dram_tensor("out", [D, NT], F32, kind="ExternalOutput").ap()
    xmid_d = nc.dram_tensor("xmid_scr" + sfx, [D, NT], F32, kind="ExternalOutput" if DEBUG else "Internal").ap()
    if DEBUG:
        gwT_dbg = nc.dram_tensor("gwT_dbg", [N_EXP, NT], F32, kind="ExternalOutput").ap()
        acc_dbg = nc.dram_tensor("acc_dbg", [D, NT], F32, kind="ExternalOutput").ap()
        h2_dbg = nc.dram_tensor("h2_dbg", [D, NT], F32, kind="ExternalOutput").ap()
        modv_dbg = nc.dram_tensor("modv_dbg", [128, 64], F32, kind="ExternalOutput").ap()
    r3 = lambda ap: ap.rearrange("(c p) n -> p c n", p=128)

    with ExitStack() as st:
        C = _begin(env, st, False)
        P = C.P
        psA = [C.pss_all[i][0] for i in (0, 1)]
        psB = [C.pss_all[i][0] for i in (2, 3)]
        psY = [C.pss_all[i][0] for i in (4, 5)]
        psM = [C.pss_all[i][0] for i in (6, 7)]
        h2b = C.sb("h2b", [128, NCH, NT], MMDT)
        gwT = C.sb("gwT", [N_EXP, NT])
        n2g = C.sb("n2g", [128, NCH]); fing = C.sb("fing", [128, NCH])
        A2 = C.sb("A2", [128, NCH, 2])
        g2v = C.sb("g2v", [128, NCH, 2])
        tmp32 = C.sb("tmp32", [N_EXP, 512])
        rw = C.sb("rw", [128, NCH, N_EXP]); rb = C.sb("rb", [1, N_EXP])
        bgu = C.sb("bgu", [128, N_EXP, NCH, 2]); bdn = C.sb("bdn", [128, N_EXP, NCH])
        P.dma("sp", lambda e: e.dma_start(out=n2g[:], in_=n2g_d), writes=["n2g"])
        P.dma("sp", lambda e: e.dma_start(out=fing[:], in_=fing_d), writes=["fing"])
        P.dma("sp", lambda e: e.dma_start(out=rw[:], in_=r3(rw_d)), writes=["rw"])
        P.dma("sp", lambda e: e.dma_start(out=rb[:], in_=rb_d), writes=["rb"])
        P.dma("sp", lambda e: e.dma_start(out=bgu[:], in_=bgu_d), writes=["bgu"])
        P.dma("sp", lambda e: e.dma_start(out=bdn[:], in_=bdn_d), writes=["bdn"])

        with ExitStack() as st2:
            C.st = st2
            modv = emit_mod(C, cv_d, modw_d, modb_d, 4096, psM[0], "psM0")
            P.op("dve", lambda e: e.tensor_copy(out=g2v[:], in_=modv[:, 24:32, :]), reads=["modv"], writes=["g2v"])
            for w in range(2):
                P.op("dve", (lambda w: lambda e: e.scalar_tensor_tensor(out=A2[:, :, w], in0=modv[:, 16:24, w], scalar=1.0, in1=n2g[:],
                                                                         op0=ALU.add, op1=ALU.mult))(w),
                     reads=["modv", "n2g"], writes=["A2"])
            xt = [C.sb("xt%d" % i, [128, NCH, 512]) for i in range(2)]
            ya = [C.sb("ya0", [128, NCH, 512])] * 2
            yb = [C.sb("yb0", [128, NCH, 512])] * 2
            sq = C.sb("sq", [128, NCH, 512])
            rstd = C.sb("rstd", [128, 512])
            h2f = C.sb("h2f", [128, NCH, 512])
            lg = C.sb("lg", [128, N_EXP]); top8 = C.sb("top8", [128, 8]); negm = C.sb("negm", [128, 1])
            exl = C.sb("exl", [128, N_EXP]); msk = C.sb("msk", [128, N_EXP]); den = C.sb("den", [128, 1])
            gw = C.sb("gw", [128, N_EXP])
            for ti, (t0, N, w) in enumerate(tiles):
                b = ti % 2
                kx, ka, kb = "xt%d" % b, "ya0", "yb0"
                P.dma("sp", (lambda b, t0, N: lambda e: e.dma_start(out=xt[b][:, :, :N], in_=r3(xT_d)[:, :, t0:t0 + N]))(b, t0, N), writes=[kx])
                P.dma("sp", (lambda b, t0, N: lambda e: e.dma_start(out=ya[b][:, :, :N], in_=r3(ypa_d)[:, :, t0:t0 + N]))(b, t0, N), writes=[ka])
                P.dma("sp", (lambda b, t0, N: lambda e: e.dma_start(out=yb[b][:, :, :N], in_=r3(ypb_d)[:, :, t0:t0 + N]))(b, t0, N), writes=[kb])
                P.op("pool", (lambda b, N: lambda e: e.tensor_tensor(out=ya[b][:, :, :N], in0=ya[b][:, :, :N], in1=yb[b][:, :, :N], op=ALU.add))(b, N),
                     reads=[ka, kb], writes=[ka])
                for c in range(NCH):
                    P.op("dve", (lambda b, N, c, w: lambda e: e.scalar_tensor_tensor(out=xt[b][:, c, :N], in0=ya[b][:, c, :N], scalar=modv[:, c, w:w + 1],
                                                                                      in1=xt[b][:, c, :N], op0=ALU.mult, op1=ALU.add))(b, N, c, w),
                         reads=[ka, kx, "modv"], writes=[kx])
                P.dma("sp", (lambda b, t0, N: lambda e: e.dma_start(out=r3(xmid_d)[:, :, t0:t0 + N], in_=xt[b][:, :, :N]))(b, t0, N),
                      reads=[kx], writes=["xmid_d%d" % ti])
                emit_rstd(C, xt[b], kx, N, sq, "sq", psM[1], "psM1", rstd, "rstd")
                for c in range(NCH):
                    P.op("pool", (lambda b, N, c: lambda e: e.tensor_tensor(out=h2f[:, c, :N], in0=xt[b][:, c, :N], in1=rstd[:, :N], op=ALU.mult))(b, N, c),
                         reads=[kx, "rstd"], writes=["h2f"])
                    P.op("dve", (lambda N, c, w: lambda e: e.tensor_scalar(out=h2f[:, c, :N], in0=h2f[:, c, :N], scalar1=A2[:, c, w:w + 1],
                                                                            scalar2=modv[:, 8 + c, w:w + 1], op0=ALU.mult, op1=ALU.add))(N, c, w),
                         reads=["h2f", "A2", "modv"], writes=["h2f"])
                for c in range(NCH):
                    P.op("act", (lambda t0, N, c: lambda e: e.copy(out=h2b[:, c, t0:t0 + N], in_=h2f[:, c, :N]))(t0, N, c), reads=["h2f"], writes=["h2b"])
                if DEBUG:
                    P.dma("sp", (lambda t0, N: lambda e: e.dma_start(out=r3(h2_dbg)[:, :, t0:t0 + N], in_=h2f[:, :, :N]))(t0, N), reads=["h2f"], writes=["dbgh%d" % ti])
                    if ti == 0:
                        P.dma("sp", lambda e: e.dma_start(out=modv_dbg, in_=modv[:].rearrange("p j w -> p (j w)")), reads=["modv"], writes=["dbgm"])
                        a2_dbg = nc.dram_tensor("a2_dbg", [128, 16], F32, kind="ExternalOutput").ap()
                        P.dma("sp", lambda e: e.dma_start(out=a2_dbg, in_=A2[:].rearrange("p j w -> p (j w)")), reads=["A2"], writes=["dbga2"])
                        rstd_dbg = nc.dram_tensor("rstd_dbg", [128, 512], F32, kind="ExternalOutput").ap()
                        P.dma("sp", lambda e: e.dma_start(out=rstd_dbg, in_=rstd[:]), reads=["rstd"], writes=["dbgr"])
                        sq_dbg = nc.dram_tensor("sq_dbg", [128, NCH, 512], F32, kind="ExternalOutput").ap()
                        P.dma("sp", lambda e: e.dma_start(out=sq_dbg, in_=sq[:]), reads=["sq"], writes=["dbgsq"])
                for blk in range(N // 128):
                    o = blk * 128
                    for c in range(NCH):
                        P.op("pe", (lambda o, c: lambda e: e.matmul(psM[0][:, 0:N_EXP], lhsT=h2f[:, c, o:o + 128], rhs=rw[:, c, :], start=(c == 0), stop=False))(o, c),
                             reads=["h2f", "rw"], writes=["psM0"])
                    P.op("pe", lambda e: e.matmul(psM[0][:, 0:N_EXP], lhsT=C.ones[0:1, :], rhs=rb[:], start=False, stop=True),
                         reads=["ones", "rb"], writes=["psM0"])
                    P.op("dve", lambda e: e.tensor_copy(out=lg[:], in_=psM[0][:, 0:N_EXP]), reads=["psM0"], writes=["lg"])
                    P.op("dve", lambda e: e.max(out=top8[:], in_=lg[:]), reads=["lg"], writes=["top8"])
                    P.op("dve", lambda e: e.tensor_scalar(out=negm[:], in0=top8[:, 0:1], scalar1=-1.0, scalar2=None, op0=ALU.mult),
                         reads=["top8"], writes=["negm"])
                    P.op("act", lambda e: e.activation(out=exl[:], in_=lg[:], func=AF.Exp, bias=negm[:], scale=1.0),
                         reads=["lg", "negm"], writes=["exl"])
                    P.op("dve", lambda e: e.tensor_scalar(out=msk[:], in0=lg[:], scalar1=top8[:, 3:4], scalar2=None, op0=ALU.is_ge),
                         reads=["lg", "top8"], writes=["msk"])
                    P.op("dve", lambda e: e.tensor_tensor(out=exl[:], in0=exl[:], in1=msk[:], op=ALU.mult), reads=["exl", "msk"], writes=["exl"])
                    P.op("dve", lambda e: e.reduce_sum(out=den[:], in_=exl[:], axis=AX.X), reads=["exl"], writes=["den"])
                    P.op("dve", lambda e: e.reciprocal(out=den[:], in_=den[:]), reads=["den"], writes=["den"])
                    P.op("dve", lambda e: e.tensor_scalar(out=gw[:], in0=exl[:], scalar1=den[:, 0:1], scalar2=None, op0=ALU.mult),
                         reads=["exl", "den"], writes=["gw"])
                    P.op("pe", lambda e: e.transpose(out=psM[1][0:N_EXP, 0:128], in_=gw[:], identity=C.ident[:]), reads=["gw", "ident"], writes=["psM1"])
                    P.op("act", (lambda t0, o: lambda e: e.copy(out=gwT[:, t0 + o:t0 + o + 128], in_=psM[1][0:N_EXP, 0:128]))(t0, o),
                         reads=["psM1"], writes=["gwT"])
            P.barrier()
        C.st = st

        stacc = st.enter_context(ExitStack())
        C.st = stacc
        acc = C.sb("acc", [128, NCH, NT])
        with ExitStack() as stb:
            C.st = stb
            bdnT = C.sb("bdnT", [N_EXP, D])
            _dma(C, "sp", bdnT[:], bdnT_d, [], ["bdnT"])
            nb_ = 0
            for ti, (t0, N, w) in enumerate(tiles):
                for dmc in range(NCH):
                    pb = nb_ % 2
                    nb_ += 1
                    _mm(C, psY[pb][:, :N], bdnT[:, dmc * 128:(dmc + 1) * 128], gwT[:, t0:t0 + N], True, True, ["bdnT", "gwT"], ["psY%d" % pb])
                    _cp(C, "act" if pb else "dve", acc[:, dmc, t0:t0 + N], psY[pb][:, :N], ["psY%d" % pb], ["acc"])
            P.barrier()
        C.st = stacc
        with ExitStack() as st3:
            C.st = st3
            NRING = 2
            wgu_r = [C.sb("wgu_r%d" % i, [128, NCH, 256], MMDT) for i in range(NRING)]
            wdn_r = [C.sb("wdn_r%d" % i, [128, NCH, 128], MMDT) for i in range(NRING)]
            wst = [C.sb("wst%d" % i, [128, NCH, 256]) for i in range(2)]
            actT = C.sb("actT", [128, NCH, NT], MMDT)
            gwb = C.sb("gwb", [128, NT])
            gt = [C.sb("gt%d" % i, [128, 512]) for i in range(2)]
            ut = [C.sb("ut%d" % i, [128, 512]) for i in range(2)]
            sg = [C.sb("sg%d" % i, [128, 512]) for i in range(2)]
            wguv = wgu_d.rearrange("e (c p) n -> e p c n", p=128)
            wdnv = wdn_d.rearrange("e (c p) n -> e p c n", p=128)
            it = 0
            pending = []
            nexp = N_EXP if DEBUG_NEXP is None else DEBUG_NEXP
            pieces = []
            for ex in range(nexp):
                for fc in range(NCH):
                    pieces.append((ex, "gu", fc, len(pieces)))
                for dmc in range(NCH):
                    pieces.append((ex, "dn", dmc, len(pieces)))

            def slots(k):
                ex, kind, j, _ = pieces[k]
                return (ex * NCH + j) % NRING, k % 2

            def prep(k):
                ex, kind, j, _ = pieces[k]
                rs, ws_ = slots(k)
                kst = "wst%d" % ws_
                if kind == "gu":
                    kw = "wgu_r%d" % rs
                    _dma(C, "sp", wst[ws_][:], wguv[ex, :, :, j * 256:(j + 1) * 256], [], [kst])
                    for c in range(NCH):
                        _cp(C, "act" if c % 2 else "pool", wgu_r[rs][:, c, :], wst[ws_][:, c, :], [kst], [kw])
                else:
                    kw = "wdn_r%d" % rs
                    _dma(C, "sp", wst[ws_][:, :, 0:128], wdnv[ex, :, :, j * 128:(j + 1) * 128], [], [kst])
                    for hh in range(2):
                        _cp(C, "pool" if hh else "act", wdn_r[rs][:, hh * 4:(hh + 1) * 4, :], wst[ws_][:, hh * 4:(hh + 1) * 4, 0:128], [kst], [kw])

            if pieces:
                prep(0)
            for k in range(len(pieces)):
                ex, kind, j, _ = pieces[k]
                rs, _ws = slots(k)
                if k + 1 < len(pieces):
                    prep(k + 1)
                if kind == "gu" and j == 0:
                    for ti, (t0, N, w) in enumerate(tiles):
                        _ts(C, "dve", tmp32[:, :N], gwT[:, t0:t0 + N], C.ident[0:N_EXP, ex:ex + 1], None, ALU.mult, None, ["gwT", "ident"], ["tmp32"])
                        _mm(C, psM[0][:, :N], C.ones[0:N_EXP, :], tmp32[:, :N], True, True, ["ones", "tmp32"], ["psM0"])
                        _cp(C, "act", gwb[:, t0:t0 + N], psM[0][:, :N], ["psM0"], ["gwb%d" % ti])
                if kind == "gu":
                    fc = j
                    kw = "wgu_r%d" % rs
                    for ti, (t0, N, w) in enumerate(tiles):
                        pb = it % 2
                        it += 1
                        ka, kb = "psA%d" % pb, "psB%d" % pb
                        for two, (pst, kk) in enumerate(((psA[pb], ka), (psB[pb], kb))):
                            for c in range(NCH):
                                _mm(C, pst[:, :N], wgu_r[rs][:, c, two:256:2], h2b[:, c, t0:t0 + N], c == 0, c == NCH - 1, [kw, "h2b"], [kk])
                        kg, ku, ks = "gt%d" % pb, "ut%d" % pb, "sg%d" % pb
                        _act(C, ut[pb][:, :N], psB[pb][:, :N], AF.Identity, [kb], [ku], bias=bgu[:, ex, fc, 1:2])
                        _ts(C, "dve", gt[pb][:, :N], psA[pb][:, :N], bgu[:, ex, fc, 0:1], 7.0, ALU.add, ALU.min, [ka, "bgu"], [kg])
                        _act(C, sg[pb][:, :N], gt[pb][:, :N], AF.Sigmoid, [kg], [ks], scale=1.702)
                        _ts(C, "dve", ut[pb][:, :N], ut[pb][:, :N], -7.0, 7.0, ALU.max, ALU.min, [ku], [ku])
                        for fn_ in pending:
                            fn_()
                        pending = [
                            (lambda pb=pb, N=N, kg=kg, ks=ks: _tt(C, "pool", gt[pb][:, :N], gt[pb][:, :N], sg[pb][:, :N], ALU.mult, [kg, ks], [kg])),
                            (lambda pb=pb, N=N, kg=kg, t0=t0, ti=ti: _tt(C, "pool", gt[pb][:, :N], gt[pb][:, :N], gwb[:, t0:t0 + N], ALU.mult, [kg, "gwb%d" % ti], [kg])),
                            (lambda pb=pb, N=N, kg=kg, ku=ku, t0=t0, ti=ti, fc=fc: _stt(C, actT[:, fc, t0:t0 + N], ut[pb][:, :N], 1.0, gt[pb][:, :N], ALU.add, ALU.mult, [ku, kg], ["actT%d_%d" % (fc, ti)])),
                        ]
                    if fc == NCH - 1:
                        for fn_ in pending:
                            fn_()
                        pending = []
                else:
                    dmc = j
                    kw = "wdn_r%d" % rs
                    for ti, (t0, N, w) in enumerate(tiles):
                        pb = it % 2
                        it += 1
                        ky = "psY%d" % pb
                        for fc in range(NCH):
                            _mm(C, psY[pb][:, :N], wdn_r[rs][:, fc, :], actT[:, fc, t0:t0 + N], fc == 0, fc == NCH - 1, [kw, "actT%d_%d" % (fc, ti)], [ky])
                        _tt(C, "dve", acc[:, dmc, t0:t0 + N], acc[:, dmc, t0:t0 + N], psY[pb][:, :N], ALU.add, [ky, "acc", "acc%d_%d" % (dmc, ti)], ["acc%d_%d" % (dmc, ti)])
            P.barrier()
        C.st = st
        if DEBUG:
            P.dma("sp", lambda e: e.dma_start(out=gwT_dbg, in_=gwT[:]), writes=["dbg1"])
            P.dma("sp", lambda e: e.dma_start(out=r3(acc_dbg), in_=acc[:]), writes=["dbg2"])
        with ExitStack() as st4:
            C.st = st4
            xm = [C.sb("xm%d" % i, [128, NCH, 512]) for i in range(2)]
            sqD = C.sb("sq2", [128, NCH, 512])
            rstdD = C.sb("rstd2", [128, 512])
            for ti, (t0, N, w) in enumerate(tiles):
                b = ti % 2
                kx = "xm%d" % b
                P.dma("sp", (lambda b, t0, N: lambda e: e.dma_start(out=xm[b][:, :, :N], in_=r3(xmid_d)[:, :, t0:t0 + N]))(b, t0, N), writes=[kx])
                for c in range(NCH):
                    P.op("dve", (lambda b, N, c, w, t0: lambda e: e.scalar_tensor_tensor(out=xm[b][:, c, :N], in0=acc[:, c, t0:t0 + N], scalar=g2v[:, c, w:w + 1],
                                                                                          in1=xm[b][:, c, :N], op0=ALU.mult, op1=ALU.add))(b, N, c, w, t0),
                         reads=[kx], writes=[kx])
                if last:
                    emit_rstd(C, xm[b], kx, N, sqD, "sq2", psM[1], "psM1", rstdD, "rstd2")
                    for c in range(NCH):
                        P.op("dve", (lambda b, N, c: lambda e: e.scalar_tensor_tensor(out=xm[b][:, c, :N], in0=xm[b][:, c, :N], scalar=fing[:, c:c + 1],
                                                                                       in1=rstdD[:, :N], op0=ALU.mult, op1=ALU.mult))(b, N, c),
                             reads=[kx, "rstd2"], writes=[kx])
                P.dma("sp", (lambda b, t0, N: lambda e: e.dma_start(out=r3(out_d)[:, :, t0:t0 + N], in_=xm[b][:, :, :N]))(b, t0, N),
                      reads=[kx], writes=["out%d" % ti])
            if env is None:
                P.finish()
            else:
                P.barrier()
        if env is None:
            P.emit()
    return nc


def ffn_inputs(layer, inputs, xT, ypa, ypb, b):
    pc = lambda v: np.ascontiguousarray(v.reshape(-1, 128).T)
    cv = np.stack([pc(inputs["c"][b]), pc(inputs["c_ctx"])], axis=-1)
    bgu = np.ascontiguousarray(inputs["moe_b_gu"][layer].reshape(N_EXP, NCH, 128, 2).transpose(2, 0, 1, 3))
    bdn = np.ascontiguousarray(inputs["moe_b_dn"][layer].reshape(N_EXP, NCH, 128).transpose(2, 0, 1))
    return {
        "xT": None if xT is None else np.ascontiguousarray(xT), "ypa": None if ypa is None else np.ascontiguousarray(ypa),
        "ypb": None if ypb is None else np.ascontiguousarray(ypb),
        "cv": np.ascontiguousarray(cv),
        "modw": np.ascontiguousarray(inputs["mod_w"][layer][:, 2048:6144]),
        "modb": pc(inputs["mod_b"][layer][2048:6144]),
        "n2g": pc(inputs["norm2_g"][layer]), "fing": pc(inputs["final_g"]),
        "rw": np.ascontiguousarray(inputs["router_w"][layer]), "rb": np.ascontiguousarray(inputs["router_b"][layer][None, :]),
        "wgu": np.ascontiguousarray(inputs["moe_w_gu"][layer]), "bgu": bgu,
        "wdn": np.ascontiguousarray(inputs["moe_w_dn"][layer]), "bdn": bdn, "bdnT": np.ascontiguousarray(inputs["moe_b_dn"][layer]),
    }


def _mm(C, out, lhsT, rhs, start, stop, r, w):
    C.P.op("pe", lambda e: e.matmul(out, lhsT=lhsT, rhs=rhs, start=start, stop=stop), reads=r, writes=w)


def _tr(C, out, in_, r, w):
    ident = C.ident[0:in_.shape[0], 0:in_.shape[0]]
    C.P.op("pe", lambda e: e.transpose(out=out, in_=in_, identity=ident), reads=list(r) + ["ident"], writes=w)


def _ts(C, eng, out, in0, s1, s2, op0, op1, r, w):
    if s2 is None:
        C.P.op(eng, lambda e: e.tensor_scalar(out=out, in0=in0, scalar1=s1, scalar2=None, op0=op0), reads=r, writes=w)
    else:
        C.P.op(eng, lambda e: e.tensor_scalar(out=out, in0=in0, scalar1=s1, scalar2=s2, op0=op0, op1=op1), reads=r, writes=w)


def _tt(C, eng, out, in0, in1, op, r, w):
    C.P.op(eng, lambda e: e.tensor_tensor(out=out, in0=in0, in1=in1, op=op), reads=r, writes=w)


def _stt(C, out, in0, scalar, in1, op0, op1, r, w):
    C.P.op("dve", lambda e: e.scalar_tensor_tensor(out=out, in0=in0, scalar=scalar, in1=in1, op0=op0, op1=op1), reads=r, writes=w)


def _act(C, out, in_, func, r, w, bias=None, scale=1.0):
    if bias is None:
        C.P.op("act", lambda e: e.activation(out=out, in_=in_, func=func, scale=scale), reads=r, writes=w)
    else:
        C.P.op("act", lambda e: e.activation(out=out, in_=in_, func=func, bias=bias, scale=scale), reads=r, writes=w)


def _cp(C, eng, out, in_, r, w):
    if eng == "act":
        C.P.op("act", lambda e: e.copy(out=out, in_=in_), reads=r, writes=w)
    else:
        C.P.op(eng, lambda e: e.tensor_copy(out=out, in_=in_), reads=r, writes=w)


def _dma(C, q, out, in_, r, w):
    C.P.dma(q, lambda e: e.dma_start(out=out, in_=in_), reads=r, writes=w)


T_ALL = 4352
NB = 34
N_CTXB = 2


def scan_order(direction):
    if direction == 0:
        return list(range(NB))
    return [1, 0] + list(range(NB - 1, N_CTXB - 1, -1))


def emit_masks(C):
    P = C.P
    C.masks = {}
    for name, op, sgn in (("F", ALU.is_ge, 1), ("Fs", ALU.is_gt, 1), ("B", ALU.is_ge, -1), ("Bs", ALU.is_gt, -1)):
        m = C.sb("mask" + name, [128, 128])
        P.op("pool", (lambda m: lambda e: e.memset(m[:], 1.0))(m), writes=["mask" + name])
        P.op("pool", (lambda m, op, sgn: lambda e: e.affine_select(out=m[:], in_=m[:], pattern=[[sgn, 128]], compare_op=op, fill=0.0,
                                                               base=0, channel_multiplier=-sgn))(m, op, sgn),
             reads=["mask" + name], writes=["mask" + name])
        C.masks[name] = m
    for name, row in (("sel0", 0), ("sel127", 127)):
        m = C.sb(name, [128, 128])
        P.op("pool", (lambda m: lambda e: e.memset(m[:], 1.0))(m), writes=[name])
        P.op("pool", (lambda m, row: lambda e: e.affine_select(out=m[:], in_=m[:], pattern=[[0, 128]], compare_op=ALU.is_equal, fill=0.0,
                                                                base=-row, channel_multiplier=1))(m, row),
             reads=[name], writes=[name])
        C.masks[name] = m


def emit_stream_tables(C, g, colb, direction, tabs, psb, pskey, sid):
    P = C.P
    kf, rowf, ctab, gF, gL, tmp = tabs
    first, last = ("sel0", "sel127") if direction == 0 else ("sel127", "sel0")
    kk = "tabs%s" % sid
    _mm(C, psb[:, 0:NB], C.masks[first][:], g[:], True, True, [first, "g" + sid], [pskey])
    _cp(C, "dve", gF[:], psb[:, 0:NB], [pskey], [kk + "gF"])
    _mm(C, psb[:, 0:NB], C.masks[last][:], g[:], True, True, [last, "g" + sid], [pskey])
    _cp(C, "dve", gL[:], psb[:, 0:NB], [pskey], [kk + "gL"])
    _tt(C, "dve", kf[:], gL[:], colb[:], ALU.add, [kk + "gL", "colb" + sid], [kk + "kf"])
    _ts(C, "dve", kf[:], kf[:], 0.0, None, ALU.min, None, [kk + "kf"], [kk + "kf"])
    _act(C, kf[:], kf[:], AF.Exp, [kk + "kf"], [kk + "kf"])
    _tt(C, "dve", rowf[:], g[:], gF[:], ALU.subtract, ["g" + sid, kk + "gF"], [kk + "rowf"])
    _ts(C, "dve", rowf[:], rowf[:], 0.0, None, ALU.min, None, [kk + "rowf"], [kk + "rowf"])
    _act(C, rowf[:], rowf[:], AF.Exp, [kk + "rowf"], [kk + "rowf"])
    _tt(C, "dve", ctab[:], gF[:, :, None].to_broadcast([128, NB, NB]), gL[:, None, :].to_broadcast([128, NB, NB]), ALU.subtract,
        [kk + "gF", kk + "gL"], [kk + "ctab"])
    for I in range(NB):
        _ts(C, "dve", ctab[:, I, :], ctab[:, I, :], 0.0, None, ALU.min, None, [kk + "ctab"], [kk + "ctab"])
        _act(C, ctab[:, I, :], ctab[:, I, :], AF.Exp, [kk + "ctab"], [kk + "ctab"])


def emit_diag_E(C, g, colb, I, direction, strict, Et, etkey, dg, psb, pskey, sid):
    mk = C.masks[("F" if direction == 0 else "B") + ("s" if strict else "")]
    mkey = "mask" + ("F" if direction == 0 else "B") + ("s" if strict else "")
    _ts(C, "dve", dg[:], C.ident[:], g[:, I:I + 1], None, ALU.mult, None, ["ident", "g" + sid], ["dg"])
    _mm(C, psb[:, 0:128], C.ones[:], dg[:], True, True, ["ones", "dg"], [pskey])
    _ts(C, "dve", Et[:], psb[:, 0:128], colb[:, I:I + 1], 0.0, ALU.add, ALU.min, [pskey, "colb" + sid], [etkey])
    _act(C, Et[:], Et[:], AF.Exp, [etkey], [etkey])
    _tt(C, "pool", Et[:], Et[:], mk[:], ALU.mult, [etkey, mkey], [etkey])


def emit_attn_block(C, I, order_pos, order, AT, BT, VP, Vraw, dvp, tabs, g, colb, direction, bufs, sid, out, outkey, skip_off_rowscale=None, kx=""):
    kf, rowf, ctab, gF, gL, tmp = tabs
    psS, psO, psD, psR, wts, Et, dg, offb = bufs
    before = order[:order_pos]
    isl = slice(I * 128, (I + 1) * 128)
    kk = "tabs%s" % sid
    for n, J in enumerate(before):
        sb_i = C.rot % len(psS)
        C.rot += 1
        ps, pk = psS[sb_i]
        wt, wk = wts[sb_i % len(wts)]
        _mm(C, ps[:, 0:128], AT[:, J * 128:(J + 1) * 128], BT[:, isl], True, True, ["AT" + kx + sid, "BT" + kx + sid], [pk])
        _ts(C, "dve", wt[:], ps[:, 0:128], ctab[:, I, J:J + 1], None, ALU.mult, None, [pk, kk + "ctab"], [wk])
        _mm(C, psO[0][:, 0:dvp], wt[:], VP[:, J, :], n == 0, n == len(before) - 1, [wk, "VP" + kx + sid], [psO[1]])
    if before:
        _ts(C, "dve", offb[:, 0:dvp], psO[0][:, 0:dvp], rowf[:, I:I + 1], None, ALU.mult, None, [psO[1], kk + "rowf"], ["offb"])
    emit_diag_E(C, g, colb, I, direction, False, Et, "Et", dg, psR[0], psR[1], sid)
    sb_i = C.rot % len(psS)
    C.rot += 1
    ps, pk = psS[sb_i]
    wt, wk = wts[sb_i % len(wts)]
    _mm(C, ps[:, 0:128], AT[:, isl], BT[:, isl], True, True, ["AT" + kx + sid, "BT" + kx + sid], [pk])
    _tt(C, "dve", wt[:], ps[:, 0:128], Et[:], ALU.mult, [pk, "Et"], [wk])
    _mm(C, psD[0][:, 0:dvp], wt[:], Vraw[:, I, :], True, True, [wk, "Vraw" + kx + sid], [psD[1]])
    if before:
        _tt(C, "dve", out, psD[0][:, 0:dvp], offb[:, 0:dvp], ALU.add, [psD[1], "offb"], [outkey])
    else:
        _cp(C, "dve", out, psD[0][:, 0:dvp], [psD[1]], [outkey])


def emit_stream_pipelined(C, order, AT, BT, VP, Vraw, dvp, tabs, g, colb, direction, bufs, kx, tail, diag=True, lookahead=3):
    kf, rowf, ctab, gF, gL, _ = tabs
    psS, psO2, psD, psR, wts, Et2, dg2, offb = bufs
    kk = "tabs"
    items = []
    for pos, I in enumerate(order):
        before = order[:pos]
        for n, J in enumerate(before):
            items.append(("off", I, pos, J, n == 0, n == len(before) - 1, False))
        if diag:
            items.append(("diag", I, pos, I, True, True, True))
        if items and items[-1][1] == I:
            it = items[-1]
            items[-1] = it[:6] + (True,)
        elif not before and not diag:
            items.append(("none", I, pos, I, True, True, True))

    def emit_s(idx):
        kind, I, pos, J, first, last, lastI = items[idx]
        if kind == "none":
            return
        ps, pk = psS[idx % len(psS)]
        if kind == "diag" or (kind == "off" and first and not diag):
            pass
        if kind == "diag":
            Et, ek = Et2[pos % 2]
            dg, dk = dg2[pos % 2]
            mk = C.masks["F" if direction == 0 else "B"]
            mkey = "maskF" if direction == 0 else "maskB"
            _ts(C, "dve", dg[:], C.ident[:], g[:, I:I + 1], None, ALU.mult, None, ["ident", "g"], [dk])
            _mm(C, psR[0][:, 0:128], C.ones[:], dg[:], True, True, ["ones", dk], [psR[1]])
            _ts(C, "dve", Et[:], psR[0][:, 0:128], colb[:, I:I + 1], 0.0, ALU.add, ALU.min, [psR[1], "colb"], [ek])
            _act(C, Et[:], Et[:], AF.Exp, [ek], [ek])
            _tt(C, "pool", Et[:], Et[:], mk[:], ALU.mult, [ek, mkey], [ek])
        _mm(C, ps[:, 0:128], AT[:, J * 128:(J + 1) * 128], BT[:, I * 128:(I + 1) * 128], True, True, ["AT" + kx, "BT" + kx], [pk])

    def emit_da(idx):
        kind, I, pos, J, first, last, lastI = items[idx]
        psO = psO2[pos % 2]
        if kind != "none":
            ps, pk = psS[idx % len(psS)]
            wt, wk = wts[idx % len(wts)]
            if kind == "off":
                _ts(C, "dve", wt[:], ps[:, 0:128], ctab[:, I, J:J + 1], None, ALU.mult, None, [pk, kk + "ctab"], [wk])
                _mm(C, psO[0][:, 0:dvp], wt[:], VP[:, J, :], first, last, [wk, "VP" + kx], [psO[1]])
            else:
                Et, ek = Et2[pos % 2]
                _tt(C, "dve", wt[:], ps[:, 0:128], Et[:], ALU.mult, [pk, ek], [wk])
                _mm(C, psD[0][:, 0:dvp], wt[:], Vraw[:, I, :], True, True, [wk, "Vraw" + kx], [psD[1]])
        if lastI:
            tail(I, pos, pos > 0, psO[0], psO[1])

    n = len(items)
    for idx in range(min(lookahead, n)):
        emit_s(idx)
    for idx in range(n):
        emit_da(idx)
        if idx + lookahead < n:
            emit_s(idx + lookahead)


def std_tail(C, tabs, bufs, dvp, out_fn):
    kf, rowf, ctab, gF, gL, _ = tabs
    psS, psO2, psD, psR, wts, Et2, dg2, offb = bufs

    def tail(I, pos, has_off, psO, psOkey):
        res = C.tailbuf
        if has_off:
            _ts(C, "dve", offb[:, 0:dvp], psO[:, 0:dvp], rowf[:, I:I + 1], None, ALU.mult, None, [psOkey, "tabsrowf"], ["offb"])
            _tt(C, "dve", res[:, 0:dvp], psD[0][:, 0:dvp], offb[:, 0:dvp], ALU.add, [psD[1], "offb"], ["tailbuf"])
        else:
            _cp(C, "dve", res[:, 0:dvp], psD[0][:, 0:dvp], [psD[1]], ["tailbuf"])
        out_fn(I, res, "tailbuf")
    return tail


def emit_norm1_tile(C, xt, kx, N, w, A1, modv, sq, rstd, psb, pskey, h1b, tmp):
    emit_rstd(C, xt, kx, N, sq, "sq", psb, pskey, rstd, "rstd")
    for c in range(NCH):
        _tt(C, "pool", tmp[:, c, :N], xt[:, c, :N], rstd[:, :N], ALU.mult, [kx, "rstd"], ["sq"])
        _ts(C, "dve", h1b[:, c, :N], tmp[:, c, :N], A1[:, c, w:w + 1], modv[:, c, w:w + 1], ALU.mult, ALU.add, ["sq", "A1", "modv"], ["h1b"])


def mixer_tiles():
    return [(0, 256, 1)] + [(256 + 512 * k, 512, 0) for k in range(8)]


def build_mixer_odd(env=None):
    T = T_ALL
    sfx = env.sfx if env else ""
    nc = env.nc if env else bass.Bass("TRN2", target_bir_lowering=False)
    env_nc_holder[0] = nc
    din = _din_factory(nc, env)
    xT_d = din("xT", [D, T]); cv_d = din("cv", [128, NCH, 2]); modw_d = din("modw", [D, 2048]); modb_d = din("modb", [128, 16])
    n1g_d = din("n1g", [128, NCH])
    wfm_d = din("wfm", [D, 1024]); wtm_d = din("wtm", [D, 2048]); wout_d = din("wout", [1024, D])
    rope_d = din("rope", [4, 128, T]); rperm_d = din("rperm", [128, 128]); gtab_d = din("gtab", [128, 8, NB]); gainb_d = din("gainb", [128, 1024])
    yT_d = None if env else nc.dram_tensor("yT", [D, T], F32, kind="ExternalOutput").ap()
    FT_d = nc.dram_tensor("FT_scr" + sfx, [8, 128, T], F32, kind="Internal").ap()
    TM_d = nc.dram_tensor("TM_scr" + sfx, [T, 2048], F32, kind="Internal").ap()
    UT_d = nc.dram_tensor("UT_scr" + sfx, [8, 128, T], MMDT, kind="Internal").ap()
    r3 = lambda ap: ap.rearrange("(c p) n -> p c n", p=128)
    tiles = mixer_tiles()
    with ExitStack() as st:
        C = _begin(env, st, True)
        P = C.P
        C.rot = 0
        pss = C.pss_all
        n1g = C.sb("n1g", [128, NCH]); A1 = C.sb("A1", [128, NCH, 2])
        _dma(C, "sp", n1g[:], n1g_d, [], ["n1g"])
        with ExitStack() as st2:
            C.st = st2
            modv = emit_mod(C, cv_d, modw_d, modb_d, 2048, pss[0][0], "ps0")
            for w in range(2):
                _stt(C, A1[:, :, w], modv[:, 8:16, w], 1.0, n1g[:], ALU.add, ALU.mult, ["modv", "n1g"], ["A1"])
            wfm = C.sb("wfm", [128, NCH, 1024], MMDT); wtm = C.sb("wtm", [128, NCH, 2048], MMDT)
            rperm = C.sb("rperm", [128, 128])
            _dma(C, "sp", rperm[:], rperm_d, [], ["rperm"])
            for c in range(NCH):
                _dma(C, "pool", wfm[:, c, :], wfm_d[c * 128:(c + 1) * 128, :], [], ["wfm"])
                _dma(C, "pool", wtm[:, c, :], wtm_d[c * 128:(c + 1) * 128, :], [], ["wtm"])
            xt = C.sb("xt", [128, NCH, 512]); sq = C.sb("sq", [128, NCH, 512]); rstd = C.sb("rstd", [128, 512])
            h1b = C.sb("h1b", [128, NCH, 512], MMDT)
            ropet = C.sb("ropet", [128, 4, 512])
            raw = [C.sb("raw%d" % i, [128, 512]) for i in range(2)]
            r1 = [C.sb("r1%d" % i, [128, 512]) for i in range(2)]
            r2 = [C.sb("r2%d" % i, [128, 512]) for i in range(2)]
            tmo = [C.sb("tmo%d" % i, [128, 512]) for i in range(2)]
            for ti, (t0, N, w) in enumerate(tiles):
                if env is not None and env.xload is not None:
                    env.xload(C, xt, t0, N)
                else:
                    _dma(C, "sp", xt[:, :, :N], r3(xT_d)[:, :, t0:t0 + N], [], ["xt"])
                _dma(C, "sp", ropet[:, :, :N], rope_d.rearrange("f p t -> p f t")[:, :, t0:t0 + N], [], ["ropet"])
                emit_norm1_tile(C, xt, "xt", N, w, A1, modv, sq, rstd, pss[1][0], "ps1", h1b, sq)
                for fm in range(8):
                    b = fm % 2
                    ps, pk = pss[2 + b]
                    ps2, pk2 = pss[4 + b]
                    for c in range(NCH):
                        _mm(C, ps[:, :N], wfm[:, c, fm * 128:(fm + 1) * 128], h1b[:, c, :N], c == 0, c == NCH - 1, ["wfm", "h1b"], [pk])
                    _cp(C, "act", raw[b][:, :N], ps[:, :N], [pk], ["raw%d" % b])
                    _mm(C, ps2[:, :N], rperm[:], raw[b][:, :N], True, True, ["rperm", "raw%d" % b], [pk2])
                    tb = 0 if fm < 4 else 2
                    _tt(C, "pool", r1[b][:, :N], raw[b][:, :N], ropet[:, tb, :N], ALU.mult, ["raw%d" % b, "ropet"], ["r1%d" % b])
                    _tt(C, "dve", r2[b][:, :N], ps2[:, :N], ropet[:, tb + 1, :N], ALU.mult, [pk2, "ropet"], ["r2%d" % b])
                    _tt(C, "pool", r1[b][:, :N], r1[b][:, :N], r2[b][:, :N], ALU.add, ["r1%d" % b, "r2%d" % b], ["r1%d" % b])
                    _dma(C, "sp", FT_d[fm, :, t0:t0 + N], r1[b][:, :N], ["r1%d" % b], ["FT_d"])
                for blk in range(N // 128):
                    for half in range(4):
                        b = (blk * 4 + half) % 2
                        ps, pk = pss[6 + b]
                        for c in range(NCH):
                            _mm(C, ps[:, :512], h1b[:, c, blk * 128:(blk + 1) * 128], wtm[:, c, half * 512:(half + 1) * 512], c == 0, c == NCH - 1, ["h1b", "wtm"], [pk])
                        _cp(C, "act" if half % 2 else "dve", tmo[b][:], ps[:, :512], [pk], ["tmo%d" % b])
                        _dma(C, "sp", TM_d[t0 + blk * 128:t0 + (blk + 1) * 128, half * 512:(half + 1) * 512], tmo[b][:], ["tmo%d" % b], ["TM_d"])
            P.barrier()
        C.st = st
        with ExitStack() as st3:
            C.st = st3
            AT = C.sb("AT", [128, T], MMDT); BT = C.sb("BT", [128, T], MMDT)
            Vraw = C.sb("Vraw", [128, NB, 256], MMDT); VP = C.sb("VP", [128, NB, 256], MMDT); OH = C.sb("OH", [128, NB, 256])
            gtab = C.sb("gtab", [128, 8, NB]); gainb = C.sb("gainb", [128, 1024])
            _dma(C, "sp", gtab[:], gtab_d, [], ["gtab", "g"])
            _dma(C, "sp", gainb[:], gainb_d, [], ["gainb"])
            colb = C.sb("colb", [128, NB])
            tabs = (C.sb("kf", [128, NB]), C.sb("rowf", [128, NB]), C.sb("ctab", [128, NB, NB]), C.sb("gF", [128, NB]), C.sb("gL", [128, NB]), None)
            wts = [(C.sb("wt%d" % i, [128, 128], MMDT), "wt%d" % i) for i in range(4)]
            Et2 = [(C.sb("Et%d" % i, [128, 128]), "Et%d" % i) for i in range(2)]; dg2 = [(C.sb("dg%d" % i, [128, 128]), "dg%d" % i) for i in range(2)]
            offb = C.sb("offb", [128, 256]); otmp = C.sb("otmp", [128, 256]); C.tailbuf = C.sb("tailbuf", [128, 256])
            bufs = (pss[0:4], [pss[4], pss[6]], pss[5], pss[7], wts, Et2, dg2, offb)
            Gt = C.sb("Gt", [128, 256]); cen = C.sb("cen", [128, 256]); st1 = C.sb("st1", [128, 4]); uT = C.sb("uT", [128, 2, 128], MMDT)
            TMv = TM_d.rearrange("(n p) w -> p n w", p=128)
            for h in range(4):
                _dma(C, "pool", AT[:], FT_d[4 + h], ["FT_d"], ["AT"])
                _dma(C, "pool", BT[:], FT_d[h], ["FT_d"], ["BT"])
                for q4 in range(2):
                    _dma(C, "pool", Vraw[:, q4 * 17:(q4 + 1) * 17, :], TMv[:, q4 * 17:(q4 + 1) * 17, h * 256:(h + 1) * 256], ["TM_d"], ["Vraw"])
                for d in range(2):
                    s = h * 2 + d
                    g = gtab[:, s, :]
                    _ts(C, "dve", colb[:], g, -1.0, None, ALU.mult, None, ["gtab"], ["colb"])
                    emit_stream_tables(C, g, colb, d, tabs, pss[7][0], "ps7", "")
                    for J in range(NB):
                        _ts(C, "pool" if J % 2 else "dve", VP[:, J, :], Vraw[:, J, :], tabs[0][:, J:J + 1], None, ALU.mult, None, ["Vraw", "tabskf"], ["VP"])
                    order = scan_order(d)

                    def out_fn(I, res, rk, d=d):
                        if d == 0:
                            _cp(C, "pool", OH[:, I, :], res[:, 0:256], [rk], ["OH%d" % I])
                        else:
                            _tt(C, "pool", OH[:, I, :], OH[:, I, :], res[:, 0:256], ALU.add, [rk, "OH%d" % I], ["OH%d" % I])
                    emit_stream_pipelined(C, order, AT, BT, VP, Vraw, 256, tabs, g, colb, d, bufs, "", std_tail(C, tabs, bufs, 256, out_fn))
                for I in range(NB):
                    k = "OH%d" % I
                    _dma(C, "sp", Gt[:], TM_d[I * 128:(I + 1) * 128, 1024 + h * 256:1024 + (h + 1) * 256], ["TM_d"], ["Gt"])
                    P.op("dve", (lambda I: lambda e: e.reduce_sum(out=st1[:, 0:1], in_=OH[:, I, :], axis=AX.X))(I), reads=[k], writes=["st1"])
                    _ts(C, "dve", st1[:, 0:1], st1[:, 0:1], -1.0 / 256, None, ALU.mult, None, ["st1"], ["st1"])
                    _ts(C, "dve", cen[:], OH[:, I, :], st1[:, 0:1], None, ALU.add, None, [k, "st1"], ["cen"])
                    P.op("act", lambda e: e.activation(out=otmp[:], in_=cen[:], func=AF.Square, accum_out=st1[:, 1:2]), reads=["cen"], writes=["otmp", "st1b"])
                    _act(C, st1[:, 2:3], st1[:, 1:2], AF.Sqrt, ["st1b"], ["st1c"], bias=EPS, scale=1.0 / 256)
                    P.op("dve", lambda e: e.reciprocal(out=st1[:, 3:4], in_=st1[:, 2:3]), reads=["st1c"], writes=["st1d"])
                    _act(C, Gt[:], Gt[:], AF.Silu, ["Gt"], ["Gt"])
                    _tt(C, "pool", Gt[:], Gt[:], gainb[:, h * 256:(h + 1) * 256], ALU.mult, ["Gt", "gainb"], ["Gt"])
                    _stt(C, cen[:], cen[:], st1[:, 3:4], Gt[:], ALU.mult, ALU.mult, ["cen", "st1d", "Gt"], ["cen"])
                    for ec in range(2):
                        _tr(C, pss[7][0][:, ec * 128:(ec + 1) * 128], cen[:, ec * 128:(ec + 1) * 128], ["cen"], ["ps7"])
                    _cp(C, "act", uT[:].rearrange("p a b -> p (a b)"), pss[7][0][:, 0:256], ["ps7"], ["uT"])
                    for ec in range(2):
                        _dma(C, "sp", UT_d[h * 2 + ec, :, I * 128:(I + 1) * 128], uT[:, ec, :], ["uT"], ["UT_d"])
            P.barrier()
        C.st = st
        with ExitStack() as st4:
            C.st = st4
            wout = C.sb("wout", [128, 8, D], MMDT)
            for hc in range(8):
                _dma(C, "pool", wout[:, hc, :], wout_d[hc * 128:(hc + 1) * 128, :], [], ["wout"])
            ut = [C.sb("utile%d" % i, [128, 8, 512], MMDT) for i in range(2)]
            yo = [C.sb("yo%d" % i, [128, 512]) for i in range(2)]
            for ti, (t0, N, w) in enumerate(tiles):
                b = ti % 2
                _dma(C, "sp", ut[b][:, :, :N], UT_d.rearrange("f p t -> p f t")[:, :, t0:t0 + N], [], ["ut%d" % b])
                for dmc in range(NCH):
                    pb = dmc % 2
                    ps, pk = pss[pb]
                    for hc in range(8):
                        _mm(C, ps[:, :N], wout[:, hc, dmc * 128:(dmc + 1) * 128], ut[b][:, hc, :N], hc == 0, hc == 7, ["wout", "ut%d" % b], [pk])
                    _cp(C, "act" if pb else "dve", yo[pb][:, :N], ps[:, :N], [pk], ["yo%d" % pb])
                    if env is not None:
                        env.ywrite(C, dmc, t0, N, yo[pb], "yo%d" % pb)
                    else:
                        _dma(C, "sp", yT_d[dmc * 128:(dmc + 1) * 128, t0:t0 + N], yo[pb][:, :N], ["yo%d" % pb], ["yT%d_%d" % (ti, dmc)])
            if env is None:
                P.finish()
            else:
                P.barrier()
        if env is None:
            P.emit()
    return nc


RET_HEADS = 8
ROPE_BASE = 10000.0


def mixer_common_inputs(layer, inputs, b, x_lat, x_ctx):
    pc = lambda v: np.ascontiguousarray(v.reshape(-1, 128).T)
    cv = np.stack([pc(inputs["c"][b]), pc(inputs["c_ctx"])], axis=-1)
    xT = np.ascontiguousarray(np.concatenate([x_ctx[b], x_lat[b]], 0).T)
    return {"xT": xT, "cv": np.ascontiguousarray(cv), "modw": np.ascontiguousarray(inputs["mod_w"][layer][:, 0:2048]),
            "modb": pc(inputs["mod_b"][layer][0:2048]), "n1g": pc(inputs["norm1_g"][layer])}


def odd_inputs(layer, inputs, b, hg, x_lat, x_ctx):
    j = layer // 2
    m = mixer_common_inputs(layer, inputs, b, x_lat, x_ctx)
    w_in = inputs["od_w_in"][j]
    hs = slice(hg * 4, hg * 4 + 4)
    wq = w_in[:, 0:1024].reshape(D, 8, 128)[:, hs].reshape(D, 512)
    wk = w_in[:, 1024:2048].reshape(D, 8, 128)[:, hs].reshape(D, 512)
    wv = w_in[:, 2048:4096].reshape(D, 8, 256)[:, hs].reshape(D, 1024)
    wg = w_in[:, 4096:6144].reshape(D, 8, 256)[:, hs].reshape(D, 1024)
    m["wfm"] = np.ascontiguousarray(np.concatenate([wq, wk], 1))
    m["wtm"] = np.ascontiguousarray(np.concatenate([wv, wg], 1))
    m["wout"] = np.ascontiguousarray(inputs["od_w_out"][j].reshape(8, 256, D)[hs].reshape(1024, D))
    half = 64
    freqs = (np.float32(ROPE_BASE) ** (-np.arange(half, dtype=np.float32) / np.float32(half))).astype(np.float32)
    pos = np.arange(T_ALL, dtype=np.float32)
    ang = (pos[:, None] * freqs[None, :]).astype(np.float32)
    cos, sin = np.cos(ang).astype(np.float32).T, np.sin(ang).astype(np.float32).T
    cosT = np.concatenate([cos, cos], 0); sinT = np.concatenate([-sin, sin], 0)
    ks = np.float32(128.0 ** -0.5)
    m["rope"] = np.ascontiguousarray(np.stack([cosT, sinT, cosT * ks, sinT * ks], 0).astype(np.float32))
    m["rperm"] = np.ascontiguousarray(np.roll(np.eye(128, dtype=np.float32), 64, axis=0))
    expo = 5.0 + np.arange(RET_HEADS, dtype=np.float32)
    lg_f = np.log1p(-np.exp2(-expo)).astype(np.float32)
    lg_b = np.log1p(-np.exp2(-expo[::-1])).astype(np.float32)
    sp_f = np.arange(T_ALL, dtype=np.float32)
    sp_b = np.concatenate([255.0 - np.arange(256), 256.0 + 4095.0 - np.arange(4096)]).astype(np.float32)
    gt = np.zeros((128, 8, NB), np.float32)
    for hl in range(4):
        h = hg * 4 + hl
        gt[:, hl * 2 + 0, :] = (sp_f * lg_f[h]).reshape(NB, 128).T
        gt[:, hl * 2 + 1, :] = (sp_b * lg_b[h]).reshape(NB, 128).T
    m["gtab"] = gt
    m["gainb"] = np.ascontiguousarray(np.broadcast_to(inputs["ret_norm_g"][j].reshape(8, 256)[hs].reshape(1, 1024), (128, 1024)))
    return m


def emit_offdiag(C, I, order_pos, order, AT, BT, VP, dvp, ctab, bufs, sid):
    psS, psO, psD, psR, wts, Et, dg, offb = bufs
    before = order[:order_pos]
    isl = slice(I * 128, (I + 1) * 128)
    kk = "tabs%s" % sid
    for n, J in enumerate(before):
        sb_i = C.rot % len(psS)
        C.rot += 1
        ps, pk = psS[sb_i]
        wt, wk = wts[sb_i % len(wts)]
        _mm(C, ps[:, 0:128], AT[:, J * 128:(J + 1) * 128], BT[:, isl], True, True, ["AT" + sid, "BT" + sid], [pk])
        _ts(C, "dve", wt[:], ps[:, 0:128], ctab[:, I, J:J + 1], None, ALU.mult, None, [pk, kk + "ctab"], [wk])
        _mm(C, psO[0][:, 0:dvp], wt[:], VP[:, J, :], n == 0, n == len(before) - 1, [wk, "VP" + sid], [psO[1]])
    return len(before) > 0


def emit_cumsum(C, x, xkey, direction, out, outkey, tmpA, tmpB, psb, pskey):
    n = NB * 4
    flat = lambda t: t[:].rearrange("p a b -> p (a b)")
    m = C.masks["F" if direction == 0 else "B"]
    mkey = "maskF" if direction == 0 else "maskB"
    _mm(C, psb[:, 0:n], C.ones[:], flat(x), True, True, ["ones", xkey], [pskey])
    _cp(C, "dve", flat(tmpA), psb[:, 0:n], [pskey], ["cs_tot"])
    _mm(C, psb[:, 0:n], m[:], flat(x), True, True, [mkey, xkey], [pskey])
    for k in range(4):
        C.P.op("dve", (lambda k: lambda e: e.tensor_tensor_scan(out=tmpB[:, :, k], data0=C.ones[:, 0:NB], data1=tmpA[:, :, k], initial=0.0,
                                                                 op0=ALU.mult, op1=ALU.add))(k), reads=["cs_tot", "ones"], writes=["cs_cs"])
    if direction == 0:
        _tt(C, "dve", flat(tmpB), flat(tmpB), flat(tmpA), ALU.subtract, ["cs_cs", "cs_tot"], ["cs_carry"])
    else:
        _tt(C, "dve", tmpA[:, 0, :], tmpB[:, 1, :], tmpB[:, NB - 1, :], ALU.add, ["cs_cs", "cs_tot"], ["cs_tot"])
        _cp(C, "dve", tmpA[:, 2, :], tmpA[:, 1, :], ["cs_tot"], ["cs_tot"])
        _tt(C, "dve", tmpB[:, 2:NB, :], tmpA[:, 0:1, :].to_broadcast([128, NB - 2, 4]), tmpB[:, 2:NB, :], ALU.subtract, ["cs_cs", "cs_tot"], ["cs_carry"])
        _cp(C, "dve", tmpB[:, 0, :], tmpA[:, 2, :], ["cs_tot", "cs_carry"], ["cs_carry"])
        C.P.op("dve", lambda e: e.memset(tmpB[:, 1, :], 0.0), reads=["cs_carry"], writes=["cs_carry"])
    _tt(C, "dve", flat(out), psb[:, 0:n], flat(tmpB), ALU.add, [pskey, "cs_carry"], [outkey])


def build_mixer_even(env=None):
    T = T_ALL
    sfx = env.sfx if env else ""
    nc = env.nc if env else bass.Bass("TRN2", target_bir_lowering=False)
    env_nc_holder[0] = nc
    din = _din_factory(nc, env)
    xT_d = din("xT", [D, T]); cv_d = din("cv", [128, NCH, 2]); modw_d = din("modw", [D, 2048]); modb_d = din("modb", [128, 16])
    n1g_d = din("n1g", [128, NCH])
    wfm_d = din("wfm", [D, 1280]); wtm_d = din("wtm", [D, 784]); wout_d = din("wout", [512, D])
    convw_d = din("convw", [128, 6, 3]); gconst_d = din("gconst", [128, 4, 4]); gainb_d = din("gainb", [128, 512])
    yT_d = None if env else nc.dram_tensor("yT", [D, T], F32, kind="ExternalOutput").ap()
    FT_d = nc.dram_tensor("FT_scr" + sfx, [10, 128, T], F32, kind="Internal").ap()
    TM_d = nc.dram_tensor("TM_scr" + sfx, [T, 784], F32, kind="Internal").ap()
    UT_d = nc.dram_tensor("UT_scr" + sfx, [4, 128, T], MMDT, kind="Internal").ap()
    r3 = lambda ap: ap.rearrange("(c p) n -> p c n", p=128)
    tiles = mixer_tiles()
    with ExitStack() as st:
        C = _begin(env, st, True)
        P = C.P
        C.rot = 0
        pss = C.pss_all
        n1g = C.sb("n1g", [128, NCH]); A1 = C.sb("A1", [128, NCH, 2])
        _dma(C, "sp", n1g[:], n1g_d, [], ["n1g"])
        with ExitStack() as st2:
            C.st = st2
            modv = emit_mod(C, cv_d, modw_d, modb_d, 2048, pss[0][0], "ps0")
            for w in range(2):
                _stt(C, A1[:, :, w], modv[:, 8:16, w], 1.0, n1g[:], ALU.add, ALU.mult, ["modv", "n1g"], ["A1"])
            wfm = C.sb("wfm", [128, NCH, 1280], MMDT); wtm = C.sb("wtm", [128, NCH, 784], MMDT)
            convw = C.sb("convw", [128, 6, 3])
            _dma(C, "sp", convw[:], convw_d, [], ["convw"])
            for c in range(NCH):
                _dma(C, "pool", wfm[:, c, :], wfm_d[c * 128:(c + 1) * 128, :], [], ["wfm"])
                _dma(C, "pool", wtm[:, c, :], wtm_d[c * 128:(c + 1) * 128, :], [], ["wtm"])
            xt = C.sb("xt", [128, NCH, 512]); sq = C.sb("sq", [128, NCH, 512]); rstd = C.sb("rstd", [128, 512])
            h1b = C.sb("h1b", [128, NCH, 512], MMDT)
            raw = [C.sb("raw%d" % i, [128, 512]) for i in range(2)]
            r1 = [C.sb("r1%d" % i, [128, 512]) for i in range(2)]
            r2 = [C.sb("r2%d" % i, [128, 512]) for i in range(2)]
            tmo = [C.sb("tmo%d" % i, [128, 512]) for i in range(2)]
            for ti, (t0, N, w) in enumerate(tiles):
                if env is not None and env.xload is not None:
                    env.xload(C, xt, t0, N)
                else:
                    _dma(C, "sp", xt[:, :, :N], r3(xT_d)[:, :, t0:t0 + N], [], ["xt"])
                emit_norm1_tile(C, xt, "xt", N, w, A1, modv, sq, rstd, pss[1][0], "ps1", h1b, sq)
                R, RL = (1, 256) if w == 1 else (N // 64, 64)
                v3 = lambda t: t[:, :N].rearrange("p (r l) -> p r l", l=RL)
                for fm in range(10):
                    b = fm % 2
                    ps, pk = pss[2 + b]
                    ps2, pk2 = pss[4 + b]
                    kr, k1, k2 = "raw%d" % b, "r1%d" % b, "r2%d" % b
                    for c in range(NCH):
                        _mm(C, ps[:, :N], wfm[:, c, fm * 128:(fm + 1) * 128], h1b[:, c, :N], c == 0, c == NCH - 1, ["wfm", "h1b"], [pk])
                    if fm < 6:
                        _cp(C, "act", raw[b][:, :N], ps[:, :N], [pk], [kr])
                        _ts(C, "dve", r1[b][:, :N], raw[b][:, :N], convw[:, fm, 1:2], None, ALU.mult, None, [kr, "convw"], [k1])
                        _stt(C, v3(r1[b])[:, :, 1:RL], v3(raw[b])[:, :, 0:RL - 1], convw[:, fm, 0:1], v3(r1[b])[:, :, 1:RL], ALU.mult, ALU.add, [kr, k1, "convw"], [k1])
                        _stt(C, v3(r1[b])[:, :, 0:RL - 1], v3(raw[b])[:, :, 1:RL], convw[:, fm, 2:3], v3(r1[b])[:, :, 0:RL - 1], ALU.mult, ALU.add, [kr, k1, "convw"], [k1])
                        _act(C, r1[b][:, :N], r1[b][:, :N], AF.Silu, [k1], [k1])
                        if fm < 4:
                            _act(C, r2[b][:, :N], r1[b][:, :N], AF.Square, [k1], [k2])
                            _mm(C, ps2[:, :N], C.ones[:], r2[b][:, :N], True, True, ["ones", k2], [pk2])
                            _act(C, r2[b][:, :N], ps2[:, :N], AF.Sqrt, [pk2], [k2], bias=EPS, scale=1.0)
                            P.op("dve", (lambda b, N: lambda e: e.reciprocal(out=r2[b][:, :N], in_=r2[b][:, :N]))(b, N), reads=[k2], writes=[k2])
                            _stt(C, r1[b][:, :N], r1[b][:, :N], (128.0 ** -0.5) if fm < 2 else 1.0, r2[b][:, :N], ALU.mult, ALU.mult, [k1, k2], [k1])
                    elif fm < 8:
                        _cp(C, "act", r1[b][:, :N], ps[:, :N], [pk], [k1])
                    else:
                        _act(C, r1[b][:, :N], ps[:, :N], AF.Identity, [pk], [k1], scale=128.0 ** -0.5)
                    _dma(C, "sp", FT_d[fm, :, t0:t0 + N], r1[b][:, :N], [k1], ["FT_d"])
                for blk in range(N // 128):
                    for half, (c0, c1) in enumerate(((0, 512), (512, 784))):
                        b = (blk * 2 + half) % 2
                        ps, pk = pss[6 + b]
                        for c in range(NCH):
                            _mm(C, ps[:, :c1 - c0], h1b[:, c, blk * 128:(blk + 1) * 128], wtm[:, c, c0:c1], c == 0, c == NCH - 1, ["h1b", "wtm"], [pk])
                        _cp(C, "act" if half % 2 else "dve", tmo[b][:, :c1 - c0], ps[:, :c1 - c0], [pk], ["tmo%d" % b])
                        _dma(C, "sp", TM_d[t0 + blk * 128:t0 + (blk + 1) * 128, c0:c1], tmo[b][:, :c1 - c0], ["tmo%d" % b], ["TM_d"])
            P.barrier()
        C.st = st
        with ExitStack() as st3:
            C.st = st3
            TMv = TM_d.rearrange("(n p) w -> p n w", p=128)
            gates = C.sb("gates", [128, NB, 16]); gconst = C.sb("gconst", [128, 4, 4]); gainb = C.sb("gainb", [128, 512])
            _dma(C, "sp", gates[:], TMv[:, :, 768:784], [], ["gates"])
            _dma(C, "sp", gconst[:], gconst_d, [], ["gconst"])
            _dma(C, "sp", gainb[:], gainb_d, [], ["gainb"])
            la = C.sb("la", [128, NB, 4]); beta = C.sb("beta", [128, NB, 4]); ic = C.sb("ic", [128, NB, 4]); lf = C.sb("lf", [128, NB, 4])
            gG = C.sb("gG", [128, NB, 4]); gF_ = C.sb("gFm", [128, NB, 4]); tA = C.sb("tA", [128, NB, 4]); tB = C.sb("tB", [128, NB, 4])
            arate = C.sb("arate", [128, 4]); cmax = C.sb("cmax", [128, 4]); ecneg = C.sb("ecneg", [128, 4]); c1 = C.sb("c1", [128, 4])
            bc = lambda col: gconst[:, col:col + 1, :].to_broadcast([128, NB, 4])
            _act(C, arate[:], gconst[:, 0, :], AF.Exp, ["gconst"], ["arate"])
            _tt(C, "dve", la[:], gates[:, :, 0:4], bc(1), ALU.add, ["gates", "gconst"], ["la"])
            _act(C, la[:], la[:], AF.Exp, ["la"], ["la"])
            _act(C, la[:], la[:], AF.Ln, ["la"], ["la"], bias=1.0)
            _tt(C, "dve", la[:], la[:], arate[:, None, :].to_broadcast([128, NB, 4]), ALU.mult, ["la", "arate"], ["la"])
            _ts(C, "dve", la[:], la[:], -1.0, None, ALU.mult, None, ["la"], ["la"])
            _act(C, beta[:], gates[:, :, 4:8], AF.Sigmoid, ["gates"], ["beta"])
            _tt(C, "dve", ic[:], gates[:, :, 8:12], bc(2), ALU.add, ["gates", "gconst"], ["ic"])
            _tt(C, "dve", lf[:], gates[:, :, 12:16], bc(3), ALU.add, ["gates", "gconst"], ["lf"])
            _act(C, lf[:], lf[:], AF.Exp, ["lf"], ["lf"], scale=-1.0)
            _act(C, lf[:], lf[:], AF.Ln, ["lf"], ["lf"], bias=1.0)
            _ts(C, "dve", lf[:], lf[:], -1.0, None, ALU.mult, None, ["lf"], ["lf"])
            P.op("dve", lambda e: e.tensor_reduce(out=c1[:], in_=ic[:].rearrange("p n k -> p k n"), axis=AX.X, op=ALU.max), reads=["ic"], writes=["c1"])
            _tr(C, pss[7][0][0:4, 0:128], c1[:], ["c1"], ["ps7"])
            c2 = C.sb("c2", [4, 1]); c3 = C.sb("c3", [4, 4])
            P.op("dve", lambda e: e.reduce_max(out=c2[:], in_=pss[7][0][0:4, 0:128], axis=AX.X), reads=["ps7"], writes=["c2"])
            _ts(C, "dve", c3[:], C.ident[0:4, 0:4], c2[:, 0:1], None, ALU.mult, None, ["ident", "c2"], ["c3"])
            _mm(C, pss[7][0][:, 0:4], C.ones[0:4, :], c3[:], True, True, ["ones", "c3"], ["ps7"])
            _cp(C, "dve", cmax[:], pss[7][0][:, 0:4], ["ps7"], ["cmax"])
            _act(C, ecneg[:], cmax[:], AF.Exp, ["cmax"], ["ecneg"], scale=-1.0)
            ncmax = C.sb("ncmax", [128, 4])
            _ts(C, "dve", ncmax[:], cmax[:], -1.0, None, ALU.mult, None, ["cmax"], ["ncmax"])

            KT = C.sb("KT", [128, T]); QT = C.sb("QT", [128, T], MMDT); VT = C.sb("VT", [128, T]); KTb = C.sb("KTb", [128, T], MMDT)
            Xb = C.sb("Xb", [128, NB, 129], MMDT); XPb = C.sb("XPb", [128, NB, 129], MMDT)
            gdn_heads = 2 if EVEN_STOP >= 3 else 0
            ml_heads = 2 if EVEN_STOP >= 7 else 0
            Vtm = C.sb("Vtm", [128, NB, 129]); X = C.sb("X", [128, NB, 129]); XP = C.sb("XP", [128, NB, 129])
            TT = C.sb("TT", [128, NB, 128]); OH = C.sb("OH", [128, NB, 128])
            g = C.sb("g", [128, NB]); colb = C.sb("colb", [128, NB]); rowfb = C.sb("rowfb", [128, NB])
            tabs = (C.sb("kf", [128, NB]), C.sb("rowf", [128, NB]), C.sb("ctab", [128, NB, NB]), C.sb("gF", [128, NB]), C.sb("gL", [128, NB]), None)
            kf, rowf, ctab = tabs[0], tabs[1], tabs[2]
            wts = [(C.sb("wt%d" % i, [128, 128]), "wt%d" % i) for i in range(4)]
            wtsb = [(C.sb("wtb%d" % i, [128, 128], MMDT), "wtb%d" % i) for i in range(4)]
            Et = C.sb("Et", [128, 128]); dg = C.sb("dg", [128, 128]); offb = C.sb("offb", [128, 129]); otmp = C.sb("otmp", [128, 129])
            Et2 = [(C.sb("Etp%d" % i, [128, 128]), "Etp%d" % i) for i in range(2)]; dg2 = [(C.sb("dgp%d" % i, [128, 128]), "dgp%d" % i) for i in range(2)]
            C.tailbuf = C.sb("tailbuf", [128, 129])
            L_Am = [[C.sb("Am%d_%d" % (i, l), [128, 128]) for i in range(2)] for l in range(2)]
            L_Bm = [[C.sb("Bm%d_%d" % (i, l), [128, 128]) for i in range(2)] for l in range(2)]
            L_Et = [C.sb("EtL%d" % l, [128, 128]) for l in range(2)]; L_dg = [C.sb("dgL%d" % l, [128, 128]) for l in range(2)]
            L_Pm = [C.sb("PmL%d" % l, [128, 128]) for l in range(2)]
            Pm = C.sb("Pm", [128, 128]); Rm = C.sb("Rm", [128, 128])
            bufs = (pss[0:4], pss[4], pss[5], pss[6], wts, Et, dg, offb)
            bufsb = (pss[0:4], pss[4], pss[5], pss[6], wtsb, Et, dg, offb)
            pbufs = (pss[0:4], [pss[4], pss[6]], pss[5], pss[7], wts, Et2, dg2, offb)
            pbufsb = (pss[0:4], [pss[4], pss[6]], pss[5], pss[7], wtsb, Et2, dg2, offb)
            Gt = C.sb("Gt", [128, 128]); cen = C.sb("cen", [128, 128]); st1 = C.sb("st1", [128, 4]); uT = C.sb("uT", [128, 128], MMDT)
            ps7 = pss[7][0]

            def head_tail(hidx, gate_col, gate_func, gain_off):
                for I in range(NB):
                    k = "OH%d" % I
                    _dma(C, "sp", Gt[:], TM_d[I * 128:(I + 1) * 128, gate_col:gate_col + 128], ["TM_d"], ["Gt"])
                    P.op("act", (lambda I: lambda e: e.activation(out=otmp[:, 0:128], in_=OH[:, I, :], func=AF.Square, accum_out=st1[:, 1:2]))(I), reads=[k], writes=["otmp", "st1b"])
                    _act(C, st1[:, 2:3], st1[:, 1:2], AF.Sqrt, ["st1b"], ["st1c"], bias=EPS, scale=1.0 / 128)
                    P.op("dve", lambda e: e.reciprocal(out=st1[:, 3:4], in_=st1[:, 2:3]), reads=["st1c"], writes=["st1d"])
                    _act(C, Gt[:], Gt[:], gate_func, ["Gt"], ["Gt"])
                    _tt(C, "pool", Gt[:], Gt[:], gainb[:, gain_off:gain_off + 128], ALU.mult, ["Gt", "gainb"], ["Gt"])
                    _stt(C, cen[:], OH[:, I, :], st1[:, 3:4], Gt[:], ALU.mult, ALU.mult, [k, "st1d", "Gt"], ["cen"])
                    _tr(C, ps7[:, 0:128], cen[:], ["cen"], ["ps7"])
                    _cp(C, "act", uT[:], ps7[:, 0:128], ["ps7"], ["uT"])
                    _dma(C, "sp", UT_d[hidx, :, I * 128:(I + 1) * 128], uT[:], ["uT"], ["UT_d"])

            for hl in range(gdn_heads):
                _dma(C, "sp", KT[:], FT_d[2 + hl], ["FT_d"], ["AT"])
                _dma(C, "pool", KTb[:], FT_d[2 + hl], ["FT_d"], ["ATb"])
                _dma(C, "pool", QT[:], FT_d[hl], ["FT_d"], ["BTb"])
                _dma(C, "sp", VT[:], FT_d[4 + hl], ["FT_d"], ["VT"])
                for I in range(NB):
                    _tr(C, ps7[:, 0:128], VT[:, I * 128:(I + 1) * 128], ["VT"], ["ps7"])
                    _cp(C, "act", Vtm[:, I, 0:128], ps7[:, 0:128], ["ps7"], ["Vtm"])
                for d in range(2):
                    col = d * 2 + hl
                    order = scan_order(d)
                    emit_cumsum(C, la, "la", d, gG, "gG", tA, tB, ps7, "ps7")
                    _cp(C, "dve", g[:], gG[:, :, col], ["gG"], ["g"])
                    _ts(C, "dve", colb[:], g[:], -1.0, None, ALU.mult, None, ["g"], ["colb"])
                    emit_stream_tables(C, g[:], colb, d, tabs, ps7, "ps7", "")
                    _tt(C, "dve", rowfb[:], rowf[:], beta[:, :, col], ALU.mult, ["tabsrowf", "beta"], ["rowfb"])
                    smask = C.masks["Bs" if d == 0 else "Fs"]
                    smkey = "maskBs" if d == 0 else "maskFs"
                    def pass0_stages(I, lane, col=col, smask=smask, smkey=smkey):
                        isl = slice(I * 128, (I + 1) * 128)
                        pa, pb_, pc = pss[lane * 3][0], pss[lane * 3 + 1][0], pss[lane * 3 + 2][0]
                        ka, kb_, kc = "ps%d" % (lane * 3), "ps%d" % (lane * 3 + 1), "ps%d" % (lane * 3 + 2)
                        Etl, dgl, Pml = L_Et[lane], L_dg[lane], L_Pm[lane]
                        Aml, Bml = L_Am[lane], L_Bm[lane]
                        tg = "_l%d" % lane
                        st_ = []
                        st_.append(lambda: (_ts(C, "dve", dgl[:], C.ident[:], g[:, I:I + 1], None, ALU.mult, None, ["ident", "g"], ["dg" + tg]),
                                            _mm(C, pa[:, 0:128], C.ones[:], dgl[:], True, True, ["ones", "dg" + tg], [ka]),
                                            _mm(C, pb_[:, 0:128], KT[:, isl], KT[:, isl], True, True, ["AT"], [kb_])))
                        st_.append(lambda: (_ts(C, "dve", Etl[:], pa[:, 0:128], colb[:, I:I + 1], 0.0, ALU.add, ALU.max, [ka, "colb"], ["Et" + tg]),
                                            _act(C, Etl[:], Etl[:], AF.Exp, ["Et" + tg], ["Et" + tg], scale=-1.0),
                                            _tt(C, "pool", Etl[:], Etl[:], smask[:], ALU.mult, ["Et" + tg, smkey], ["Et" + tg])))
                        st_.append(lambda: (_stt(C, Bml[0][:], pb_[:, 0:128], beta[:, I, col:col + 1], Etl[:], ALU.mult, ALU.mult, [kb_, "beta", "Et" + tg], ["Bm0" + tg]),
                                            _tr(C, pa[:, 0:128], Bml[0][:], ["Bm0" + tg], [ka])))
                        st_.append(lambda: (_cp(C, "act", Aml[0][:], pa[:, 0:128], [ka], ["Am0" + tg]),
                                            _tt(C, "dve", Pml[:], C.ident[:], Aml[0][:], ALU.subtract, ["ident", "Am0" + tg], ["Pm" + tg])))
                        for lev in range(1, 7):
                            a0, a1 = (lev - 1) % 2, lev % 2
                            st_.append(lambda a0=a0: (_mm(C, pb_[:, 0:128], Bml[a0][:], Aml[a0][:], True, True, ["Bm%d" % a0 + tg, "Am%d" % a0 + tg], [kb_]),
                                                      _mm(C, pc[:, 0:128], Aml[a0][:], Bml[a0][:], True, True, ["Bm%d" % a0 + tg, "Am%d" % a0 + tg], [kc])))
                            st_.append(lambda a1=a1: (_cp(C, "act", Aml[a1][:], pb_[:, 0:128], [kb_], ["Am%d" % a1 + tg]),
                                                      _cp(C, "dve", Bml[a1][:], pc[:, 0:128], [kc], ["Bm%d" % a1 + tg])))
                            st_.append(lambda a1=a1: _mm(C, pa[:, 0:128], Bml[a1][:], Pml[:], True, True, ["Bm%d" % a1 + tg, "Pm" + tg], [ka]))
                            st_.append(lambda: _tt(C, "dve", Pml[:], Pml[:], pa[:, 0:128], ALU.add, ["Pm" + tg, ka], ["Pm" + tg]))
                        st_.append(lambda: _cp(C, "pool", TT[:, I, :], Pml[:], ["Pm" + tg], ["TT"]))
                        return st_

                    for I0 in range(0, NB, 2):
                        lanes_ = [pass0_stages(I0 + ln, ln) for ln in range(2) if I0 + ln < NB]
                        for sidx in range(len(lanes_[0])):
                            for st_ in lanes_:
                                st_[sidx]()
                    def tail1(I, pos, has_off, psO, psOkey, col=col):
                        _ts(C, "dve", Rm[:], Vtm[:, I, 0:128], beta[:, I, col:col + 1], None, ALU.mult, None, ["Vtm", "beta"], ["Rm"])
                        if has_off:
                            _ts(C, "dve", offb[:, 0:128], psO[:, 0:128], rowfb[:, I:I + 1], None, ALU.mult, None, [psOkey, "rowfb"], ["offb"])
                            _tt(C, "pool", Rm[:], Rm[:], offb[:, 0:128], ALU.subtract, ["Rm", "offb"], ["Rm"])
                        _mm(C, pss[5][0][:, 0:128], TT[:, I, :], Rm[:], True, True, ["TT", "Rm"], ["ps5"])
                        _cp(C, "act", X[:, I, 0:128], pss[5][0][:, 0:128], ["ps5"], ["Vraw"])
                        _ts(C, "dve", XP[:, I, 0:128], pss[5][0][:, 0:128], kf[:, I:I + 1], None, ALU.mult, None, ["ps5", "tabskf"], ["VP"])
                        _cp(C, "pool", Xb[:, I, 0:128], X[:, I, 0:128], ["Vraw"], ["Vrawb"])
                        _cp(C, "pool", XPb[:, I, 0:128], XP[:, I, 0:128], ["VP"], ["VPb"])
                    emit_stream_pipelined(C, order, KT, KT, XP[:, :, 0:128], X[:, :, 0:128], 128, tabs, g, colb, d, pbufs, "", tail1, diag=False)
                    def out2(I, res, rk, d=d):
                        if d == 0:
                            _cp(C, "pool", OH[:, I, :], res[:, 0:128], [rk], ["OH%d" % I])
                        else:
                            _tt(C, "pool", OH[:, I, :], OH[:, I, :], res[:, 0:128], ALU.add, [rk, "OH%d" % I], ["OH%d" % I])
                    emit_stream_pipelined(C, order, KTb, QT, XPb[:, :, 0:128], Xb[:, :, 0:128], 128, tabs, g, colb, d, pbufsb, "b", std_tail(C, tabs, pbufsb, 128, out2))
                if EVEN_STOP >= 6:
                    head_tail(hl, hl * 128, AF.Silu, 0)
            for hl in range(ml_heads):
                _dma(C, "pool", KTb[:], FT_d[8 + hl], ["FT_d"], ["ATb"])
                _dma(C, "pool", QT[:], FT_d[6 + hl], ["FT_d"], ["BTb"])
                _dma(C, "sp", Vtm[:, :, 0:128], TMv[:, :, 256 + hl * 128:256 + (hl + 1) * 128], ["TM_d"], ["Vraw", "Vtm"])
                P.op("pool", lambda e: e.memset(Vtm[:, :, 128:129], 1.0), reads=["Vraw"], writes=["Vraw"])
                for J in range(NB):
                    _cp(C, "act", Xb[:, J, :], Vtm[:, J, :], ["Vraw"], ["Vrawb"])
                for d in range(2):
                    col = d * 2 + hl
                    order = scan_order(d)
                    emit_cumsum(C, lf, "lf", d, gF_, "gFm", tA, tB, ps7, "ps7")
                    _cp(C, "dve", g[:], gF_[:, :, col], ["gFm"], ["g"])
                    _tt(C, "dve", colb[:], ic[:, :, col], g[:], ALU.subtract, ["ic", "g"], ["colb"])
                    _ts(C, "dve", colb[:], colb[:], ncmax[:, col:col + 1], None, ALU.add, None, ["colb", "ncmax"], ["colb"])
                    emit_stream_tables(C, g[:], colb, d, tabs, ps7, "ps7", "")
                    for J in range(NB):
                        _ts(C, "pool" if J % 2 else "dve", XPb[:, J, :], Vtm[:, J, :], kf[:, J:J + 1], None, ALU.mult, None, ["Vraw", "tabskf"], ["VPb"])
                    def out3(I, res, rk, d=d, col=col):
                        _act(C, st1[:, 0:1], res[:, 128:129], AF.Abs, [rk], ["st1"])
                        _ts(C, "dve", st1[:, 0:1], st1[:, 0:1], ecneg[:, col:col + 1], None, ALU.max, None, ["st1", "ecneg"], ["st1"])
                        P.op("dve", lambda e: e.reciprocal(out=st1[:, 0:1], in_=st1[:, 0:1]), reads=["st1"], writes=["st1"])
                        if d == 0:
                            _ts(C, "dve", OH[:, I, :], res[:, 0:128], st1[:, 0:1], None, ALU.mult, None, [rk, "st1"], ["OH%d" % I])
                        else:
                            _stt(C, OH[:, I, :], res[:, 0:128], st1[:, 0:1], OH[:, I, :], ALU.mult, ALU.add, [rk, "st1", "OH%d" % I], ["OH%d" % I])
                    emit_stream_pipelined(C, order, KTb, QT, XPb, Xb, 129, tabs, g, colb, d, pbufsb, "b", std_tail(C, tabs, pbufsb, 129, out3))
                head_tail(2 + hl, 512 + hl * 128, AF.Sigmoid, 128 + hl * 128)
            P.barrier()
        C.st = st
        with ExitStack() as st4:
            C.st = st4
            wout = C.sb("wout", [128, 4, D], MMDT)
            for hc in range(4):
                _dma(C, "pool", wout[:, hc, :], wout_d[hc * 128:(hc + 1) * 128, :], [], ["wout"])
            ut = [C.sb("utile%d" % i, [128, 4, 512], MMDT) for i in range(2)]
            yo = [C.sb("yo%d" % i, [128, 512]) for i in range(2)]
            for ti, (t0, N, w) in enumerate(tiles):
                b = ti % 2
                _dma(C, "sp", ut[b][:, :, :N], UT_d.rearrange("f p t -> p f t")[:, :, t0:t0 + N], [], ["ut%d" % b])
                for dmc in range(NCH):
                    pb = dmc % 2
                    ps, pk = pss[pb]
                    for hc in range(4):
                        _mm(C, ps[:, :N], wout[:, hc, dmc * 128:(dmc + 1) * 128], ut[b][:, hc, :N], hc == 0, hc == 3, ["wout", "ut%d" % b], [pk])
                    _cp(C, "act" if pb else "dve", yo[pb][:, :N], ps[:, :N], [pk], ["yo%d" % pb])
                    if env is not None:
                        env.ywrite(C, dmc, t0, N, yo[pb], "yo%d" % pb)
                    else:
                        _dma(C, "sp", yT_d[dmc * 128:(dmc + 1) * 128, t0:t0 + N], yo[pb][:, :N], ["yo%d" % pb], ["yT%d_%d" % (ti, dmc)])
            if env is None:
                P.finish()
            else:
                P.barrier()
        if env is None:
            P.emit()
    return nc


def even_inputs(layer, inputs, b, hg, x_lat, x_ctx):
    j = layer // 2
    m = mixer_common_inputs(layer, inputs, b, x_lat, x_ctx)
    w_in = inputs["ev_w_in"][j]
    hs = [hg * 2, hg * 2 + 1]
    hcols = lambda off, h: w_in[:, off + h * 128: off + (h + 1) * 128]
    fm = [hcols(0, h) for h in hs] + [hcols(512, h) for h in hs] + [hcols(1024, h) for h in hs] + \
         [hcols(2064, h) for h in hs] + [hcols(2576, h) for h in hs]
    m["wfm"] = np.ascontiguousarray(np.concatenate(fm, 1))
    gate_cols = []
    for off in (2048, 2056, 4112, 4120):
        for d in range(2):
            for h in hs:
                gate_cols.append(w_in[:, off + d * 4 + h: off + d * 4 + h + 1])
    tm = [hcols(1536, h) for h in hs] + [hcols(3088, h) for h in hs] + [hcols(3600, h) for h in hs] + gate_cols
    m["wtm"] = np.ascontiguousarray(np.concatenate(tm, 1))
    w_out = inputs["ev_w_out"][j]
    m["wout"] = np.ascontiguousarray(np.concatenate([w_out[h * 128:(h + 1) * 128] for h in hs] + [w_out[512 + h * 128:512 + (h + 1) * 128] for h in hs], 0))
    cw = inputs["ev_conv_w"][j]
    conv = np.zeros((128, 6, 3), np.float32)
    for qi, off in enumerate((0, 512, 1024)):
        for hi, h in enumerate(hs):
            conv[:, qi * 2 + hi, :] = cw[:, off + h * 128: off + (h + 1) * 128].T
    m["convw"] = conv
    gc = np.zeros((128, 4, 4), np.float32)
    for r, name in enumerate(("gdn_a_log", "gdn_dt_bias", "ml_i_bias", "ml_f_bias")):
        for d in range(2):
            for hi, h in enumerate(hs):
                gc[:, r, d * 2 + hi] = inputs[name][j][d, h]
    m["gconst"] = gc
    gb = np.concatenate([inputs["gdn_norm_g"][j]] + [inputs["ml_norm_g"][j][h * 128:(h + 1) * 128] for h in hs] + [np.zeros(128, np.float32)])
    m["gainb"] = np.ascontiguousarray(np.broadcast_to(gb[None, :], (128, 512)).astype(np.float32))
    return m


NTH = 2176
PAIRS = [[0, 1], [2, 3], [4, 5], [6, 7]]


def build_fused():
    nc = bass.Bass("TRN2", target_bir_lowering=False)
    env_nc_holder[0] = nc
    idx_d = nc.dram_tensor("idx_tab", [128, 16], I32, kind="ExternalInput").ap()
    ysend = nc.dram_tensor("ysend", [2, D, NTH], F32, kind="Internal").ap()
    yrecv = nc.dram_tensor("yrecv", [16 * 256, NTH], F32, kind="Internal", addr_space="Local").ap()
    ysel = nc.dram_tensor("ysel", [2, D, NTH], F32, kind="Internal").ap()
    xo0 = nc.dram_tensor("xo0", [D, NTH], F32, kind="Internal").ap()
    g2 = nc.dram_tensor("g2", [8 * 256, NTH], F32, kind="Internal", addr_space="Local").ap()
    with ExitStack() as gst:
        C = Ctx(nc, gst)
        C.sfx = "_g"
        P = C.P
        emit_consts(C)
        emit_masks(C)
        C.pss_all = [(C.ps("ps%d" % i), "ps%d" % i) for i in range(8)]
        idx_sb = C.sb("idx", [128, 16], I32)
        _dma(C, "sp", idx_sb[:], idx_d, [], ["idx"])
        env = Env(nc, C)

        def ywrite(C, dmc, t0, N, yo, yokey):
            rows = slice(dmc * 128, (dmc + 1) * 128)
            if t0 == 0:
                _dma(C, "sp", ysend[0, rows, 2048:2176], yo[:, 0:128], [yokey], ["ysend"])
                _dma(C, "sp", ysend[1, rows, 2048:2176], yo[:, 128:256], [yokey], ["ysend"])
            else:
                j0 = t0 - 256
                _dma(C, "sp", ysend[j0 // 2048, rows, j0 % 2048:j0 % 2048 + N], yo[:, :N], [yokey], ["ysend"])

        def exchange_y(tag):
            for k in range(16):
                h, c = k // 8, k % 8
                P.coll((lambda h, c, k: lambda e: e.collective_compute("AllGather", ALU.bypass, replica_groups=PAIRS,
                                                                         ins=[ysend[h, c * 128:(c + 1) * 128, :]],
                                                                         outs=[yrecv[k * 256:(k + 1) * 256, :]]))(h, c, k),
                       reads=["ysend"], writes=["yrecv"])
            with ExitStack() as stx:
                C.st = stx
                C.sfx = "_x" + tag
                selb = [C.sb("selb%d" % i, [128, NTH]) for i in range(2)]
                for c in range(8):
                    for r in range(2):
                        b = (c * 2 + r) % 2
                        col = c * 2 + r
                        P.dma("pool", (lambda b, col: lambda e: e.indirect_dma_start(
                            out=selb[b][:, :], out_offset=None, in_=yrecv[:, :],
                            in_offset=bass.IndirectOffsetOnAxis(ap=idx_sb[:, col:col + 1], axis=0)))(b, col),
                            reads=["yrecv", "idx"], writes=["selb%d" % b])
                        _dma(C, "sp", ysel[r, c * 128:(c + 1) * 128, :], selb[b][:], ["selb%d" % b], ["ysel"])
                P.barrier()
            C.st = gst

        def exchange_x():
            for c in range(8):
                P.coll((lambda c: lambda e: e.collective_compute("AllGather", ALU.bypass, replica_groups=PAIRS,
                                                                   ins=[xo0[c * 128:(c + 1) * 128, :]],
                                                                   outs=[g2[c * 256:(c + 1) * 256, :]]))(c),
                       reads=["xo0"], writes=["g2"])
            P.barrier()

        g2v = g2.rearrange("(c r p) n -> p c r n", c=8, r=2, p=128)

        def xload_g2(C, xt, t0, N):
            if t0 == 0:
                _dma(C, "sp", xt[:, :, 0:128], g2v[:, :, 0, 2048:2176], [], ["xt"])
                _dma(C, "sp", xt[:, :, 128:256], g2v[:, :, 1, 2048:2176], [], ["xt"])
            else:
                j0 = t0 - 256
                _dma(C, "sp", xt[:, :, :N], g2v[:, :, j0 // 2048, j0 % 2048:j0 % 2048 + N], [], ["xt"])

        env.sfx, env.over, env.xload, env.ywrite = "_m0", {}, None, ywrite
        build_mixer_even(env)
        C.st = gst
        exchange_y("0")
        env.sfx, env.over = "_f0", {"ypa": ysel[0], "ypb": ysel[1], "out": xo0}
        build_ffn(2048, 128, False, env)
        C.st = gst
        exchange_x()
        env.sfx, env.over, env.xload = "_m1", {"xT": None}, xload_g2
        build_mixer_odd(env)
        C.st = gst
        exchange_y("1")
        env.sfx, env.over = "_f1", {"xT": xo0[:, 0:2048], "ypa": ysel[0][:, 0:2048], "ypb": ysel[1][:, 0:2048]}
        build_ffn(2048, 0, True, env)
        C.st = gst
        P.finish()
        P.emit()
    return nc


def fused_inputs(inputs, c):
    b, r = c // 2, c % 2
    x_lat, x_ctx = inputs["x"], inputs["ctx"]
    m = {}
    for k, v in even_inputs(0, inputs, b, r, x_lat, x_ctx).items():
        m[k + "_m0"] = v
    xh = np.concatenate([x_lat[b, r * 2048:(r + 1) * 2048], x_ctx[b, r * 128:(r + 1) * 128]], 0).T
    for k, v in ffn_inputs(0, inputs, xh, None, None, b).items():
        if k not in ("ypa", "ypb"):
            m[k + "_f0"] = v
    for k, v in odd_inputs(1, inputs, b, r, x_lat, x_ctx).items():
        if k != "xT":
            m[k + "_m1"] = v
    for k, v in ffn_inputs(1, inputs, None, None, None, b).items():
        if k not in ("xT", "ypa", "ypb"):
            m[k + "_f1"] = v
    idx = np.zeros((128, 16), np.int32)
    for cc in range(8):
        for rr in range(2):
            idx[:, cc * 2 + rr] = ((r * 8 + cc) * 2 + rr) * 128 + np.arange(128)
    m["idx_tab"] = idx
    return m


_CACHE = {}


def _prog(name, fn):
    if name not in _CACHE:
        _CACHE[name] = fn()
    return _CACHE[name]


def _run(nc, maps):
    res = run_bass_kernel_spmd(nc, maps, core_ids=list(range(8)))
    return res.results


FUSED = True


def kernel(**inputs):
    inputs = {k: np.asarray(v) for k, v in inputs.items()}
    if FUSED:
        nc = _prog("fused", build_fused)
        maps = [fused_inputs(inputs, c) for c in range(8)]
        res = _run(nc, maps)
        out = np.empty((4, 4096, D), np.float32)
        for c in range(8):
            out[c // 2, (c % 2) * 2048:(c % 2 + 1) * 2048] = res[c]["out"].T
        return out
    x_lat = np.ascontiguousarray(inputs["x"], dtype=np.float32)
    x_ctx = np.ascontiguousarray(inputs["ctx"], dtype=np.float32)
    B = x_lat.shape[0]
    depth = inputs["mod_w"].shape[0]
    out_final = None
    for layer in range(depth):
        last = layer == depth - 1
        if layer % 2 == 0:
            nc = _prog("even", build_mixer_even)
            maps = [even_inputs(layer, inputs, c // 2, c % 2, x_lat, x_ctx) for c in range(8)]
        else:
            nc = _prog("odd", build_mixer_odd)
            maps = [odd_inputs(layer, inputs, c // 2, c % 2, x_lat, x_ctx) for c in range(8)]
        res = _run(nc, maps)
        yparts = [res[c]["yT"] for c in range(8)]
        n_lat, n_ctx = 2048, (0 if last else 128)
        nc = _prog("ffn_last" if last else "ffn", lambda: build_ffn(n_lat, n_ctx, last))
        maps = []
        for c in range(8):
            b, hf = c // 2, c % 2
            cols = [np.arange(256 + hf * 2048, 256 + (hf + 1) * 2048)]
            xs = [x_lat[b, hf * 2048:(hf + 1) * 2048]]
            if not last:
                cols.append(np.arange(hf * 128, (hf + 1) * 128))
                xs.append(x_ctx[b, hf * 128:(hf + 1) * 128])
            cols = np.concatenate(cols)
            xT = np.concatenate(xs, 0).T
            maps.append(ffn_inputs(layer, inputs, xT, yparts[2 * b][:, cols], yparts[2 * b + 1][:, cols], b))
        res = _run(nc, maps)
        if last:
            out_final = np.empty_like(x_lat)
            for c in range(8):
                b, hf = c // 2, c % 2
                out_final[b, hf * 2048:(hf + 1) * 2048] = res[c]["out"].T
        else:
            nx_lat = np.empty_like(x_lat)
            nx_ctx = np.empty_like(x_ctx)
            for c in range(8):
                b, hf = c // 2, c % 2
                o = res[c]["out"]
                nx_lat[b, hf * 2048:(hf + 1) * 2048] = o[:, :2048].T
                nx_ctx[b, hf * 128:(hf + 1) * 128] = o[:, 2048:].T
            x_lat, x_ctx = nx_lat, nx_ctx
    return out_final.astype(np.float32)
```
